# Optimizing a Trainium2 kernel written in Bass

```python
import jax, jax.numpy as jnp
from jax import lax
import numpy as np

D_MODEL = 1024
BATCH = 4
SEQ = 4096
DEPTH = 2

GRID_W = 64
CTX_LEN = 256

HEAD_DIM = 64
ROPE_THETA = 10000.0

ATTN_HEADS = 8
ATTN_KV_HEADS = 2
ATTN_GROUP = ATTN_HEADS // ATTN_KV_HEADS
Q_BLOCK = 128

GLA_HEADS = 4
GLA_DK = 64
GLA_DV = 128
GLA_GATE_RANK = 16
GLA_TAU = 16.0
GLA_CHUNK = 64

NA_HEADS = 8
NA_KH = 8
NA_KW = 16

A_Q = ATTN_HEADS * HEAD_DIM
A_KV = ATTN_KV_HEADS * HEAD_DIM
B_QK = GLA_HEADS * GLA_DK
B_V = GLA_HEADS * GLA_DV
B_A = 2 * GLA_GATE_RANK
C_W = NA_HEADS * HEAD_DIM
N_BRANCH = 3
IN_SPLITS = (A_Q, A_KV, A_KV, B_QK, B_QK, B_V, B_V, B_A, C_W, C_W, C_W, N_BRANCH * D_MODEL)
N_IN = sum(IN_SPLITS)
N_MOD = 6 * D_MODEL

N_GROUPS = 4
EXPERTS_PER_GROUP = 8
N_EXPERTS = N_GROUPS * EXPERTS_PER_GROUP
TOP_K = 2
D_EXPERT = 512
MOE_BLOCK = 128

LN_EPS = 1e-6
RMS_EPS = 1e-6
DEEPNORM_ALPHA = (2 * DEPTH) ** 0.25
DEEPNORM_BETA = (8 * DEPTH) ** -0.25

kernel_name = "hybrid_gated_mixers_hmoe_dit"


def _f32(a):
    return a.astype(jnp.float32)


def layer_norm0(x):
    xf = _f32(x)
    mu = xf.mean(-1, keepdims=True)
    var = jnp.square(xf - mu).mean(-1, keepdims=True)
    return ((xf - mu) * lax.rsqrt(var + LN_EPS)).astype(x.dtype)


def layer_norm(x, g, b):
    return layer_norm0(x) * g + b


def rms_norm(x, g):
    xf = _f32(x)
    return (xf * lax.rsqrt(jnp.mean(xf * xf, -1, keepdims=True) + RMS_EPS)).astype(x.dtype) * g


def modulate(x, shift, scale):
    return layer_norm0(x) * (1.0 + scale) + shift


def split_cols(z):
    idx = [int(i) for i in np.cumsum(IN_SPLITS)[:-1]]
    return jnp.split(z, idx, axis=-1)


def axial_rope(L):
    t = jnp.arange(L)
    row = (t // GRID_W).astype(jnp.float32)
    col = (t % GRID_W).astype(jnp.float32)
    n_freq = HEAD_DIM // 4
    inv = ROPE_THETA ** (-jnp.arange(n_freq, dtype=jnp.float32) / n_freq)
    ar = row[:, None] * inv
    ac = col[:, None] * inv
    ang = jnp.concatenate([ar, ar, ac, ac], axis=-1)
    return jnp.cos(ang), jnp.sin(ang)


def apply_rope(x, cos, sin):
    xf = _f32(x)
    x1, x2, x3, x4 = jnp.split(xf, 4, axis=-1)
    rot = jnp.concatenate([-x2, x1, -x4, x3], axis=-1)
    return (xf * cos[:, None, :] + rot * sin[:, None, :]).astype(x.dtype)


def softmax_attend(q, k, v):
    s = _f32(jnp.einsum('bqkgd,bskd->bkgqs', q, k)) * (HEAD_DIM ** -0.5)
    p = jax.nn.softmax(s, axis=-1).astype(v.dtype)
    return jnp.einsum('bkgqs,bskd->bqkgd', p, v)


def gqa_axial(q_lat, k_lat, v_lat, q_ctx, k_ctx, v_ctx, q_norm, k_norm, with_ctx_out):
    B, L = q_lat.shape[:2]
    Lc = q_ctx.shape[1]
    cos, sin = axial_rope(L)
    q = apply_rope(rms_norm(q_lat.reshape(B, L, ATTN_HEADS, HEAD_DIM), q_norm), cos, sin)
    k = apply_rope(rms_norm(k_lat.reshape(B, L, ATTN_KV_HEADS, HEAD_DIM), k_norm), cos, sin)
    k_c = rms_norm(k_ctx.reshape(B, Lc, ATTN_KV_HEADS, HEAD_DIM), k_norm)
    v_c = v_ctx.reshape(B, Lc, ATTN_KV_HEADS, HEAD_DIM)
    k_all = jnp.concatenate([k_c, k], axis=1)
    v_all = jnp.concatenate([v_c, v_lat.reshape(B, L, ATTN_KV_HEADS, HEAD_DIM)], axis=1)
    n_blk = L // Q_BLOCK
    q_blocks = q.reshape(B, n_blk, Q_BLOCK, ATTN_KV_HEADS, ATTN_GROUP, HEAD_DIM).transpose(1, 0, 2, 3, 4, 5)
    o = lax.map(lambda qb: softmax_attend(qb, k_all, v_all), q_blocks)
    o_lat = o.transpose(1, 0, 2, 3, 4, 5).reshape(B, L, A_Q)
    if not with_ctx_out:
        return o_lat, None
    q_c = rms_norm(q_ctx.reshape(B, Lc, ATTN_KV_HEADS, ATTN_GROUP, HEAD_DIM), q_norm)
    return o_lat, softmax_attend(q_c, k_c, v_c).reshape(B, Lc, A_Q)


def gla_chunk_scan(q, k, v, log_g, s0):
    B, H, T, dk = q.shape
    dv = v.shape[-1]
    nc = T // GLA_CHUNK

    def to_chunks(a):
        return jnp.moveaxis(a.reshape(B, H, nc, GLA_CHUNK, a.shape[-1]), 2, 0)

    causal = jnp.tril(jnp.ones((GLA_CHUNK, GLA_CHUNK), dtype=bool))[:, :, None]

    def step(S, inp):
        qc, kc, vc, gc = (_f32(a) for a in inp)
        b = jnp.cumsum(gc, axis=2)
        o_inter = jnp.einsum('bhtd,bhde->bhte', qc * jnp.exp(b), S)
        diff = b[:, :, :, None, :] - b[:, :, None, :, :]
        decay = jnp.where(causal, jnp.exp(jnp.where(causal, diff, 0.0)), 0.0)
        scores = jnp.einsum('bhtsd,bhsd->bhts', qc[:, :, :, None, :] * decay, kc)
        o_intra = jnp.einsum('bhts,bhse->bhte', scores, vc)
        b_end = b[:, :, -1:, :]
        S_new = jnp.exp(b_end[:, :, 0, :])[..., None] * S + jnp.einsum('bhsd,bhse->bhde', kc * jnp.exp(b_end - b), vc)
        return S_new, o_inter + o_intra

    S, o = lax.scan(step, s0, (to_chunks(q), to_chunks(k), to_chunks(v), to_chunks(log_g)))
    o = jnp.moveaxis(o, 0, 2).reshape(B, H, T, dv).astype(v.dtype)
    return S, o


def gla_bidir(z_lat, z_ctx, w_gate, b_gate, norm_g, with_ctx_out):
    def prep(z):
        q, k, v, r, a = z
        B, T = q.shape[:2]

        def heads(t, d):
            return t.reshape(B, T, GLA_HEADS, d).transpose(0, 2, 1, 3)

        def log_gate(a_dir, d):
            return heads(jax.nn.log_sigmoid(_f32(a_dir @ w_gate[d] + b_gate[d])) / GLA_TAU, GLA_DK)

        a_f, a_b = jnp.split(a, 2, axis=-1)
        return (heads(q, GLA_DK) * (GLA_DK ** -0.5), heads(k, GLA_DK), heads(v, GLA_DV), r,
                log_gate(a_f, 0), log_gate(a_b, 1))

    q_l, k_l, v_l, r_l, gf_l, gb_l = prep(z_lat)
    q_c, k_c, v_c, r_c, gf_c, gb_c = prep(z_ctx)
    B = q_l.shape[0]
    s0 = jnp.zeros((B, GLA_HEADS, GLA_DK, GLA_DV), jnp.float32)

    def flip(t):
        return jnp.flip(t, axis=2)

    s_cf, o_cf = gla_chunk_scan(q_c, k_c, v_c, gf_c, s0)
    _, o_lf = gla_chunk_scan(q_l, k_l, v_l, gf_l, s_cf)
    s_cb, o_cb = gla_chunk_scan(flip(q_c), flip(k_c), flip(v_c), flip(gb_c), s0)
    _, o_lb = gla_chunk_scan(flip(q_l), flip(k_l), flip(v_l), flip(gb_l), s_cb)

    def readout(o, r):
        Bo, H, T, dv = o.shape
        o = rms_norm(o.transpose(0, 2, 1, 3), norm_g).reshape(Bo, T, H * dv)
        return o * jax.nn.silu(r)

    o_lat = readout(o_lf + flip(o_lb), r_l)
    o_ctx = readout(o_cf + flip(o_cb), r_c) if with_ctx_out else None
    return o_lat, o_ctx


def neighbourhood_attn(q_lat, k_lat, v_lat, q_ctx, k_ctx, v_ctx, rpb, with_ctx_out):
    B, L = q_lat.shape[:2]
    Lc = q_ctx.shape[1]
    rows = L // GRID_W
    kh = min(NA_KH, rows)
    scale = HEAD_DIM ** -0.5

    def grid(t):
        return t.reshape(B, rows, GRID_W, NA_HEADS, HEAD_DIM).transpose(0, 3, 1, 2, 4)

    q_rows = q_lat.reshape(B, rows, GRID_W, NA_HEADS, HEAD_DIM).transpose(1, 0, 3, 2, 4)
    k_grid, v_grid = grid(k_lat), grid(v_lat)
    k_c = k_ctx.reshape(B, Lc, NA_HEADS, HEAD_DIM)
    v_c = v_ctx.reshape(B, Lc, NA_HEADS, HEAD_DIM)

    r = np.arange(rows)
    row_start = np.clip(r - kh // 2, 0, rows - kh)
    cidx = np.arange(GRID_W)
    col_start = np.clip(cidx - NA_KW // 2, 0, GRID_W - NA_KW)
    col_idx = col_start[:, None] + np.arange(NA_KW)
    row_rel = row_start[:, None] + np.arange(kh) - r[:, None] + (NA_KH - 1)
    col_rel = col_idx - cidx[:, None] + (NA_KW - 1)
    bias = rpb[:, row_rel][:, :, :, col_rel].transpose(1, 0, 3, 2, 4)

    def row_fn(inp):
        qr, rs, br = inp
        kb = lax.dynamic_slice_in_dim(k_grid, rs, kh, axis=2)
        vb = lax.dynamic_slice_in_dim(v_grid, rs, kh, axis=2)
        kg = kb[:, :, :, col_idx]
        vg = vb[:, :, :, col_idx]
        s_nb = _f32(jnp.einsum('bhwd,bhiwjd->bhwij', qr, kg)) * scale + br
        s_cx = _f32(jnp.einsum('bhwd,bchd->bhwc', qr, k_c)) * scale
        s = jnp.concatenate([s_nb.reshape(B, NA_HEADS, GRID_W, kh * NA_KW), s_cx], axis=-1)
        p = jax.nn.softmax(s, axis=-1).astype(vg.dtype)
        p_nb = p[..., :kh * NA_KW].reshape(B, NA_HEADS, GRID_W, kh, NA_KW)
        p_cx = p[..., kh * NA_KW:]
        return jnp.einsum('bhwij,bhiwjd->bhwd', p_nb, vg) + jnp.einsum('bhwc,bchd->bhwd', p_cx, v_c)

    o = lax.map(row_fn, (q_rows, jnp.asarray(row_start, jnp.int32), bias))
    o_lat = o.transpose(1, 0, 3, 2, 4).reshape(B, L, C_W)
    if not with_ctx_out:
        return o_lat, None
    q_c = q_ctx.reshape(B, Lc, NA_HEADS, 1, HEAD_DIM)
    return o_lat, softmax_attend(q_c, k_c, v_c).reshape(B, Lc, C_W)


def token_mixers(h_lat, h_ctx, w_in, b_in, q_norm, k_norm, gla_w_gate, gla_b_gate, gla_norm, na_rpb,
                 w_br_attn, w_br_gla, w_br_na, w_out, with_ctx_out):
    z_lat = split_cols(h_lat @ w_in + b_in)
    z_ctx = split_cols(h_ctx @ w_in + b_in)
    oa_l, oa_c = gqa_axial(*z_lat[0:3], *z_ctx[0:3], q_norm, k_norm, with_ctx_out)
    ob_l, ob_c = gla_bidir(z_lat[3:8], z_ctx[3:8], gla_w_gate, gla_b_gate, gla_norm, with_ctx_out)
    oc_l, oc_c = neighbourhood_attn(*z_lat[8:11], *z_ctx[8:11], na_rpb, with_ctx_out)

    def merge(o_a, o_b, o_c, gate_cols):
        g = jax.nn.sigmoid(gate_cols.reshape(gate_cols.shape[:-1] + (N_BRANCH, D_MODEL)))
        y = g[..., 0, :] * (o_a @ w_br_attn) + g[..., 1, :] * (o_b @ w_br_gla) + g[..., 2, :] * (o_c @ w_br_na)
        return y @ w_out

    y_lat = merge(oa_l, ob_l, oc_l, z_lat[11])
    y_ctx = merge(oa_c, ob_c, oc_c, z_ctx[11]) if with_ctx_out else None
    return y_lat, y_ctx


def hier_moe(h, w_rg, b_rg, w_re, b_re, w1, w3, w2):
    T, D = h.shape
    hf = _f32(h)
    g_logits = hf @ _f32(w_rg) + _f32(b_rg)
    grp = jnp.argmax(g_logits, axis=-1)
    p_grp = jax.nn.softmax(g_logits, axis=-1).max(-1, keepdims=True)
    e_logits = (hf @ _f32(w_re) + _f32(b_re)).reshape(T, N_GROUPS, EXPERTS_PER_GROUP)
    sel = jnp.einsum('tge,tg->te', e_logits, jax.nn.one_hot(grp, N_GROUPS, dtype=jnp.float32))
    top_v, top_i = lax.top_k(sel, TOP_K)
    gate = p_grp * jax.nn.softmax(top_v, axis=-1)
    expert = grp[:, None] * EXPERTS_PER_GROUP + top_i

    n_assign = T * TOP_K
    n_blocks = -(-n_assign // MOE_BLOCK) + N_EXPERTS
    flat_e = expert.reshape(-1)
    flat_t = jnp.repeat(jnp.arange(T), TOP_K)
    order = jnp.argsort(flat_e)
    se, st, sw = flat_e[order], flat_t[order], gate.reshape(-1)[order]
    counts = jnp.bincount(flat_e, length=N_EXPERTS)
    padded = ((counts + MOE_BLOCK - 1) // MOE_BLOCK) * MOE_BLOCK
    pad_end = jnp.cumsum(padded)
    pad_start = pad_end - padded
    start = jnp.cumsum(counts) - counts
    pos = pad_start[se] + (jnp.arange(n_assign) - start[se])
    block_expert = jnp.minimum(
        jnp.searchsorted(pad_end, jnp.arange(n_blocks) * MOE_BLOCK, side='right'), N_EXPERTS - 1)
    x_slots = jnp.zeros((n_blocks * MOE_BLOCK, D), h.dtype).at[pos].set(h[st])

    def expert_block(inp):
        xb, e = inp
        return (jax.nn.silu(xb @ w1[e]) * (xb @ w3[e])) @ w2[e]

    y_slots = lax.map(expert_block, (x_slots.reshape(n_blocks, MOE_BLOCK, D), block_expert)).reshape(-1, D)
    y = y_slots[pos] * sw[:, None].astype(h.dtype)
    return jax.ops.segment_sum(y, st, num_segments=T)


def setup_inputs(seed: int = 0) -> dict:
    key = jax.random.key(seed)
    ks = jax.random.split(key, 29)
    D = D_MODEL

    def n(k, shape, s):
        return jax.random.normal(k, shape, jnp.float32) * s

    return {
        "x": n(ks[0], (BATCH, SEQ, D), 1.0),
        "c": n(ks[1], (BATCH, D), 1.0),
        "ctx": n(ks[2], (BATCH, CTX_LEN, D), 1.0),
        "c_ctx": n(ks[3], (D,), 1.0),
        "w_mod": n(ks[4], (DEPTH, D, N_MOD), 0.5 * D ** -0.5),
        "b_mod": n(ks[5], (DEPTH, N_MOD), 0.01),
        "w_in": n(ks[6], (DEPTH, D, N_IN), D ** -0.5),
        "b_in": n(ks[7], (DEPTH, N_IN), 0.01),
        "attn_q_norm": 1.0 + n(ks[8], (DEPTH, HEAD_DIM), 0.02),
        "attn_k_norm": 1.0 + n(ks[9], (DEPTH, HEAD_DIM), 0.02),
        "gla_w_gate": n(ks[10], (DEPTH, 2, GLA_GATE_RANK, B_QK), GLA_GATE_RANK ** -0.5),
        "gla_b_gate": n(ks[11], (DEPTH, 2, B_QK), 0.1),
        "gla_norm": 1.0 + n(ks[12], (DEPTH, GLA_DV), 0.02),
        "na_rpb": n(ks[13], (DEPTH, NA_HEADS, 2 * NA_KH - 1, 2 * NA_KW - 1), 0.02),
        "w_br_attn": n(ks[14], (DEPTH, A_Q, D), DEEPNORM_BETA * A_Q ** -0.5),
        "w_br_gla": n(ks[15], (DEPTH, B_V, D), DEEPNORM_BETA * B_V ** -0.5),
        "w_br_na": n(ks[16], (DEPTH, C_W, D), DEEPNORM_BETA * C_W ** -0.5),
        "w_out": n(ks[17], (DEPTH, D, D), DEEPNORM_BETA * D ** -0.5),
        "ln1_g": 1.0 + n(ks[18], (DEPTH, D), 0.02),
        "ln1_b": n(ks[19], (DEPTH, D), 0.01),
        "w_router_group": n(ks[20], (DEPTH, D, N_GROUPS), D ** -0.5),
        "b_router_group": n(ks[21], (DEPTH, N_GROUPS), 0.01),
        "w_router_expert": n(ks[22], (DEPTH, D, N_EXPERTS), D ** -0.5),
        "b_router_expert": n(ks[23], (DEPTH, N_EXPERTS), 0.01),
        "moe_w1": n(ks[24], (DEPTH, N_EXPERTS, D, D_EXPERT), D ** -0.5),
        "moe_w3": n(ks[25], (DEPTH, N_EXPERTS, D, D_EXPERT), D ** -0.5),
        "moe_w2": n(ks[26], (DEPTH, N_EXPERTS, D_EXPERT, D), DEEPNORM_BETA * D_EXPERT ** -0.5),
        "ln2_g": 1.0 + n(ks[27], (DEPTH, D), 0.02),
        "ln2_b": n(ks[28], (DEPTH, D), 0.01),
    }


def reference(x, c, ctx, c_ctx, w_mod, b_mod, w_in, b_in, attn_q_norm, attn_k_norm, gla_w_gate, gla_b_gate,
              gla_norm, na_rpb, w_br_attn, w_br_gla, w_br_na, w_out, ln1_g, ln1_b, w_router_group,
              b_router_group, w_router_expert, b_router_expert, moe_w1, moe_w3, moe_w2, ln2_g, ln2_b):
    B, L, D = x.shape
    Lc = ctx.shape[1]
    silu_c = jax.nn.silu(c)
    silu_cc = jax.nn.silu(c_ctx)
    x_ctx = ctx
    for l in range(DEPTH):
        last = l == DEPTH - 1
        sh1, sc1, g1, sh2, sc2, g2 = jnp.split((silu_c @ w_mod[l] + b_mod[l])[:, None, :], 6, axis=-1)
        csh1, csc1, cg1, csh2, csc2, cg2 = jnp.split(silu_cc @ w_mod[l] + b_mod[l], 6, axis=-1)

        y_lat, y_ctx = token_mixers(modulate(x, sh1, sc1), modulate(x_ctx, csh1, csc1), w_in[l], b_in[l],
                                    attn_q_norm[l], attn_k_norm[l], gla_w_gate[l], gla_b_gate[l], gla_norm[l],
                                    na_rpb[l], w_br_attn[l], w_br_gla[l], w_br_na[l], w_out[l], not last)
        x = layer_norm(DEEPNORM_ALPHA * x + g1 * y_lat, ln1_g[l], ln1_b[l])

        h_lat = modulate(x, sh2, sc2).reshape(B * L, D)
        if last:
            tokens = h_lat
        else:
            x_ctx = layer_norm(DEEPNORM_ALPHA * x_ctx + cg1 * y_ctx, ln1_g[l], ln1_b[l])
            tokens = jnp.concatenate([h_lat, modulate(x_ctx, csh2, csc2).reshape(B * Lc, D)], axis=0)
        y = hier_moe(tokens, w_router_group[l], b_router_group[l], w_router_expert[l], b_router_expert[l],
                     moe_w1[l], moe_w3[l], moe_w2[l])
        x = layer_norm(DEEPNORM_ALPHA * x + g2 * y[:B * L].reshape(B, L, D), ln2_g[l], ln2_b[l])
        if not last:
            x_ctx = layer_norm(DEEPNORM_ALPHA * x_ctx + cg2 * y[B * L:].reshape(B, Lc, D), ln2_g[l], ln2_b[l])
    return x
```

```python
import os
import numpy as np
import ml_dtypes
import concourse.bass as bass
import concourse.mybir as mybir
from concourse.bass_utils import run_bass_kernel_spmd

F32 = mybir.dt.float32
BF16 = mybir.dt.bfloat16
I32 = mybir.dt.int32
AF = mybir.ActivationFunctionType
ALU = mybir.AluOpType
AX = mybir.AxisListType


class Res:
    __slots__ = ("name", "w", "r")

    def __init__(self, name=""):
        self.name = name
        self.w = None
        self.r = {}


class Buf:
    def __init__(self, t, nres=1, name=""):
        self.t = t
        self.rs = [Res(f"{name}{i}") for i in range(nres)]

    @property
    def r(self):
        return self.rs[0]

    def __getitem__(self, k):
        return self.t[k]


class Prog:
    ENGS = ("pe", "act", "dve", "pool", "sp")
    SAME_SYNC = {"pe": False, "act": True, "dve": True, "pool": True, "sp": False}

    def __init__(self, nc, n_dma_sems=48):
        self.nc = nc
        self.lists = {k: [] for k in self.ENGS}
        self.semobj = {}
        self.cnt = {}
        for k in self.ENGS:
            self.semobj[k] = nc.alloc_semaphore(f"sem_{k}")
            self.cnt[k] = 0
        self.seen = {k: {} for k in self.ENGS}
        self.nd = n_dma_sems
        self.dval = [0] * n_dma_sems
        for i in range(n_dma_sems):
            self.semobj[f"d{i}"] = nc.alloc_semaphore(f"sem_d{i}")
        self.dnext = 0
        self.dnext_sw = 0
        self.n_ops = 0
        self.arena0, self.arena1 = nc.bump_sbuf(212000)
        self.sb_off = self.arena0
        self.sb_peak = self.arena0
        self.n_alloc = 0

    def sbuf(self, name, shape, dtype, nres=1):
        esz = {F32: 4, BF16: 2, I32: 4}[dtype]
        nb = esz
        for d in shape[1:]:
            nb *= d
        nb = (nb + 31) // 32 * 32
        assert self.sb_off + nb <= self.arena1, f"SBUF arena overflow at {name}: {self.sb_off + nb - self.arena0}"
        self.n_alloc += 1
        t = self.nc.alloc_sbuf_tensor_at(f"s{self.n_alloc}_{name}", list(shape), dtype, offset=self.sb_off)
        self.sb_off += nb
        self.sb_peak = max(self.sb_peak, self.sb_off)
        return Buf(t, nres, name)

    def mark(self):
        return self.sb_off

    def release(self, mark):
        self.barrier()
        self.sb_off = mark

    def barrier(self):
        ev = [(k, self.cnt[k]) for k in self.ENGS if self.cnt[k] > 0]
        ev += [(f"d{i}", self.dval[i]) for i in range(self.nd) if self.dval[i] > 0]
        if "cc" in self.semobj:
            ev.append(("cc", self.ccval))
        for k in self.ENGS:
            self._wait(k, ev)

    def psum(self, name, shape, dtype=F32, nres=1):
        return Buf(self.nc.alloc_psum_tensor("p_" + name, list(shape), dtype), nres, name)

    def dram(self, name, shape, dtype, kind="Internal", nres=1):
        return Buf(self.nc.dram_tensor(name, list(shape), dtype, kind=kind), nres, name)

    def _wait(self, eng, deps):
        for key, val in deps:
            if key == eng and not self.SAME_SYNC[eng]:
                continue
            if self.seen[eng].get(key, 0) >= val:
                continue
            self.seen[eng][key] = val
            self.lists[eng].append(("w", key, val))

    @staticmethod
    def _deps(reads, writes):
        deps = []
        for r in reads:
            if r.w is not None:
                deps.append(r.w)
        for w in writes:
            if w.w is not None:
                deps.append(w.w)
            deps.extend(w.r.items())
        return deps

    @staticmethod
    def _commit(ev, reads, writes):
        for r in reads:
            if r.r.get(ev[0], 0) < ev[1]:
                r.r[ev[0]] = ev[1]
        for w in writes:
            w.w = ev
            w.r = {}

    def op(self, eng, fn, reads=(), writes=()):
        self._wait(eng, self._deps(reads, writes))
        self.cnt[eng] += 1
        ev = (eng, self.cnt[eng])
        self.lists[eng].append(("o", fn, eng, 1))
        self._commit(ev, reads, writes)
        self.n_ops += 1
        return ev

    def dma(self, q, fn, reads=(), writes=()):
        half = self.nd // 2
        if q == "pool":
            i = half + self.dnext_sw
            self.dnext_sw = (self.dnext_sw + 1) % (self.nd - half)
        else:
            i = self.dnext
            self.dnext = (self.dnext + 1) % half
        key = f"d{i}"
        deps = self._deps(reads, writes)
        if self.dval[i] > 0:
            deps.append((key, self.dval[i]))
        self._wait(q, deps)
        self.dval[i] += 16
        ev = (key, self.dval[i])
        self.lists[q].append(("o", fn, key, 16))
        self._commit(ev, reads, writes)
        self.n_ops += 1
        return ev

    def coll(self, fn, reads=(), writes=()):
        if "cc" not in self.semobj:
            self.semobj["cc"] = self.nc.alloc_semaphore("sem_cc")
            self.ccval = 0
        deps = self._deps(reads, writes)
        if self.ccval > 0:
            deps.append(("cc", self.ccval))
        self._wait("pool", deps)
        self.ccval += 1
        ev = ("cc", self.ccval)
        self.lists["pool"].append(("o", fn, "cc", 1))
        self._commit(ev, reads, writes)
        return ev

    def dma_copy(self, q, out, in_, reads=(), writes=(), **kw):
        return self.dma(q, lambda e: e.dma_start(out=out, in_=in_, **kw), reads, writes)

    def finalize(self):
        final = [(k, self.cnt[k]) for k in self.ENGS if self.cnt[k] > 0 and k != "sp"]
        final += [(f"d{i}", self.dval[i]) for i in range(self.nd) if self.dval[i] > 0]
        if "cc" in self.semobj:
            final.append(("cc", self.ccval))
        self._wait("sp", final)
        nc = self.nc
        lists = self.lists
        semobj = self.semobj

        def run(e, items):
            for it in items:
                if it[0] == "w":
                    e.wait_ge(semobj[it[1]], it[2])
                else:
                    ins = it[1](e)
                    ins.then_inc(semobj[it[2]], it[3])

        with nc.Block() as block:
            @block.tensor
            def _(e):
                run(e, lists["pe"])

            @block.scalar
            def _(e):
                run(e, lists["act"])

            @block.vector
            def _(e):
                run(e, lists["dve"])

            @block.gpsimd
            def _(e):
                run(e, lists["pool"])

            @block.sync
            def _(e):
                run(e, lists["sp"])


PENG = os.environ.get('PENG', 'pool')
GM = int(os.environ.get('GM', '9'))
NT = 18
NTOK = 2304
BLKS = [(0, 512), (512, 512), (1024, 512), (1536, 512), (2048, 256)]
LN_EPS = 1e-6
RMS_EPS = 1e-6

C_AQ, C_AK, C_AV, C_BQ, C_BK, C_BV, C_BR, C_BA, C_CQ, C_CK, C_CV, C_G = (
    0, 512, 640, 768, 1024, 1280, 1792, 2304, 2336, 2848, 3360, 3872)


class Ctx:
    pass


def mk_helpers(P):
    H = Ctx()

    def MM(out, lhsT, rhs, start, stop, reads, writes):
        P.op("pe", lambda e: e.matmul(out, lhsT=lhsT, rhs=rhs, start=start, stop=stop), reads, writes)

    def TR(out, in_, ident, reads, writes):
        P.op("pe", lambda e: e.transpose(out, in_, ident), reads, writes)

    def ACT(out, in_, func, reads, writes, bias=None, scale=None):
        kw = {}
        if bias is not None:
            kw["bias"] = bias
        if scale is not None:
            kw["scale"] = scale
        P.op("act", lambda e: e.activation(out=out, in_=in_, func=func, **kw), reads, writes)

    def TT(eng, out, in0, in1, op, reads, writes):
        P.op(eng, lambda e: e.tensor_tensor(out=out, in0=in0, in1=in1, op=op), reads, writes)

    def TS(eng, out, in0, s1, s2, op0, op1, reads, writes):
        if op1 is None:
            P.op(eng, lambda e: e.tensor_scalar(out=out, in0=in0, scalar1=s1, scalar2=None, op0=op0), reads, writes)
        else:
            P.op(eng, lambda e: e.tensor_scalar(out=out, in0=in0, scalar1=s1, scalar2=s2, op0=op0, op1=op1), reads, writes)

    def STT(eng, out, in0, scalar, in1, op0, op1, reads, writes):
        P.op(eng, lambda e: e.scalar_tensor_tensor(out=out, in0=in0, scalar=scalar, in1=in1, op0=op0, op1=op1), reads, writes)

    def CP(eng, out, in_, reads, writes):
        P.op(eng, lambda e: e.tensor_copy(out=out, in_=in_), reads, writes)

    def MS(eng, ap, val, writes):
        P.op(eng, lambda e: e.memset(ap, val), (), writes)

    H.MM, H.TR, H.ACT, H.TT, H.TS, H.STT, H.CP, H.MS = MM, TR, ACT, TT, TS, STT, CP, MS
    return H


class Banks:
    def __init__(self, P, n=8, bufs=None):
        self.b = list(bufs) if bufs is not None else [P.psum(f"bank{i}", [128, 512], F32) for i in range(n)]
        self.i = 0
        self.n = len(self.b)

    def get(self):
        b = self.b[self.i]
        self.i = (self.i + 1) % self.n
        return b


class Ring:
    def __init__(self, bufs):
        self.bufs = bufs
        self.i = 0

    def get(self):
        b = self.bufs[self.i]
        self.i = (self.i + 1) % len(self.bufs)
        return b


def tile_res(buf, t0, n):
    return [buf.rs[i] for i in range(t0 // 128, (t0 + n + 127) // 128)]


ALPHA = 4 ** 0.25
NKT = 34
NBLK = 68
NSLOT = NBLK * 128
BIG = 1.0e9
def emit_l1(P, E, L):
    nc = P.nc
    H = mk_helpers(P)
    MM, TR, ACT, TT, TS, STT, CP, MS = H.MM, H.TR, H.ACT, H.TT, H.TS, H.STT, H.CP, H.MS
    din = lambda name, shape, dt=F32: E.din(name, shape, dt, L)
    dout = lambda name, shape, dt: E.dout(name, shape, dt, L)
    m_stage = P.mark()

    x_in = din("x_in", [NTOK, 1024])
    cvec = din("cvec", [2, 1024])
    w_mod = din("w_mod", [1024, 6144])
    b_mod = din("b_mod", [1, 6144])
    w_in = din("w_in", [1024, 6944])
    b_in = din("b_in", [1, 6944])
    qn_g = din("qn_g", [64, 1])
    kn_g = din("kn_g", [64, 1])
    wgate_d = din("wgate", [2, 16, 256])
    bgate_d = din("bgate", [2, 256])
    cos_d = din("cosT", [128, NTOK])
    sin_d = din("sinT", [128, NTOK])
    ident_d = din("ident", [128, 128])
    rot_d = din("rotM", [128, 128])
    oblk_d = din("onesblk", [128, 128])

    gT = dout("gT", [3072, NTOK], BF16)
    rT = dout("rT", [512, NTOK], BF16)
    cqT = dout("cqT", [512, NTOK], BF16)
    ckT = dout("ckT", [512, NTOK], BF16)
    cv = dout("cv", [NTOK, 512], BF16)
    bqT = dout("bqT", [256, NTOK], F32)
    bkT = dout("bkT", [256, NTOK], F32)
    bk = dout("bk", [NTOK, 256], F32)
    bv = dout("bv", [NTOK, 512], BF16)
    Gd = dout("Gd", [NTOK, 2, 256], F32)
    aqT = dout("aqT", [512, NTOK], BF16)
    akT = dout("akT", [128, NTOK], BF16)
    av = dout("av", [NTOK, 128], BF16)
    modD = dout("modD", [2, 6144], F32)

    banks = Banks(P, 0, E.fb)
    pT = E.bb

    ident = P.sbuf("ident", [128, 128], BF16)
    P.dma_copy("pool", ident[:], ident_d.t[:, :], writes=[ident.r])
    oblk = P.sbuf("oblk", [128, 128], BF16)
    P.dma_copy("pool", oblk[:], oblk_d.t[:, :], writes=[oblk.r])
    rotM = P.sbuf("rotM", [128, 128], F32)
    P.dma_copy("sp", rotM[:], rot_d.t[:, :], writes=[rotM.r])
    cosT = P.sbuf("cosT", [128, NTOK], F32)
    sinT = P.sbuf("sinT", [128, NTOK], F32)
    P.dma_copy("sp", cosT[:], cos_d.t[:, :], writes=[cosT.r])
    P.dma_copy("sp", sinT[:], sin_d.t[:, :], writes=[sinT.r])
    g8 = P.sbuf("g8", [128, 2], F32)
    for hh in range(2):
        P.dma_copy("sp", g8[hh * 64:(hh + 1) * 64, 0:1], qn_g.t[:, :], writes=[g8.r])
        P.dma_copy("sp", g8[hh * 64:(hh + 1) * 64, 1:2], kn_g.t[:, :], writes=[g8.r])
    TS("dve", g8[:], g8[:], 8.0, None, ALU.mult, None, [g8.r], [g8.r])
    wgate = P.sbuf("wgate", [16, 2, 256], BF16)
    P.dma_copy("pool", wgate[:], wgate_d.t.rearrange("d r c -> r d c"), writes=[wgate.r])
    bg_bc = P.sbuf("bg_bc", [128, 2, 256], F32)
    for d in range(2):
        P.dma_copy("sp", bg_bc[:, d, :], bgate_d.t[d:d + 1, :].partition_broadcast(128), writes=[bg_bc.r])

    epsc = P.sbuf("epsc", [128, 2], F32)
    MS("dve", epsc[:, 0:1], LN_EPS, [epsc.r])
    MS("dve", epsc[:, 1:2], 64.0 * RMS_EPS, [epsc.r])
    cT = P.sbuf("cT", [128, 8, 2], F32)
    for j in range(2):
        P.dma_copy("sp", cT[:, :, j], cvec.t[j:j + 1, :].rearrange("o (k p) -> p (o k)", p=128), writes=[cT.r],
                   allow_slow_non_contiguous=True)
    scT = P.sbuf("scT", [128, 8, 2], BF16)
    ACT(scT[:], cT[:], AF.Silu, [cT.r], [scT.r])
    bm_r = Ring([P.sbuf(f"bm{i}", [2, 512], F32) for i in range(2)])
    mr_r = Ring([P.sbuf(f"mr{i}", [2, 512], F32) for i in range(2)])
    wsl = Ring([P.sbuf(f"wsg{i}", [128, 8, 512], BF16) for i in range(2)])
    for g in range(12):
        wb = wsl.get()
        P.dma_copy("pool", wb[:], w_mod.t[:, g * 512:(g + 1) * 512].rearrange("(k p) c -> p k c", p=128),
                   writes=[wb.r])
        bm = bm_r.get()
        for j in range(2):
            P.dma_copy("sp", bm[j:j + 1, :], b_mod.t[0:1, g * 512:(g + 1) * 512], writes=[bm.r])
        ps = banks.get()
        for k in range(8):
            MM(ps[0:2, 0:512], scT[:, k, :], wb[:, k, :], k == 0, k == 7, [scT.r, wb.r], [ps.r])
        mr = mr_r.get()
        TT("dve", mr[:], ps[0:2, 0:512], bm[:], ALU.add, [ps.r, bm.r], [mr.r])
        P.dma_copy("sp", modD.t[:, g * 512:(g + 1) * 512], mr[:], reads=[mr.r], writes=[modD.r])
    modc = P.sbuf("modc", [128, 2, 6, 8], F32)
    for j in range(2):
        for m in range(2):
            P.dma_copy("sp", modc[:, j, m, :],
                       modD.t[j:j + 1, m * 1024:(m + 1) * 1024].rearrange("o (k p) -> p (o k)", p=128),
                       reads=[modD.r], writes=[modc.r], allow_slow_non_contiguous=True)
    TS("dve", modc[:, :, 1, :], modc[:, :, 1, :], 1.0, None, ALU.add, None, [modc.r], [modc.r])

    hT = P.sbuf("hT", [128, 8, NTOK], BF16, nres=NT)
    xin = Ring([P.sbuf(f"xin{i}", [128, 1024], F32) for i in range(2)])
    xnr = Ring([P.sbuf(f"xn{i}", [128, 1024], BF16) for i in range(2)])
    str_ = Ring([P.sbuf(f"st{i}", [128, 2, 6], F32) for i in range(2)])
    mvr = Ring([P.sbuf(f"mv{i}", [128, 2], F32) for i in range(2)])
    for i in range(NT):
        xt = xin.get()
        P.dma_copy("sp", xt[:], x_in.t[i * 128:(i + 1) * 128, :], writes=[xt.r])
        st = str_.get()
        for c in range(2):
            P.op("dve", lambda e, st=st, xt=xt, c=c: e.bn_stats(out=st[:, c, :], in_=xt[:, c * 512:(c + 1) * 512]),
                 [xt.r], [st.r])
        mv = mvr.get()
        P.op("dve", lambda e, st=st, mv=mv: e.bn_aggr(out=mv[:], in_=st[:].rearrange("p a b -> p (a b)")),
             [st.r], [mv.r])
        ACT(mv[:, 1:2], mv[:, 1:2], AF.Sqrt, [mv.r], [mv.r], bias=epsc[:, 0:1])
        P.op("dve", lambda e, mv=mv: e.reciprocal(out=mv[:, 1:2], in_=mv[:, 1:2]), [mv.r], [mv.r])
        xn = xnr.get()
        TS("dve", xn[:], xt[:], mv[:, 0:1], mv[:, 1:2], ALU.subtract, ALU.mult, [xt.r, mv.r], [xn.r])
        for k in range(8):
            TR(pT[:, k, :], xn[:, k * 128:(k + 1) * 128], ident[:], [xn.r, ident.r], [pT.r])
        j = 0 if i < 16 else 1
        for k in range(8):
            o = hT[:, k, i * 128:(i + 1) * 128]
            if k % 2 == 0:
                ACT(o, pT[:, k, :], AF.Identity, [pT.r, modc.r], [hT.rs[i]],
                    bias=modc[:, j, 0, k:k + 1], scale=modc[:, j, 1, k:k + 1])
            else:
                TS("dve", o, pT[:, k, :], modc[:, j, 1, k:k + 1], modc[:, j, 0, k:k + 1], ALU.mult, ALU.add,
                   [pT.r, modc.r], [hT.rs[i]])

    bcols = P.sbuf("bcols", [128, 48], F32)
    bcol_idx = {}
    nb = 0
    for (c0, ng) in [(C_AQ, 4), (C_AK, 1), (C_BQ, 2), (C_BK, 2), (C_BR, 4), (C_CQ, 4), (C_CK, 4), (C_G, 24)]:
        P.dma_copy("sp", bcols[:, nb:nb + ng],
                   b_in.t[0:1, c0:c0 + ng * 128].rearrange("o (g p) -> p (o g)", p=128),
                   writes=[bcols.r], allow_slow_non_contiguous=True)
        for g in range(ng):
            bcol_idx[c0 + g * 128] = nb + g
        nb += ng
    bacol = P.sbuf("bacol", [16, 2], F32)
    P.dma_copy("sp", bacol[:], b_in.t[0:1, C_BA:C_BA + 32].rearrange("o (d p) -> p (o d)", p=16),
               writes=[bacol.r], allow_slow_non_contiguous=True)
    bias_bc = P.sbuf("bias_bc", [128, 1408], F32)
    tm_groups = [(C_AV, 128, 0), (C_BK, 256, 128), (C_BV, 512, 384), (C_CV, 512, 896)]
    for (c0, n, o) in tm_groups:
        P.dma_copy("sp", bias_bc[:, o:o + n], b_in.t[0:1, c0:c0 + n].partition_broadcast(128), writes=[bias_bc.r])

    mark1b = P.mark()
    stg_bf = Ring([P.sbuf(f"stgbf{i}", [128, NTOK], BF16) for i in range(3)])
    stg_f = Ring([P.sbuf(f"stgf{i}", [128, NTOK], F32) for i in range(2)])
    aux = {n: Ring([P.sbuf(f"{n}{i}", [128, 512], dt) for i in range(2)])
           for n, dt in [("zq", F32), ("sq", BF16), ("rs", F32), ("qn", F32), ("t1", F32), ("t2", F32)]}
    stm_bf = Ring([P.sbuf(f"stmbf{i}", [128, 512], BF16) for i in range(3)])
    stm_f = Ring([P.sbuf(f"stmf{i}", [128, 256], F32) for i in range(2)])
    aT = P.sbuf("aT", [16, 2, NTOK], BF16)

    def load_w(c0, n):
        wb = wsl.get()
        P.dma_copy("pool", wb[:, :, 0:n], w_in.t[:, c0:c0 + n].rearrange("(k p) c -> p k c", p=128),
                   writes=[wb.r])
        return wb

    def fm_mm(wb, off, m, t0, n):
        ps = banks.get()
        rd = [wb.r] + tile_res(hT, t0, n)
        for k in range(8):
            MM(ps[0:m, 0:n], wb[:, k, off:off + m], hT[:, k, t0:t0 + n], k == 0, k == 7, rd, [ps.r])
        return ps

    def fm_simple(wb, off, c0, func, dst, row0, f32=False, scale=None):
        stg = stg_f.get() if f32 else stg_bf.get()
        bc = bcols[:, bcol_idx[c0]:bcol_idx[c0] + 1]
        for (t0, n) in BLKS:
            ps = fm_mm(wb, off, 128, t0, n)
            ACT(stg[:, t0:t0 + n], ps[:, 0:n], func, [ps.r, bcols.r], [stg.r], bias=bc)
        P.dma_copy("sp", dst.t[row0:row0 + 128, :], stg[:], reads=[stg.r], writes=[dst.r])

    def fm_qk(wb, off, c0, gcol, dst, row0):
        stg = stg_bf.get()
        bc = bcols[:, bcol_idx[c0]:bcol_idx[c0] + 1]
        for (t0, n) in BLKS:
            ps = fm_mm(wb, off, 128, t0, n)
            zq, sq, rs, qn, t1, t2 = (aux[k].get() for k in ("zq", "sq", "rs", "qn", "t1", "t2"))
            ACT(zq[:, 0:n], ps[:, 0:n], AF.Identity, [ps.r, bcols.r], [zq.r], bias=bc)
            ACT(sq[:, 0:n], ps[:, 0:n], AF.Square, [ps.r, bcols.r], [sq.r], bias=bc)
            ss = banks.get()
            MM(ss[:, 0:n], oblk[:], sq[:, 0:n], True, True, [oblk.r, sq.r], [ss.r])
            ACT(rs[:, 0:n], ss[:, 0:n], AF.Sqrt, [ss.r], [rs.r], bias=epsc[:, 1:2])
            P.op("dve", lambda e, rs=rs, n=n: e.reciprocal(out=rs[:, 0:n], in_=rs[:, 0:n]), [rs.r], [rs.r])
            STT("dve", qn[:, 0:n], zq[:, 0:n], g8[:, gcol:gcol + 1], rs[:, 0:n], ALU.mult, ALU.mult,
                [zq.r, g8.r, rs.r], [qn.r])
            rot = banks.get()
            MM(rot[:, 0:n], rotM[:], qn[:, 0:n], True, True, [rotM.r, qn.r], [rot.r])
            TT("pool", t1[:, 0:n], qn[:, 0:n], cosT[:, t0:t0 + n], ALU.mult, [qn.r, cosT.r], [t1.r])
            TT("dve", t2[:, 0:n], rot[:, 0:n], sinT[:, t0:t0 + n], ALU.mult, [rot.r, sinT.r], [t2.r])
            TT("dve", stg[:, t0:t0 + n], t1[:, 0:n], t2[:, 0:n], ALU.add, [t1.r, t2.r], [stg.r])
        P.dma_copy("sp", dst.t[row0:row0 + 128, :], stg[:], reads=[stg.r], writes=[dst.r])

    def tm_group(wb, off, n, bo, dst, f32=False):
        for i in range(NT):
            ps = banks.get()
            rd = [wb.r, hT.rs[i]]
            for k in range(8):
                MM(ps[:, 0:n], hT[:, k, i * 128:(i + 1) * 128], wb[:, k, off:off + n], k == 0, k == 7, rd, [ps.r])
            stg = stm_f.get() if f32 else stm_bf.get()
            TT("dve", stg[:, 0:n], ps[:, 0:n], bias_bc[:, bo:bo + n], ALU.add, [ps.r, bias_bc.r], [stg.r])
            P.dma_copy("sp", dst.t[i * 128:(i + 1) * 128, :], stg[:, 0:n], reads=[stg.r], writes=[dst.r])

    wb = load_w(C_AQ, 512)
    for s in range(4):
        fm_qk(wb, s * 128, C_AQ + s * 128, 0, aqT, s * 128)
    wb = load_w(C_AK, 256)
    fm_qk(wb, 0, C_AK, 1, akT, 0)
    tm_group(wb, 128, 128, 0, av)
    wb = load_w(C_BQ, 512)
    for s in range(2):
        fm_simple(wb, s * 128, C_BQ + s * 128, AF.Identity, bqT, s * 128, f32=True)
    for s in range(2):
        fm_simple(wb, 256 + s * 128, C_BK + s * 128, AF.Identity, bkT, s * 128, f32=True)
    tm_group(wb, 256, 256, 128, bk, f32=True)
    wb = load_w(C_BV, 512)
    tm_group(wb, 0, 512, 384, bv)
    wb = load_w(C_BR, 512)
    for s in range(4):
        fm_simple(wb, s * 128, C_BR + s * 128, AF.Silu, rT, s * 128)
    wb = load_w(C_BA, 32)
    for d in range(2):
        for (t0, n) in BLKS:
            ps = fm_mm(wb, d * 16, 16, t0, n)
            ACT(aT[0:16, d, t0:t0 + n], ps[0:16, 0:n], AF.Identity, [ps.r, bacol.r], [aT.r], bias=bacol[:, d:d + 1])
    tg_r = Ring([P.sbuf(f"tg{i}", [128, 256], F32) for i in range(2)])
    te_r = Ring([P.sbuf(f"te{i}", [128, 256], F32) for i in range(2)])
    gst_r = Ring([P.sbuf(f"gst{i}", [128, 2, 256], F32) for i in range(2)])
    for i in range(NT):
        gst = gst_r.get()
        for d in range(2):
            ps = banks.get()
            MM(ps[:, 0:256], aT[0:16, d, i * 128:(i + 1) * 128], wgate[0:16, d, :], True, True,
               [aT.r, wgate.r], [ps.r])
            tg = tg_r.get()
            te = te_r.get()
            TT("dve", tg[:], ps[:, 0:256], bg_bc[:, d, :], ALU.add, [ps.r, bg_bc.r], [tg.r])
            ACT(te[:], tg[:], AF.Exp, [tg.r], [te.r], scale=-1.0)
            ACT(gst[:, d, :], te[:], AF.Ln, [te.r], [gst.r], bias=1.0)
        P.dma_copy("sp", Gd.t[i * 128:(i + 1) * 128, :, :], gst[:], reads=[gst.r], writes=[Gd.r])
    wb = load_w(C_CQ, 512)
    for s in range(4):
        fm_simple(wb, s * 128, C_CQ + s * 128, AF.Identity, cqT, s * 128)
    wb = load_w(C_CK, 512)
    for s in range(4):
        fm_simple(wb, s * 128, C_CK + s * 128, AF.Identity, ckT, s * 128)
    wb = load_w(C_CV, 512)
    tm_group(wb, 0, 512, 896, cv)
    for gsup in range(6):
        wb = load_w(C_G + gsup * 512, 512)
        for s in range(4):
            fm_simple(wb, s * 128, C_G + gsup * 512 + s * 128, AF.Sigmoid, gT, gsup * 512 + s * 128)

    P.release(mark1b)
    tri_d = {n: din(n, [128, 128]) for n in ("triInc", "triDec", "triSgt", "triSlt")}
    flags_d = din("flags", [64, 2])
    Og = dout("Og", [2, 512, NTOK], F32)
    qBT = dout("qBT", [2, 256, 2048], BF16)
    Sfin = dout("Sfin", [2, 64, 512], F32)
    tri = {}
    for n in tri_d:
        tri[n] = P.sbuf("c_" + n, [128, 128], F32)
        P.dma_copy("sp", tri[n][:], tri_d[n].t[:, :], writes=[tri[n].r])
    flags = P.sbuf("flags", [64, 2], F32)
    P.dma_copy("sp", flags[:], flags_d.t[:, :], writes=[flags.r])
    Sst = [P.sbuf(f"S{d}", [64, 4, 128], F32) for d in range(2)]
    Sbf = [P.sbuf(f"Sbf{d}", [64, 4, 128], BF16) for d in range(2)]
    Dcum = [P.sbuf(f"Dcum{d}", [64, 4], F32) for d in range(2)]
    rq = [Ring([P.sbuf(f"gq{d}{i}", [64, 4, 128], F32) for i in range(2)]) for d in range(2)]
    rk = [Ring([P.sbuf(f"gk{d}{i}", [64, 4, 128], F32) for i in range(2)]) for d in range(2)]
    rkt = [Ring([P.sbuf(f"gkt{d}{i}", [128, 256], F32) for i in range(2)]) for d in range(2)]
    rv = [Ring([P.sbuf(f"gv{d}{i}", [128, 512], BF16) for i in range(2)]) for d in range(2)]
    rG = [Ring([P.sbuf(f"gG{d}{i}", [128, 256], F32) for i in range(2)]) for d in range(2)]
    reb = [Ring([P.sbuf(f"geb{d}{i}", [64, 4, 128], F32) for i in range(2)]) for d in range(2)]
    rei = [Ring([P.sbuf(f"gei{d}{i}", [64, 4, 128], F32) for i in range(2)]) for d in range(2)]
    rqb = [Ring([P.sbuf(f"gqb{d}{i}", [64, 4, 128], BF16) for i in range(2)]) for d in range(2)]
    rkb = [Ring([P.sbuf(f"gkb{d}{i}", [64, 4, 128], BF16) for i in range(2)]) for d in range(2)]
    rkr = [Ring([P.sbuf(f"gkr{d}{i}", [128, 256], F32) for i in range(2)]) for d in range(2)]
    rke = [Ring([P.sbuf(f"gke{d}{i}", [128, 256], BF16) for i in range(2)]) for d in range(2)]
    rsc = [Ring([P.sbuf(f"gsc{d}{i}", [128, 4, 128], BF16) for i in range(2)]) for d in range(2)]
    rO = [Ring([P.sbuf(f"gO{d}{i}", [128, 4, 128], F32) for i in range(2)]) for d in range(2)]
    rqB = [Ring([P.sbuf(f"gqB{d}{i}", [64, 4, 128], BF16) for i in range(2)]) for d in range(2)]

    def gla_step(d, i, lat):
        cumM = tri["triInc"] if d == 0 else tri["triDec"]
        remM = tri["triSgt"] if d == 0 else tri["triSlt"]
        endc = 127 if d == 0 else 0
        t0 = i * 128
        S, Sb, Dc = Sst[d], Sbf[d], Dcum[d]
        q_t, k_t, kt_t, v_t, G_t = rq[d].get(), rk[d].get(), rkt[d].get(), rv[d].get(), rG[d].get()
        P.dma_copy("sp", q_t[:], bqT.t[:, t0:t0 + 128].rearrange("(h p) t -> p h t", p=64), reads=[bqT.r], writes=[q_t.r])
        P.dma_copy("sp", k_t[:], bkT.t[:, t0:t0 + 128].rearrange("(h p) t -> p h t", p=64), reads=[bkT.r], writes=[k_t.r])
        P.dma_copy("sp", kt_t[:], bk.t[t0:t0 + 128, :], reads=[bk.r], writes=[kt_t.r])
        P.dma_copy("sp", v_t[:], bv.t[t0:t0 + 128, :], reads=[bv.r], writes=[v_t.r])
        P.dma_copy("sp", G_t[:], Gd.t[t0:t0 + 128, d, :], reads=[Gd.r], writes=[G_t.r])
        cps = banks.get()
        for h in range(4):
            MM(cps[0:64, h * 128:(h + 1) * 128], G_t[:, h * 64:(h + 1) * 64], cumM[:], True, True, [G_t.r, cumM.r], [cps.r])
        eb, ei = reb[d].get(), rei[d].get()
        ACT(eb[:].rearrange("p a t -> p (a t)"), cps[0:64, :], AF.Exp, [cps.r], [eb.r], scale=-1.0 / 16)
        ACT(ei[:].rearrange("p a t -> p (a t)"), cps[0:64, :], AF.Exp, [cps.r], [ei.r], scale=1.0 / 16)
        qb, kb = rqb[d].get(), rkb[d].get()
        STT("dve", qb[:], q_t[:], 0.125, eb[:], ALU.mult, ALU.mult, [q_t.r, eb.r], [qb.r])
        TT(PENG, kb[:], k_t[:], ei[:], ALU.mult, [k_t.r, ei.r], [kb.r])
        rps = banks.get()
        MM(rps[:, 0:256], remM[:], G_t[:], True, True, [remM.r, G_t.r], [rps.r])
        kr = rkr[d].get()
        ACT(kr[:], rps[:, 0:256], AF.Exp, [rps.r], [kr.r], scale=-1.0 / 16)
        ke = rke[d].get()
        TT(PENG, ke[:], kt_t[:], kr[:], ALU.mult, [kt_t.r, kr.r], [ke.r])
        sps = banks.get()
        for h in range(4):
            MM(sps[:, h * 128:(h + 1) * 128], kb[:, h, :], qb[:, h, :], True, True, [kb.r, qb.r], [sps.r])
        sc = rsc[d].get()
        TT("dve", sc[:], sps[:].rearrange("p (h t) -> p h t", h=4),
           cumM[:].unsqueeze(1).to_broadcast([128, 4, 128]), ALU.mult, [sps.r, cumM.r], [sc.r])
        ops_ = banks.get()
        for h in range(4):
            MM(ops_[:, h * 128:(h + 1) * 128], Sb[:, h, :], qb[:, h, :], True, False, [Sb.r, qb.r], [ops_.r])
            MM(ops_[:, h * 128:(h + 1) * 128], v_t[:, h * 128:(h + 1) * 128], sc[:, h, :],
               False, True, [v_t.r, sc.r], [ops_.r])
        Ot = rO[d].get()
        ACT(Ot[:].rearrange("p h t -> p (h t)"), ops_[:, :], AF.Identity, [ops_.r], [Ot.r])
        P.dma_copy("sp", Og.t[d, :, t0:t0 + 128].rearrange("(h p) t -> p h t", p=128), Ot[:], reads=[Ot.r], writes=[Og.r])
        if lat:
            qB = rqB[d].get()
            TT(PENG, qB[:], qb[:], Dc[:].unsqueeze(2).to_broadcast([64, 4, 128]), ALU.mult, [qb.r, Dc.r], [qB.r])
            P.dma_copy("sp", qBT.t[d, :, t0:t0 + 128].rearrange("(h p) t -> p h t", p=64), qB[:], reads=[qB.r], writes=[qBT.r])
            TT("dve", Dc[:], Dc[:], eb[:, :, endc], ALU.mult, [Dc.r, eb.r], [Dc.r])
        ups = banks.get()
        for h in range(4):
            MM(ups[0:64, h * 128:(h + 1) * 128], ke[:, h * 64:(h + 1) * 64], v_t[:, h * 128:(h + 1) * 128], True, True,
               [ke.r, v_t.r], [ups.r])
        TT("dve", S[:], S[:], eb[:, :, endc:endc + 1].to_broadcast([64, 4, 128]), ALU.mult, [S.r, eb.r], [S.r])
        TT("dve", S[:], S[:], ups[0:64, :].rearrange("p (h e) -> p h e", h=4), ALU.add, [S.r, ups.r], [S.r])
        CP(PENG, Sb[:], S[:], [S.r], [Sb.r])

    for d in range(2):
        MS("dve", Sst[d][:], 0.0, [Sst[d].r])
        MS(PENG, Sbf[d][:], 0.0, [Sbf[d].r])
        MS("dve", Dcum[d][:], 1.0, [Dcum[d].r])
    for j in range(2 if GM > 0 else 0):
        gla_step(0, 16 + j, False)
        gla_step(1, 17 - j, False)
    for d in range(2):
        TS("dve", Sst[d][:], Sst[d][:], flags[:, d:d + 1], None, ALU.mult, None, [Sst[d].r, flags.r], [Sst[d].r])
        CP(PENG, Sbf[d][:], Sst[d][:], [Sst[d].r], [Sbf[d].r])
    for j in range(16 if GM > 0 else 0):
        gla_step(0, j, True)
        gla_step(1, 15 - j, True)
    for d in range(2):
        P.dma_copy("sp", Sfin.t[d, :, :], Sst[d][:].rearrange("p h e -> p (h e)"), reads=[Sst[d].r], writes=[Sfin.r])

    P.release(m_stage)


def emit_l2(P, E, L):
    nc = P.nc
    H = mk_helpers(P)
    MM, TR, ACT, TT, TS, STT, CP, MS = H.MM, H.TR, H.ACT, H.TT, H.TS, H.STT, H.CP, H.MS
    din = lambda name, shape, dt=F32: E.din(name, shape, dt, L)
    dout = lambda name, shape, dt: E.dout(name, shape, dt, L)
    m_stage = P.mark()

    x_in = din("x_in", [NTOK, 1024])
    modD = din("modD", [2, 6144])
    aqT = din("aqT", [512, NTOK], BF16)
    akT = din("akT", [128, NTOK], BF16)
    av = din("av", [NTOK, 128], BF16)
    g_ak = din("g_ak", [256, 2048], BF16)
    g_av = din("g_av", [4096, 128], BF16)
    g_ck = din("g_ck", [1024, 768], BF16)
    g_cv = din("g_cv", [1536, 512], BF16)
    g_S = din("g_S", [256, 512])
    cqT = din("cqT", [512, NTOK], BF16)
    ckT = din("ckT", [512, NTOK], BF16)
    cv = din("cv", [NTOK, 512], BF16)
    tabNA = din("tabNA", [5, 7, 2, 128, 512], BF16)
    Og = din("Og", [2, 512, NTOK])
    qBT = din("qBT", [2, 256, 2048], BF16)
    fl1m = din("fl1m", [64, 2])
    rT = din("rT", [512, NTOK], BF16)
    gT = din("gT", [3072, NTOK], BF16)
    gn_d = din("gla_norm", [128, 1])
    wba_d = din("w_br_attn", [512, 1024])
    wbg_d = din("w_br_gla", [512, 1024])
    wbn_d = din("w_br_na", [512, 1024])
    wout_d = din("w_out", [1024, 1024])
    ln1g_d = din("ln1_g", [1, 1024])
    ln1b_d = din("ln1_b", [1, 1024])
    ones_d = din("ones128", [128, 128])
    x1_o = dout("x1", [NTOK, 1024], F32)
    h2_o = dout("h2", [NTOK, 1024], F32)

    bankS = Banks(P, 0, E.fb[0:3])
    bankA = Ring(E.fb[3:5])
    bankM = Ring(E.fb[5:7])

    oaD = dout("oaD", [512, NTOK], BF16)
    ocD = dout("ocD", [512, NTOK], BF16)
    obD = dout("obD", [512, NTOK], BF16)
    stgr = Ring([P.sbuf(f"ostg{i}", [128, 512], BF16) for i in range(3)])
    epsc = P.sbuf("epsc", [128, 2], F32)
    MS("dve", epsc[:, 0:1], LN_EPS, [epsc.r])
    MS("dve", epsc[:, 1:2], RMS_EPS, [epsc.r])
    rdr = Ring([P.sbuf(f"rd{i}", [128, 512], F32) for i in range(2)])
    rd0r = Ring([P.sbuf(f"rd0{i}", [64, 512], F32) for i in range(2)])
    ptr = Ring([P.sbuf(f"pt{i}", [128, 512], BF16) for i in range(4)])

    def blk_of(t0):
        return min(t0 // 512, 4)

    def normalize(po, n, dest, dres, view=None):
        rd = rdr.get()
        P.op("dve", lambda e: e.reciprocal(out=rd[64:128, 0:n], in_=po[64:128, 0:n]), [po.r], [rd.r])
        rd0 = rd0r.get()
        P.dma_copy("sp", rd0[0:64, 0:n], rd[64:128, 0:n], reads=[rd.r], writes=[rd0.r])
        a, b_ = po[0:64, 0:n], rd0[0:64, 0:n]
        if view is not None:
            a, b_ = view(a), view(b_)
        TT("dve", dest, a, b_, ALU.mult, [po.r, rd0.r], [dres])

    markA = P.mark()
    KT = [P.sbuf(f"KT{g}", [64, NKT * 128], BF16) for g in range(2)]
    V1 = [P.sbuf(f"V1{g}", [128, NKT, 128], BF16) for g in range(2)]
    for g in range(2):
        for r_ in range(2):
            P.dma_copy("sp", KT[g][:, r_ * 2048:(r_ + 1) * 2048], g_ak.t[r_ * 128 + g * 64:r_ * 128 + (g + 1) * 64, :],
                       reads=[g_ak.r], writes=[KT[g].r])
        P.dma_copy("sp", KT[g][:, 4096:4352], akT.t[g * 64:(g + 1) * 64, 2048:2304], reads=[akT.r], writes=[KT[g].r])
        MS("pool", V1[g][:, :, 64:128], 1.0, [V1[g].r])
        P.dma_copy("sp", V1[g][:, 0:32, 0:64], g_av.t[:, g * 64:(g + 1) * 64].rearrange("(kt p) d -> p kt d", p=128),
                   reads=[g_av.r], writes=[V1[g].r])
        P.dma_copy("sp", V1[g][:, 32:34, 0:64], av.t[2048:2304, g * 64:(g + 1) * 64].rearrange("(kt p) d -> p kt d", p=128),
                   reads=[av.r], writes=[V1[g].r])
    qbr = Ring([P.sbuf(f"qblk{i}", [64, 8, 512], BF16) for i in range(2)])
    for bi, (t0, n) in enumerate(BLKS):
        qb = qbr.get()
        P.dma_copy("sp", qb[:, :, 0:n], aqT.t[:, t0:t0 + n].rearrange("(h p) t -> p h t", p=64), writes=[qb.r])
        kts = list(range(NKT)) if bi < 4 else [32, 33]
        for h in range(8):
            g = h // 4
            po = bankA.get()
            for idx, kt in enumerate(kts):
                ps = bankS.get()
                MM(ps[:, 0:n], KT[g][:, kt * 128:(kt + 1) * 128], qb[:, h, 0:n], True, True, [KT[g].r, qb.r], [ps.r])
                pt = ptr.get()
                ACT(pt[:, 0:n], ps[:, 0:n], AF.Exp, [ps.r], [pt.r], scale=0.125)
                MM(po[:, 0:n], V1[g][:, kt, :], pt[:, 0:n], idx == 0, idx == len(kts) - 1, [V1[g].r, pt.r], [po.r])
            stg = stgr.get()
            normalize(po, n, stg[0:64, 0:n], stg.r)
            P.dma_copy("sp", oaD.t[h * 64:(h + 1) * 64, t0:t0 + n], stg[0:64, 0:n], reads=[stg.r], writes=[oaD.r])
    P.release(markA)

    markN = P.mark()
    KTc = P.sbuf("KTc", [64, 8, 256], BF16)
    V1c = P.sbuf("V1c", [128, 2, 8, 128], BF16)
    P.dma_copy("sp", KTc[:], ckT.t[:, 2048:2304].rearrange("(h p) t -> p h t", p=64), writes=[KTc.r])
    MS("pool", V1c[:, :, :, 64:128], 1.0, [V1c.r])
    for kt in range(2):
        P.dma_copy("sp", V1c[:, kt, :, 0:64],
                   cv.t[2048 + kt * 128:2048 + (kt + 1) * 128, :].rearrange("p (h d) -> p h d", d=64), writes=[V1c.r])
    qtr = Ring([P.sbuf(f"nq{i}", [64, 8, 128], BF16) for i in range(2)])
    ktr = Ring([P.sbuf(f"nk{i}", [64, 8, 896], BF16) for i in range(2)])
    vwr = Ring([P.sbuf(f"nv{i}", [128, 7, 8, 128], BF16) for i in range(2)])
    for vb in vwr.bufs:
        MS("pool", vb[:, :, :, 64:128], 1.0, [vb.r])
    tbr = Ring([P.sbuf(f"ntb{i}", [128, 512], BF16) for i in range(3)])
    tmr = Ring([P.sbuf(f"ntm{i}", [128, 512], F32) for i in range(2)])
    nptr = Ring([P.sbuf(f"npt{i}", [128, 512], BF16) for i in range(18)])
    for j in range(18):
        qt = qtr.get()
        P.dma_copy("sp", qt[:], cqT.t[:, j * 128:(j + 1) * 128].rearrange("(h p) t -> p h t", p=64), writes=[qt.r])
        if j < 16:
            kw, vw = ktr.get(), vwr.get()
            for kt in range(7):
                t_ = j + kt
                if t_ < 3:
                    ksrc, kres = g_ck.t[0:512, 384 + t_ * 128:384 + (t_ + 1) * 128], g_ck.r
                    vsrc, vres = g_cv.t[384 + t_ * 128:384 + (t_ + 1) * 128, :], g_cv.r
                elif t_ < 19:
                    ksrc, kres = ckT.t[:, (t_ - 3) * 128:(t_ - 2) * 128], ckT.r
                    vsrc, vres = cv.t[(t_ - 3) * 128:(t_ - 2) * 128, :], cv.r
                else:
                    ksrc, kres = g_ck.t[512:1024, (t_ - 19) * 128:(t_ - 18) * 128], g_ck.r
                    vsrc, vres = g_cv.t[768 + (t_ - 19) * 128:768 + (t_ - 18) * 128, :], g_cv.r
                P.dma_copy("sp", kw[:, :, kt * 128:(kt + 1) * 128], ksrc.rearrange("(h p) t -> p h t", p=64),
                           reads=[kres], writes=[kw.r])
                P.dma_copy("sp", vw[:, kt, :, 0:64], vsrc.rearrange("p (h d) -> p h d", d=64), reads=[vres], writes=[vw.r])
            kts = list(range(9))
            slot = j if j < 2 else (j - 12 if j >= 14 else 4)
        else:
            kts = [7, 8]
        for grp in range(2):
            pts = {}
            for idx, kt in enumerate(kts):
                sps = bankS.get()
                for hh in range(4):
                    h = grp * 4 + hh
                    if kt < 7:
                        lhs, rd_ = kw[:, h, kt * 128:(kt + 1) * 128], kw.r
                    else:
                        lhs, rd_ = KTc[:, h, (kt - 7) * 128:(kt - 6) * 128], KTc.r
                    MM(sps[:, hh * 128:(hh + 1) * 128], lhs, qt[:, h, :], True, True, [rd_, qt.r], [sps.r])
                pt = nptr.get()
                pts[kt] = pt
                if kt < 7:
                    tb = tbr.get()
                    P.dma_copy("sp", tb[:], tabNA.t[slot, kt, grp], writes=[tb.r])
                    tm = tmr.get()
                    STT("dve", tm[:], sps[:], 0.125, tb[:], ALU.mult, ALU.add, [sps.r, tb.r], [tm.r])
                    ACT(pt[:], tm[:], AF.Exp, [tm.r], [pt.r])
                else:
                    ACT(pt[:], sps[:], AF.Exp, [sps.r], [pt.r], scale=0.125)
            po = bankA.get()
            for hh in range(4):
                h = grp * 4 + hh
                for idx, kt in enumerate(kts):
                    pt = pts[kt]
                    if kt < 7:
                        lhs, rd_ = vw[:, kt, h, :], vw.r
                    else:
                        lhs, rd_ = V1c[:, kt - 7, h, :], V1c.r
                    MM(po[:, hh * 128:(hh + 1) * 128], lhs, pt[:, hh * 128:(hh + 1) * 128], idx == 0, idx == len(kts) - 1,
                       [rd_, pt.r], [po.r])
            stg = stgr.get()
            normalize(po, 512, stg[0:64, :], stg.r)
            P.dma_copy("sp", ocD.t[grp * 256:(grp + 1) * 256, j * 128:(j + 1) * 128].rearrange("(h p) t -> p h t", p=64),
                       stg[0:64, :].rearrange("p (h t) -> p h t", h=4), reads=[stg.r], writes=[ocD.r])
    P.release(markN)

    markG = P.mark()
    ones128 = P.sbuf("ones128", [128, 128], BF16)
    P.dma_copy("pool", ones128[:], ones_d.t[:, :], writes=[ones128.r])
    gn = P.sbuf("gn", [128, 1], F32)
    P.dma_copy("sp", gn[:], gn_d.t[:, :], writes=[gn.r])
    f1 = P.sbuf("f1", [64, 2], F32)
    P.dma_copy("sp", f1[:], fl1m.t[:, :], writes=[f1.r])
    Sin = []
    for d in range(2):
        sp_ = P.sbuf(f"Spart{d}", [64, 512], F32)
        P.dma_copy("sp", sp_[:], g_S.t[d * 128 + d * 64:d * 128 + (d + 1) * 64, :], reads=[g_S.r], writes=[sp_.r])
        sb = P.sbuf(f"Sin{d}", [64, 4, 128], BF16)
        TS("dve", sb[:].rearrange("p h e -> p (h e)"), sp_[:], f1[:, d:d + 1], None, ALU.mult, None, [sp_.r, f1.r], [sb.r])
        Sin.append(sb)
    qBr = [Ring([P.sbuf(f"qB{d}{i}", [64, 4, 512], BF16) for i in range(2)]) for d in range(2)]
    ogr = [Ring([P.sbuf(f"og{d}{i}", [128, 512], F32) for i in range(2)]) for d in range(2)]
    osr = Ring([P.sbuf(f"os{i}", [128, 512], F32) for i in range(2)])
    sqr = Ring([P.sbuf(f"sq{i}", [128, 512], BF16) for i in range(2)])
    rsr = Ring([P.sbuf(f"rs{i}", [128, 512], F32) for i in range(2)])
    rtr = Ring([P.sbuf(f"rt{i}", [128, 512], BF16) for i in range(2)])
    t3r = Ring([P.sbuf(f"t3{i}", [128, 512], F32) for i in range(2)])
    for bi, (t0, n) in enumerate(BLKS):
        lat = bi < 4
        if lat:
            qB = [qBr[d].get() for d in range(2)]
            for d in range(2):
                P.dma_copy("sp", qB[d][:, :, 0:n], qBT.t[d, :, t0:t0 + n].rearrange("(h p) t -> p h t", p=64),
                           writes=[qB[d].r])
        for h in range(4):
            og = [ogr[d].get() for d in range(2)]
            for d in range(2):
                P.dma_copy("sp", og[d][:, 0:n], Og.t[d, h * 128:(h + 1) * 128, t0:t0 + n], writes=[og[d].r])
            osum = osr.get()
            TT("pool", osum[:, 0:n], og[0][:, 0:n], og[1][:, 0:n], ALU.add, [og[0].r, og[1].r], [osum.r])
            if lat:
                pc = bankM.get()
                MM(pc[:, 0:n], Sin[0][:, h, :], qB[0][:, h, 0:n], True, False, [Sin[0].r, qB[0].r], [pc.r])
                MM(pc[:, 0:n], Sin[1][:, h, :], qB[1][:, h, 0:n], False, True, [Sin[1].r, qB[1].r], [pc.r])
                TT("dve", osum[:, 0:n], osum[:, 0:n], pc[:, 0:n], ALU.add, [osum.r, pc.r], [osum.r])
            sq = sqr.get()
            ACT(sq[:, 0:n], osum[:, 0:n], AF.Square, [osum.r], [sq.r])
            ss = bankM.get()
            MM(ss[:, 0:n], ones128[:], sq[:, 0:n], True, True, [ones128.r, sq.r], [ss.r])
            rs = rsr.get()
            ACT(rs[:, 0:n], ss[:, 0:n], AF.Sqrt, [ss.r, epsc.r], [rs.r], bias=epsc[:, 1:2], scale=1.0 / 128)
            P.op("dve", lambda e, rs=rs, n=n: e.reciprocal(out=rs[:, 0:n], in_=rs[:, 0:n]), [rs.r], [rs.r])
            rt = rtr.get()
            P.dma_copy("sp", rt[:, 0:n], rT.t[h * 128:(h + 1) * 128, t0:t0 + n], writes=[rt.r])
            t3 = t3r.get()
            STT("dve", t3[:, 0:n], osum[:, 0:n], gn[:, 0:1], rs[:, 0:n], ALU.mult, ALU.mult, [osum.r, gn.r, rs.r], [t3.r])
            stg = stgr.get()
            TT("pool", stg[:, 0:n], t3[:, 0:n], rt[:, 0:n], ALU.mult, [t3.r, rt.r], [stg.r])
            P.dma_copy("sp", obD.t[h * 128:(h + 1) * 128, t0:t0 + n], stg[:, 0:n], reads=[stg.r], writes=[obD.r])
    P.release(markG)

    wba = P.sbuf("wba", [64, 8, 1024], BF16)
    wbn = P.sbuf("wbn", [64, 8, 1024], BF16)
    wbg = P.sbuf("wbg", [128, 4, 1024], BF16)
    wo = P.sbuf("wo", [128, 8, 1024], BF16)
    P.dma_copy("pool", wba[:], wba_d.t.rearrange("(h p) f -> p h f", p=64), writes=[wba.r])
    P.dma_copy("pool", wbn[:], wbn_d.t.rearrange("(h p) f -> p h f", p=64), writes=[wbn.r])
    P.dma_copy("pool", wbg[:], wbg_d.t.rearrange("(h p) f -> p h f", p=128), writes=[wbg.r])
    P.dma_copy("pool", wo[:], wout_d.t.rearrange("(k p) f -> p k f", p=128), writes=[wo.r])
    bc = {}
    for nm, m in (("g1", 2), ("sh2", 3), ("sc2", 4)):
        for j in range(2):
            t = P.sbuf(f"bc_{nm}{j}", [128, 1024], F32)
            P.dma_copy("sp", t[:], modD.t[j:j + 1, m * 1024:(m + 1) * 1024].partition_broadcast(128), writes=[t.r])
            bc[(nm, j)] = t
    for j in range(2):
        TS("pool", bc[("sc2", j)][:], bc[("sc2", j)][:], 1.0, None, ALU.add, None, [bc[("sc2", j)].r], [bc[("sc2", j)].r])
    lg = P.sbuf("bc_ln1g", [128, 1024], F32)
    lb = P.sbuf("bc_ln1b", [128, 1024], F32)
    P.dma_copy("sp", lg[:], ln1g_d.t[0:1, :].partition_broadcast(128), writes=[lg.r])
    P.dma_copy("sp", lb[:], ln1b_d.t[0:1, :].partition_broadcast(128), writes=[lb.r])
    ymT = Ring([P.sbuf(f"ymT{i}", [128, 8, 512], BF16) for i in range(1)])
    ggr = Ring([P.sbuf(f"gg{i}", [128, 3, 512], BF16) for i in range(3)])
    tar = Ring([P.sbuf(f"ta{i}", [128, 512], F32) for i in range(2)])
    tbr2 = Ring([P.sbuf(f"tb2{i}", [128, 512], F32) for i in range(2)])
    xr = Ring([P.sbuf(f"xr{i}", [128, 1024], F32) for i in range(1)])
    ur = Ring([P.sbuf(f"ur{i}", [128, 1024], F32) for i in range(1)])
    x1r = Ring([P.sbuf(f"x1r{i}", [128, 1024], F32) for i in range(1)])
    h2r = Ring([P.sbuf(f"h2r{i}", [128, 1024], F32) for i in range(1)])
    str_ = Ring([P.sbuf(f"st{i}", [128, 2, 6], F32) for i in range(2)])
    mvr = Ring([P.sbuf(f"mv{i}", [128, 2], F32) for i in range(2)])

    def ln_stats(src):
        st = str_.get()
        for c in range(2):
            P.op("dve", lambda e, st=st, c=c: e.bn_stats(out=st[:, c, :], in_=src[:, c * 512:(c + 1) * 512]),
                 [src.r], [st.r])
        mv = mvr.get()
        P.op("dve", lambda e, st=st, mv=mv: e.bn_aggr(out=mv[:], in_=st[:].rearrange("p a b -> p (a b)")),
             [st.r], [mv.r])
        ACT(mv[:, 1:2], mv[:, 1:2], AF.Sqrt, [mv.r, epsc.r], [mv.r], bias=epsc[:, 0:1])
        P.op("dve", lambda e, mv=mv: e.reciprocal(out=mv[:, 1:2], in_=mv[:, 1:2]), [mv.r], [mv.r])
        return mv

    oabr = Ring([P.sbuf(f"oab{i}", [64, 8, 512], BF16) for i in range(2)])
    ocbr = Ring([P.sbuf(f"ocb{i}", [64, 8, 512], BF16) for i in range(2)])
    obbr = Ring([P.sbuf(f"obb{i}", [128, 4, 512], BF16) for i in range(2)])
    for bi, (t0, n) in enumerate(BLKS):
        ym = ymT.get()
        oab, ocb, obb = oabr.get(), ocbr.get(), obbr.get()
        P.dma_copy("sp", oab[:, :, 0:n], oaD.t[:, t0:t0 + n].rearrange("(h p) t -> p h t", p=64), reads=[oaD.r], writes=[oab.r])
        P.dma_copy("sp", ocb[:, :, 0:n], ocD.t[:, t0:t0 + n].rearrange("(h p) t -> p h t", p=64), reads=[ocD.r], writes=[ocb.r])
        P.dma_copy("sp", obb[:, :, 0:n], obD.t[:, t0:t0 + n].rearrange("(h p) t -> p h t", p=128), reads=[obD.r], writes=[obb.r])
        for fc in range(8):
            gg = ggr.get()
            P.dma_copy("sp", gg[:, :, 0:n],
                       gT.t[:, t0:t0 + n].rearrange("(b r) t -> r b t", b=3)[fc * 128:(fc + 1) * 128],
                       writes=[gg.r])
            fs = slice(fc * 128, (fc + 1) * 128)
            pa = bankS.get()
            for h in range(8):
                MM(pa[:, 0:n], wba[:, h, fs], oab[:, h, 0:n], h == 0, h == 7, [wba.r, oab.r], [pa.r])
            pb = bankS.get()
            for h in range(4):
                MM(pb[:, 0:n], wbg[:, h, fs], obb[:, h, 0:n], h == 0, h == 3, [wbg.r, obb.r], [pb.r])
            pcn = bankS.get()
            for h in range(8):
                MM(pcn[:, 0:n], wbn[:, h, fs], ocb[:, h, 0:n], h == 0, h == 7, [wbn.r, ocb.r], [pcn.r])
            ta, tb = tar.get(), tbr2.get()
            TT("dve", ta[:, 0:n], pa[:, 0:n], gg[:, 0, 0:n], ALU.mult, [pa.r, gg.r], [ta.r])
            TT("dve", tb[:, 0:n], pb[:, 0:n], gg[:, 1, 0:n], ALU.mult, [pb.r, gg.r], [tb.r])
            TT("pool", ta[:, 0:n], ta[:, 0:n], tb[:, 0:n], ALU.add, [ta.r, tb.r], [ta.r])
            TT("dve", tb[:, 0:n], pcn[:, 0:n], gg[:, 2, 0:n], ALU.mult, [pcn.r, gg.r], [tb.r])
            TT("pool", ym[:, fc, 0:n], ta[:, 0:n], tb[:, 0:n], ALU.add, [ta.r, tb.r], [ym.r])
        for ti in range(n // 128):
            i = t0 // 128 + ti
            j = 0 if i < 16 else 1
            xt = xr.get()
            P.dma_copy("sp", xt[:], x_in.t[i * 128:(i + 1) * 128, :], writes=[xt.r])
            u = ur.get()
            for hf in range(2):
                py = bankM.get()
                for fc in range(8):
                    MM(py[:, :], ym[:, fc, ti * 128:(ti + 1) * 128], wo[:, fc, hf * 512:(hf + 1) * 512], fc == 0, fc == 7,
                       [ym.r, wo.r], [py.r])
                TT("dve", u[:, hf * 512:(hf + 1) * 512], py[:, :], bc[("g1", j)][:, hf * 512:(hf + 1) * 512], ALU.mult,
                   [py.r, bc[("g1", j)].r], [u.r])
            STT("dve", u[:], xt[:], ALPHA, u[:], ALU.mult, ALU.add, [xt.r, u.r], [u.r])
            mv = ln_stats(u)
            x1 = x1r.get()
            TS("dve", x1[:], u[:], mv[:, 0:1], mv[:, 1:2], ALU.subtract, ALU.mult, [u.r, mv.r], [x1.r])
            TT("pool", x1[:], x1[:], lg[:], ALU.mult, [x1.r, lg.r], [x1.r])
            TT("pool", x1[:], x1[:], lb[:], ALU.add, [x1.r, lb.r], [x1.r])
            P.dma_copy("sp", x1_o.t[i * 128:(i + 1) * 128, :], x1[:], reads=[x1.r], writes=[x1_o.r])
            mv2 = ln_stats(x1)
            h2 = h2r.get()
            TS("dve", h2[:], x1[:], mv2[:, 0:1], mv2[:, 1:2], ALU.subtract, ALU.mult, [x1.r, mv2.r], [h2.r])
            TT("pool", h2[:], h2[:], bc[("sc2", j)][:], ALU.mult, [h2.r, bc[("sc2", j)].r], [h2.r])
            TT("pool", h2[:], h2[:], bc[("sh2", j)][:], ALU.add, [h2.r, bc[("sh2", j)].r], [h2.r])
            P.dma_copy("sp", h2_o.t[i * 128:(i + 1) * 128, :], h2[:], reads=[h2.r], writes=[h2_o.r])

    P.release(m_stage)


def emit_l3(P, E, L):
    nc = P.nc
    H = mk_helpers(P)
    MM, TR, ACT, TT, TS, STT, CP, MS = H.MM, H.TR, H.ACT, H.TT, H.TS, H.STT, H.CP, H.MS
    din = lambda name, shape, dt=F32: E.din(name, shape, dt, L)
    dout = lambda name, shape, dt: E.dout(name, shape, dt, L)
    m_stage = P.mark()

    x1_d = din("x1", [NTOK, 1024])
    h2_d = din("h2", [NTOK, 1024])
    modD = din("modD", [2, 6144])
    wr_d = din("w_r", [1024, 36])
    br_d = din("b_r", [1, 36])
    w1_d = din("moe_w1", [32 * 1024, 512])
    w3_d = din("moe_w3", [32 * 1024, 512])
    w2_d = din("moe_w2", [32 * 512, 1024])
    ln2g_d = din("ln2_g", [1, 1024])
    ln2b_d = din("ln2_b", [1, 1024])
    id32_d = din("ident", [128, 128])
    tris_d = din("triSlt", [128, 128])
    ones_d = din("ones128", [128, 128])
    thr_d = din("thr18", [128, 32, 18])
    thrj_d = din("thrj", [128, NBLK, 32])
    tokidx_d = din("tokidx", [128, NT], I32)
    wbase_d = din("wbase", [128, 8])
    x2_o = dout("x2", [NTOK, 1024], F32)
    h2b = dout("h2b", [NTOK + 128, 1024], BF16)
    tokslot = dout("tokslot", [NSLOT, 1], I32)
    yslot = dout("yslot", [NSLOT, 1024], F32)
    be_o = dout("be_o", [1, NBLK], I32)
    pos_o = dout("pos_o", [128, NT, 2], I32)
    gate_o = dout("gate_o", [128, NT, 2], F32)

    banks = Banks(P, 0, E.fb)
    pTb = E.bb

    id32 = P.sbuf("id32", [128, 128], F32)
    P.dma_copy("sp", id32[:], id32_d.t[:, :], writes=[id32.r])
    idb = P.sbuf("idb", [128, 128], BF16)
    P.dma_copy("pool", idb[:], id32_d.t[:, :], writes=[idb.r])
    tris = P.sbuf("tris", [128, 128], F32)
    P.dma_copy("sp", tris[:], tris_d.t[:, :], writes=[tris.r])
    ones = P.sbuf("ones", [128, 128], F32)
    P.dma_copy("sp", ones[:], ones_d.t[:, :], writes=[ones.r])
    thr = P.sbuf("thr", [128, 32, 18], F32)
    P.dma_copy("sp", thr[:], thr_d.t[:, :, :], writes=[thr.r])
    thrj = P.sbuf("thrj", [128, NBLK, 32], F32)
    P.dma_copy("sp", thrj[:], thrj_d.t[:, :, :], writes=[thrj.r])
    tokidx = P.sbuf("tokidx", [128, NT], I32)
    P.dma_copy("sp", tokidx[:], tokidx_d.t[:, :], writes=[tokidx.r])
    wr = P.sbuf("wr", [128, 8, 36], F32)
    P.dma_copy("sp", wr[:], wr_d.t.rearrange("(k p) c -> p k c", p=128), writes=[wr.r])
    br_bc = P.sbuf("br_bc", [128, 36], F32)
    P.dma_copy("sp", br_bc[:], br_d.t[0:1, :].partition_broadcast(128), writes=[br_bc.r])
    epsc = P.sbuf("epsc", [128, 1], F32)
    MS("dve", epsc[:], LN_EPS, [epsc.r])
    dum = P.sbuf("dum", [128, NBLK], I32)
    MS("pool", dum[:], NTOK, [dum.r])
    P.dma_copy("sp", tokslot.t.rearrange("(p j) o -> p (j o)", p=128), dum[:], reads=[dum.r], writes=[tokslot.r])
    zt = P.sbuf("zt", [128, 1024], BF16)
    MS("pool", zt[:], 0.0, [zt.r])
    P.dma_copy("sp", h2b.t[NTOK:NTOK + 128, :], zt[:], reads=[zt.r], writes=[h2b.r])

    oh1a = P.sbuf("oh1a", [128, NT, 32], F32)
    oh2a = P.sbuf("oh2a", [128, NT, 32], F32)
    Aall = P.sbuf("Aall", [128, NT, 32], F32)
    gates = P.sbuf("gates", [128, NT, 2], F32)
    posI = P.sbuf("posI", [128, NT, 2], I32)
    beI = P.sbuf("beI", [1, NBLK], I32)
    widx = P.sbuf("widx", [128, NBLK, 8], I32)
    widx2 = P.sbuf("widx2", [128, NBLK, 4], I32)
    wbase = P.sbuf("wbase", [128, 8], F32)
    P.dma_copy("sp", wbase[:], wbase_d.t[:, :], writes=[wbase.r])
    carry = P.sbuf("carry", [128, 32], F32)
    MS("dve", carry[:], 0.0, [carry.r])
    excl = P.sbuf("excl", [128, NT, 32], F32)

    markR = P.mark()
    h2r = Ring([P.sbuf(f"h2t{i}", [128, 1024], F32) for i in range(2)])
    hTr = Ring([P.sbuf(f"h2T{i}", [128, 8, 128], F32) for i in range(2)])
    lgr = Ring([P.sbuf(f"lg{i}", [128, 36], F32) for i in range(2)])
    sm = Ring([P.sbuf(f"sm{i}", [128, 16], F32) for i in range(2)])
    elr = Ring([P.sbuf(f"elm{i}", [128, 32], F32) for i in range(2)])
    el2r = Ring([P.sbuf(f"elm2{i}", [128, 32], F32) for i in range(2)])
    for i in range(NT):
        ht = h2r.get()
        P.dma_copy("sp", ht[:], h2_d.t[i * 128:(i + 1) * 128, :], writes=[ht.r])
        P.dma_copy("pool", h2b.t[i * 128:(i + 1) * 128, :], ht[:], reads=[ht.r], writes=[h2b.r])
        hT = hTr.get()
        for half in range(2):
            pt = banks.get()
            for kk in range(4):
                k = half * 4 + kk
                TR(pt[:, kk * 128:(kk + 1) * 128], ht[:, k * 128:(k + 1) * 128], id32[:], [ht.r, id32.r], [pt.r])
            if half == 0:
                ACT(hT[:, 0:4, :].rearrange("p k t -> p (k t)"), pt[:, :], AF.Identity, [pt.r], [hT.r])
            else:
                CP("dve", hT[:, 4:8, :].rearrange("p k t -> p (k t)"), pt[:, :], [pt.r], [hT.r])
        pl = banks.get()
        for k in range(8):
            MM(pl[:, 0:36], hT[:, k, :], wr[:, k, :], k == 0, k == 7, [hT.r, wr.r], [pl.r])
        lg = lgr.get()
        TT("dve", lg[:], pl[:, 0:36], br_bc[:], ALU.add, [pl.r, br_bc.r], [lg.r])
        s = sm.get()
        P.op("dve", lambda e, s=s, lg=lg: e.reduce_max(out=s[:, 0:1], in_=lg[:, 0:4], axis=AX.X), [lg.r], [s.r])
        TS("dve", s[:, 1:2], s[:, 0:1], -1.0, None, ALU.mult, None, [s.r], [s.r])
        TS("dve", s[:, 8:12], lg[:, 0:4], s[:, 0:1], None, ALU.is_equal, None, [lg.r, s.r], [s.r])
        ACT(s[:, 12:16], lg[:, 0:4], AF.Exp, [lg.r, s.r], [s.r], bias=s[:, 1:2])
        P.op("dve", lambda e, s=s: e.reduce_sum(out=s[:, 2:3], in_=s[:, 12:16], axis=AX.X), [s.r], [s.r])
        P.op("dve", lambda e, s=s: e.reciprocal(out=s[:, 3:4], in_=s[:, 2:3]), [s.r], [s.r])
        TS("dve", s[:, 12:16], s[:, 8:12], 1.0, BIG, ALU.subtract, ALU.mult, [s.r], [s.r])
        elm = elr.get()
        TT("dve", elm[:].rearrange("p (g e) -> p g e", g=4), lg[:, 4:36].rearrange("p (g e) -> p g e", g=4),
           s[:, 12:16].unsqueeze(2).to_broadcast([128, 4, 8]), ALU.add, [lg.r, s.r], [elm.r])
        P.op("dve", lambda e, s=s, elm=elm: e.reduce_max(out=s[:, 4:5], in_=elm[:], axis=AX.X), [elm.r], [s.r])
        TS("dve", oh1a[:, i, :], elm[:], s[:, 4:5], None, ALU.is_equal, None, [elm.r, s.r], [oh1a.r])
        elm2 = el2r.get()
        STT("dve", elm2[:], oh1a[:, i, :], -BIG, elm[:], ALU.mult, ALU.add, [oh1a.r, elm.r], [elm2.r])
        P.op("dve", lambda e, s=s, elm2=elm2: e.reduce_max(out=s[:, 5:6], in_=elm2[:], axis=AX.X), [elm2.r], [s.r])
        TS("dve", oh2a[:, i, :], elm2[:], s[:, 5:6], None, ALU.is_equal, None, [elm2.r, s.r], [oh2a.r])
        TT("dve", Aall[:, i, :], oh1a[:, i, :], oh2a[:, i, :], ALU.add, [oh1a.r, oh2a.r], [Aall.r])
        TT("dve", s[:, 6:7], s[:, 5:6], s[:, 4:5], ALU.subtract, [s.r], [s.r])
        ACT(s[:, 6:7], s[:, 6:7], AF.Exp, [s.r], [s.r])
        TS("dve", s[:, 6:7], s[:, 6:7], 1.0, None, ALU.add, None, [s.r], [s.r])
        P.op("dve", lambda e, s=s: e.reciprocal(out=s[:, 7:8], in_=s[:, 6:7]), [s.r], [s.r])
        TT("dve", gates[:, i, 0:1], s[:, 3:4], s[:, 7:8], ALU.mult, [s.r], [gates.r])
        TT("dve", gates[:, i, 1:2], s[:, 3:4], gates[:, i, 0:1], ALU.subtract, [s.r, gates.r], [gates.r])
        pex = banks.get()
        MM(pex[:, 0:32], tris[:], Aall[:, i, :], True, True, [tris.r, Aall.r], [pex.r])
        TT("dve", excl[:, i, :], pex[:, 0:32], carry[:], ALU.add, [pex.r, carry.r], [excl.r])
        pcs = banks.get()
        MM(pcs[:, 0:32], ones[:], Aall[:, i, :], True, True, [ones.r, Aall.r], [pcs.r])
        TT("dve", carry[:], carry[:], pcs[:, 0:32], ALU.add, [carry.r, pcs.r], [carry.r])
    cmp18 = P.sbuf("cmp18", [128, 32, 18], F32)
    TT("dve", cmp18[:], carry[:].unsqueeze(2).to_broadcast([128, 32, 18]), thr[:], ALU.is_gt, [carry.r, thr.r], [cmp18.r])
    padded = P.sbuf("padded", [128, 32], F32)
    P.op("dve", lambda e: e.reduce_sum(out=padded[:], in_=cmp18[:], axis=AX.X), [cmp18.r], [padded.r])
    TS("dve", padded[:], padded[:], 128.0, None, ALU.mult, None, [padded.r], [padded.r])
    cs = [P.sbuf(f"cs{i}", [128, 32], F32) for i in range(2)]
    CP("dve", cs[0][:], padded[:], [padded.r], [cs[0].r])
    cur = 0
    for sh in (1, 2, 4, 8, 16):
        a, b_ = cs[cur], cs[1 - cur]
        CP("dve", b_[:, 0:sh], a[:, 0:sh], [a.r], [b_.r])
        TT("dve", b_[:, sh:32], a[:, sh:32], a[:, 0:32 - sh], ALU.add, [a.r], [b_.r])
        cur = 1 - cur
    pend = cs[cur]
    pstart = P.sbuf("pstart", [128, 32], F32)
    TT("dve", pstart[:], pend[:], padded[:], ALU.subtract, [pend.r, padded.r], [pstart.r])
    posf = P.sbuf("posf", [128, NT, 2], F32)
    tmp32 = Ring([P.sbuf(f"tmp32{i}", [128, 32], F32) for i in range(2)])
    for i in range(NT):
        sb_ = tmp32.get()
        TT("dve", sb_[:], excl[:, i, :], pstart[:], ALU.add, [excl.r, pstart.r], [sb_.r])
        for k, oh in enumerate((oh1a, oh2a)):
            t2 = tmp32.get()
            TT("dve", t2[:], sb_[:], oh[:, i, :], ALU.mult, [sb_.r, oh.r], [t2.r])
            P.op("dve", lambda e, t2=t2, i=i, k=k: e.reduce_sum(out=posf[:, i, k:k + 1], in_=t2[:], axis=AX.X),
                 [t2.r], [posf.r])
    CP("dve", posI[:], posf[:], [posf.r], [posI.r])
    cmpj = P.sbuf("cmpj", [128, NBLK, 32], F32)
    TT("dve", cmpj[:], pend[:].unsqueeze(1).to_broadcast([128, NBLK, 32]), thrj[:], ALU.is_le, [pend.r, thrj.r], [cmpj.r])
    bef = P.sbuf("bef", [128, NBLK], F32)
    P.op("dve", lambda e: e.reduce_sum(out=bef[:], in_=cmpj[:], axis=AX.X), [cmpj.r], [bef.r])
    TS("dve", bef[:], bef[:], 31.0, None, ALU.min, None, [bef.r], [bef.r])
    CP("dve", beI[:], bef[0:1, :], [bef.r], [beI.r])
    wif = P.sbuf("wif", [128, NBLK, 8], F32)
    TS("dve", bef[:], bef[:], 1024.0, None, ALU.mult, None, [bef.r], [bef.r])
    TT("dve", wif[:], bef[:].unsqueeze(2).to_broadcast([128, NBLK, 8]), wbase[:].unsqueeze(1).to_broadcast([128, NBLK, 8]),
       ALU.add, [bef.r, wbase.r], [wif.r])
    CP("dve", widx[:], wif[:], [wif.r], [widx.r])
    TS("dve", bef[:], bef[:], 0.5, None, ALU.mult, None, [bef.r], [bef.r])
    TT("dve", wif[:, :, 0:4], bef[:].unsqueeze(2).to_broadcast([128, NBLK, 4]), wbase[:, 0:4].unsqueeze(1).to_broadcast([128, NBLK, 4]),
       ALU.add, [bef.r, wbase.r], [wif.r])
    CP("dve", widx2[:], wif[:, :, 0:4], [wif.r], [widx2.r])
    P.dma_copy("sp", be_o.t[:, :], beI[:], reads=[beI.r], writes=[be_o.r])
    P.dma_copy("sp", pos_o.t[:, :, :], posI[:], reads=[posI.r], writes=[pos_o.r])
    P.dma_copy("sp", gate_o.t[:, :, :], gates[:], reads=[gates.r], writes=[gate_o.r])
    for i in range(NT):
        for k in range(2):
            P.dma("pool", lambda e, i=i, k=k: e.indirect_dma_start(
                out=tokslot.t[:, :], out_offset=bass.IndirectOffsetOnAxis(ap=posI[:, i, k:k + 1], axis=0),
                in_=tokidx[:, i:i + 1], in_offset=None), reads=[posI.r, tokidx.r], writes=[tokslot.r])
    P.release(markR)

    markE = P.mark()
    w1r = Ring([P.sbuf(f"w1b{i}", [128, 8, 512], BF16) for i in range(2)])
    w3r = Ring([P.sbuf(f"w3b{i}", [128, 8, 512], BF16) for i in range(2)])
    w2r = Ring([P.sbuf(f"w2b{i}", [128, 4, 1024], BF16) for i in range(2)])
    idxr = Ring([P.sbuf(f"idx{i}", [128, 1], I32) for i in range(2)])
    xgr = Ring([P.sbuf(f"xg{i}", [128, 1024], BF16) for i in range(2)])
    xTr = Ring([P.sbuf(f"xT{i}", [128, 8, 128], BF16) for i in range(2)])
    s1r = Ring([P.sbuf(f"s1{i}", [128, 512], F32) for i in range(2)])
    aTr = Ring([P.sbuf(f"aT{i}", [128, 4, 128], BF16) for i in range(2)])
    ysr = Ring([P.sbuf(f"ys{i}", [128, 1024], F32) for i in range(2)])

    for j in range(NBLK):
        w1b, w3b, w2b = w1r.get(), w3r.get(), w2r.get()

        for (wb_, wd_, wi_, nk) in ((w1b, w1_d, widx, 8), (w3b, w3_d, widx, 8), (w2b, w2_d, widx2, 4)):
            for k in range(nk):
                P.dma("pool", lambda e, wb_=wb_, wd_=wd_, wi_=wi_, k=k, j=j: e.indirect_dma_start(
                    out=wb_[:, k, :], out_offset=None, in_=wd_.t[:, :],
                    in_offset=bass.IndirectOffsetOnAxis(ap=wi_[:, j, k:k + 1], axis=0)),
                    reads=[wi_.r], writes=[wb_.r])
        idx = idxr.get()
        P.dma_copy("sp", idx[:], tokslot.t[j * 128:(j + 1) * 128, :], reads=[tokslot.r], writes=[idx.r])
        xg = xgr.get()
        P.dma("pool", lambda e, xg=xg, idx=idx: e.indirect_dma_start(
            out=xg[:, :], out_offset=None, in_=h2b.t[:, :],
            in_offset=bass.IndirectOffsetOnAxis(ap=idx[:, 0:1], axis=0)), reads=[idx.r, h2b.r], writes=[xg.r])
        for k in range(8):
            TR(pTb[:, k, :], xg[:, k * 128:(k + 1) * 128], idb[:], [xg.r, idb.r], [pTb.r])
        xT = xTr.get()
        ACT(xT[:, 0:4, :], pTb[:, 0:4, :], AF.Identity, [pTb.r], [xT.r])
        CP("dve", xT[:, 4:8, :], pTb[:, 4:8, :], [pTb.r], [xT.r])
        p1, p3 = banks.get(), banks.get()
        for (pp, wb) in ((p1, w1b), (p3, w3b)):
            for fc in range(4):
                for k in range(8):
                    MM(pp[:, fc * 128:(fc + 1) * 128], wb[:, k, fc * 128:(fc + 1) * 128], xT[:, k, :], k == 0, k == 7,
                       [wb.r, xT.r], [pp.r])
        s1 = s1r.get()
        ACT(s1[:], p1[:, :], AF.Silu, [p1.r], [s1.r])
        aT = aTr.get()
        TT("dve", aT[:].rearrange("p f t -> p (f t)"), s1[:], p3[:, :], ALU.mult, [s1.r, p3.r], [aT.r])
        ys = ysr.get()
        for half in range(2):
            py = banks.get()
            for fc in range(4):
                MM(py[:, :], aT[:, fc, :], w2b[:, fc, half * 512:(half + 1) * 512], fc == 0, fc == 3, [aT.r, w2b.r], [py.r])
            if half == 0:
                ACT(ys[:, 0:512], py[:, :], AF.Identity, [py.r], [ys.r])
            else:
                CP("dve", ys[:, 512:1024], py[:, :], [py.r], [ys.r])
        P.dma_copy("sp", yslot.t[j * 128:(j + 1) * 128, :], ys[:], reads=[ys.r], writes=[yslot.r])
    P.release(markE)

    g2bc = []
    for j in range(2):
        t = P.sbuf(f"g2bc{j}", [128, 1024], F32)
        P.dma_copy("sp", t[:], modD.t[j:j + 1, 5 * 1024:6 * 1024].partition_broadcast(128), writes=[t.r])
        g2bc.append(t)
    lg2 = P.sbuf("lg2", [128, 1024], F32)
    lb2 = P.sbuf("lb2", [128, 1024], F32)
    P.dma_copy("sp", lg2[:], ln2g_d.t[0:1, :].partition_broadcast(128), writes=[lg2.r])
    P.dma_copy("sp", lb2[:], ln2b_d.t[0:1, :].partition_broadcast(128), writes=[lb2.r])
    y1r = Ring([P.sbuf(f"y1{i}", [128, 1024], F32) for i in range(2)])
    y2r = Ring([P.sbuf(f"y2{i}", [128, 1024], F32) for i in range(2)])
    x1r = Ring([P.sbuf(f"x1t{i}", [128, 1024], F32) for i in range(2)])
    ur = Ring([P.sbuf(f"u{i}", [128, 1024], F32) for i in range(2)])
    str_ = Ring([P.sbuf(f"st{i}", [128, 2, 6], F32) for i in range(2)])
    mvr = Ring([P.sbuf(f"mv{i}", [128, 2], F32) for i in range(2)])
    for i in range(NT):
        j = 0 if i < 16 else 1
        y1, y2 = y1r.get(), y2r.get()
        for k, yy in enumerate((y1, y2)):
            P.dma("pool", lambda e, yy=yy, i=i, k=k: e.indirect_dma_start(
                out=yy[:, :], out_offset=None, in_=yslot.t[:, :],
                in_offset=bass.IndirectOffsetOnAxis(ap=posI[:, i, k:k + 1], axis=0)),
                reads=[posI.r, yslot.r], writes=[yy.r])
        xt = x1r.get()
        P.dma_copy("sp", xt[:], x1_d.t[i * 128:(i + 1) * 128, :], writes=[xt.r])
        TS("dve", y1[:], y1[:], gates[:, i, 0:1], None, ALU.mult, None, [y1.r, gates.r], [y1.r])
        STT("dve", y1[:], y2[:], gates[:, i, 1:2], y1[:], ALU.mult, ALU.add, [y2.r, gates.r, y1.r], [y1.r])
        u = ur.get()
        TT("pool", u[:], y1[:], g2bc[j][:], ALU.mult, [y1.r, g2bc[j].r], [u.r])
        STT("dve", u[:], xt[:], ALPHA, u[:], ALU.mult, ALU.add, [xt.r, u.r], [u.r])
        st = str_.get()
        for c in range(2):
            P.op("dve", lambda e, st=st, u=u, c=c: e.bn_stats(out=st[:, c, :], in_=u[:, c * 512:(c + 1) * 512]),
                 [u.r], [st.r])
        mv = mvr.get()
        P.op("dve", lambda e, st=st, mv=mv: e.bn_aggr(out=mv[:], in_=st[:].rearrange("p a b -> p (a b)")),
             [st.r], [mv.r])
        ACT(mv[:, 1:2], mv[:, 1:2], AF.Sqrt, [mv.r, epsc.r], [mv.r], bias=epsc[:, 0:1])
        P.op("dve", lambda e, mv=mv: e.reciprocal(out=mv[:, 1:2], in_=mv[:, 1:2]), [mv.r], [mv.r])
        TS("dve", u[:], u[:], mv[:, 0:1], mv[:, 1:2], ALU.subtract, ALU.mult, [u.r, mv.r], [u.r])
        TT("pool", u[:], u[:], lg2[:], ALU.mult, [u.r, lg2.r], [u.r])
        TT("pool", u[:], u[:], lb2[:], ALU.add, [u.r, lb2.r], [u.r])
        P.dma_copy("sp", x2_o.t[i * 128:(i + 1) * 128, :], u[:], reads=[u.r], writes=[x2_o.r])

    P.release(m_stage)


LAYER_W = {"w_mod", "b_mod", "w_in", "b_in", "qn_g", "kn_g", "wgate", "bgate", "gla_norm", "w_br_attn", "w_br_gla",
           "w_br_na", "w_out", "ln1_g", "ln1_b", "w_r", "b_r", "moe_w1", "moe_w3", "moe_w2", "ln2_g", "ln2_b", "tabNA"}


class Env:
    def __init__(self, P):
        self.P = P
        self.scratch = {}
        self.ext = {}
        self.last = 1
        self.fb = [P.psum(f"fb{i}", [128, 512], F32) for i in range(7)]
        self.bb = P.psum("bb", [128, 8, 128], BF16)

    def _mk(self, name, shape, dt, kind):
        b = self.P.dram(name, shape, dt, kind=kind)
        b.t = b.t.ap()
        return b

    def din(self, name, shape, dt, L):
        if name == "x_in":
            if L == 0:
                if "x_in" not in self.ext:
                    self.ext["x_in"] = self._mk("x_in", shape, dt, "ExternalInput")
                return self.ext["x_in"]
            return self.scratch["x2"]
        if name in self.scratch:
            return self.scratch[name]
        key = f"{name}_{L}" if name in LAYER_W else name
        if key not in self.ext:
            self.ext[key] = self._mk(key, shape, dt, "ExternalInput")
        return self.ext[key]

    def dout(self, name, shape, dt, L):
        if name == "x2" and L == self.last:
            return self._mk("x2out", shape, dt, "ExternalOutput")
        if name not in self.scratch:
            self.scratch[name] = self._mk("sc_" + name, shape, dt, "Internal")
        return self.scratch[name]


def emit_exchange(P, E, groups):
    S = E.scratch

    def sc(name, shape, dt):
        if name not in S:
            S[name] = E._mk("sc_" + name, shape, dt, "Internal")
        return S[name]
    pk_ak, g_ak = sc("pk_ak", [128, 2048], BF16), sc("g_ak", [256, 2048], BF16)
    pk_av, g_av = sc("pk_av", [2048, 128], BF16), sc("g_av", [4096, 128], BF16)
    pk_ck, g_ck = sc("pk_ck", [512, 768], BF16), sc("g_ck", [1024, 768], BF16)
    pk_cv, g_cv = sc("pk_cv", [768, 512], BF16), sc("g_cv", [1536, 512], BF16)
    pk_S, g_S = sc("pk_S", [128, 512], F32), sc("g_S", [256, 512], F32)
    P.dma_copy("sp", pk_ak.t[:, :], S["akT"].t[:, 0:2048], reads=[S["akT"].r], writes=[pk_ak.r])
    P.dma_copy("sp", pk_av.t[:, :], S["av"].t[0:2048, :], reads=[S["av"].r], writes=[pk_av.r])
    P.dma_copy("sp", pk_ck.t[:, 0:384], S["ckT"].t[:, 0:384], reads=[S["ckT"].r], writes=[pk_ck.r])
    P.dma_copy("sp", pk_ck.t[:, 384:768], S["ckT"].t[:, 1664:2048], reads=[S["ckT"].r], writes=[pk_ck.r])
    P.dma_copy("sp", pk_cv.t[0:384, :], S["cv"].t[0:384, :], reads=[S["cv"].r], writes=[pk_cv.r])
    P.dma_copy("sp", pk_cv.t[384:768, :], S["cv"].t[1664:2048, :], reads=[S["cv"].r], writes=[pk_cv.r])
    P.dma_copy("sp", pk_S.t[:, :], S["Sfin"].t.rearrange("d p f -> (d p) f"), reads=[S["Sfin"].r], writes=[pk_S.r])
    for a, b_ in ((pk_ak, g_ak), (pk_av, g_av), (pk_ck, g_ck), (pk_cv, g_cv), (pk_S, g_S)):
        P.coll(lambda e, a=a, b_=b_: e.collective_compute("AllGather", ALU.bypass, replica_groups=groups,
                                                           ins=[a.t[:, :]], outs=[b_.t[:, :]]),
               reads=[a.r], writes=[b_.r])


def build_fused(groups=None, n_layers=2):
    if groups is None:
        groups = [[0, 1], [2, 3], [4, 5], [6, 7]]
    nc = bass.Bass("TRN2", target_bir_lowering=False)
    P = Prog(nc)
    E = Env(P)
    E.last = n_layers - 1
    for L in range(n_layers):
        emit_l1(P, E, L)
        emit_exchange(P, E, groups)
        emit_l2(P, E, L)
        emit_l3(P, E, L)
    P.finalize()
    return nc, P


_BF = ml_dtypes.bfloat16


def _rope_tables(tok):
    row = (tok // 64).astype(np.float32)
    col = (tok % 64).astype(np.float32)
    inv = (10000.0 ** (-np.arange(16, dtype=np.float32) / 16)).astype(np.float32)
    ar = row[:, None] * inv
    ac = col[:, None] * inv
    ang = np.concatenate([ar, ar, ac, ac], -1)
    return np.cos(ang).astype(np.float32), np.sin(ang).astype(np.float32)


def _consts(s):
    tok = s * 2048 + np.arange(2048)
    cos, sin = _rope_tables(tok)
    cosf = np.concatenate([cos, np.ones((256, 64), np.float32)], 0).T
    sinf = np.concatenate([sin, np.zeros((256, 64), np.float32)], 0).T
    R = np.zeros((64, 64), np.float32)
    for d in range(16):
        R[d, d + 16] = -1
        R[d + 16, d] = 1
        R[d + 32, d + 48] = -1
        R[d + 48, d + 32] = 1
    rotM = np.zeros((128, 128), np.float32)
    rotM[:64, :64] = R.T
    rotM[64:, 64:] = R.T
    ob = np.zeros((128, 128), np.float32)
    ob[:64, :64] = 1
    ob[64:, 64:] = 1
    ii = np.arange(128)
    sI, tI = np.meshgrid(ii, ii, indexing="ij")
    f32 = np.float32
    return dict(cosT=np.ascontiguousarray(np.concatenate([cosf, cosf], 0)),
                sinT=np.ascontiguousarray(np.concatenate([sinf, sinf], 0)),
                rotM=rotM, onesblk=ob, ident=np.eye(128, dtype=f32),
                triInc=(sI <= tI).astype(f32), triDec=(sI >= tI).astype(f32),
                triSgt=(sI > tI).astype(f32), triSlt=(sI < tI).astype(f32),
                flags=np.tile(np.array([[1.0 if s == 0 else 0.0, 1.0 if s == 1 else 0.0]], f32), (64, 1)),
                fl1m=np.tile(np.array([[0.0 if s == 0 else 1.0, 0.0 if s == 1 else 1.0]], f32), (64, 1)),
                ones128=np.ones((128, 128), f32),
                thr18=np.tile((128.0 * np.arange(18, dtype=f32))[None, None, :], (128, 32, 1)),
                thrj=np.tile((128.0 * np.arange(NBLK, dtype=f32))[None, :, None], (128, 1, 32)),
                wbase=(np.arange(8)[None, :] * 128 + np.arange(128)[:, None]).astype(f32),
                tokidx=(np.arange(18)[None, :] * 128 + np.arange(128)[:, None]).astype(np.int32))


def _na_table7(rpb, s):
    tab = np.full((5, 7, 2, 128, 512), -30000.0, np.float32)
    kp = np.arange(128)
    qp = np.arange(128)
    for slot, j in enumerate((0, 1, 14, 15, 5)):
        J = 16 * s + j
        qr = 2 * J + qp // 64
        qc = qp % 64
        rs = np.clip(qr - 4, 0, 56)
        cs = np.clip(qc - 8, 0, 48)
        for kt in range(7):
            T = J - 3 + kt
            if T < 0 or T > 31:
                continue
            kr = 2 * T + kp // 64
            kc = kp % 64
            valid = ((kr[:, None] >= rs[None, :]) & (kr[:, None] < rs[None, :] + 8)
                     & (kc[:, None] >= cs[None, :]) & (kc[:, None] < cs[None, :] + 16))
            ri = np.clip(kr[:, None] - qr[None, :] + 7, 0, 14)
            ci = np.clip(kc[:, None] - qc[None, :] + 15, 0, 30)
            for h in range(8):
                tab[slot, kt, h // 4, :, (h % 4) * 128:(h % 4 + 1) * 128] = np.where(valid, rpb[h][ri, ci], -30000.0)
    return tab.astype(_BF)


def _core_map(inp, c, n_layers=2):
    f32 = np.float32
    b, s = c // 2, c % 2
    m = dict(x_in=np.concatenate([inp["x"][b, s * 2048:(s + 1) * 2048], inp["ctx"][b]], 0),
             cvec=np.stack([inp["c"][b], inp["c_ctx"]], 0), **_consts(s))
    for l in range(n_layers):
        lw = dict(w_mod=inp["w_mod"][l], b_mod=inp["b_mod"][l][None], w_in=inp["w_in"][l], b_in=inp["b_in"][l][None],
                  qn_g=inp["attn_q_norm"][l][:, None], kn_g=inp["attn_k_norm"][l][:, None],
                  wgate=inp["gla_w_gate"][l], bgate=inp["gla_b_gate"][l], gla_norm=inp["gla_norm"][l][:, None],
                  w_br_attn=inp["w_br_attn"][l], w_br_gla=inp["w_br_gla"][l], w_br_na=inp["w_br_na"][l],
                  w_out=inp["w_out"][l], ln1_g=inp["ln1_g"][l][None], ln1_b=inp["ln1_b"][l][None],
                  w_r=np.concatenate([inp["w_router_group"][l], inp["w_router_expert"][l]], 1),
                  b_r=np.concatenate([inp["b_router_group"][l], inp["b_router_expert"][l]])[None],
                  moe_w1=inp["moe_w1"][l].reshape(-1, 512), moe_w3=inp["moe_w3"][l].reshape(-1, 512),
                  moe_w2=inp["moe_w2"][l].reshape(-1, 1024), ln2_g=inp["ln2_g"][l][None], ln2_b=inp["ln2_b"][l][None],
                  tabNA=_na_table7(inp["na_rpb"][l].astype(f32), s))
        for k, v in lw.items():
            m[f"{k}_{l}"] = v
    out = {}
    for k, v in m.items():
        v = np.asarray(v)
        if v.dtype not in (np.int32, _BF):
            v = v.astype(f32)
        out[k] = np.ascontiguousarray(v)
    return out


_PROG = []


def kernel(**inp):
    inp = {k: np.asarray(v) for k, v in inp.items()}
    if not _PROG:
        _PROG.append(build_fused()[0])
    cores = list(range(8))
    maps = [_core_map(inp, c) for c in cores]
    res = run_bass_kernel_spmd(_PROG[0], maps, core_ids=cores).results
    out = np.zeros((4, 4096, 1024), np.float32)
    for c in cores:
        out[c // 2, (c % 2) * 2048:(c % 2 + 1) * 2048] = np.asarray(res[c]["x2out"], dtype=np.float32)[:2048]
    return out
```

```python
import os
import numpy as np
import ml_dtypes
import concourse.bass as bass
import concourse.mybir as mybir
from concourse.bass_utils import run_bass_kernel_spmd

F32 = mybir.dt.float32
BF16 = mybir.dt.bfloat16
I32 = mybir.dt.int32
AF = mybir.ActivationFunctionType
ALU = mybir.AluOpType
AX = mybir.AxisListType


class Res:
    __slots__ = ("name", "w", "r")

    def __init__(self, name=""):
        self.name = name
        self.w = None
        self.r = {}


class Buf:
    def __init__(self, t, nres=1, name=""):
        self.t = t
        self.rs = [Res(f"{name}{i}") for i in range(nres)]

    @property
    def r(self):
        return self.rs[0]

    def __getitem__(self, k):
        return self.t[k]


class Prog:
    ENGS = ("pe", "act", "dve", "pool", "sp")
    SAME_SYNC = {"pe": False, "act": True, "dve": True, "pool": True, "sp": False}

    def __init__(self, nc, n_dma_sems=48):
        self.nc = nc
        self.lists = {k: [] for k in self.ENGS}
        self.semobj = {}
        self.cnt = {}
        for k in self.ENGS:
            self.semobj[k] = nc.alloc_semaphore(f"sem_{k}")
            self.cnt[k] = 0
        self.seen = {k: {} for k in self.ENGS}
        self.nd = n_dma_sems
        self.dval = [0] * n_dma_sems
        for i in range(n_dma_sems):
            self.semobj[f"d{i}"] = nc.alloc_semaphore(f"sem_d{i}")
        self.dnext = 0
        self.dnext_sw = 0
        self.n_ops = 0
        self.arena0, self.arena1 = nc.bump_sbuf(212000)
        self.sb_off = self.arena0
        self.sb_peak = self.arena0
        self.n_alloc = 0

    def sbuf(self, name, shape, dtype, nres=1):
        esz = {F32: 4, BF16: 2, I32: 4}[dtype]
        nb = esz
        for d in shape[1:]:
            nb *= d
        nb = (nb + 31) // 32 * 32
        assert self.sb_off + nb <= self.arena1, f"SBUF arena overflow at {name}: {self.sb_off + nb - self.arena0}"
        self.n_alloc += 1
        t = self.nc.alloc_sbuf_tensor_at(f"s{self.n_alloc}_{name}", list(shape), dtype, offset=self.sb_off)
        self.sb_off += nb
        self.sb_peak = max(self.sb_peak, self.sb_off)
        return Buf(t, nres, name)

    def mark(self):
        return self.sb_off

    def release(self, mark):
        self.barrier()
        self.sb_off = mark

    def barrier(self):
        ev = [(k, self.cnt[k]) for k in self.ENGS if self.cnt[k] > 0]
        ev += [(f"d{i}", self.dval[i]) for i in range(self.nd) if self.dval[i] > 0]
        if "cc" in self.semobj:
            ev.append(("cc", self.ccval))
        for k in self.ENGS:
            self._wait(k, ev)

    def psum(self, name, shape, dtype=F32, nres=1):
        return Buf(self.nc.alloc_psum_tensor("p_" + name, list(shape), dtype), nres, name)

    def dram(self, name, shape, dtype, kind="Internal", nres=1):
        return Buf(self.nc.dram_tensor(name, list(shape), dtype, kind=kind), nres, name)

    def _wait(self, eng, deps):
        for key, val in deps:
            if key == eng and not self.SAME_SYNC[eng]:
                continue
            if self.seen[eng].get(key, 0) >= val:
                continue
            self.seen[eng][key] = val
            self.lists[eng].append(("w", key, val))

    @staticmethod
    def _deps(reads, writes):
        deps = []
        for r in reads:
            if r.w is not None:
                deps.append(r.w)
        for w in writes:
            if w.w is not None:
                deps.append(w.w)
            deps.extend(w.r.items())
        return deps

    @staticmethod
    def _commit(ev, reads, writes):
        for r in reads:
            if r.r.get(ev[0], 0) < ev[1]:
                r.r[ev[0]] = ev[1]
        for w in writes:
            w.w = ev
            w.r = {}

    def op(self, eng, fn, reads=(), writes=()):
        self._wait(eng, self._deps(reads, writes))
        self.cnt[eng] += 1
        ev = (eng, self.cnt[eng])
        self.lists[eng].append(("o", fn, eng, 1))
        self._commit(ev, reads, writes)
        self.n_ops += 1
        return ev

    def dma(self, q, fn, reads=(), writes=()):
        half = self.nd // 2
        if q == "pool":
            i = half + self.dnext_sw
            self.dnext_sw = (self.dnext_sw + 1) % (self.nd - half)
        else:
            i = self.dnext
            self.dnext = (self.dnext + 1) % half
        key = f"d{i}"
        deps = self._deps(reads, writes)
        if self.dval[i] > 0:
            deps.append((key, self.dval[i]))
        self._wait(q, deps)
        self.dval[i] += 16
        ev = (key, self.dval[i])
        self.lists[q].append(("o", fn, key, 16))
        self._commit(ev, reads, writes)
        self.n_ops += 1
        return ev

    def coll(self, fn, reads=(), writes=()):
        if "cc" not in self.semobj:
            self.semobj["cc"] = self.nc.alloc_semaphore("sem_cc")
            self.ccval = 0
        deps = self._deps(reads, writes)
        if self.ccval > 0:
            deps.append(("cc", self.ccval))
        self._wait("pool", deps)
        self.ccval += 1
        ev = ("cc", self.ccval)
        self.lists["pool"].append(("o", fn, "cc", 1))
        self._commit(ev, reads, writes)
        return ev

    def dma_copy(self, q, out, in_, reads=(), writes=(), **kw):
        return self.dma(q, lambda e: e.dma_start(out=out, in_=in_, **kw), reads, writes)

    def finalize(self):
        final = [(k, self.cnt[k]) for k in self.ENGS if self.cnt[k] > 0 and k != "sp"]
        final += [(f"d{i}", self.dval[i]) for i in range(self.nd) if self.dval[i] > 0]
        if "cc" in self.semobj:
            final.append(("cc", self.ccval))
        self._wait("sp", final)
        nc = self.nc
        lists = self.lists
        semobj = self.semobj

        def run(e, items):
            for it in items:
                if it[0] == "w":
                    e.wait_ge(semobj[it[1]], it[2])
                else:
                    ins = it[1](e)
                    ins.then_inc(semobj[it[2]], it[3])

        with nc.Block() as block:
            @block.tensor
            def _(e):
                run(e, lists["pe"])

            @block.scalar
            def _(e):
                run(e, lists["act"])

            @block.vector
            def _(e):
                run(e, lists["dve"])

            @block.gpsimd
            def _(e):
                run(e, lists["pool"])

            @block.sync
            def _(e):
                run(e, lists["sp"])


PENG = os.environ.get('PENG', 'pool')
GM = int(os.environ.get('GM', '9'))
NT = 18
NTOK = 2304
BLKS = [(0, 512), (512, 512), (1024, 512), (1536, 512), (2048, 256)]
LN_EPS = 1e-6
RMS_EPS = 1e-6

C_AQ, C_AK, C_AV, C_BQ, C_BK, C_BV, C_BR, C_BA, C_CQ, C_CK, C_CV, C_G = (
    0, 512, 640, 768, 1024, 1280, 1792, 2304, 2336, 2848, 3360, 3872)


class Ctx:
    pass


def mk_helpers(P):
    H = Ctx()

    def MM(out, lhsT, rhs, start, stop, reads, writes):
        P.op("pe", lambda e: e.matmul(out, lhsT=lhsT, rhs=rhs, start=start, stop=stop), reads, writes)

    def TR(out, in_, ident, reads, writes):
        P.op("pe", lambda e: e.transpose(out, in_, ident), reads, writes)

    def ACT(out, in_, func, reads, writes, bias=None, scale=None):
        kw = {}
        if bias is not None:
            kw["bias"] = bias
        if scale is not None:
            kw["scale"] = scale
        P.op("act", lambda e: e.activation(out=out, in_=in_, func=func, **kw), reads, writes)

    def TT(eng, out, in0, in1, op, reads, writes):
        P.op(eng, lambda e: e.tensor_tensor(out=out, in0=in0, in1=in1, op=op), reads, writes)

    def TS(eng, out, in0, s1, s2, op0, op1, reads, writes):
        if op1 is None:
            P.op(eng, lambda e: e.tensor_scalar(out=out, in0=in0, scalar1=s1, scalar2=None, op0=op0), reads, writes)
        else:
            P.op(eng, lambda e: e.tensor_scalar(out=out, in0=in0, scalar1=s1, scalar2=s2, op0=op0, op1=op1), reads, writes)

    def STT(eng, out, in0, scalar, in1, op0, op1, reads, writes):
        P.op(eng, lambda e: e.scalar_tensor_tensor(out=out, in0=in0, scalar=scalar, in1=in1, op0=op0, op1=op1), reads, writes)

    def CP(eng, out, in_, reads, writes):
        P.op(eng, lambda e: e.tensor_copy(out=out, in_=in_), reads, writes)

    def MS(eng, ap, val, writes):
        P.op(eng, lambda e: e.memset(ap, val), (), writes)

    H.MM, H.TR, H.ACT, H.TT, H.TS, H.STT, H.CP, H.MS = MM, TR, ACT, TT, TS, STT, CP, MS
    return H


class Banks:
    def __init__(self, P, n=8, bufs=None):
        self.b = list(bufs) if bufs is not None else [P.psum(f"bank{i}", [128, 512], F32) for i in range(n)]
        self.i = 0
        self.n = len(self.b)

    def get(self):
        b = self.b[self.i]
        self.i = (self.i + 1) % self.n
        return b


class Ring:
    def __init__(self, bufs):
        self.bufs = bufs
        self.i = 0

    def get(self):
        b = self.bufs[self.i]
        self.i = (self.i + 1) % len(self.bufs)
        return b


def tile_res(buf, t0, n):
    return [buf.rs[i] for i in range(t0 // 128, (t0 + n + 127) // 128)]


ALPHA = 4 ** 0.25
NKT = 34
NBLK = 68
NSLOT = NBLK * 128
BIG = 1.0e9
def emit_l1(P, E, L):
    nc = P.nc
    H = mk_helpers(P)
    MM, TR, ACT, TT, TS, STT, CP, MS = H.MM, H.TR, H.ACT, H.TT, H.TS, H.STT, H.CP, H.MS
    din = lambda name, shape, dt=F32: E.din(name, shape, dt, L)
    dout = lambda name, shape, dt: E.dout(name, shape, dt, L)
    m_stage = P.mark()

    x_in = din("x_in", [NTOK, 1024])
    cvec = din("cvec", [2, 1024])
    w_mod = din("w_mod", [1024, 6144])
    b_mod = din("b_mod", [1, 6144])
    w_in = din("w_in", [1024, 6944])
    b_in = din("b_in", [1, 6944])
    qn_g = din("qn_g", [64, 1])
    kn_g = din("kn_g", [64, 1])
    wgate_d = din("wgate", [2, 16, 256])
    bgate_d = din("bgate", [2, 256])
    cos_d = din("cosT", [128, NTOK])
    sin_d = din("sinT", [128, NTOK])
    ident_d = din("ident", [128, 128])
    rot_d = din("rotM", [128, 128])
    oblk_d = din("onesblk", [128, 128])

    gT = dout("gT", [3072, NTOK], BF16)
    rT = dout("rT", [512, NTOK], BF16)
    cqT = dout("cqT", [512, NTOK], BF16)
    ckT = dout("ckT", [512, NTOK], BF16)
    cv = dout("cv", [NTOK, 512], BF16)
    bqT = dout("bqT", [256, NTOK], F32)
    bkT = dout("bkT", [256, NTOK], F32)
    bk = dout("bk", [NTOK, 256], F32)
    bv = dout("bv", [NTOK, 512], BF16)
    Gd = dout("Gd", [NTOK, 2, 256], F32)
    aqT = dout("aqT", [512, NTOK], BF16)
    akT = dout("akT", [128, NTOK], BF16)
    av = dout("av", [NTOK, 128], BF16)
    modD = dout("modD", [2, 6144], F32)

    banks = Banks(P, 0, E.fb)
    pT = E.bb

    ident = P.sbuf("ident", [128, 128], BF16)
    P.dma_copy("pool", ident[:], ident_d.t[:, :], writes=[ident.r])
    oblk = P.sbuf("oblk", [128, 128], BF16)
    P.dma_copy("pool", oblk[:], oblk_d.t[:, :], writes=[oblk.r])
    rotM = P.sbuf("rotM", [128, 128], F32)
    P.dma_copy("sp", rotM[:], rot_d.t[:, :], writes=[rotM.r])
    cosT = P.sbuf("cosT", [128, NTOK], F32)
    sinT = P.sbuf("sinT", [128, NTOK], F32)
    P.dma_copy("sp", cosT[:], cos_d.t[:, :], writes=[cosT.r])
    P.dma_copy("sp", sinT[:], sin_d.t[:, :], writes=[sinT.r])
    g8 = P.sbuf("g8", [128, 2], F32)
    for hh in range(2):
        P.dma_copy("sp", g8[hh * 64:(hh + 1) * 64, 0:1], qn_g.t[:, :], writes=[g8.r])
        P.dma_copy("sp", g8[hh * 64:(hh + 1) * 64, 1:2], kn_g.t[:, :], writes=[g8.r])
    TS("dve", g8[:], g8[:], 8.0, None, ALU.mult, None, [g8.r], [g8.r])
    wgate = P.sbuf("wgate", [16, 2, 256], BF16)
    P.dma_copy("pool", wgate[:], wgate_d.t.rearrange("d r c -> r d c"), writes=[wgate.r])
    bg_bc = P.sbuf("bg_bc", [128, 2, 256], F32)
    for d in range(2):
        P.dma_copy("sp", bg_bc[:, d, :], bgate_d.t[d:d + 1, :].partition_broadcast(128), writes=[bg_bc.r])

    epsc = P.sbuf("epsc", [128, 2], F32)
    MS("dve", epsc[:, 0:1], LN_EPS, [epsc.r])
    MS("dve", epsc[:, 1:2], 64.0 * RMS_EPS, [epsc.r])
    cT = P.sbuf("cT", [128, 8, 2], F32)
    for j in range(2):
        P.dma_copy("sp", cT[:, :, j], cvec.t[j:j + 1, :].rearrange("o (k p) -> p (o k)", p=128), writes=[cT.r],
                   allow_slow_non_contiguous=True)
    scT = P.sbuf("scT", [128, 8, 2], BF16)
    ACT(scT[:], cT[:], AF.Silu, [cT.r], [scT.r])
    bm_r = Ring([P.sbuf(f"bm{i}", [2, 512], F32) for i in range(2)])
    mr_r = Ring([P.sbuf(f"mr{i}", [2, 512], F32) for i in range(2)])
    wsl = Ring([P.sbuf(f"wsg{i}", [128, 8, 512], BF16) for i in range(2)])
    for g in range(12):
        wb = wsl.get()
        P.dma_copy("pool", wb[:], w_mod.t[:, g * 512:(g + 1) * 512].rearrange("(k p) c -> p k c", p=128),
                   writes=[wb.r])
        bm = bm_r.get()
        for j in range(2):
            P.dma_copy("sp", bm[j:j + 1, :], b_mod.t[0:1, g * 512:(g + 1) * 512], writes=[bm.r])
        ps = banks.get()
        for k in range(8):
            MM(ps[0:2, 0:512], scT[:, k, :], wb[:, k, :], k == 0, k == 7, [scT.r, wb.r], [ps.r])
        mr = mr_r.get()
        TT("dve", mr[:], ps[0:2, 0:512], bm[:], ALU.add, [ps.r, bm.r], [mr.r])
        P.dma_copy("sp", modD.t[:, g * 512:(g + 1) * 512], mr[:], reads=[mr.r], writes=[modD.r])
    modc = P.sbuf("modc", [128, 2, 6, 8], F32)
    for j in range(2):
        for m in range(2):
            P.dma_copy("sp", modc[:, j, m, :],
                       modD.t[j:j + 1, m * 1024:(m + 1) * 1024].rearrange("o (k p) -> p (o k)", p=128),
                       reads=[modD.r], writes=[modc.r], allow_slow_non_contiguous=True)
    TS("dve", modc[:, :, 1, :], modc[:, :, 1, :], 1.0, None, ALU.add, None, [modc.r], [modc.r])

    hT = P.sbuf("hT", [128, 8, NTOK], BF16, nres=NT)
    xin = Ring([P.sbuf(f"xin{i}", [128, 1024], F32) for i in range(2)])
    xnr = Ring([P.sbuf(f"xn{i}", [128, 1024], BF16) for i in range(2)])
    str_ = Ring([P.sbuf(f"st{i}", [128, 2, 6], F32) for i in range(2)])
    mvr = Ring([P.sbuf(f"mv{i}", [128, 2], F32) for i in range(2)])
    for i in range(NT):
        xt = xin.get()
        P.dma_copy("sp", xt[:], x_in.t[i * 128:(i + 1) * 128, :], writes=[xt.r])
        st = str_.get()
        for c in range(2):
            P.op("dve", lambda e, st=st, xt=xt, c=c: e.bn_stats(out=st[:, c, :], in_=xt[:, c * 512:(c + 1) * 512]),
                 [xt.r], [st.r])
        mv = mvr.get()
        P.op("dve", lambda e, st=st, mv=mv: e.bn_aggr(out=mv[:], in_=st[:].rearrange("p a b -> p (a b)")),
             [st.r], [mv.r])
        ACT(mv[:, 1:2], mv[:, 1:2], AF.Sqrt, [mv.r], [mv.r], bias=epsc[:, 0:1])
        P.op("dve", lambda e, mv=mv: e.reciprocal(out=mv[:, 1:2], in_=mv[:, 1:2]), [mv.r], [mv.r])
        xn = xnr.get()
        TS("dve", xn[:], xt[:], mv[:, 0:1], mv[:, 1:2], ALU.subtract, ALU.mult, [xt.r, mv.r], [xn.r])
        for k in range(8):
            TR(pT[:, k, :], xn[:, k * 128:(k + 1) * 128], ident[:], [xn.r, ident.r], [pT.r])
        j = 0 if i < 16 else 1
        for k in range(8):
            o = hT[:, k, i * 128:(i + 1) * 128]
            if k % 2 == 0:
                ACT(o, pT[:, k, :], AF.Identity, [pT.r, modc.r], [hT.rs[i]],
                    bias=modc[:, j, 0, k:k + 1], scale=modc[:, j, 1, k:k + 1])
            else:
                TS("dve", o, pT[:, k, :], modc[:, j, 1, k:k + 1], modc[:, j, 0, k:k + 1], ALU.mult, ALU.add,
                   [pT.r, modc.r], [hT.rs[i]])

    bcols = P.sbuf("bcols", [128, 48], F32)
    bcol_idx = {}
    nb = 0
    for (c0, ng) in [(C_AQ, 4), (C_AK, 1), (C_BQ, 2), (C_BK, 2), (C_BR, 4), (C_CQ, 4), (C_CK, 4), (C_G, 24)]:
        P.dma_copy("sp", bcols[:, nb:nb + ng],
                   b_in.t[0:1, c0:c0 + ng * 128].rearrange("o (g p) -> p (o g)", p=128),
                   writes=[bcols.r], allow_slow_non_contiguous=True)
        for g in range(ng):
            bcol_idx[c0 + g * 128] = nb + g
        nb += ng
    bacol = P.sbuf("bacol", [16, 2], F32)
    P.dma_copy("sp", bacol[:], b_in.t[0:1, C_BA:C_BA + 32].rearrange("o (d p) -> p (o d)", p=16),
               writes=[bacol.r], allow_slow_non_contiguous=True)
    bias_bc = P.sbuf("bias_bc", [128, 1408], F32)
    tm_groups = [(C_AV, 128, 0), (C_BK, 256, 128), (C_BV, 512, 384), (C_CV, 512, 896)]
    for (c0, n, o) in tm_groups:
        P.dma_copy("sp", bias_bc[:, o:o + n], b_in.t[0:1, c0:c0 + n].partition_broadcast(128), writes=[bias_bc.r])

    mark1b = P.mark()
    stg_bf = Ring([P.sbuf(f"stgbf{i}", [128, NTOK], BF16) for i in range(3)])
    stg_f = Ring([P.sbuf(f"stgf{i}", [128, NTOK], F32) for i in range(2)])
    aux = {n: Ring([P.sbuf(f"{n}{i}", [128, 512], dt) for i in range(2)])
           for n, dt in [("zq", F32), ("sq", BF16), ("rs", F32), ("qn", F32), ("t1", F32), ("t2", F32)]}
    stm_bf = Ring([P.sbuf(f"stmbf{i}", [128, 512], BF16) for i in range(3)])
    stm_f = Ring([P.sbuf(f"stmf{i}", [128, 256], F32) for i in range(2)])
    aT = P.sbuf("aT", [16, 2, NTOK], BF16)

    def load_w(c0, n):
        wb = wsl.get()
        P.dma_copy("pool", wb[:, :, 0:n], w_in.t[:, c0:c0 + n].rearrange("(k p) c -> p k c", p=128),
                   writes=[wb.r])
        return wb

    def fm_mm(wb, off, m, t0, n):
        ps = banks.get()
        rd = [wb.r] + tile_res(hT, t0, n)
        for k in range(8):
            MM(ps[0:m, 0:n], wb[:, k, off:off + m], hT[:, k, t0:t0 + n], k == 0, k == 7, rd, [ps.r])
        return ps

    def fm_simple(wb, off, c0, func, dst, row0, f32=False, scale=None):
        stg = stg_f.get() if f32 else stg_bf.get()
        bc = bcols[:, bcol_idx[c0]:bcol_idx[c0] + 1]
        for (t0, n) in BLKS:
            ps = fm_mm(wb, off, 128, t0, n)
            ACT(stg[:, t0:t0 + n], ps[:, 0:n], func, [ps.r, bcols.r], [stg.r], bias=bc)
        P.dma_copy("sp", dst.t[row0:row0 + 128, :], stg[:], reads=[stg.r], writes=[dst.r])

    def fm_qk(wb, off, c0, gcol, dst, row0):
        stg = stg_bf.get()
        bc = bcols[:, bcol_idx[c0]:bcol_idx[c0] + 1]
        for (t0, n) in BLKS:
            ps = fm_mm(wb, off, 128, t0, n)
            zq, sq, rs, qn, t1, t2 = (aux[k].get() for k in ("zq", "sq", "rs", "qn", "t1", "t2"))
            ACT(zq[:, 0:n], ps[:, 0:n], AF.Identity, [ps.r, bcols.r], [zq.r], bias=bc)
            ACT(sq[:, 0:n], ps[:, 0:n], AF.Square, [ps.r, bcols.r], [sq.r], bias=bc)
            ss = banks.get()
            MM(ss[:, 0:n], oblk[:], sq[:, 0:n], True, True, [oblk.r, sq.r], [ss.r])
            ACT(rs[:, 0:n], ss[:, 0:n], AF.Sqrt, [ss.r], [rs.r], bias=epsc[:, 1:2])
            P.op("dve", lambda e, rs=rs, n=n: e.reciprocal(out=rs[:, 0:n], in_=rs[:, 0:n]), [rs.r], [rs.r])
            STT("dve", qn[:, 0:n], zq[:, 0:n], g8[:, gcol:gcol + 1], rs[:, 0:n], ALU.mult, ALU.mult,
                [zq.r, g8.r, rs.r], [qn.r])
            rot = banks.get()
            MM(rot[:, 0:n], rotM[:], qn[:, 0:n], True, True, [rotM.r, qn.r], [rot.r])
            TT("pool", t1[:, 0:n], qn[:, 0:n], cosT[:, t0:t0 + n], ALU.mult, [qn.r, cosT.r], [t1.r])
            TT("dve", t2[:, 0:n], rot[:, 0:n], sinT[:, t0:t0 + n], ALU.mult, [rot.r, sinT.r], [t2.r])
            TT("dve", stg[:, t0:t0 + n], t1[:, 0:n], t2[:, 0:n], ALU.add, [t1.r, t2.r], [stg.r])
        P.dma_copy("sp", dst.t[row0:row0 + 128, :], stg[:], reads=[stg.r], writes=[dst.r])

    def tm_group(wb, off, n, bo, dst, f32=False):
        for i in range(NT):
            ps = banks.get()
            rd = [wb.r, hT.rs[i]]
            for k in range(8):
                MM(ps[:, 0:n], hT[:, k, i * 128:(i + 1) * 128], wb[:, k, off:off + n], k == 0, k == 7, rd, [ps.r])
            stg = stm_f.get() if f32 else stm_bf.get()
            TT("dve", stg[:, 0:n], ps[:, 0:n], bias_bc[:, bo:bo + n], ALU.add, [ps.r, bias_bc.r], [stg.r])
            P.dma_copy("sp", dst.t[i * 128:(i + 1) * 128, :], stg[:, 0:n], reads=[stg.r], writes=[dst.r])

    wb = load_w(C_AQ, 512)
    for s in range(4):
        fm_qk(wb, s * 128, C_AQ + s * 128, 0, aqT, s * 128)
    wb = load_w(C_AK, 256)
    fm_qk(wb, 0, C_AK, 1, akT, 0)
    tm_group(wb, 128, 128, 0, av)
    wb = load_w(C_BQ, 512)
    for s in range(2):
        fm_simple(wb, s * 128, C_BQ + s * 128, AF.Identity, bqT, s * 128, f32=True)
    for s in range(2):
        fm_simple(wb, 256 + s * 128, C_BK + s * 128, AF.Identity, bkT, s * 128, f32=True)
    tm_group(wb, 256, 256, 128, bk, f32=True)
    wb = load_w(C_BV, 512)
    tm_group(wb, 0, 512, 384, bv)
    wb = load_w(C_BR, 512)
    for s in range(4):
        fm_simple(wb, s * 128, C_BR + s * 128, AF.Silu, rT, s * 128)
    wb = load_w(C_BA, 32)
    for d in range(2):
        for (t0, n) in BLKS:
            ps = fm_mm(wb, d * 16, 16, t0, n)
            ACT(aT[0:16, d, t0:t0 + n], ps[0:16, 0:n], AF.Identity, [ps.r, bacol.r], [aT.r], bias=bacol[:, d:d + 1])
    tg_r = Ring([P.sbuf(f"tg{i}", [128, 256], F32) for i in range(2)])
    te_r = Ring([P.sbuf(f"te{i}", [128, 256], F32) for i in range(2)])
    gst_r = Ring([P.sbuf(f"gst{i}", [128, 2, 256], F32) for i in range(2)])
    for i in range(NT):
        gst = gst_r.get()
        for d in range(2):
            ps = banks.get()
            MM(ps[:, 0:256], aT[0:16, d, i * 128:(i + 1) * 128], wgate[0:16, d, :], True, True,
               [aT.r, wgate.r], [ps.r])
            tg = tg_r.get()
            te = te_r.get()
            TT("dve", tg[:], ps[:, 0:256], bg_bc[:, d, :], ALU.add, [ps.r, bg_bc.r], [tg.r])
            ACT(te[:], tg[:], AF.Exp, [tg.r], [te.r], scale=-1.0)
            ACT(gst[:, d, :], te[:], AF.Ln, [te.r], [gst.r], bias=1.0)
        P.dma_copy("sp", Gd.t[i * 128:(i + 1) * 128, :, :], gst[:], reads=[gst.r], writes=[Gd.r])
    wb = load_w(C_CQ, 512)
    for s in range(4):
        fm_simple(wb, s * 128, C_CQ + s * 128, AF.Identity, cqT, s * 128)
    wb = load_w(C_CK, 512)
    for s in range(4):
        fm_simple(wb, s * 128, C_CK + s * 128, AF.Identity, ckT, s * 128)
    wb = load_w(C_CV, 512)
    tm_group(wb, 0, 512, 896, cv)
    for gsup in range(6):
        wb = load_w(C_G + gsup * 512, 512)
        for s in range(4):
            fm_simple(wb, s * 128, C_G + gsup * 512 + s * 128, AF.Sigmoid, gT, gsup * 512 + s * 128)

    P.release(mark1b)
    tri_d = {n: din(n, [128, 128]) for n in ("triInc", "triDec", "triSgt", "triSlt")}
    flags_d = din("flags", [64, 2])
    Og = dout("Og", [2, 512, NTOK], F32)
    qBT = dout("qBT", [2, 256, 2048], BF16)
    Sfin = dout("Sfin", [2, 64, 512], F32)
    tri = {}
    for n in tri_d:
        tri[n] = P.sbuf("c_" + n, [128, 128], F32)
        P.dma_copy("sp", tri[n][:], tri_d[n].t[:, :], writes=[tri[n].r])
    flags = P.sbuf("flags", [64, 2], F32)
    P.dma_copy("sp", flags[:], flags_d.t[:, :], writes=[flags.r])
    Sst = [P.sbuf(f"S{d}", [64, 4, 128], F32) for d in range(2)]
    Sbf = [P.sbuf(f"Sbf{d}", [64, 4, 128], BF16) for d in range(2)]
    Dcum = [P.sbuf(f"Dcum{d}", [64, 4], F32) for d in range(2)]
    rq = [Ring([P.sbuf(f"gq{d}{i}", [64, 4, 128], F32) for i in range(2)]) for d in range(2)]
    rk = [Ring([P.sbuf(f"gk{d}{i}", [64, 4, 128], F32) for i in range(2)]) for d in range(2)]
    rkt = [Ring([P.sbuf(f"gkt{d}{i}", [128, 256], F32) for i in range(2)]) for d in range(2)]
    rv = [Ring([P.sbuf(f"gv{d}{i}", [128, 512], BF16) for i in range(2)]) for d in range(2)]
    rG = [Ring([P.sbuf(f"gG{d}{i}", [128, 256], F32) for i in range(2)]) for d in range(2)]
    reb = [Ring([P.sbuf(f"geb{d}{i}", [64, 4, 128], F32) for i in range(2)]) for d in range(2)]
    rei = [Ring([P.sbuf(f"gei{d}{i}", [64, 4, 128], F32) for i in range(2)]) for d in range(2)]
    rqb = [Ring([P.sbuf(f"gqb{d}{i}", [64, 4, 128], BF16) for i in range(2)]) for d in range(2)]
    rkb = [Ring([P.sbuf(f"gkb{d}{i}", [64, 4, 128], BF16) for i in range(2)]) for d in range(2)]
    rkr = [Ring([P.sbuf(f"gkr{d}{i}", [128, 256], F32) for i in range(2)]) for d in range(2)]
    rke = [Ring([P.sbuf(f"gke{d}{i}", [128, 256], BF16) for i in range(2)]) for d in range(2)]
    rsc = [Ring([P.sbuf(f"gsc{d}{i}", [128, 4, 128], BF16) for i in range(2)]) for d in range(2)]
    rO = [Ring([P.sbuf(f"gO{d}{i}", [128, 4, 128], F32) for i in range(2)]) for d in range(2)]
    rqB = [Ring([P.sbuf(f"gqB{d}{i}", [64, 4, 128], BF16) for i in range(2)]) for d in range(2)]

    def gla_step(d, i, lat):
        cumM = tri["triInc"] if d == 0 else tri["triDec"]
        remM = tri["triSgt"] if d == 0 else tri["triSlt"]
        endc = 127 if d == 0 else 0
        t0 = i * 128
        S, Sb, Dc = Sst[d], Sbf[d], Dcum[d]
        q_t, k_t, kt_t, v_t, G_t = rq[d].get(), rk[d].get(), rkt[d].get(), rv[d].get(), rG[d].get()
        P.dma_copy("sp", q_t[:], bqT.t[:, t0:t0 + 128].rearrange("(h p) t -> p h t", p=64), reads=[bqT.r], writes=[q_t.r])
        P.dma_copy("sp", k_t[:], bkT.t[:, t0:t0 + 128].rearrange("(h p) t -> p h t", p=64), reads=[bkT.r], writes=[k_t.r])
        P.dma_copy("sp", kt_t[:], bk.t[t0:t0 + 128, :], reads=[bk.r], writes=[kt_t.r])
        P.dma_copy("sp", v_t[:], bv.t[t0:t0 + 128, :], reads=[bv.r], writes=[v_t.r])
        P.dma_copy("sp", G_t[:], Gd.t[t0:t0 + 128, d, :], reads=[Gd.r], writes=[G_t.r])
        cps = banks.get()
        for h in range(4):
            MM(cps[0:64, h * 128:(h + 1) * 128], G_t[:, h * 64:(h + 1) * 64], cumM[:], True, True, [G_t.r, cumM.r], [cps.r])
        eb, ei = reb[d].get(), rei[d].get()
        ACT(eb[:].rearrange("p a t -> p (a t)"), cps[0:64, :], AF.Exp, [cps.r], [eb.r], scale=-1.0 / 16)
        ACT(ei[:].rearrange("p a t -> p (a t)"), cps[0:64, :], AF.Exp, [cps.r], [ei.r], scale=1.0 / 16)
        qb, kb = rqb[d].get(), rkb[d].get()
        STT("dve", qb[:], q_t[:], 0.125, eb[:], ALU.mult, ALU.mult, [q_t.r, eb.r], [qb.r])
        TT(PENG, kb[:], k_t[:], ei[:], ALU.mult, [k_t.r, ei.r], [kb.r])
        rps = banks.get()
        MM(rps[:, 0:256], remM[:], G_t[:], True, True, [remM.r, G_t.r], [rps.r])
        kr = rkr[d].get()
        ACT(kr[:], rps[:, 0:256], AF.Exp, [rps.r], [kr.r], scale=-1.0 / 16)
        ke = rke[d].get()
        TT(PENG, ke[:], kt_t[:], kr[:], ALU.mult, [kt_t.r, kr.r], [ke.r])
        sps = banks.get()
        for h in range(4):
            MM(sps[:, h * 128:(h + 1) * 128], kb[:, h, :], qb[:, h, :], True, True, [kb.r, qb.r], [sps.r])
        sc = rsc[d].get()
        TT("dve", sc[:], sps[:].rearrange("p (h t) -> p h t", h=4),
           cumM[:].unsqueeze(1).to_broadcast([128, 4, 128]), ALU.mult, [sps.r, cumM.r], [sc.r])
        ops_ = banks.get()
        for h in range(4):
            MM(ops_[:, h * 128:(h + 1) * 128], Sb[:, h, :], qb[:, h, :], True, False, [Sb.r, qb.r], [ops_.r])
            MM(ops_[:, h * 128:(h + 1) * 128], v_t[:, h * 128:(h + 1) * 128], sc[:, h, :],
               False, True, [v_t.r, sc.r], [ops_.r])
        Ot = rO[d].get()
        ACT(Ot[:].rearrange("p h t -> p (h t)"), ops_[:, :], AF.Identity, [ops_.r], [Ot.r])
        P.dma_copy("sp", Og.t[d, :, t0:t0 + 128].rearrange("(h p) t -> p h t", p=128), Ot[:], reads=[Ot.r], writes=[Og.r])
        if lat:
            qB = rqB[d].get()
            TT(PENG, qB[:], qb[:], Dc[:].unsqueeze(2).to_broadcast([64, 4, 128]), ALU.mult, [qb.r, Dc.r], [qB.r])
            P.dma_copy("sp", qBT.t[d, :, t0:t0 + 128].rearrange("(h p) t -> p h t", p=64), qB[:], reads=[qB.r], writes=[qBT.r])
            TT("dve", Dc[:], Dc[:], eb[:, :, endc], ALU.mult, [Dc.r, eb.r], [Dc.r])
        ups = banks.get()
        for h in range(4):
            MM(ups[0:64, h * 128:(h + 1) * 128], ke[:, h * 64:(h + 1) * 64], v_t[:, h * 128:(h + 1) * 128], True, True,
               [ke.r, v_t.r], [ups.r])
        TT("dve", S[:], S[:], eb[:, :, endc:endc + 1].to_broadcast([64, 4, 128]), ALU.mult, [S.r, eb.r], [S.r])
        TT("dve", S[:], S[:], ups[0:64, :].rearrange("p (h e) -> p h e", h=4), ALU.add, [S.r, ups.r], [S.r])
        CP(PENG, Sb[:], S[:], [S.r], [Sb.r])

    for d in range(2):
        MS("dve", Sst[d][:], 0.0, [Sst[d].r])
        MS(PENG, Sbf[d][:], 0.0, [Sbf[d].r])
        MS("dve", Dcum[d][:], 1.0, [Dcum[d].r])
    for j in range(2 if GM > 0 else 0):
        gla_step(0, 16 + j, False)
        gla_step(1, 17 - j, False)
    for d in range(2):
        TS("dve", Sst[d][:], Sst[d][:], flags[:, d:d + 1], None, ALU.mult, None, [Sst[d].r, flags.r], [Sst[d].r])
        CP(PENG, Sbf[d][:], Sst[d][:], [Sst[d].r], [Sbf[d].r])
    for j in range(16 if GM > 0 else 0):
        gla_step(0, j, True)
        gla_step(1, 15 - j, True)
    for d in range(2):
        P.dma_copy("sp", Sfin.t[d, :, :], Sst[d][:].rearrange("p h e -> p (h e)"), reads=[Sst[d].r], writes=[Sfin.r])

    P.release(m_stage)


def emit_l2(P, E, L):
    nc = P.nc
    H = mk_helpers(P)
    MM, TR, ACT, TT, TS, STT, CP, MS = H.MM, H.TR, H.ACT, H.TT, H.TS, H.STT, H.CP, H.MS
    din = lambda name, shape, dt=F32: E.din(name, shape, dt, L)
    dout = lambda name, shape, dt: E.dout(name, shape, dt, L)
    m_stage = P.mark()

    x_in = din("x_in", [NTOK, 1024])
    modD = din("modD", [2, 6144])
    aqT = din("aqT", [512, NTOK], BF16)
    akT = din("akT", [128, NTOK], BF16)
    av = din("av", [NTOK, 128], BF16)
    g_ak = din("g_ak", [256, 2048], BF16)
    g_av = din("g_av", [4096, 128], BF16)
    g_ck = din("g_ck", [1024, 768], BF16)
    g_cv = din("g_cv", [1536, 512], BF16)
    g_S = din("g_S", [256, 512])
    cqT = din("cqT", [512, NTOK], BF16)
    ckT = din("ckT", [512, NTOK], BF16)
    cv = din("cv", [NTOK, 512], BF16)
    tabNA = din("tabNA", [5, 7, 2, 128, 512], BF16)
    Og = din("Og", [2, 512, NTOK])
    qBT = din("qBT", [2, 256, 2048], BF16)
    fl1m = din("fl1m", [64, 2])
    rT = din("rT", [512, NTOK], BF16)
    gT = din("gT", [3072, NTOK], BF16)
    gn_d = din("gla_norm", [128, 1])
    wba_d = din("w_br_attn", [512, 1024])
    wbg_d = din("w_br_gla", [512, 1024])
    wbn_d = din("w_br_na", [512, 1024])
    wout_d = din("w_out", [1024, 1024])
    ln1g_d = din("ln1_g", [1, 1024])
    ln1b_d = din("ln1_b", [1, 1024])
    ones_d = din("ones128", [128, 128])
    x1_o = dout("x1", [NTOK, 1024], F32)
    h2_o = dout("h2", [NTOK, 1024], F32)

    bankS = Banks(P, 0, E.fb[0:3])
    bankA = Ring(E.fb[3:5])
    bankM = Ring(E.fb[5:7])

    oaD = dout("oaD", [512, NTOK], BF16)
    ocD = dout("ocD", [512, NTOK], BF16)
    obD = dout("obD", [512, NTOK], BF16)
    stgr = Ring([P.sbuf(f"ostg{i}", [128, 512], BF16) for i in range(3)])
    epsc = P.sbuf("epsc", [128, 2], F32)
    MS("dve", epsc[:, 0:1], LN_EPS, [epsc.r])
    MS("dve", epsc[:, 1:2], RMS_EPS, [epsc.r])
    rdr = Ring([P.sbuf(f"rd{i}", [128, 512], F32) for i in range(2)])
    rd0r = Ring([P.sbuf(f"rd0{i}", [64, 512], F32) for i in range(2)])
    ptr = Ring([P.sbuf(f"pt{i}", [128, 512], BF16) for i in range(4)])

    def blk_of(t0):
        return min(t0 // 512, 4)

    def normalize(po, n, dest, dres, view=None):
        rd = rdr.get()
        P.op("dve", lambda e: e.reciprocal(out=rd[64:128, 0:n], in_=po[64:128, 0:n]), [po.r], [rd.r])
        rd0 = rd0r.get()
        P.dma_copy("sp", rd0[0:64, 0:n], rd[64:128, 0:n], reads=[rd.r], writes=[rd0.r])
        a, b_ = po[0:64, 0:n], rd0[0:64, 0:n]
        if view is not None:
            a, b_ = view(a), view(b_)
        TT("dve", dest, a, b_, ALU.mult, [po.r, rd0.r], [dres])

    markA = P.mark()
    KT = [P.sbuf(f"KT{g}", [64, NKT * 128], BF16) for g in range(2)]
    V1 = [P.sbuf(f"V1{g}", [128, NKT, 128], BF16) for g in range(2)]
    for g in range(2):
        for r_ in range(2):
            P.dma_copy("sp", KT[g][:, r_ * 2048:(r_ + 1) * 2048], g_ak.t[r_ * 128 + g * 64:r_ * 128 + (g + 1) * 64, :],
                       reads=[g_ak.r], writes=[KT[g].r])
        P.dma_copy("sp", KT[g][:, 4096:4352], akT.t[g * 64:(g + 1) * 64, 2048:2304], reads=[akT.r], writes=[KT[g].r])
        MS("pool", V1[g][:, :, 64:128], 1.0, [V1[g].r])
        P.dma_copy("sp", V1[g][:, 0:32, 0:64], g_av.t[:, g * 64:(g + 1) * 64].rearrange("(kt p) d -> p kt d", p=128),
                   reads=[g_av.r], writes=[V1[g].r])
        P.dma_copy("sp", V1[g][:, 32:34, 0:64], av.t[2048:2304, g * 64:(g + 1) * 64].rearrange("(kt p) d -> p kt d", p=128),
                   reads=[av.r], writes=[V1[g].r])
    qbr = Ring([P.sbuf(f"qblk{i}", [64, 8, 512], BF16) for i in range(2)])
    for bi, (t0, n) in enumerate(BLKS):
        qb = qbr.get()
        P.dma_copy("sp", qb[:, :, 0:n], aqT.t[:, t0:t0 + n].rearrange("(h p) t -> p h t", p=64), writes=[qb.r])
        kts = list(range(NKT)) if bi < 4 else [32, 33]
        for h in range(8):
            g = h // 4
            po = bankA.get()
            for idx, kt in enumerate(kts):
                ps = bankS.get()
                MM(ps[:, 0:n], KT[g][:, kt * 128:(kt + 1) * 128], qb[:, h, 0:n], True, True, [KT[g].r, qb.r], [ps.r])
                pt = ptr.get()
                ACT(pt[:, 0:n], ps[:, 0:n], AF.Exp, [ps.r], [pt.r], scale=0.125)
                MM(po[:, 0:n], V1[g][:, kt, :], pt[:, 0:n], idx == 0, idx == len(kts) - 1, [V1[g].r, pt.r], [po.r])
            stg = stgr.get()
            normalize(po, n, stg[0:64, 0:n], stg.r)
            P.dma_copy("sp", oaD.t[h * 64:(h + 1) * 64, t0:t0 + n], stg[0:64, 0:n], reads=[stg.r], writes=[oaD.r])
    P.release(markA)

    markN = P.mark()
    KTc = P.sbuf("KTc", [64, 8, 256], BF16)
    V1c = P.sbuf("V1c", [128, 2, 8, 128], BF16)
    P.dma_copy("sp", KTc[:], ckT.t[:, 2048:2304].rearrange("(h p) t -> p h t", p=64), writes=[KTc.r])
    MS("pool", V1c[:, :, :, 64:128], 1.0, [V1c.r])
    for kt in range(2):
        P.dma_copy("sp", V1c[:, kt, :, 0:64],
                   cv.t[2048 + kt * 128:2048 + (kt + 1) * 128, :].rearrange("p (h d) -> p h d", d=64), writes=[V1c.r])
    qtr = Ring([P.sbuf(f"nq{i}", [64, 8, 128], BF16) for i in range(2)])
    ktr = Ring([P.sbuf(f"nk{i}", [64, 8, 896], BF16) for i in range(2)])
    vwr = Ring([P.sbuf(f"nv{i}", [128, 7, 8, 128], BF16) for i in range(2)])
    for vb in vwr.bufs:
        MS("pool", vb[:, :, :, 64:128], 1.0, [vb.r])
    tbr = Ring([P.sbuf(f"ntb{i}", [128, 512], BF16) for i in range(3)])
    tmr = Ring([P.sbuf(f"ntm{i}", [128, 512], F32) for i in range(2)])
    nptr = Ring([P.sbuf(f"npt{i}", [128, 512], BF16) for i in range(18)])
    for j in range(18):
        qt = qtr.get()
        P.dma_copy("sp", qt[:], cqT.t[:, j * 128:(j + 1) * 128].rearrange("(h p) t -> p h t", p=64), writes=[qt.r])
        if j < 16:
            kw, vw = ktr.get(), vwr.get()
            for kt in range(7):
                t_ = j + kt
                if t_ < 3:
                    ksrc, kres = g_ck.t[0:512, 384 + t_ * 128:384 + (t_ + 1) * 128], g_ck.r
                    vsrc, vres = g_cv.t[384 + t_ * 128:384 + (t_ + 1) * 128, :], g_cv.r
                elif t_ < 19:
                    ksrc, kres = ckT.t[:, (t_ - 3) * 128:(t_ - 2) * 128], ckT.r
                    vsrc, vres = cv.t[(t_ - 3) * 128:(t_ - 2) * 128, :], cv.r
                else:
                    ksrc, kres = g_ck.t[512:1024, (t_ - 19) * 128:(t_ - 18) * 128], g_ck.r
                    vsrc, vres = g_cv.t[768 + (t_ - 19) * 128:768 + (t_ - 18) * 128, :], g_cv.r
                P.dma_copy("sp", kw[:, :, kt * 128:(kt + 1) * 128], ksrc.rearrange("(h p) t -> p h t", p=64),
                           reads=[kres], writes=[kw.r])
                P.dma_copy("sp", vw[:, kt, :, 0:64], vsrc.rearrange("p (h d) -> p h d", d=64), reads=[vres], writes=[vw.r])
            kts = list(range(9))
            slot = j if j < 2 else (j - 12 if j >= 14 else 4)
        else:
            kts = [7, 8]
        for grp in range(2):
            pts = {}
            for idx, kt in enumerate(kts):
                sps = bankS.get()
                for hh in range(4):
                    h = grp * 4 + hh
                    if kt < 7:
                        lhs, rd_ = kw[:, h, kt * 128:(kt + 1) * 128], kw.r
                    else:
                        lhs, rd_ = KTc[:, h, (kt - 7) * 128:(kt - 6) * 128], KTc.r
                    MM(sps[:, hh * 128:(hh + 1) * 128], lhs, qt[:, h, :], True, True, [rd_, qt.r], [sps.r])
                pt = nptr.get()
                pts[kt] = pt
                if kt < 7:
                    tb = tbr.get()
                    P.dma_copy("sp", tb[:], tabNA.t[slot, kt, grp], writes=[tb.r])
                    tm = tmr.get()
                    STT("dve", tm[:], sps[:], 0.125, tb[:], ALU.mult, ALU.add, [sps.r, tb.r], [tm.r])
                    ACT(pt[:], tm[:], AF.Exp, [tm.r], [pt.r])
                else:
                    ACT(pt[:], sps[:], AF.Exp, [sps.r], [pt.r], scale=0.125)
            po = bankA.get()
            for hh in range(4):
                h = grp * 4 + hh
                for idx, kt in enumerate(kts):
                    pt = pts[kt]
                    if kt < 7:
                        lhs, rd_ = vw[:, kt, h, :], vw.r
                    else:
                        lhs, rd_ = V1c[:, kt - 7, h, :], V1c.r
                    MM(po[:, hh * 128:(hh + 1) * 128], lhs, pt[:, hh * 128:(hh + 1) * 128], idx == 0, idx == len(kts) - 1,
                       [rd_, pt.r], [po.r])
            stg = stgr.get()
            normalize(po, 512, stg[0:64, :], stg.r)
            P.dma_copy("sp", ocD.t[grp * 256:(grp + 1) * 256, j * 128:(j + 1) * 128].rearrange("(h p) t -> p h t", p=64),
                       stg[0:64, :].rearrange("p (h t) -> p h t", h=4), reads=[stg.r], writes=[ocD.r])
    P.release(markN)

    markG = P.mark()
    ones128 = P.sbuf("ones128", [128, 128], BF16)
    P.dma_copy("pool", ones128[:], ones_d.t[:, :], writes=[ones128.r])
    gn = P.sbuf("gn", [128, 1], F32)
    P.dma_copy("sp", gn[:], gn_d.t[:, :], writes=[gn.r])
    f1 = P.sbuf("f1", [64, 2], F32)
    P.dma_copy("sp", f1[:], fl1m.t[:, :], writes=[f1.r])
    Sin = []
    for d in range(2):
        sp_ = P.sbuf(f"Spart{d}", [64, 512], F32)
        P.dma_copy("sp", sp_[:], g_S.t[d * 128 + d * 64:d * 128 + (d + 1) * 64, :], reads=[g_S.r], writes=[sp_.r])
        sb = P.sbuf(f"Sin{d}", [64, 4, 128], BF16)
        TS("dve", sb[:].rearrange("p h e -> p (h e)"), sp_[:], f1[:, d:d + 1], None, ALU.mult, None, [sp_.r, f1.r], [sb.r])
        Sin.append(sb)
    qBr = [Ring([P.sbuf(f"qB{d}{i}", [64, 4, 512], BF16) for i in range(2)]) for d in range(2)]
    ogr = [Ring([P.sbuf(f"og{d}{i}", [128, 512], F32) for i in range(2)]) for d in range(2)]
    osr = Ring([P.sbuf(f"os{i}", [128, 512], F32) for i in range(2)])
    sqr = Ring([P.sbuf(f"sq{i}", [128, 512], BF16) for i in range(2)])
    rsr = Ring([P.sbuf(f"rs{i}", [128, 512], F32) for i in range(2)])
    rtr = Ring([P.sbuf(f"rt{i}", [128, 512], BF16) for i in range(2)])
    t3r = Ring([P.sbuf(f"t3{i}", [128, 512], F32) for i in range(2)])
    for bi, (t0, n) in enumerate(BLKS):
        lat = bi < 4
        if lat:
            qB = [qBr[d].get() for d in range(2)]
            for d in range(2):
                P.dma_copy("sp", qB[d][:, :, 0:n], qBT.t[d, :, t0:t0 + n].rearrange("(h p) t -> p h t", p=64),
                           writes=[qB[d].r])
        for h in range(4):
            og = [ogr[d].get() for d in range(2)]
            for d in range(2):
                P.dma_copy("sp", og[d][:, 0:n], Og.t[d, h * 128:(h + 1) * 128, t0:t0 + n], writes=[og[d].r])
            osum = osr.get()
            TT("pool", osum[:, 0:n], og[0][:, 0:n], og[1][:, 0:n], ALU.add, [og[0].r, og[1].r], [osum.r])
            if lat:
                pc = bankM.get()
                MM(pc[:, 0:n], Sin[0][:, h, :], qB[0][:, h, 0:n], True, False, [Sin[0].r, qB[0].r], [pc.r])
                MM(pc[:, 0:n], Sin[1][:, h, :], qB[1][:, h, 0:n], False, True, [Sin[1].r, qB[1].r], [pc.r])
                TT("dve", osum[:, 0:n], osum[:, 0:n], pc[:, 0:n], ALU.add, [osum.r, pc.r], [osum.r])
            sq = sqr.get()
            ACT(sq[:, 0:n], osum[:, 0:n], AF.Square, [osum.r], [sq.r])
            ss = bankM.get()
            MM(ss[:, 0:n], ones128[:], sq[:, 0:n], True, True, [ones128.r, sq.r], [ss.r])
            rs = rsr.get()
            ACT(rs[:, 0:n], ss[:, 0:n], AF.Sqrt, [ss.r, epsc.r], [rs.r], bias=epsc[:, 1:2], scale=1.0 / 128)
            P.op("dve", lambda e, rs=rs, n=n: e.reciprocal(out=rs[:, 0:n], in_=rs[:, 0:n]), [rs.r], [rs.r])
            rt = rtr.get()
            P.dma_copy("sp", rt[:, 0:n], rT.t[h * 128:(h + 1) * 128, t0:t0 + n], writes=[rt.r])
            t3 = t3r.get()
            STT("dve", t3[:, 0:n], osum[:, 0:n], gn[:, 0:1], rs[:, 0:n], ALU.mult, ALU.mult, [osum.r, gn.r, rs.r], [t3.r])
            stg = stgr.get()
            TT("pool", stg[:, 0:n], t3[:, 0:n], rt[:, 0:n], ALU.mult, [t3.r, rt.r], [stg.r])
            P.dma_copy("sp", obD.t[h * 128:(h + 1) * 128, t0:t0 + n], stg[:, 0:n], reads=[stg.r], writes=[obD.r])
    P.release(markG)

    wba = P.sbuf("wba", [64, 8, 1024], BF16)
    wbn = P.sbuf("wbn", [64, 8, 1024], BF16)
    wbg = P.sbuf("wbg", [128, 4, 1024], BF16)
    wo = P.sbuf("wo", [128, 8, 1024], BF16)
    P.dma_copy("pool", wba[:], wba_d.t.rearrange("(h p) f -> p h f", p=64), writes=[wba.r])
    P.dma_copy("pool", wbn[:], wbn_d.t.rearrange("(h p) f -> p h f", p=64), writes=[wbn.r])
    P.dma_copy("pool", wbg[:], wbg_d.t.rearrange("(h p) f -> p h f", p=128), writes=[wbg.r])
    P.dma_copy("pool", wo[:], wout_d.t.rearrange("(k p) f -> p k f", p=128), writes=[wo.r])
    bc = {}
    for nm, m in (("g1", 2), ("sh2", 3), ("sc2", 4)):
        for j in range(2):
            t = P.sbuf(f"bc_{nm}{j}", [128, 1024], F32)
            P.dma_copy("sp", t[:], modD.t[j:j + 1, m * 1024:(m + 1) * 1024].partition_broadcast(128), writes=[t.r])
            bc[(nm, j)] = t
    for j in range(2):
        TS("pool", bc[("sc2", j)][:], bc[("sc2", j)][:], 1.0, None, ALU.add, None, [bc[("sc2", j)].r], [bc[("sc2", j)].r])
    lg = P.sbuf("bc_ln1g", [128, 1024], F32)
    lb = P.sbuf("bc_ln1b", [128, 1024], F32)
    P.dma_copy("sp", lg[:], ln1g_d.t[0:1, :].partition_broadcast(128), writes=[lg.r])
    P.dma_copy("sp", lb[:], ln1b_d.t[0:1, :].partition_broadcast(128), writes=[lb.r])
    ymT = Ring([P.sbuf(f"ymT{i}", [128, 8, 512], BF16) for i in range(1)])
    ggr = Ring([P.sbuf(f"gg{i}", [128, 3, 512], BF16) for i in range(3)])
    tar = Ring([P.sbuf(f"ta{i}", [128, 512], F32) for i in range(2)])
    tbr2 = Ring([P.sbuf(f"tb2{i}", [128, 512], F32) for i in range(2)])
    xr = Ring([P.sbuf(f"xr{i}", [128, 1024], F32) for i in range(1)])
    ur = Ring([P.sbuf(f"ur{i}", [128, 1024], F32) for i in range(1)])
    x1r = Ring([P.sbuf(f"x1r{i}", [128, 1024], F32) for i in range(1)])
    h2r = Ring([P.sbuf(f"h2r{i}", [128, 1024], F32) for i in range(1)])
    str_ = Ring([P.sbuf(f"st{i}", [128, 2, 6], F32) for i in range(2)])
    mvr = Ring([P.sbuf(f"mv{i}", [128, 2], F32) for i in range(2)])

    def ln_stats(src):
        st = str_.get()
        for c in range(2):
            P.op("dve", lambda e, st=st, c=c: e.bn_stats(out=st[:, c, :], in_=src[:, c * 512:(c + 1) * 512]),
                 [src.r], [st.r])
        mv = mvr.get()
        P.op("dve", lambda e, st=st, mv=mv: e.bn_aggr(out=mv[:], in_=st[:].rearrange("p a b -> p (a b)")),
             [st.r], [mv.r])
        ACT(mv[:, 1:2], mv[:, 1:2], AF.Sqrt, [mv.r, epsc.r], [mv.r], bias=epsc[:, 0:1])
        P.op("dve", lambda e, mv=mv: e.reciprocal(out=mv[:, 1:2], in_=mv[:, 1:2]), [mv.r], [mv.r])
        return mv

    oabr = Ring([P.sbuf(f"oab{i}", [64, 8, 512], BF16) for i in range(2)])
    ocbr = Ring([P.sbuf(f"ocb{i}", [64, 8, 512], BF16) for i in range(2)])
    obbr = Ring([P.sbuf(f"obb{i}", [128, 4, 512], BF16) for i in range(2)])
    for bi, (t0, n) in enumerate(BLKS):
        ym = ymT.get()
        oab, ocb, obb = oabr.get(), ocbr.get(), obbr.get()
        P.dma_copy("sp", oab[:, :, 0:n], oaD.t[:, t0:t0 + n].rearrange("(h p) t -> p h t", p=64), reads=[oaD.r], writes=[oab.r])
        P.dma_copy("sp", ocb[:, :, 0:n], ocD.t[:, t0:t0 + n].rearrange("(h p) t -> p h t", p=64), reads=[ocD.r], writes=[ocb.r])
        P.dma_copy("sp", obb[:, :, 0:n], obD.t[:, t0:t0 + n].rearrange("(h p) t -> p h t", p=128), reads=[obD.r], writes=[obb.r])
        for fc in range(8):
            gg = ggr.get()
            P.dma_copy("sp", gg[:, :, 0:n],
                       gT.t[:, t0:t0 + n].rearrange("(b r) t -> r b t", b=3)[fc * 128:(fc + 1) * 128],
                       writes=[gg.r])
            fs = slice(fc * 128, (fc + 1) * 128)
            pa = bankS.get()
            for h in range(8):
                MM(pa[:, 0:n], wba[:, h, fs], oab[:, h, 0:n], h == 0, h == 7, [wba.r, oab.r], [pa.r])
            pb = bankS.get()
            for h in range(4):
                MM(pb[:, 0:n], wbg[:, h, fs], obb[:, h, 0:n], h == 0, h == 3, [wbg.r, obb.r], [pb.r])
            pcn = bankS.get()
            for h in range(8):
                MM(pcn[:, 0:n], wbn[:, h, fs], ocb[:, h, 0:n], h == 0, h == 7, [wbn.r, ocb.r], [pcn.r])
            ta, tb = tar.get(), tbr2.get()
            TT("dve", ta[:, 0:n], pa[:, 0:n], gg[:, 0, 0:n], ALU.mult, [pa.r, gg.r], [ta.r])
            TT("dve", tb[:, 0:n], pb[:, 0:n], gg[:, 1, 0:n], ALU.mult, [pb.r, gg.r], [tb.r])
            TT("pool", ta[:, 0:n], ta[:, 0:n], tb[:, 0:n], ALU.add, [ta.r, tb.r], [ta.r])
            TT("dve", tb[:, 0:n], pcn[:, 0:n], gg[:, 2, 0:n], ALU.mult, [pcn.r, gg.r], [tb.r])
            TT("pool", ym[:, fc, 0:n], ta[:, 0:n], tb[:, 0:n], ALU.add, [ta.r, tb.r], [ym.r])
        for ti in range(n // 128):
            i = t0 // 128 + ti
            j = 0 if i < 16 else 1
            xt = xr.get()
            P.dma_copy("sp", xt[:], x_in.t[i * 128:(i + 1) * 128, :], writes=[xt.r])
            u = ur.get()
            for hf in range(2):
                py = bankM.get()
                for fc in range(8):
                    MM(py[:, :], ym[:, fc, ti * 128:(ti + 1) * 128], wo[:, fc, hf * 512:(hf + 1) * 512], fc == 0, fc == 7,
                       [ym.r, wo.r], [py.r])
                TT("dve", u[:, hf * 512:(hf + 1) * 512], py[:, :], bc[("g1", j)][:, hf * 512:(hf + 1) * 512], ALU.mult,
                   [py.r, bc[("g1", j)].r], [u.r])
            STT("dve", u[:], xt[:], ALPHA, u[:], ALU.mult, ALU.add, [xt.r, u.r], [u.r])
            mv = ln_stats(u)
            x1 = x1r.get()
            TS("dve", x1[:], u[:], mv[:, 0:1], mv[:, 1:2], ALU.subtract, ALU.mult, [u.r, mv.r], [x1.r])
            TT("pool", x1[:], x1[:], lg[:], ALU.mult, [x1.r, lg.r], [x1.r])
            TT("pool", x1[:], x1[:], lb[:], ALU.add, [x1.r, lb.r], [x1.r])
            P.dma_copy("sp", x1_o.t[i * 128:(i + 1) * 128, :], x1[:], reads=[x1.r], writes=[x1_o.r])
            mv2 = ln_stats(x1)
            h2 = h2r.get()
            TS("dve", h2[:], x1[:], mv2[:, 0:1], mv2[:, 1:2], ALU.subtract, ALU.mult, [x1.r, mv2.r], [h2.r])
            TT("pool", h2[:], h2[:], bc[("sc2", j)][:], ALU.mult, [h2.r, bc[("sc2", j)].r], [h2.r])
            TT("pool", h2[:], h2[:], bc[("sh2", j)][:], ALU.add, [h2.r, bc[("sh2", j)].r], [h2.r])
            P.dma_copy("sp", h2_o.t[i * 128:(i + 1) * 128, :], h2[:], reads=[h2.r], writes=[h2_o.r])

    P.release(m_stage)


def emit_l3(P, E, L):
    nc = P.nc
    H = mk_helpers(P)
    MM, TR, ACT, TT, TS, STT, CP, MS = H.MM, H.TR, H.ACT, H.TT, H.TS, H.STT, H.CP, H.MS
    din = lambda name, shape, dt=F32: E.din(name, shape, dt, L)
    dout = lambda name, shape, dt: E.dout(name, shape, dt, L)
    m_stage = P.mark()

    x1_d = din("x1", [NTOK, 1024])
    h2_d = din("h2", [NTOK, 1024])
    modD = din("modD", [2, 6144])
    wr_d = din("w_r", [1024, 36])
    br_d = din("b_r", [1, 36])
    w1_d = din("moe_w1", [32 * 128, 4096])
    w3_d = din("moe_w3", [32 * 128, 4096])
    w2_d = din("moe_w2", [32 * 128, 4096])
    ln2g_d = din("ln2_g", [1, 1024])
    ln2b_d = din("ln2_b", [1, 1024])
    id32_d = din("ident", [128, 128])
    tris_d = din("triSlt", [128, 128])
    ones_d = din("ones128", [128, 128])
    thr_d = din("thr18", [128, 32, 18])
    thrj_d = din("thrj", [128, NBLK, 32])
    tokidx_d = din("tokidx", [128, NT], I32)
    wbase_d = din("wbase", [128, 8])
    x2_o = dout("x2", [NTOK, 1024], F32)
    h2b = dout("h2b", [NTOK + 128, 1024], BF16)
    tokslot = dout("tokslot", [NSLOT, 1], I32)
    yslot = dout("yslot", [NSLOT, 1024], F32)
    be_o = dout("be_o", [1, NBLK], I32)
    pos_o = dout("pos_o", [128, NT, 2], I32)
    gate_o = dout("gate_o", [128, NT, 2], F32)

    banks = Banks(P, 0, E.fb)
    pTb = E.bb

    id32 = P.sbuf("id32", [128, 128], F32)
    P.dma_copy("sp", id32[:], id32_d.t[:, :], writes=[id32.r])
    idb = P.sbuf("idb", [128, 128], BF16)
    P.dma_copy("pool", idb[:], id32_d.t[:, :], writes=[idb.r])
    tris = P.sbuf("tris", [128, 128], F32)
    P.dma_copy("sp", tris[:], tris_d.t[:, :], writes=[tris.r])
    ones = P.sbuf("ones", [128, 128], F32)
    P.dma_copy("sp", ones[:], ones_d.t[:, :], writes=[ones.r])
    thr = P.sbuf("thr", [128, 32, 18], F32)
    P.dma_copy("sp", thr[:], thr_d.t[:, :, :], writes=[thr.r])
    thrj = P.sbuf("thrj", [128, NBLK, 32], F32)
    P.dma_copy("sp", thrj[:], thrj_d.t[:, :, :], writes=[thrj.r])
    tokidx = P.sbuf("tokidx", [128, NT], I32)
    P.dma_copy("sp", tokidx[:], tokidx_d.t[:, :], writes=[tokidx.r])
    wr = P.sbuf("wr", [128, 8, 36], F32)
    P.dma_copy("sp", wr[:], wr_d.t.rearrange("(k p) c -> p k c", p=128), writes=[wr.r])
    br_bc = P.sbuf("br_bc", [128, 36], F32)
    P.dma_copy("sp", br_bc[:], br_d.t[0:1, :].partition_broadcast(128), writes=[br_bc.r])
    epsc = P.sbuf("epsc", [128, 1], F32)
    MS("dve", epsc[:], LN_EPS, [epsc.r])
    dum = P.sbuf("dum", [128, NBLK], I32)
    MS("pool", dum[:], NTOK, [dum.r])
    P.dma_copy("sp", tokslot.t.rearrange("(p j) o -> p (j o)", p=128), dum[:], reads=[dum.r], writes=[tokslot.r])
    zt = P.sbuf("zt", [128, 1024], BF16)
    MS("pool", zt[:], 0.0, [zt.r])
    P.dma_copy("sp", h2b.t[NTOK:NTOK + 128, :], zt[:], reads=[zt.r], writes=[h2b.r])

    oh1a = P.sbuf("oh1a", [128, NT, 32], F32)
    oh2a = P.sbuf("oh2a", [128, NT, 32], F32)
    Aall = P.sbuf("Aall", [128, NT, 32], F32)
    gates = P.sbuf("gates", [128, NT, 2], F32)
    posI = P.sbuf("posI", [128, NT, 2], I32)
    beI = P.sbuf("beI", [1, NBLK], I32)
    widx = P.sbuf("widx", [128, NBLK], I32)
    wbase = P.sbuf("wbase", [128, 8], F32)
    P.dma_copy("sp", wbase[:], wbase_d.t[:, :], writes=[wbase.r])
    carry = P.sbuf("carry", [128, 32], F32)
    MS("dve", carry[:], 0.0, [carry.r])
    excl = P.sbuf("excl", [128, NT, 32], F32)

    markR = P.mark()
    h2r = Ring([P.sbuf(f"h2t{i}", [128, 1024], F32) for i in range(2)])
    hTr = Ring([P.sbuf(f"h2T{i}", [128, 8, 128], F32) for i in range(2)])
    lgr = Ring([P.sbuf(f"lg{i}", [128, 36], F32) for i in range(2)])
    sm = Ring([P.sbuf(f"sm{i}", [128, 16], F32) for i in range(2)])
    elr = Ring([P.sbuf(f"elm{i}", [128, 32], F32) for i in range(2)])
    el2r = Ring([P.sbuf(f"elm2{i}", [128, 32], F32) for i in range(2)])
    for i in range(NT):
        ht = h2r.get()
        P.dma_copy("sp", ht[:], h2_d.t[i * 128:(i + 1) * 128, :], writes=[ht.r])
        P.dma_copy("pool", h2b.t[i * 128:(i + 1) * 128, :], ht[:], reads=[ht.r], writes=[h2b.r])
        hT = hTr.get()
        for half in range(2):
            pt = banks.get()
            for kk in range(4):
                k = half * 4 + kk
                TR(pt[:, kk * 128:(kk + 1) * 128], ht[:, k * 128:(k + 1) * 128], id32[:], [ht.r, id32.r], [pt.r])
            if half == 0:
                ACT(hT[:, 0:4, :].rearrange("p k t -> p (k t)"), pt[:, :], AF.Identity, [pt.r], [hT.r])
            else:
                CP("dve", hT[:, 4:8, :].rearrange("p k t -> p (k t)"), pt[:, :], [pt.r], [hT.r])
        pl = banks.get()
        for k in range(8):
            MM(pl[:, 0:36], hT[:, k, :], wr[:, k, :], k == 0, k == 7, [hT.r, wr.r], [pl.r])
        lg = lgr.get()
        TT("dve", lg[:], pl[:, 0:36], br_bc[:], ALU.add, [pl.r, br_bc.r], [lg.r])
        s = sm.get()
        P.op("dve", lambda e, s=s, lg=lg: e.reduce_max(out=s[:, 0:1], in_=lg[:, 0:4], axis=AX.X), [lg.r], [s.r])
        TS("dve", s[:, 1:2], s[:, 0:1], -1.0, None, ALU.mult, None, [s.r], [s.r])
        TS("dve", s[:, 8:12], lg[:, 0:4], s[:, 0:1], None, ALU.is_equal, None, [lg.r, s.r], [s.r])
        ACT(s[:, 12:16], lg[:, 0:4], AF.Exp, [lg.r, s.r], [s.r], bias=s[:, 1:2])
        P.op("dve", lambda e, s=s: e.reduce_sum(out=s[:, 2:3], in_=s[:, 12:16], axis=AX.X), [s.r], [s.r])
        P.op("dve", lambda e, s=s: e.reciprocal(out=s[:, 3:4], in_=s[:, 2:3]), [s.r], [s.r])
        TS("dve", s[:, 12:16], s[:, 8:12], 1.0, BIG, ALU.subtract, ALU.mult, [s.r], [s.r])
        elm = elr.get()
        TT("dve", elm[:].rearrange("p (g e) -> p g e", g=4), lg[:, 4:36].rearrange("p (g e) -> p g e", g=4),
           s[:, 12:16].unsqueeze(2).to_broadcast([128, 4, 8]), ALU.add, [lg.r, s.r], [elm.r])
        P.op("dve", lambda e, s=s, elm=elm: e.reduce_max(out=s[:, 4:5], in_=elm[:], axis=AX.X), [elm.r], [s.r])
        TS("dve", oh1a[:, i, :], elm[:], s[:, 4:5], None, ALU.is_equal, None, [elm.r, s.r], [oh1a.r])
        elm2 = el2r.get()
        STT("dve", elm2[:], oh1a[:, i, :], -BIG, elm[:], ALU.mult, ALU.add, [oh1a.r, elm.r], [elm2.r])
        P.op("dve", lambda e, s=s, elm2=elm2: e.reduce_max(out=s[:, 5:6], in_=elm2[:], axis=AX.X), [elm2.r], [s.r])
        TS("dve", oh2a[:, i, :], elm2[:], s[:, 5:6], None, ALU.is_equal, None, [elm2.r, s.r], [oh2a.r])
        TT("dve", Aall[:, i, :], oh1a[:, i, :], oh2a[:, i, :], ALU.add, [oh1a.r, oh2a.r], [Aall.r])
        TT("dve", s[:, 6:7], s[:, 5:6], s[:, 4:5], ALU.subtract, [s.r], [s.r])
        ACT(s[:, 6:7], s[:, 6:7], AF.Exp, [s.r], [s.r])
        TS("dve", s[:, 6:7], s[:, 6:7], 1.0, None, ALU.add, None, [s.r], [s.r])
        P.op("dve", lambda e, s=s: e.reciprocal(out=s[:, 7:8], in_=s[:, 6:7]), [s.r], [s.r])
        TT("dve", gates[:, i, 0:1], s[:, 3:4], s[:, 7:8], ALU.mult, [s.r], [gates.r])
        TT("dve", gates[:, i, 1:2], s[:, 3:4], gates[:, i, 0:1], ALU.subtract, [s.r, gates.r], [gates.r])
        pex = banks.get()
        MM(pex[:, 0:32], tris[:], Aall[:, i, :], True, True, [tris.r, Aall.r], [pex.r])
        TT("dve", excl[:, i, :], pex[:, 0:32], carry[:], ALU.add, [pex.r, carry.r], [excl.r])
        pcs = banks.get()
        MM(pcs[:, 0:32], ones[:], Aall[:, i, :], True, True, [ones.r, Aall.r], [pcs.r])
        TT("dve", carry[:], carry[:], pcs[:, 0:32], ALU.add, [carry.r, pcs.r], [carry.r])
    cmp18 = P.sbuf("cmp18", [128, 32, 18], F32)
    TT("dve", cmp18[:], carry[:].unsqueeze(2).to_broadcast([128, 32, 18]), thr[:], ALU.is_gt, [carry.r, thr.r], [cmp18.r])
    padded = P.sbuf("padded", [128, 32], F32)
    P.op("dve", lambda e: e.reduce_sum(out=padded[:], in_=cmp18[:], axis=AX.X), [cmp18.r], [padded.r])
    TS("dve", padded[:], padded[:], 128.0, None, ALU.mult, None, [padded.r], [padded.r])
    cs = [P.sbuf(f"cs{i}", [128, 32], F32) for i in range(2)]
    CP("dve", cs[0][:], padded[:], [padded.r], [cs[0].r])
    cur = 0
    for sh in (1, 2, 4, 8, 16):
        a, b_ = cs[cur], cs[1 - cur]
        CP("dve", b_[:, 0:sh], a[:, 0:sh], [a.r], [b_.r])
        TT("dve", b_[:, sh:32], a[:, sh:32], a[:, 0:32 - sh], ALU.add, [a.r], [b_.r])
        cur = 1 - cur
    pend = cs[cur]
    pstart = P.sbuf("pstart", [128, 32], F32)
    TT("dve", pstart[:], pend[:], padded[:], ALU.subtract, [pend.r, padded.r], [pstart.r])
    posf = P.sbuf("posf", [128, NT, 2], F32)
    tmp32 = Ring([P.sbuf(f"tmp32{i}", [128, 32], F32) for i in range(2)])
    for i in range(NT):
        sb_ = tmp32.get()
        TT("dve", sb_[:], excl[:, i, :], pstart[:], ALU.add, [excl.r, pstart.r], [sb_.r])
        for k, oh in enumerate((oh1a, oh2a)):
            t2 = tmp32.get()
            TT("dve", t2[:], sb_[:], oh[:, i, :], ALU.mult, [sb_.r, oh.r], [t2.r])
            P.op("dve", lambda e, t2=t2, i=i, k=k: e.reduce_sum(out=posf[:, i, k:k + 1], in_=t2[:], axis=AX.X),
                 [t2.r], [posf.r])
    CP("dve", posI[:], posf[:], [posf.r], [posI.r])
    cmpj = P.sbuf("cmpj", [128, NBLK, 32], F32)
    TT("dve", cmpj[:], pend[:].unsqueeze(1).to_broadcast([128, NBLK, 32]), thrj[:], ALU.is_le, [pend.r, thrj.r], [cmpj.r])
    bef = P.sbuf("bef", [128, NBLK], F32)
    P.op("dve", lambda e: e.reduce_sum(out=bef[:], in_=cmpj[:], axis=AX.X), [cmpj.r], [bef.r])
    TS("dve", bef[:], bef[:], 31.0, None, ALU.min, None, [bef.r], [bef.r])
    CP("dve", beI[:], bef[0:1, :], [bef.r], [beI.r])
    used = P.sbuf("used", [128, NBLK], F32)
    TS("dve", used[:], thrj[:, :, 0], pend[:, 31:32], None, ALU.is_lt, None, [thrj.r, pend.r], [used.r])
    TS("dve", used[:], used[:], -8192.0, 8192.0, ALU.mult, ALU.add, [used.r], [used.r])
    wif = P.sbuf("wif", [128, NBLK], F32)
    TS("dve", wif[:], bef[:], 128.0, wbase[:, 0:1], ALU.mult, ALU.add, [bef.r, wbase.r], [wif.r])
    TT("dve", wif[:], wif[:], used[:], ALU.add, [wif.r, used.r], [wif.r])
    CP("dve", widx[:], wif[:], [wif.r], [widx.r])
    P.dma_copy("sp", be_o.t[:, :], beI[:], reads=[beI.r], writes=[be_o.r])
    P.dma_copy("sp", pos_o.t[:, :, :], posI[:], reads=[posI.r], writes=[pos_o.r])
    P.dma_copy("sp", gate_o.t[:, :, :], gates[:], reads=[gates.r], writes=[gate_o.r])
    for i in range(NT):
        for k in range(2):
            P.dma("pool", lambda e, i=i, k=k: e.indirect_dma_start(
                out=tokslot.t[:, :], out_offset=bass.IndirectOffsetOnAxis(ap=posI[:, i, k:k + 1], axis=0),
                in_=tokidx[:, i:i + 1], in_offset=None), reads=[posI.r, tokidx.r], writes=[tokslot.r])
    P.release(markR)

    markE = P.mark()
    w1r = Ring([P.sbuf(f"w1b{i}", [128, 8, 512], BF16) for i in range(2)])
    w3r = Ring([P.sbuf(f"w3b{i}", [128, 8, 512], BF16) for i in range(2)])
    w2r = Ring([P.sbuf(f"w2b{i}", [128, 4, 1024], BF16) for i in range(2)])
    idxr = Ring([P.sbuf(f"idx{i}", [128, 1], I32) for i in range(2)])
    xgr = Ring([P.sbuf(f"xg{i}", [128, 1024], BF16) for i in range(2)])
    xTr = Ring([P.sbuf(f"xT{i}", [128, 8, 128], BF16) for i in range(2)])
    s1r = Ring([P.sbuf(f"s1{i}", [128, 512], F32) for i in range(2)])
    aTr = Ring([P.sbuf(f"aT{i}", [128, 4, 128], BF16) for i in range(2)])
    ysr = Ring([P.sbuf(f"ys{i}", [128, 1024], F32) for i in range(2)])

    bcreg = {}
    for j in range(NBLK):
        w1b, w3b, w2b = w1r.get(), w3r.get(), w2r.get()

        for (wb_, wd_) in ((w1b, w1_d), (w3b, w3_d), (w2b, w2_d)):
            def wgather(e, wb_=wb_, wd_=wd_, j=j):
                if not hasattr(P, "bnd_val"):
                    r_ = e.alloc_register("bnd")
                    e.reg_mov(r_, 32 * 128 - 1)
                    P.bnd_val = e.snap(r_)
                bcreg["v"] = P.bnd_val
                return e.indirect_dma_start(
                    out=wb_[:].rearrange("p a f -> p (a f)"), out_offset=None, in_=wd_.t[:, :],
                    in_offset=bass.IndirectOffsetOnAxis(ap=widx[:, j:j + 1], axis=0),
                    bounds_check=bcreg["v"], oob_is_err=False)
            P.dma("pool", wgather, reads=[widx.r], writes=[wb_.r])
        idx = idxr.get()
        P.dma_copy("sp", idx[:], tokslot.t[j * 128:(j + 1) * 128, :], reads=[tokslot.r], writes=[idx.r])
        xg = xgr.get()
        P.dma("pool", lambda e, xg=xg, idx=idx: e.indirect_dma_start(
            out=xg[:, :], out_offset=None, in_=h2b.t[:, :],
            in_offset=bass.IndirectOffsetOnAxis(ap=idx[:, 0:1], axis=0)), reads=[idx.r, h2b.r], writes=[xg.r])
        for k in range(8):
            TR(pTb[:, k, :], xg[:].rearrange("p (a i) -> p a i", i=8)[:, :, k], idb[:], [xg.r, idb.r], [pTb.r])
        xT = xTr.get()
        ACT(xT[:, 0:4, :], pTb[:, 0:4, :], AF.Identity, [pTb.r], [xT.r])
        CP("dve", xT[:, 4:8, :], pTb[:, 4:8, :], [pTb.r], [xT.r])
        p1, p3 = banks.get(), banks.get()
        for (pp, wb) in ((p1, w1b), (p3, w3b)):
            for fc in range(4):
                for k in range(8):
                    MM(pp[:, fc * 128:(fc + 1) * 128], wb[:, k, :].rearrange("p (a c) -> p a c", c=4)[:, :, fc], xT[:, k, :], k == 0, k == 7,
                       [wb.r, xT.r], [pp.r])
        s1 = s1r.get()
        ACT(s1[:], p1[:, :], AF.Silu, [p1.r], [s1.r])
        aT = aTr.get()
        TT("dve", aT[:].rearrange("p f t -> p (f t)"), s1[:], p3[:, :], ALU.mult, [s1.r, p3.r], [aT.r])
        ys = ysr.get()
        for half in range(2):
            py = banks.get()
            for fc in range(4):
                MM(py[:, :], aT[:, fc, :], w2b[:, fc, half * 512:(half + 1) * 512], fc == 0, fc == 3, [aT.r, w2b.r], [py.r])
            if half == 0:
                ACT(ys[:, 0:512], py[:, :], AF.Identity, [py.r], [ys.r])
            else:
                CP("dve", ys[:, 512:1024], py[:, :], [py.r], [ys.r])
        P.dma_copy("sp", yslot.t[j * 128:(j + 1) * 128, :], ys[:], reads=[ys.r], writes=[yslot.r])
    P.release(markE)

    g2bc = []
    for j in range(2):
        t = P.sbuf(f"g2bc{j}", [128, 1024], F32)
        P.dma_copy("sp", t[:], modD.t[j:j + 1, 5 * 1024:6 * 1024].partition_broadcast(128), writes=[t.r])
        g2bc.append(t)
    lg2 = P.sbuf("lg2", [128, 1024], F32)
    lb2 = P.sbuf("lb2", [128, 1024], F32)
    P.dma_copy("sp", lg2[:], ln2g_d.t[0:1, :].partition_broadcast(128), writes=[lg2.r])
    P.dma_copy("sp", lb2[:], ln2b_d.t[0:1, :].partition_broadcast(128), writes=[lb2.r])
    y1r = Ring([P.sbuf(f"y1{i}", [128, 1024], F32) for i in range(2)])
    y2r = Ring([P.sbuf(f"y2{i}", [128, 1024], F32) for i in range(2)])
    x1r = Ring([P.sbuf(f"x1t{i}", [128, 1024], F32) for i in range(2)])
    ur = Ring([P.sbuf(f"u{i}", [128, 1024], F32) for i in range(2)])
    str_ = Ring([P.sbuf(f"st{i}", [128, 2, 6], F32) for i in range(2)])
    mvr = Ring([P.sbuf(f"mv{i}", [128, 2], F32) for i in range(2)])
    for i in range(NT):
        j = 0 if i < 16 else 1
        y1, y2 = y1r.get(), y2r.get()
        for k, yy in enumerate((y1, y2)):
            P.dma("pool", lambda e, yy=yy, i=i, k=k: e.indirect_dma_start(
                out=yy[:, :], out_offset=None, in_=yslot.t[:, :],
                in_offset=bass.IndirectOffsetOnAxis(ap=posI[:, i, k:k + 1], axis=0)),
                reads=[posI.r, yslot.r], writes=[yy.r])
        xt = x1r.get()
        P.dma_copy("sp", xt[:], x1_d.t[i * 128:(i + 1) * 128, :], writes=[xt.r])
        TS("dve", y1[:], y1[:], gates[:, i, 0:1], None, ALU.mult, None, [y1.r, gates.r], [y1.r])
        STT("dve", y1[:], y2[:], gates[:, i, 1:2], y1[:], ALU.mult, ALU.add, [y2.r, gates.r, y1.r], [y1.r])
        u = ur.get()
        TT("pool", u[:], y1[:], g2bc[j][:], ALU.mult, [y1.r, g2bc[j].r], [u.r])
        STT("dve", u[:], xt[:], ALPHA, u[:], ALU.mult, ALU.add, [xt.r, u.r], [u.r])
        st = str_.get()
        for c in range(2):
            P.op("dve", lambda e, st=st, u=u, c=c: e.bn_stats(out=st[:, c, :], in_=u[:, c * 512:(c + 1) * 512]),
                 [u.r], [st.r])
        mv = mvr.get()
        P.op("dve", lambda e, st=st, mv=mv: e.bn_aggr(out=mv[:], in_=st[:].rearrange("p a b -> p (a b)")),
             [st.r], [mv.r])
        ACT(mv[:, 1:2], mv[:, 1:2], AF.Sqrt, [mv.r, epsc.r], [mv.r], bias=epsc[:, 0:1])
        P.op("dve", lambda e, mv=mv: e.reciprocal(out=mv[:, 1:2], in_=mv[:, 1:2]), [mv.r], [mv.r])
        TS("dve", u[:], u[:], mv[:, 0:1], mv[:, 1:2], ALU.subtract, ALU.mult, [u.r, mv.r], [u.r])
        TT("pool", u[:], u[:], lg2[:], ALU.mult, [u.r, lg2.r], [u.r])
        TT("pool", u[:], u[:], lb2[:], ALU.add, [u.r, lb2.r], [u.r])
        P.dma_copy("sp", x2_o.t[i * 128:(i + 1) * 128, :], u[:], reads=[u.r], writes=[x2_o.r])

    P.release(m_stage)


LAYER_W = {"w_mod", "b_mod", "w_in", "b_in", "qn_g", "kn_g", "wgate", "bgate", "gla_norm", "w_br_attn", "w_br_gla",
           "w_br_na", "w_out", "ln1_g", "ln1_b", "w_r", "b_r", "moe_w1", "moe_w3", "moe_w2", "ln2_g", "ln2_b", "tabNA"}


class Env:
    def __init__(self, P):
        self.P = P
        self.scratch = {}
        self.ext = {}
        self.last = 1
        self.fb = [P.psum(f"fb{i}", [128, 512], F32) for i in range(7)]
        self.bb = P.psum("bb", [128, 8, 128], BF16)

    def _mk(self, name, shape, dt, kind):
        b = self.P.dram(name, shape, dt, kind=kind)
        b.t = b.t.ap()
        return b

    def din(self, name, shape, dt, L):
        if name == "x_in":
            if L == 0:
                if "x_in" not in self.ext:
                    self.ext["x_in"] = self._mk("x_in", shape, dt, "ExternalInput")
                return self.ext["x_in"]
            return self.scratch["x2"]
        if name in self.scratch:
            return self.scratch[name]
        key = f"{name}_{L}" if name in LAYER_W else name
        if key not in self.ext:
            self.ext[key] = self._mk(key, shape, dt, "ExternalInput")
        return self.ext[key]

    def dout(self, name, shape, dt, L):
        if name == "x2" and L == self.last:
            return self._mk("x2out", shape, dt, "ExternalOutput")
        if name not in self.scratch:
            self.scratch[name] = self._mk("sc_" + name, shape, dt, "Internal")
        return self.scratch[name]


def emit_exchange(P, E, groups):
    S = E.scratch

    def sc(name, shape, dt):
        if name not in S:
            S[name] = E._mk("sc_" + name, shape, dt, "Internal")
        return S[name]
    pk_ak, g_ak = sc("pk_ak", [128, 2048], BF16), sc("g_ak", [256, 2048], BF16)
    pk_av, g_av = sc("pk_av", [2048, 128], BF16), sc("g_av", [4096, 128], BF16)
    pk_ck, g_ck = sc("pk_ck", [512, 768], BF16), sc("g_ck", [1024, 768], BF16)
    pk_cv, g_cv = sc("pk_cv", [768, 512], BF16), sc("g_cv", [1536, 512], BF16)
    pk_S, g_S = sc("pk_S", [128, 512], F32), sc("g_S", [256, 512], F32)
    P.dma_copy("sp", pk_ak.t[:, :], S["akT"].t[:, 0:2048], reads=[S["akT"].r], writes=[pk_ak.r])
    P.dma_copy("sp", pk_av.t[:, :], S["av"].t[0:2048, :], reads=[S["av"].r], writes=[pk_av.r])
    P.dma_copy("sp", pk_ck.t[:, 0:384], S["ckT"].t[:, 0:384], reads=[S["ckT"].r], writes=[pk_ck.r])
    P.dma_copy("sp", pk_ck.t[:, 384:768], S["ckT"].t[:, 1664:2048], reads=[S["ckT"].r], writes=[pk_ck.r])
    P.dma_copy("sp", pk_cv.t[0:384, :], S["cv"].t[0:384, :], reads=[S["cv"].r], writes=[pk_cv.r])
    P.dma_copy("sp", pk_cv.t[384:768, :], S["cv"].t[1664:2048, :], reads=[S["cv"].r], writes=[pk_cv.r])
    P.dma_copy("sp", pk_S.t[:, :], S["Sfin"].t.rearrange("d p f -> (d p) f"), reads=[S["Sfin"].r], writes=[pk_S.r])
    for a, b_ in ((pk_ak, g_ak), (pk_av, g_av), (pk_ck, g_ck), (pk_cv, g_cv), (pk_S, g_S)):
        P.coll(lambda e, a=a, b_=b_: e.collective_compute("AllGather", ALU.bypass, replica_groups=groups,
                                                           ins=[a.t[:, :]], outs=[b_.t[:, :]]),
               reads=[a.r], writes=[b_.r])


def build_fused(groups=None, n_layers=2):
    if groups is None:
        groups = [[0, 1], [2, 3], [4, 5], [6, 7]]
    nc = bass.Bass("TRN2", target_bir_lowering=False)
    P = Prog(nc)
    E = Env(P)
    E.last = n_layers - 1
    for L in range(n_layers):
        emit_l1(P, E, L)
        emit_exchange(P, E, groups)
        emit_l2(P, E, L)
        emit_l3(P, E, L)
    P.finalize()
    return nc, P


_BF = ml_dtypes.bfloat16


def _rope_tables(tok):
    row = (tok // 64).astype(np.float32)
    col = (tok % 64).astype(np.float32)
    inv = (10000.0 ** (-np.arange(16, dtype=np.float32) / 16)).astype(np.float32)
    ar = row[:, None] * inv
    ac = col[:, None] * inv
    ang = np.concatenate([ar, ar, ac, ac], -1)
    return np.cos(ang).astype(np.float32), np.sin(ang).astype(np.float32)


def _consts(s):
    tok = s * 2048 + np.arange(2048)
    cos, sin = _rope_tables(tok)
    cosf = np.concatenate([cos, np.ones((256, 64), np.float32)], 0).T
    sinf = np.concatenate([sin, np.zeros((256, 64), np.float32)], 0).T
    R = np.zeros((64, 64), np.float32)
    for d in range(16):
        R[d, d + 16] = -1
        R[d + 16, d] = 1
        R[d + 32, d + 48] = -1
        R[d + 48, d + 32] = 1
    rotM = np.zeros((128, 128), np.float32)
    rotM[:64, :64] = R.T
    rotM[64:, 64:] = R.T
    ob = np.zeros((128, 128), np.float32)
    ob[:64, :64] = 1
    ob[64:, 64:] = 1
    ii = np.arange(128)
    sI, tI = np.meshgrid(ii, ii, indexing="ij")
    f32 = np.float32
    return dict(cosT=np.ascontiguousarray(np.concatenate([cosf, cosf], 0)),
                sinT=np.ascontiguousarray(np.concatenate([sinf, sinf], 0)),
                rotM=rotM, onesblk=ob, ident=np.eye(128, dtype=f32),
                triInc=(sI <= tI).astype(f32), triDec=(sI >= tI).astype(f32),
                triSgt=(sI > tI).astype(f32), triSlt=(sI < tI).astype(f32),
                flags=np.tile(np.array([[1.0 if s == 0 else 0.0, 1.0 if s == 1 else 0.0]], f32), (64, 1)),
                fl1m=np.tile(np.array([[0.0 if s == 0 else 1.0, 0.0 if s == 1 else 1.0]], f32), (64, 1)),
                ones128=np.ones((128, 128), f32),
                thr18=np.tile((128.0 * np.arange(18, dtype=f32))[None, None, :], (128, 32, 1)),
                thrj=np.tile((128.0 * np.arange(NBLK, dtype=f32))[None, :, None], (128, 1, 32)),
                wbase=(np.arange(8)[None, :] * 128 + np.arange(128)[:, None]).astype(f32),
                tokidx=(np.arange(18)[None, :] * 128 + np.arange(128)[:, None]).astype(np.int32))


def _na_table7(rpb, s):
    tab = np.full((5, 7, 2, 128, 512), -30000.0, np.float32)
    kp = np.arange(128)
    qp = np.arange(128)
    for slot, j in enumerate((0, 1, 14, 15, 5)):
        J = 16 * s + j
        qr = 2 * J + qp // 64
        qc = qp % 64
        rs = np.clip(qr - 4, 0, 56)
        cs = np.clip(qc - 8, 0, 48)
        for kt in range(7):
            T = J - 3 + kt
            if T < 0 or T > 31:
                continue
            kr = 2 * T + kp // 64
            kc = kp % 64
            valid = ((kr[:, None] >= rs[None, :]) & (kr[:, None] < rs[None, :] + 8)
                     & (kc[:, None] >= cs[None, :]) & (kc[:, None] < cs[None, :] + 16))
            ri = np.clip(kr[:, None] - qr[None, :] + 7, 0, 14)
            ci = np.clip(kc[:, None] - qc[None, :] + 15, 0, 30)
            for h in range(8):
                tab[slot, kt, h // 4, :, (h % 4) * 128:(h % 4 + 1) * 128] = np.where(valid, rpb[h][ri, ci], -30000.0)
    return tab.astype(_BF)


def _core_map(inp, c, n_layers=2):
    f32 = np.float32
    b, s = c // 2, c % 2
    m = dict(x_in=np.concatenate([inp["x"][b, s * 2048:(s + 1) * 2048], inp["ctx"][b]], 0),
             cvec=np.stack([inp["c"][b], inp["c_ctx"]], 0), **_consts(s))
    for l in range(n_layers):
        lw = dict(w_mod=inp["w_mod"][l], b_mod=inp["b_mod"][l][None], w_in=inp["w_in"][l], b_in=inp["b_in"][l][None],
                  qn_g=inp["attn_q_norm"][l][:, None], kn_g=inp["attn_k_norm"][l][:, None],
                  wgate=inp["gla_w_gate"][l], bgate=inp["gla_b_gate"][l], gla_norm=inp["gla_norm"][l][:, None],
                  w_br_attn=inp["w_br_attn"][l], w_br_gla=inp["w_br_gla"][l], w_br_na=inp["w_br_na"][l],
                  w_out=inp["w_out"][l], ln1_g=inp["ln1_g"][l][None], ln1_b=inp["ln1_b"][l][None],
                  w_r=np.concatenate([inp["w_router_group"][l], inp["w_router_expert"][l]], 1),
                  b_r=np.concatenate([inp["b_router_group"][l], inp["b_router_expert"][l]])[None],
                  moe_w1=inp["moe_w1"][l].reshape(-1, 4096), moe_w3=inp["moe_w3"][l].reshape(-1, 4096),
                  moe_w2=inp["moe_w2"][l].reshape(-1, 4096), ln2_g=inp["ln2_g"][l][None], ln2_b=inp["ln2_b"][l][None],
                  tabNA=_na_table7(inp["na_rpb"][l].astype(f32), s))
        for k, v in lw.items():
            m[f"{k}_{l}"] = v
    out = {}
    for k, v in m.items():
        v = np.asarray(v)
        if v.dtype not in (np.int32, _BF):
            v = v.astype(f32)
        out[k] = np.ascontiguousarray(v)
    return out


_PROG = []


def kernel(**inp):
    inp = {k: np.asarray(v) for k, v in inp.items()}
    if not _PROG:
        _PROG.append(build_fused()[0])
    cores = list(range(8))
    maps = [_core_map(inp, c) for c in cores]
    res = run_bass_kernel_spmd(_PROG[0], maps, core_ids=cores).results
    out = np.zeros((4, 4096, 1024), np.float32)
    for c in cores:
        out[c // 2, (c % 2) * 2048:(c % 2 + 1) * 2048] = np.asarray(res[c]["x2out"], dtype=np.float32)[:2048]
    return out
```

```python
import os
import numpy as np
import ml_dtypes
import concourse.bass as bass
import concourse.mybir as mybir
from concourse.bass_utils import run_bass_kernel_spmd

F32 = mybir.dt.float32
BF16 = mybir.dt.bfloat16
I32 = mybir.dt.int32
AF = mybir.ActivationFunctionType
ALU = mybir.AluOpType
AX = mybir.AxisListType


class Res:
    __slots__ = ("name", "w", "r")

    def __init__(self, name=""):
        self.name = name
        self.w = None
        self.r = {}


class Buf:
    def __init__(self, t, nres=1, name=""):
        self.t = t
        self.rs = [Res(f"{name}{i}") for i in range(nres)]

    @property
    def r(self):
        return self.rs[0]

    def __getitem__(self, k):
        return self.t[k]


class Prog:
    ENGS = ("pe", "act", "dve", "pool", "sp")
    SAME_SYNC = {"pe": False, "act": os.environ.get("SS","1")=="1", "dve": os.environ.get("SS","1")=="1", "pool": os.environ.get("SS","1")=="1", "sp": False}

    def __init__(self, nc, n_dma_sems=48):
        self.nc = nc
        self.lists = {k: [] for k in self.ENGS}
        self.semobj = {}
        self.cnt = {}
        for k in self.ENGS:
            self.semobj[k] = nc.alloc_semaphore(f"sem_{k}")
            self.cnt[k] = 0
        self.seen = {k: {} for k in self.ENGS}
        self.nd = n_dma_sems
        self.dval = [0] * n_dma_sems
        for i in range(n_dma_sems):
            self.semobj[f"d{i}"] = nc.alloc_semaphore(f"sem_d{i}")
        self.dnext = 0
        self.dnext_sw = 0
        self.n_ops = 0
        self.arena0, self.arena1 = nc.bump_sbuf(212000)
        self.sb_off = self.arena0
        self.sb_peak = self.arena0
        self.n_alloc = 0

    def sbuf(self, name, shape, dtype, nres=1):
        esz = {F32: 4, BF16: 2, I32: 4}[dtype]
        nb = esz
        for d in shape[1:]:
            nb *= d
        nb = (nb + 31) // 32 * 32
        assert self.sb_off + nb <= self.arena1, f"SBUF arena overflow at {name}: {self.sb_off + nb - self.arena0}"
        self.n_alloc += 1
        t = self.nc.alloc_sbuf_tensor_at(f"s{self.n_alloc}_{name}", list(shape), dtype, offset=self.sb_off)
        self.sb_off += nb
        self.sb_peak = max(self.sb_peak, self.sb_off)
        return Buf(t, nres, name)

    def mark(self):
        return self.sb_off

    def release(self, mark):
        self.barrier()
        self.sb_off = mark

    def barrier(self):
        ev = [(k, self.cnt[k]) for k in self.ENGS if self.cnt[k] > 0]
        ev += [(f"d{i}", self.dval[i]) for i in range(self.nd) if self.dval[i] > 0]
        if "cc" in self.semobj:
            ev.append(("cc", self.ccval))
        for k in self.ENGS:
            self._wait(k, ev)

    def psum(self, name, shape, dtype=F32, nres=1):
        return Buf(self.nc.alloc_psum_tensor("p_" + name, list(shape), dtype), nres, name)

    def dram(self, name, shape, dtype, kind="Internal", nres=1):
        return Buf(self.nc.dram_tensor(name, list(shape), dtype, kind=kind), nres, name)

    def _wait(self, eng, deps):
        for key, val in deps:
            if key == eng and not self.SAME_SYNC[eng]:
                continue
            if self.seen[eng].get(key, 0) >= val:
                continue
            self.seen[eng][key] = val
            self.lists[eng].append(("w", key, val))

    @staticmethod
    def _deps(reads, writes):
        deps = []
        for r in reads:
            if r.w is not None:
                deps.append(r.w)
        for w in writes:
            if w.w is not None:
                deps.append(w.w)
            deps.extend(w.r.items())
        return deps

    @staticmethod
    def _commit(ev, reads, writes):
        for r in reads:
            if r.r.get(ev[0], 0) < ev[1]:
                r.r[ev[0]] = ev[1]
        for w in writes:
            w.w = ev
            w.r = {}

    def op(self, eng, fn, reads=(), writes=()):
        self._wait(eng, self._deps(reads, writes))
        self.cnt[eng] += 1
        ev = (eng, self.cnt[eng])
        self.lists[eng].append(("o", fn, eng, 1))
        self._commit(ev, reads, writes)
        self.n_ops += 1
        return ev

    def dma(self, q, fn, reads=(), writes=()):
        half = self.nd // 2
        if q == "pool":
            i = half + self.dnext_sw
            self.dnext_sw = (self.dnext_sw + 1) % (self.nd - half)
        else:
            i = self.dnext
            self.dnext = (self.dnext + 1) % half
        key = f"d{i}"
        deps = self._deps(reads, writes)
        if self.dval[i] > 0:
            deps.append((key, self.dval[i]))
        self._wait(q, deps)
        self.dval[i] += 16
        ev = (key, self.dval[i])
        self.lists[q].append(("o", fn, key, 16))
        self._commit(ev, reads, writes)
        self.n_ops += 1
        return ev

    def coll(self, fn, reads=(), writes=()):
        if "cc" not in self.semobj:
            self.semobj["cc"] = self.nc.alloc_semaphore("sem_cc")
            self.ccval = 0
        deps = self._deps(reads, writes)
        if self.ccval > 0:
            deps.append(("cc", self.ccval))
        self._wait("pool", deps)
        self.ccval += 1
        ev = ("cc", self.ccval)
        self.lists["pool"].append(("o", fn, "cc", 1))
        self._commit(ev, reads, writes)
        return ev

    def dma_copy(self, q, out, in_, reads=(), writes=(), **kw):
        return self.dma(q, lambda e: e.dma_start(out=out, in_=in_, **kw), reads, writes)

    def finalize(self):
        final = [(k, self.cnt[k]) for k in self.ENGS if self.cnt[k] > 0 and k != "sp"]
        final += [(f"d{i}", self.dval[i]) for i in range(self.nd) if self.dval[i] > 0]
        if "cc" in self.semobj:
            final.append(("cc", self.ccval))
        self._wait("sp", final)
        nc = self.nc
        lists = self.lists
        semobj = self.semobj

        def run(e, items):
            for it in items:
                if it[0] == "w":
                    e.wait_ge(semobj[it[1]], it[2])
                else:
                    ins = it[1](e)
                    ins.then_inc(semobj[it[2]], it[3])

        with nc.Block() as block:
            @block.tensor
            def _(e):
                run(e, lists["pe"])

            @block.scalar
            def _(e):
                run(e, lists["act"])

            @block.vector
            def _(e):
                run(e, lists["dve"])

            @block.gpsimd
            def _(e):
                run(e, lists["pool"])

            @block.sync
            def _(e):
                run(e, lists["sp"])


PENG = os.environ.get('PENG', 'pool')
GM = int(os.environ.get('GM', '9'))
NT = 18
NTOK = 2304
BLKS = [(0, 512), (512, 512), (1024, 512), (1536, 512), (2048, 256)]
LN_EPS = 1e-6
RMS_EPS = 1e-6

C_AQ, C_AK, C_AV, C_BQ, C_BK, C_BV, C_BR, C_BA, C_CQ, C_CK, C_CV, C_G = (
    0, 512, 640, 768, 1024, 1280, 1792, 2304, 2336, 2848, 3360, 3872)


class Ctx:
    pass


def mk_helpers(P):
    H = Ctx()

    def MM(out, lhsT, rhs, start, stop, reads, writes):
        P.op("pe", lambda e: e.matmul(out, lhsT=lhsT, rhs=rhs, start=start, stop=stop), reads, writes)

    def TR(out, in_, ident, reads, writes):
        P.op("pe", lambda e: e.transpose(out, in_, ident), reads, writes)

    def ACT(out, in_, func, reads, writes, bias=None, scale=None):
        kw = {}
        if bias is not None:
            kw["bias"] = bias
        if scale is not None:
            kw["scale"] = scale
        P.op("act", lambda e: e.activation(out=out, in_=in_, func=func, **kw), reads, writes)

    def TT(eng, out, in0, in1, op, reads, writes):
        P.op(eng, lambda e: e.tensor_tensor(out=out, in0=in0, in1=in1, op=op), reads, writes)

    def TS(eng, out, in0, s1, s2, op0, op1, reads, writes):
        if op1 is None:
            P.op(eng, lambda e: e.tensor_scalar(out=out, in0=in0, scalar1=s1, scalar2=None, op0=op0), reads, writes)
        else:
            P.op(eng, lambda e: e.tensor_scalar(out=out, in0=in0, scalar1=s1, scalar2=s2, op0=op0, op1=op1), reads, writes)

    def STT(eng, out, in0, scalar, in1, op0, op1, reads, writes):
        P.op(eng, lambda e: e.scalar_tensor_tensor(out=out, in0=in0, scalar=scalar, in1=in1, op0=op0, op1=op1), reads, writes)

    def CP(eng, out, in_, reads, writes):
        P.op(eng, lambda e: e.tensor_copy(out=out, in_=in_), reads, writes)

    def MS(eng, ap, val, writes):
        P.op(eng, lambda e: e.memset(ap, val), (), writes)

    H.MM, H.TR, H.ACT, H.TT, H.TS, H.STT, H.CP, H.MS = MM, TR, ACT, TT, TS, STT, CP, MS
    return H


class Banks:
    def __init__(self, P, n=8, bufs=None):
        self.b = list(bufs) if bufs is not None else [P.psum(f"bank{i}", [128, 512], F32) for i in range(n)]
        self.i = 0
        self.n = len(self.b)

    def get(self):
        b = self.b[self.i]
        self.i = (self.i + 1) % self.n
        return b


class Ring:
    def __init__(self, bufs):
        self.bufs = bufs
        self.i = 0

    def get(self):
        b = self.bufs[self.i]
        self.i = (self.i + 1) % len(self.bufs)
        return b


def tile_res(buf, t0, n):
    return [buf.rs[i] for i in range(t0 // 128, (t0 + n + 127) // 128)]


ALPHA = 4 ** 0.25
NKT = 34
NBLK = 68
NSLOT = NBLK * 128
BIG = 1.0e9
def emit_l1(P, E, L):
    nc = P.nc
    H = mk_helpers(P)
    MM, TR, ACT, TT, TS, STT, CP, MS = H.MM, H.TR, H.ACT, H.TT, H.TS, H.STT, H.CP, H.MS
    din = lambda name, shape, dt=F32: E.din(name, shape, dt, L)
    dout = lambda name, shape, dt: E.dout(name, shape, dt, L)
    m_stage = P.mark()

    x_in = din("x_in", [NTOK, 1024])
    cvec = din("cvec", [2, 1024])
    w_mod = din("w_mod", [1024, 6144])
    b_mod = din("b_mod", [1, 6144])
    w_in = din("w_in", [1024, 6944])
    b_in = din("b_in", [1, 6944])
    qn_g = din("qn_g", [64, 1])
    kn_g = din("kn_g", [64, 1])
    wgate_d = din("wgate", [2, 16, 256])
    bgate_d = din("bgate", [2, 256])
    cos_d = din("cosT", [128, NTOK])
    sin_d = din("sinT", [128, NTOK])
    ident_d = din("ident", [128, 128])
    rot_d = din("rotM", [128, 128])
    oblk_d = din("onesblk", [128, 128])

    gT = dout("gT", [3072, NTOK], BF16)
    rT = dout("rT", [512, NTOK], BF16)
    cqT = dout("cqT", [512, NTOK], BF16)
    ckT = dout("ckT", [512, NTOK], BF16)
    cv = dout("cv", [NTOK, 512], BF16)
    bqT = dout("bqT", [256, NTOK], F32)
    bkT = dout("bkT", [256, NTOK], F32)
    bk = dout("bk", [NTOK, 256], F32)
    bv = dout("bv", [NTOK, 512], BF16)
    Gd = dout("Gd", [NTOK, 2, 256], F32)
    aqT = dout("aqT", [512, NTOK], BF16)
    akT = dout("akT", [128, NTOK], BF16)
    av = dout("av", [NTOK, 128], BF16)
    modD = dout("modD", [2, 6144], F32)

    banks = Banks(P, 0, E.fb)
    pT = E.bb

    ident = P.sbuf("ident", [128, 128], BF16)
    P.dma_copy("pool", ident[:], ident_d.t[:, :], writes=[ident.r])
    oblk = P.sbuf("oblk", [128, 128], BF16)
    P.dma_copy("pool", oblk[:], oblk_d.t[:, :], writes=[oblk.r])
    rotM = P.sbuf("rotM", [128, 128], F32)
    P.dma_copy("sp", rotM[:], rot_d.t[:, :], writes=[rotM.r])
    cosT = P.sbuf("cosT", [128, NTOK], F32)
    sinT = P.sbuf("sinT", [128, NTOK], F32)
    P.dma_copy("sp", cosT[:], cos_d.t[:, :], writes=[cosT.r])
    P.dma_copy("sp", sinT[:], sin_d.t[:, :], writes=[sinT.r])
    g8 = P.sbuf("g8", [128, 2], F32)
    for hh in range(2):
        P.dma_copy("sp", g8[hh * 64:(hh + 1) * 64, 0:1], qn_g.t[:, :], writes=[g8.r])
        P.dma_copy("sp", g8[hh * 64:(hh + 1) * 64, 1:2], kn_g.t[:, :], writes=[g8.r])
    TS("dve", g8[:], g8[:], 8.0, None, ALU.mult, None, [g8.r], [g8.r])
    wgate = P.sbuf("wgate", [16, 2, 256], BF16)
    P.dma_copy("pool", wgate[:], wgate_d.t.rearrange("d r c -> r d c"), writes=[wgate.r])
    bg_bc = P.sbuf("bg_bc", [128, 2, 256], F32)
    for d in range(2):
        P.dma_copy("sp", bg_bc[:, d, :], bgate_d.t[d:d + 1, :].partition_broadcast(128), writes=[bg_bc.r])

    epsc = P.sbuf("epsc", [128, 2], F32)
    MS("dve", epsc[:, 0:1], LN_EPS, [epsc.r])
    MS("dve", epsc[:, 1:2], 64.0 * RMS_EPS, [epsc.r])
    cT = P.sbuf("cT", [128, 8, 2], F32)
    for j in range(2):
        P.dma_copy("sp", cT[:, :, j], cvec.t[j:j + 1, :].rearrange("o (k p) -> p (o k)", p=128), writes=[cT.r],
                   allow_slow_non_contiguous=True)
    scT = P.sbuf("scT", [128, 8, 2], BF16)
    ACT(scT[:], cT[:], AF.Silu, [cT.r], [scT.r])
    bm_r = Ring([P.sbuf(f"bm{i}", [2, 512], F32) for i in range(2)])
    mr_r = Ring([P.sbuf(f"mr{i}", [2, 512], F32) for i in range(2)])
    wsl = Ring([P.sbuf(f"wsg{i}", [128, 8, 512], BF16) for i in range(2)])
    for g in range(12):
        wb = wsl.get()
        P.dma_copy("pool", wb[:], w_mod.t[:, g * 512:(g + 1) * 512].rearrange("(k p) c -> p k c", p=128),
                   writes=[wb.r])
        bm = bm_r.get()
        for j in range(2):
            P.dma_copy("sp", bm[j:j + 1, :], b_mod.t[0:1, g * 512:(g + 1) * 512], writes=[bm.r])
        ps = banks.get()
        for k in range(8):
            MM(ps[0:2, 0:512], scT[:, k, :], wb[:, k, :], k == 0, k == 7, [scT.r, wb.r], [ps.r])
        mr = mr_r.get()
        TT("dve", mr[:], ps[0:2, 0:512], bm[:], ALU.add, [ps.r, bm.r], [mr.r])
        P.dma_copy("sp", modD.t[:, g * 512:(g + 1) * 512], mr[:], reads=[mr.r], writes=[modD.r])
    modc = P.sbuf("modc", [128, 2, 6, 8], F32)
    for j in range(2):
        for m in range(2):
            P.dma_copy("sp", modc[:, j, m, :],
                       modD.t[j:j + 1, m * 1024:(m + 1) * 1024].rearrange("o (k p) -> p (o k)", p=128),
                       reads=[modD.r], writes=[modc.r], allow_slow_non_contiguous=True)
    TS("dve", modc[:, :, 1, :], modc[:, :, 1, :], 1.0, None, ALU.add, None, [modc.r], [modc.r])

    hT = P.sbuf("hT", [128, 8, NTOK], BF16, nres=NT)
    xin = Ring([P.sbuf(f"xin{i}", [128, 1024], F32) for i in range(2)])
    xnr = Ring([P.sbuf(f"xn{i}", [128, 1024], BF16) for i in range(2)])
    str_ = Ring([P.sbuf(f"st{i}", [128, 2, 6], F32) for i in range(2)])
    mvr = Ring([P.sbuf(f"mv{i}", [128, 2], F32) for i in range(2)])
    for i in range(NT):
        xt = xin.get()
        P.dma_copy("sp", xt[:], x_in.t[i * 128:(i + 1) * 128, :], writes=[xt.r])
        st = str_.get()
        for c in range(2):
            P.op("dve", lambda e, st=st, xt=xt, c=c: e.bn_stats(out=st[:, c, :], in_=xt[:, c * 512:(c + 1) * 512]),
                 [xt.r], [st.r])
        mv = mvr.get()
        P.op("dve", lambda e, st=st, mv=mv: e.bn_aggr(out=mv[:], in_=st[:].rearrange("p a b -> p (a b)")),
             [st.r], [mv.r])
        ACT(mv[:, 1:2], mv[:, 1:2], AF.Sqrt, [mv.r], [mv.r], bias=epsc[:, 0:1])
        P.op("dve", lambda e, mv=mv: e.reciprocal(out=mv[:, 1:2], in_=mv[:, 1:2]), [mv.r], [mv.r])
        xn = xnr.get()
        TS("dve", xn[:], xt[:], mv[:, 0:1], mv[:, 1:2], ALU.subtract, ALU.mult, [xt.r, mv.r], [xn.r])
        for k in range(8):
            TR(pT[:, k, :], xn[:, k * 128:(k + 1) * 128], ident[:], [xn.r, ident.r], [pT.r])
        j = 0 if i < 16 else 1
        for k in range(8):
            o = hT[:, k, i * 128:(i + 1) * 128]
            if k % 2 == 0:
                ACT(o, pT[:, k, :], AF.Identity, [pT.r, modc.r], [hT.rs[i]],
                    bias=modc[:, j, 0, k:k + 1], scale=modc[:, j, 1, k:k + 1])
            else:
                TS("dve", o, pT[:, k, :], modc[:, j, 1, k:k + 1], modc[:, j, 0, k:k + 1], ALU.mult, ALU.add,
                   [pT.r, modc.r], [hT.rs[i]])

    bcols = P.sbuf("bcols", [128, 48], F32)
    bcol_idx = {}
    nb = 0
    for (c0, ng) in [(C_AQ, 4), (C_AK, 1), (C_BQ, 2), (C_BK, 2), (C_BR, 4), (C_CQ, 4), (C_CK, 4), (C_G, 24)]:
        P.dma_copy("sp", bcols[:, nb:nb + ng],
                   b_in.t[0:1, c0:c0 + ng * 128].rearrange("o (g p) -> p (o g)", p=128),
                   writes=[bcols.r], allow_slow_non_contiguous=True)
        for g in range(ng):
            bcol_idx[c0 + g * 128] = nb + g
        nb += ng
    bacol = P.sbuf("bacol", [16, 2], F32)
    P.dma_copy("sp", bacol[:], b_in.t[0:1, C_BA:C_BA + 32].rearrange("o (d p) -> p (o d)", p=16),
               writes=[bacol.r], allow_slow_non_contiguous=True)
    bias_bc = P.sbuf("bias_bc", [128, 1408], F32)
    tm_groups = [(C_AV, 128, 0), (C_BK, 256, 128), (C_BV, 512, 384), (C_CV, 512, 896)]
    for (c0, n, o) in tm_groups:
        P.dma_copy("sp", bias_bc[:, o:o + n], b_in.t[0:1, c0:c0 + n].partition_broadcast(128), writes=[bias_bc.r])

    mark1b = P.mark()
    stg_bf = Ring([P.sbuf(f"stgbf{i}", [128, NTOK], BF16) for i in range(3)])
    stg_f = Ring([P.sbuf(f"stgf{i}", [128, NTOK], F32) for i in range(2)])
    aux = {n: Ring([P.sbuf(f"{n}{i}", [128, 512], dt) for i in range(2)])
           for n, dt in [("zq", F32), ("sq", BF16), ("rs", F32), ("qn", F32), ("t1", F32), ("t2", F32)]}
    stm_bf = Ring([P.sbuf(f"stmbf{i}", [128, 512], BF16) for i in range(3)])
    stm_f = Ring([P.sbuf(f"stmf{i}", [128, 256], F32) for i in range(2)])
    aT = P.sbuf("aT", [16, 2, NTOK], BF16)

    def load_w(c0, n):
        wb = wsl.get()
        P.dma_copy("pool", wb[:, :, 0:n], w_in.t[:, c0:c0 + n].rearrange("(k p) c -> p k c", p=128),
                   writes=[wb.r])
        return wb

    def fm_mm(wb, off, m, t0, n):
        ps = banks.get()
        rd = [wb.r] + tile_res(hT, t0, n)
        for k in range(8):
            MM(ps[0:m, 0:n], wb[:, k, off:off + m], hT[:, k, t0:t0 + n], k == 0, k == 7, rd, [ps.r])
        return ps

    def fm_simple(wb, off, c0, func, dst, row0, f32=False, scale=None):
        stg = stg_f.get() if f32 else stg_bf.get()
        bc = bcols[:, bcol_idx[c0]:bcol_idx[c0] + 1]
        for (t0, n) in BLKS:
            ps = fm_mm(wb, off, 128, t0, n)
            ACT(stg[:, t0:t0 + n], ps[:, 0:n], func, [ps.r, bcols.r], [stg.r], bias=bc)
        P.dma_copy("sp", dst.t[row0:row0 + 128, :], stg[:], reads=[stg.r], writes=[dst.r])

    def fm_qk(wb, off, c0, gcol, dst, row0):
        stg = stg_bf.get()
        bc = bcols[:, bcol_idx[c0]:bcol_idx[c0] + 1]
        for (t0, n) in BLKS:
            ps = fm_mm(wb, off, 128, t0, n)
            zq, sq, rs, qn, t1, t2 = (aux[k].get() for k in ("zq", "sq", "rs", "qn", "t1", "t2"))
            ACT(zq[:, 0:n], ps[:, 0:n], AF.Identity, [ps.r, bcols.r], [zq.r], bias=bc)
            ACT(sq[:, 0:n], ps[:, 0:n], AF.Square, [ps.r, bcols.r], [sq.r], bias=bc)
            ss = banks.get()
            MM(ss[:, 0:n], oblk[:], sq[:, 0:n], True, True, [oblk.r, sq.r], [ss.r])
            ACT(rs[:, 0:n], ss[:, 0:n], AF.Sqrt, [ss.r], [rs.r], bias=epsc[:, 1:2])
            P.op("dve", lambda e, rs=rs, n=n: e.reciprocal(out=rs[:, 0:n], in_=rs[:, 0:n]), [rs.r], [rs.r])
            STT("dve", qn[:, 0:n], zq[:, 0:n], g8[:, gcol:gcol + 1], rs[:, 0:n], ALU.mult, ALU.mult,
                [zq.r, g8.r, rs.r], [qn.r])
            rot = banks.get()
            MM(rot[:, 0:n], rotM[:], qn[:, 0:n], True, True, [rotM.r, qn.r], [rot.r])
            TT("pool", t1[:, 0:n], qn[:, 0:n], cosT[:, t0:t0 + n], ALU.mult, [qn.r, cosT.r], [t1.r])
            TT("dve", t2[:, 0:n], rot[:, 0:n], sinT[:, t0:t0 + n], ALU.mult, [rot.r, sinT.r], [t2.r])
            TT("dve", stg[:, t0:t0 + n], t1[:, 0:n], t2[:, 0:n], ALU.add, [t1.r, t2.r], [stg.r])
        P.dma_copy("sp", dst.t[row0:row0 + 128, :], stg[:], reads=[stg.r], writes=[dst.r])

    def tm_group(wb, off, n, bo, dst, f32=False):
        for i in range(NT):
            ps = banks.get()
            rd = [wb.r, hT.rs[i]]
            for k in range(8):
                MM(ps[:, 0:n], hT[:, k, i * 128:(i + 1) * 128], wb[:, k, off:off + n], k == 0, k == 7, rd, [ps.r])
            stg = stm_f.get() if f32 else stm_bf.get()
            TT("dve", stg[:, 0:n], ps[:, 0:n], bias_bc[:, bo:bo + n], ALU.add, [ps.r, bias_bc.r], [stg.r])
            P.dma_copy("sp", dst.t[i * 128:(i + 1) * 128, :], stg[:, 0:n], reads=[stg.r], writes=[dst.r])

    wb = load_w(C_AQ, 512)
    for s in range(4):
        fm_qk(wb, s * 128, C_AQ + s * 128, 0, aqT, s * 128)
    wb = load_w(C_AK, 256)
    fm_qk(wb, 0, C_AK, 1, akT, 0)
    tm_group(wb, 128, 128, 0, av)
    wb = load_w(C_BQ, 512)
    for s in range(2):
        fm_simple(wb, s * 128, C_BQ + s * 128, AF.Identity, bqT, s * 128, f32=True)
    for s in range(2):
        fm_simple(wb, 256 + s * 128, C_BK + s * 128, AF.Identity, bkT, s * 128, f32=True)
    tm_group(wb, 256, 256, 128, bk, f32=True)
    wb = load_w(C_BV, 512)
    tm_group(wb, 0, 512, 384, bv)
    wb = load_w(C_BR, 512)
    for s in range(4):
        fm_simple(wb, s * 128, C_BR + s * 128, AF.Silu, rT, s * 128)
    wb = load_w(C_BA, 32)
    for d in range(2):
        for (t0, n) in BLKS:
            ps = fm_mm(wb, d * 16, 16, t0, n)
            ACT(aT[0:16, d, t0:t0 + n], ps[0:16, 0:n], AF.Identity, [ps.r, bacol.r], [aT.r], bias=bacol[:, d:d + 1])
    tg_r = Ring([P.sbuf(f"tg{i}", [128, 256], F32) for i in range(2)])
    te_r = Ring([P.sbuf(f"te{i}", [128, 256], F32) for i in range(2)])
    gst_r = Ring([P.sbuf(f"gst{i}", [128, 2, 256], F32) for i in range(2)])
    for i in range(NT):
        gst = gst_r.get()
        for d in range(2):
            ps = banks.get()
            MM(ps[:, 0:256], aT[0:16, d, i * 128:(i + 1) * 128], wgate[0:16, d, :], True, True,
               [aT.r, wgate.r], [ps.r])
            tg = tg_r.get()
            te = te_r.get()
            TT("dve", tg[:], ps[:, 0:256], bg_bc[:, d, :], ALU.add, [ps.r, bg_bc.r], [tg.r])
            ACT(te[:], tg[:], AF.Exp, [tg.r], [te.r], scale=-1.0)
            ACT(gst[:, d, :], te[:], AF.Ln, [te.r], [gst.r], bias=1.0)
        P.dma_copy("sp", Gd.t[i * 128:(i + 1) * 128, :, :], gst[:], reads=[gst.r], writes=[Gd.r])
    wb = load_w(C_CQ, 512)
    for s in range(4):
        fm_simple(wb, s * 128, C_CQ + s * 128, AF.Identity, cqT, s * 128)
    wb = load_w(C_CK, 512)
    for s in range(4):
        fm_simple(wb, s * 128, C_CK + s * 128, AF.Identity, ckT, s * 128)
    wb = load_w(C_CV, 512)
    tm_group(wb, 0, 512, 896, cv)
    for gsup in range(6):
        wb = load_w(C_G + gsup * 512, 512)
        for s in range(4):
            fm_simple(wb, s * 128, C_G + gsup * 512 + s * 128, AF.Sigmoid, gT, gsup * 512 + s * 128)

    P.release(mark1b)
    tri_d = {n: din(n, [128, 128]) for n in ("triInc", "triDec", "triSgt", "triSlt")}
    flags_d = din("flags", [64, 2])
    Og = dout("Og", [2, 512, NTOK], F32)
    qBT = dout("qBT", [2, 256, 2048], BF16)
    Sfin = dout("Sfin", [2, 64, 512], F32)
    tri = {}
    for n in tri_d:
        tri[n] = P.sbuf("c_" + n, [128, 128], F32)
        P.dma_copy("sp", tri[n][:], tri_d[n].t[:, :], writes=[tri[n].r])
    flags = P.sbuf("flags", [64, 2], F32)
    P.dma_copy("sp", flags[:], flags_d.t[:, :], writes=[flags.r])
    Sst = [P.sbuf(f"S{d}", [64, 4, 128], F32) for d in range(2)]
    Sbf = [P.sbuf(f"Sbf{d}", [64, 4, 128], BF16) for d in range(2)]
    Dcum = [P.sbuf(f"Dcum{d}", [64, 4], F32) for d in range(2)]
    rq = [Ring([P.sbuf(f"gq{d}{i}", [64, 4, 128], F32) for i in range(2)]) for d in range(2)]
    rk = [Ring([P.sbuf(f"gk{d}{i}", [64, 4, 128], F32) for i in range(2)]) for d in range(2)]
    rkt = [Ring([P.sbuf(f"gkt{d}{i}", [128, 256], F32) for i in range(2)]) for d in range(2)]
    rv = [Ring([P.sbuf(f"gv{d}{i}", [128, 512], BF16) for i in range(2)]) for d in range(2)]
    rG = [Ring([P.sbuf(f"gG{d}{i}", [128, 256], F32) for i in range(2)]) for d in range(2)]
    reb = [Ring([P.sbuf(f"geb{d}{i}", [64, 4, 128], F32) for i in range(2)]) for d in range(2)]
    rei = [Ring([P.sbuf(f"gei{d}{i}", [64, 4, 128], F32) for i in range(2)]) for d in range(2)]
    rqb = [Ring([P.sbuf(f"gqb{d}{i}", [64, 4, 128], BF16) for i in range(2)]) for d in range(2)]
    rkb = [Ring([P.sbuf(f"gkb{d}{i}", [64, 4, 128], BF16) for i in range(2)]) for d in range(2)]
    rkr = [Ring([P.sbuf(f"gkr{d}{i}", [128, 256], F32) for i in range(2)]) for d in range(2)]
    rke = [Ring([P.sbuf(f"gke{d}{i}", [128, 256], BF16) for i in range(2)]) for d in range(2)]
    rsc = [Ring([P.sbuf(f"gsc{d}{i}", [128, 4, 128], BF16) for i in range(2)]) for d in range(2)]
    rO = [Ring([P.sbuf(f"gO{d}{i}", [128, 4, 128], F32) for i in range(2)]) for d in range(2)]
    rqB = [Ring([P.sbuf(f"gqB{d}{i}", [64, 4, 128], BF16) for i in range(2)]) for d in range(2)]

    def gla_step(d, i, lat):
        cumM = tri["triInc"] if d == 0 else tri["triDec"]
        remM = tri["triSgt"] if d == 0 else tri["triSlt"]
        endc = 127 if d == 0 else 0
        t0 = i * 128
        S, Sb, Dc = Sst[d], Sbf[d], Dcum[d]
        q_t, k_t, kt_t, v_t, G_t = rq[d].get(), rk[d].get(), rkt[d].get(), rv[d].get(), rG[d].get()
        P.dma_copy("sp", q_t[:], bqT.t[:, t0:t0 + 128].rearrange("(h p) t -> p h t", p=64), reads=[bqT.r], writes=[q_t.r])
        P.dma_copy("sp", k_t[:], bkT.t[:, t0:t0 + 128].rearrange("(h p) t -> p h t", p=64), reads=[bkT.r], writes=[k_t.r])
        P.dma_copy("sp", kt_t[:], bk.t[t0:t0 + 128, :], reads=[bk.r], writes=[kt_t.r])
        P.dma_copy("sp", v_t[:], bv.t[t0:t0 + 128, :], reads=[bv.r], writes=[v_t.r])
        P.dma_copy("sp", G_t[:], Gd.t[t0:t0 + 128, d, :], reads=[Gd.r], writes=[G_t.r])
        cps = banks.get()
        for h in range(4):
            MM(cps[0:64, h * 128:(h + 1) * 128], G_t[:, h * 64:(h + 1) * 64], cumM[:], True, True, [G_t.r, cumM.r], [cps.r])
        eb, ei = reb[d].get(), rei[d].get()
        ACT(eb[:].rearrange("p a t -> p (a t)"), cps[0:64, :], AF.Exp, [cps.r], [eb.r], scale=-1.0 / 16)
        ACT(ei[:].rearrange("p a t -> p (a t)"), cps[0:64, :], AF.Exp, [cps.r], [ei.r], scale=1.0 / 16)
        qb, kb = rqb[d].get(), rkb[d].get()
        STT("dve", qb[:], q_t[:], 0.125, eb[:], ALU.mult, ALU.mult, [q_t.r, eb.r], [qb.r])
        TT(PENG, kb[:], k_t[:], ei[:], ALU.mult, [k_t.r, ei.r], [kb.r])
        rps = banks.get()
        MM(rps[:, 0:256], remM[:], G_t[:], True, True, [remM.r, G_t.r], [rps.r])
        kr = rkr[d].get()
        ACT(kr[:], rps[:, 0:256], AF.Exp, [rps.r], [kr.r], scale=-1.0 / 16)
        ke = rke[d].get()
        TT(PENG, ke[:], kt_t[:], kr[:], ALU.mult, [kt_t.r, kr.r], [ke.r])
        sps = banks.get()
        for h in range(4):
            MM(sps[:, h * 128:(h + 1) * 128], kb[:, h, :], qb[:, h, :], True, True, [kb.r, qb.r], [sps.r])
        sc = rsc[d].get()
        TT("dve", sc[:], sps[:].rearrange("p (h t) -> p h t", h=4),
           cumM[:].unsqueeze(1).to_broadcast([128, 4, 128]), ALU.mult, [sps.r, cumM.r], [sc.r])
        ops_ = banks.get()
        for h in range(4):
            MM(ops_[:, h * 128:(h + 1) * 128], Sb[:, h, :], qb[:, h, :], True, False, [Sb.r, qb.r], [ops_.r])
            MM(ops_[:, h * 128:(h + 1) * 128], v_t[:, h * 128:(h + 1) * 128], sc[:, h, :],
               False, True, [v_t.r, sc.r], [ops_.r])
        Ot = rO[d].get()
        ACT(Ot[:].rearrange("p h t -> p (h t)"), ops_[:, :], AF.Identity, [ops_.r], [Ot.r])
        P.dma_copy("sp", Og.t[d, :, t0:t0 + 128].rearrange("(h p) t -> p h t", p=128), Ot[:], reads=[Ot.r], writes=[Og.r])
        if lat:
            qB = rqB[d].get()
            TT(PENG, qB[:], qb[:], Dc[:].unsqueeze(2).to_broadcast([64, 4, 128]), ALU.mult, [qb.r, Dc.r], [qB.r])
            P.dma_copy("sp", qBT.t[d, :, t0:t0 + 128].rearrange("(h p) t -> p h t", p=64), qB[:], reads=[qB.r], writes=[qBT.r])
            TT("dve", Dc[:], Dc[:], eb[:, :, endc], ALU.mult, [Dc.r, eb.r], [Dc.r])
        ups = banks.get()
        for h in range(4):
            MM(ups[0:64, h * 128:(h + 1) * 128], ke[:, h * 64:(h + 1) * 64], v_t[:, h * 128:(h + 1) * 128], True, True,
               [ke.r, v_t.r], [ups.r])
        TT("dve", S[:], S[:], eb[:, :, endc:endc + 1].to_broadcast([64, 4, 128]), ALU.mult, [S.r, eb.r], [S.r])
        TT("dve", S[:], S[:], ups[0:64, :].rearrange("p (h e) -> p h e", h=4), ALU.add, [S.r, ups.r], [S.r])
        CP(PENG, Sb[:], S[:], [S.r], [Sb.r])

    for d in range(2):
        MS("dve", Sst[d][:], 0.0, [Sst[d].r])
        MS(PENG, Sbf[d][:], 0.0, [Sbf[d].r])
        MS("dve", Dcum[d][:], 1.0, [Dcum[d].r])
    for j in range(2 if GM > 0 else 0):
        gla_step(0, 16 + j, False)
        gla_step(1, 17 - j, False)
    for d in range(2):
        TS("dve", Sst[d][:], Sst[d][:], flags[:, d:d + 1], None, ALU.mult, None, [Sst[d].r, flags.r], [Sst[d].r])
        CP(PENG, Sbf[d][:], Sst[d][:], [Sst[d].r], [Sbf[d].r])
    for j in range(16 if GM > 0 else 0):
        gla_step(0, j, True)
        gla_step(1, 15 - j, True)
    for d in range(2):
        P.dma_copy("sp", Sfin.t[d, :, :], Sst[d][:].rearrange("p h e -> p (h e)"), reads=[Sst[d].r], writes=[Sfin.r])

    P.release(m_stage)


def emit_l2(P, E, L):
    nc = P.nc
    H = mk_helpers(P)
    MM, TR, ACT, TT, TS, STT, CP, MS = H.MM, H.TR, H.ACT, H.TT, H.TS, H.STT, H.CP, H.MS
    din = lambda name, shape, dt=F32: E.din(name, shape, dt, L)
    dout = lambda name, shape, dt: E.dout(name, shape, dt, L)
    m_stage = P.mark()

    x_in = din("x_in", [NTOK, 1024])
    modD = din("modD", [2, 6144])
    aqT = din("aqT", [512, NTOK], BF16)
    akT = din("akT", [128, NTOK], BF16)
    av = din("av", [NTOK, 128], BF16)
    g_ak = din("g_ak", [256, 2048], BF16)
    g_av = din("g_av", [4096, 128], BF16)
    g_ck = din("g_ck", [1024, 768], BF16)
    g_cv = din("g_cv", [1536, 512], BF16)
    g_S = din("g_S", [256, 512])
    cqT = din("cqT", [512, NTOK], BF16)
    ckT = din("ckT", [512, NTOK], BF16)
    cv = din("cv", [NTOK, 512], BF16)
    tabNA = din("tabNA", [5, 7, 2, 128, 512], BF16)
    Og = din("Og", [2, 512, NTOK])
    qBT = din("qBT", [2, 256, 2048], BF16)
    fl1m = din("fl1m", [64, 2])
    rT = din("rT", [512, NTOK], BF16)
    gT = din("gT", [3072, NTOK], BF16)
    gn_d = din("gla_norm", [128, 1])
    wba_d = din("w_br_attn", [512, 1024])
    wbg_d = din("w_br_gla", [512, 1024])
    wbn_d = din("w_br_na", [512, 1024])
    wout_d = din("w_out", [1024, 1024])
    ln1g_d = din("ln1_g", [1, 1024])
    ln1b_d = din("ln1_b", [1, 1024])
    ones_d = din("ones128", [128, 128])
    x1_o = dout("x1", [NTOK, 1024], F32)
    h2_o = dout("h2", [NTOK, 1024], F32)

    bankS = Banks(P, 0, E.fb[0:3])
    bankA = Ring(E.fb[3:5])
    bankM = Ring(E.fb[5:7])

    oaD = dout("oaD", [512, NTOK], BF16)
    ocD = dout("ocD", [512, NTOK], BF16)
    obD = dout("obD", [512, NTOK], BF16)
    stgr = Ring([P.sbuf(f"ostg{i}", [128, 512], BF16) for i in range(3)])
    epsc = P.sbuf("epsc", [128, 2], F32)
    MS("dve", epsc[:, 0:1], LN_EPS, [epsc.r])
    MS("dve", epsc[:, 1:2], RMS_EPS, [epsc.r])
    rdr = Ring([P.sbuf(f"rd{i}", [128, 512], F32) for i in range(2)])
    rd0r = Ring([P.sbuf(f"rd0{i}", [64, 512], F32) for i in range(2)])
    ptr = Ring([P.sbuf(f"pt{i}", [128, 512], BF16) for i in range(8)])

    def blk_of(t0):
        return min(t0 // 512, 4)

    def normalize(po, n, dest, dres, view=None):
        rd = rdr.get()
        P.op("dve", lambda e: e.reciprocal(out=rd[64:128, 0:n], in_=po[64:128, 0:n]), [po.r], [rd.r])
        rd0 = rd0r.get()
        P.dma_copy("sp", rd0[0:64, 0:n], rd[64:128, 0:n], reads=[rd.r], writes=[rd0.r])
        a, b_ = po[0:64, 0:n], rd0[0:64, 0:n]
        if view is not None:
            a, b_ = view(a), view(b_)
        TT("dve", dest, a, b_, ALU.mult, [po.r, rd0.r], [dres])

    markA = P.mark()
    bankSA = Banks(P, 0, list(bankS.b) + list(bankM.bufs))
    KT = [P.sbuf(f"KT{g}", [64, NKT * 128], BF16) for g in range(2)]
    V1 = [P.sbuf(f"V1{g}", [128, NKT, 128], BF16) for g in range(2)]
    for g in range(2):
        for r_ in range(2):
            P.dma_copy("sp", KT[g][:, r_ * 2048:(r_ + 1) * 2048], g_ak.t[r_ * 128 + g * 64:r_ * 128 + (g + 1) * 64, :],
                       reads=[g_ak.r], writes=[KT[g].r])
        P.dma_copy("sp", KT[g][:, 4096:4352], akT.t[g * 64:(g + 1) * 64, 2048:2304], reads=[akT.r], writes=[KT[g].r])
        MS("pool", V1[g][:, :, 64:128], 1.0, [V1[g].r])
        P.dma_copy("sp", V1[g][:, 0:32, 0:64], g_av.t[:, g * 64:(g + 1) * 64].rearrange("(kt p) d -> p kt d", p=128),
                   reads=[g_av.r], writes=[V1[g].r])
        P.dma_copy("sp", V1[g][:, 32:34, 0:64], av.t[2048:2304, g * 64:(g + 1) * 64].rearrange("(kt p) d -> p kt d", p=128),
                   reads=[av.r], writes=[V1[g].r])
    qbr = Ring([P.sbuf(f"qblk{i}", [64, 8, 512], BF16) for i in range(2)])
    for bi, (t0, n) in enumerate(BLKS if 'A' not in os.environ.get('L2SKIP', '') else []):
        qb = qbr.get()
        P.dma_copy("sp", qb[:, :, 0:n], aqT.t[:, t0:t0 + n].rearrange("(h p) t -> p h t", p=64), writes=[qb.r])
        kts = list(range(NKT)) if bi < 4 else [32, 33]
        for h in range(8):
            g = h // 4
            po = bankA.get()
            LOOK = 4
            pend_ = []

            def issue_qk(kt):
                ps = bankSA.get()
                MM(ps[:, 0:n], KT[g][:, kt * 128:(kt + 1) * 128], qb[:, h, 0:n], True, True, [KT[g].r, qb.r], [ps.r])
                pt = ptr.get()
                ACT(pt[:, 0:n], ps[:, 0:n], AF.Exp, [ps.r], [pt.r], scale=0.125)
                pend_.append(pt)
            for kt in kts[:LOOK]:
                issue_qk(kt)
            for idx, kt in enumerate(kts):
                if idx + LOOK < len(kts):
                    issue_qk(kts[idx + LOOK])
                pt = pend_.pop(0)
                MM(po[:, 0:n], V1[g][:, kt, :], pt[:, 0:n], idx == 0, idx == len(kts) - 1, [V1[g].r, pt.r], [po.r])
            stg = stgr.get()
            normalize(po, n, stg[0:64, 0:n], stg.r)
            P.dma_copy("sp", oaD.t[h * 64:(h + 1) * 64, t0:t0 + n], stg[0:64, 0:n], reads=[stg.r], writes=[oaD.r])
    P.release(markA)

    markN = P.mark()
    KTc = P.sbuf("KTc", [64, 8, 256], BF16)
    V1c = P.sbuf("V1c", [128, 2, 8, 128], BF16)
    P.dma_copy("sp", KTc[:], ckT.t[:, 2048:2304].rearrange("(h p) t -> p h t", p=64), writes=[KTc.r])
    MS("pool", V1c[:, :, :, 64:128], 1.0, [V1c.r])
    for kt in range(2):
        P.dma_copy("sp", V1c[:, kt, :, 0:64],
                   cv.t[2048 + kt * 128:2048 + (kt + 1) * 128, :].rearrange("p (h d) -> p h d", d=64), writes=[V1c.r])
    qtr = Ring([P.sbuf(f"nq{i}", [64, 8, 128], BF16) for i in range(2)])
    kwin = P.sbuf("kwin", [64, 8, 8 * 128], BF16, nres=8)
    vwin = P.sbuf("vwin", [128, 8, 8, 128], BF16, nres=8)
    MS("pool", vwin[:, :, :, 64:128], 1.0, vwin.rs)
    tabI = P.sbuf("tabI", [128, 7, 2, 512], BF16)
    for kt in range(7):
        for grp in range(2):
            P.dma_copy("sp", tabI[:, kt, grp, :], tabNA.t[4, kt, grp], writes=[tabI.r])
    nloaded = [-1]

    def load_win_tile(t_):
        sl = t_ % 8
        if t_ < 3:
            ksrc, kres = g_ck.t[0:512, 384 + t_ * 128:384 + (t_ + 1) * 128], g_ck.r
            vsrc, vres = g_cv.t[384 + t_ * 128:384 + (t_ + 1) * 128, :], g_cv.r
        elif t_ < 19:
            ksrc, kres = ckT.t[:, (t_ - 3) * 128:(t_ - 2) * 128], ckT.r
            vsrc, vres = cv.t[(t_ - 3) * 128:(t_ - 2) * 128, :], cv.r
        else:
            ksrc, kres = g_ck.t[512:1024, (t_ - 19) * 128:(t_ - 18) * 128], g_ck.r
            vsrc, vres = g_cv.t[768 + (t_ - 19) * 128:768 + (t_ - 18) * 128, :], g_cv.r
        P.dma_copy("sp", kwin[:, :, sl * 128:(sl + 1) * 128], ksrc.rearrange("(h p) t -> p h t", p=64),
                   reads=[kres], writes=[kwin.rs[sl]])
        P.dma_copy("sp", vwin[:, sl, :, 0:64], vsrc.rearrange("p (h d) -> p h d", d=64), reads=[vres], writes=[vwin.rs[sl]])
    tbr = Ring([P.sbuf(f"ntb{i}", [128, 512], BF16) for i in range(3)])
    tmr = Ring([P.sbuf(f"ntm{i}", [128, 512], F32) for i in range(2)])
    nptr = Ring([P.sbuf(f"npt{i}", [128, 512], BF16) for i in range(18)])
    for j in range(18 if 'N' not in os.environ.get('L2SKIP', '') else 0):
        qt = qtr.get()
        P.dma_copy("sp", qt[:], cqT.t[:, j * 128:(j + 1) * 128].rearrange("(h p) t -> p h t", p=64), writes=[qt.r])
        if j < 16:
            while nloaded[0] < j + 6:
                nloaded[0] += 1
                load_win_tile(nloaded[0])
            kts = list(range(9))
            slot = j if j < 2 else (j - 12 if j >= 14 else 4)
        else:
            kts = [7, 8]
        for grp in range(2):
            pts = {}
            for idx, kt in enumerate(kts):
                sps = bankS.get()
                for hh in range(4):
                    h = grp * 4 + hh
                    if kt < 7:
                        lhs, rd_ = kwin[:, h, ((j + kt) % 8) * 128:((j + kt) % 8 + 1) * 128], kwin.rs[(j + kt) % 8]
                    else:
                        lhs, rd_ = KTc[:, h, (kt - 7) * 128:(kt - 6) * 128], KTc.r
                    MM(sps[:, hh * 128:(hh + 1) * 128], lhs, qt[:, h, :], True, True, [rd_, qt.r], [sps.r])
                pt = nptr.get()
                pts[kt] = pt
                if kt < 7:
                    tm = tmr.get()
                    if slot == 4:
                        STT("dve", tm[:], sps[:], 0.125, tabI[:, kt, grp, :], ALU.mult, ALU.add, [sps.r, tabI.r], [tm.r])
                    else:
                        tb = tbr.get()
                        P.dma_copy("sp", tb[:], tabNA.t[slot, kt, grp], writes=[tb.r])
                        STT("dve", tm[:], sps[:], 0.125, tb[:], ALU.mult, ALU.add, [sps.r, tb.r], [tm.r])
                    ACT(pt[:], tm[:], AF.Exp, [tm.r], [pt.r])
                else:
                    ACT(pt[:], sps[:], AF.Exp, [sps.r], [pt.r], scale=0.125)
            po = bankA.get()
            for hh in range(4):
                h = grp * 4 + hh
                for idx, kt in enumerate(kts):
                    pt = pts[kt]
                    if kt < 7:
                        lhs, rd_ = vwin[:, (j + kt) % 8, h, :], vwin.rs[(j + kt) % 8]
                    else:
                        lhs, rd_ = V1c[:, kt - 7, h, :], V1c.r
                    MM(po[:, hh * 128:(hh + 1) * 128], lhs, pt[:, hh * 128:(hh + 1) * 128], idx == 0, idx == len(kts) - 1,
                       [rd_, pt.r], [po.r])
            stg = stgr.get()
            normalize(po, 512, stg[0:64, :], stg.r)
            P.dma_copy("sp", ocD.t[grp * 256:(grp + 1) * 256, j * 128:(j + 1) * 128].rearrange("(h p) t -> p h t", p=64),
                       stg[0:64, :].rearrange("p (h t) -> p h t", h=4), reads=[stg.r], writes=[ocD.r])
    P.release(markN)

    markG = P.mark()
    ones128 = P.sbuf("ones128", [128, 128], BF16)
    P.dma_copy("pool", ones128[:], ones_d.t[:, :], writes=[ones128.r])
    gn = P.sbuf("gn", [128, 1], F32)
    P.dma_copy("sp", gn[:], gn_d.t[:, :], writes=[gn.r])
    f1 = P.sbuf("f1", [64, 2], F32)
    P.dma_copy("sp", f1[:], fl1m.t[:, :], writes=[f1.r])
    Sin = []
    for d in range(2):
        sp_ = P.sbuf(f"Spart{d}", [64, 512], F32)
        P.dma_copy("sp", sp_[:], g_S.t[d * 128 + d * 64:d * 128 + (d + 1) * 64, :], reads=[g_S.r], writes=[sp_.r])
        sb = P.sbuf(f"Sin{d}", [64, 4, 128], BF16)
        TS("dve", sb[:].rearrange("p h e -> p (h e)"), sp_[:], f1[:, d:d + 1], None, ALU.mult, None, [sp_.r, f1.r], [sb.r])
        Sin.append(sb)
    qBr = [Ring([P.sbuf(f"qB{d}{i}", [64, 4, 512], BF16) for i in range(2)]) for d in range(2)]
    ogr = [Ring([P.sbuf(f"og{d}{i}", [128, 512], F32) for i in range(2)]) for d in range(2)]
    osr = Ring([P.sbuf(f"os{i}", [128, 512], F32) for i in range(2)])
    sqr = Ring([P.sbuf(f"sq{i}", [128, 512], BF16) for i in range(2)])
    rsr = Ring([P.sbuf(f"rs{i}", [128, 512], F32) for i in range(2)])
    rtr = Ring([P.sbuf(f"rt{i}", [128, 512], BF16) for i in range(2)])
    t3r = Ring([P.sbuf(f"t3{i}", [128, 512], F32) for i in range(2)])
    for bi, (t0, n) in enumerate(BLKS):
        lat = bi < 4
        if lat:
            qB = [qBr[d].get() for d in range(2)]
            for d in range(2):
                P.dma_copy("sp", qB[d][:, :, 0:n], qBT.t[d, :, t0:t0 + n].rearrange("(h p) t -> p h t", p=64),
                           writes=[qB[d].r])
        for h in range(4):
            og = [ogr[d].get() for d in range(2)]
            for d in range(2):
                P.dma_copy("sp", og[d][:, 0:n], Og.t[d, h * 128:(h + 1) * 128, t0:t0 + n], writes=[og[d].r])
            osum = osr.get()
            TT("pool", osum[:, 0:n], og[0][:, 0:n], og[1][:, 0:n], ALU.add, [og[0].r, og[1].r], [osum.r])
            if lat:
                pc = bankM.get()
                MM(pc[:, 0:n], Sin[0][:, h, :], qB[0][:, h, 0:n], True, False, [Sin[0].r, qB[0].r], [pc.r])
                MM(pc[:, 0:n], Sin[1][:, h, :], qB[1][:, h, 0:n], False, True, [Sin[1].r, qB[1].r], [pc.r])
                TT("dve", osum[:, 0:n], osum[:, 0:n], pc[:, 0:n], ALU.add, [osum.r, pc.r], [osum.r])
            sq = sqr.get()
            ACT(sq[:, 0:n], osum[:, 0:n], AF.Square, [osum.r], [sq.r])
            ss = bankM.get()
            MM(ss[:, 0:n], ones128[:], sq[:, 0:n], True, True, [ones128.r, sq.r], [ss.r])
            rs = rsr.get()
            ACT(rs[:, 0:n], ss[:, 0:n], AF.Sqrt, [ss.r, epsc.r], [rs.r], bias=epsc[:, 1:2], scale=1.0 / 128)
            P.op("dve", lambda e, rs=rs, n=n: e.reciprocal(out=rs[:, 0:n], in_=rs[:, 0:n]), [rs.r], [rs.r])
            rt = rtr.get()
            P.dma_copy("sp", rt[:, 0:n], rT.t[h * 128:(h + 1) * 128, t0:t0 + n], writes=[rt.r])
            t3 = t3r.get()
            STT("dve", t3[:, 0:n], osum[:, 0:n], gn[:, 0:1], rs[:, 0:n], ALU.mult, ALU.mult, [osum.r, gn.r, rs.r], [t3.r])
            stg = stgr.get()
            TT("pool", stg[:, 0:n], t3[:, 0:n], rt[:, 0:n], ALU.mult, [t3.r, rt.r], [stg.r])
            P.dma_copy("sp", obD.t[h * 128:(h + 1) * 128, t0:t0 + n], stg[:, 0:n], reads=[stg.r], writes=[obD.r])
    P.release(markG)

    wba = P.sbuf("wba", [64, 8, 1024], BF16)
    wbn = P.sbuf("wbn", [64, 8, 1024], BF16)
    wbg = P.sbuf("wbg", [128, 4, 1024], BF16)
    wo = P.sbuf("wo", [128, 8, 1024], BF16)
    P.dma_copy("pool", wba[:], wba_d.t.rearrange("(h p) f -> p h f", p=64), writes=[wba.r])
    P.dma_copy("pool", wbn[:], wbn_d.t.rearrange("(h p) f -> p h f", p=64), writes=[wbn.r])
    P.dma_copy("pool", wbg[:], wbg_d.t.rearrange("(h p) f -> p h f", p=128), writes=[wbg.r])
    P.dma_copy("pool", wo[:], wout_d.t.rearrange("(k p) f -> p k f", p=128), writes=[wo.r])
    bc = {}
    for nm, m in (("g1", 2), ("sh2", 3), ("sc2", 4)):
        for j in range(2):
            t = P.sbuf(f"bc_{nm}{j}", [128, 1024], F32)
            P.dma_copy("sp", t[:], modD.t[j:j + 1, m * 1024:(m + 1) * 1024].partition_broadcast(128), writes=[t.r])
            bc[(nm, j)] = t
    for j in range(2):
        TS("pool", bc[("sc2", j)][:], bc[("sc2", j)][:], 1.0, None, ALU.add, None, [bc[("sc2", j)].r], [bc[("sc2", j)].r])
    lg = P.sbuf("bc_ln1g", [128, 1024], F32)
    lb = P.sbuf("bc_ln1b", [128, 1024], F32)
    P.dma_copy("sp", lg[:], ln1g_d.t[0:1, :].partition_broadcast(128), writes=[lg.r])
    P.dma_copy("sp", lb[:], ln1b_d.t[0:1, :].partition_broadcast(128), writes=[lb.r])
    ymT = Ring([P.sbuf(f"ymT{i}", [128, 8, 512], BF16) for i in range(1)])
    ggr = Ring([P.sbuf(f"gg{i}", [128, 3, 512], BF16) for i in range(3)])
    tar = Ring([P.sbuf(f"ta{i}", [128, 512], F32) for i in range(2)])
    tbr2 = Ring([P.sbuf(f"tb2{i}", [128, 512], F32) for i in range(2)])
    xr = Ring([P.sbuf(f"xr{i}", [128, 1024], F32) for i in range(2)])
    ur = Ring([P.sbuf(f"ur{i}", [128, 1024], F32) for i in range(2)])
    x1r = Ring([P.sbuf(f"x1r{i}", [128, 1024], F32) for i in range(2)])
    h2r = Ring([P.sbuf(f"h2r{i}", [128, 1024], F32) for i in range(2)])
    str_ = Ring([P.sbuf(f"st{i}", [128, 2, 6], F32) for i in range(2)])
    mvr = Ring([P.sbuf(f"mv{i}", [128, 2], F32) for i in range(2)])

    def ln_stats(src):
        st = str_.get()
        for c in range(2):
            P.op("dve", lambda e, st=st, c=c: e.bn_stats(out=st[:, c, :], in_=src[:, c * 512:(c + 1) * 512]),
                 [src.r], [st.r])
        mv = mvr.get()
        P.op("dve", lambda e, st=st, mv=mv: e.bn_aggr(out=mv[:], in_=st[:].rearrange("p a b -> p (a b)")),
             [st.r], [mv.r])
        ACT(mv[:, 1:2], mv[:, 1:2], AF.Sqrt, [mv.r, epsc.r], [mv.r], bias=epsc[:, 0:1])
        P.op("dve", lambda e, mv=mv: e.reciprocal(out=mv[:, 1:2], in_=mv[:, 1:2]), [mv.r], [mv.r])
        return mv

    oabr = Ring([P.sbuf(f"oab{i}", [64, 8, 512], BF16) for i in range(2)])
    ocbr = Ring([P.sbuf(f"ocb{i}", [64, 8, 512], BF16) for i in range(2)])
    obbr = Ring([P.sbuf(f"obb{i}", [128, 4, 512], BF16) for i in range(2)])
    for bi, (t0, n) in enumerate(BLKS):
        ym = ymT.get()
        oab, ocb, obb = oabr.get(), ocbr.get(), obbr.get()
        P.dma_copy("sp", oab[:, :, 0:n], oaD.t[:, t0:t0 + n].rearrange("(h p) t -> p h t", p=64), reads=[oaD.r], writes=[oab.r])
        P.dma_copy("sp", ocb[:, :, 0:n], ocD.t[:, t0:t0 + n].rearrange("(h p) t -> p h t", p=64), reads=[ocD.r], writes=[ocb.r])
        P.dma_copy("sp", obb[:, :, 0:n], obD.t[:, t0:t0 + n].rearrange("(h p) t -> p h t", p=128), reads=[obD.r], writes=[obb.r])
        for fc in range(8):
            gg = ggr.get()
            P.dma_copy("sp", gg[:, :, 0:n],
                       gT.t[:, t0:t0 + n].rearrange("(b r) t -> r b t", b=3)[fc * 128:(fc + 1) * 128],
                       writes=[gg.r])
            fs = slice(fc * 128, (fc + 1) * 128)
            pa = bankS.get()
            for h in range(8):
                MM(pa[:, 0:n], wba[:, h, fs], oab[:, h, 0:n], h == 0, h == 7, [wba.r, oab.r], [pa.r])
            pb = bankS.get()
            for h in range(4):
                MM(pb[:, 0:n], wbg[:, h, fs], obb[:, h, 0:n], h == 0, h == 3, [wbg.r, obb.r], [pb.r])
            pcn = bankS.get()
            for h in range(8):
                MM(pcn[:, 0:n], wbn[:, h, fs], ocb[:, h, 0:n], h == 0, h == 7, [wbn.r, ocb.r], [pcn.r])
            ta, tb = tar.get(), tbr2.get()
            TT("dve", ta[:, 0:n], pa[:, 0:n], gg[:, 0, 0:n], ALU.mult, [pa.r, gg.r], [ta.r])
            TT("dve", tb[:, 0:n], pb[:, 0:n], gg[:, 1, 0:n], ALU.mult, [pb.r, gg.r], [tb.r])
            TT("pool", ta[:, 0:n], ta[:, 0:n], tb[:, 0:n], ALU.add, [ta.r, tb.r], [ta.r])
            TT("dve", tb[:, 0:n], pcn[:, 0:n], gg[:, 2, 0:n], ALU.mult, [pcn.r, gg.r], [tb.r])
            TT("pool", ym[:, fc, 0:n], ta[:, 0:n], tb[:, 0:n], ALU.add, [ta.r, tb.r], [ym.r])
        for ti in range(n // 128):
            i = t0 // 128 + ti
            j = 0 if i < 16 else 1
            xt = xr.get()
            P.dma_copy("sp", xt[:], x_in.t[i * 128:(i + 1) * 128, :], writes=[xt.r])
            u = ur.get()
            for hf in range(2):
                py = bankM.get()
                for fc in range(8):
                    MM(py[:, :], ym[:, fc, ti * 128:(ti + 1) * 128], wo[:, fc, hf * 512:(hf + 1) * 512], fc == 0, fc == 7,
                       [ym.r, wo.r], [py.r])
                TT("dve", u[:, hf * 512:(hf + 1) * 512], py[:, :], bc[("g1", j)][:, hf * 512:(hf + 1) * 512], ALU.mult,
                   [py.r, bc[("g1", j)].r], [u.r])
            STT("dve", u[:], xt[:], ALPHA, u[:], ALU.mult, ALU.add, [xt.r, u.r], [u.r])
            mv = ln_stats(u)
            x1 = x1r.get()
            TS("dve", x1[:], u[:], mv[:, 0:1], mv[:, 1:2], ALU.subtract, ALU.mult, [u.r, mv.r], [x1.r])
            TT("pool", x1[:], x1[:], lg[:], ALU.mult, [x1.r, lg.r], [x1.r])
            TT("pool", x1[:], x1[:], lb[:], ALU.add, [x1.r, lb.r], [x1.r])
            P.dma_copy("sp", x1_o.t[i * 128:(i + 1) * 128, :], x1[:], reads=[x1.r], writes=[x1_o.r])
            mv2 = ln_stats(x1)
            h2 = h2r.get()
            TS("dve", h2[:], x1[:], mv2[:, 0:1], mv2[:, 1:2], ALU.subtract, ALU.mult, [x1.r, mv2.r], [h2.r])
            TT("pool", h2[:], h2[:], bc[("sc2", j)][:], ALU.mult, [h2.r, bc[("sc2", j)].r], [h2.r])
            TT("pool", h2[:], h2[:], bc[("sh2", j)][:], ALU.add, [h2.r, bc[("sh2", j)].r], [h2.r])
            P.dma_copy("sp", h2_o.t[i * 128:(i + 1) * 128, :], h2[:], reads=[h2.r], writes=[h2_o.r])

    P.release(m_stage)


def emit_l3(P, E, L):
    nc = P.nc
    H = mk_helpers(P)
    MM, TR, ACT, TT, TS, STT, CP, MS = H.MM, H.TR, H.ACT, H.TT, H.TS, H.STT, H.CP, H.MS
    din = lambda name, shape, dt=F32: E.din(name, shape, dt, L)
    dout = lambda name, shape, dt: E.dout(name, shape, dt, L)
    m_stage = P.mark()

    x1_d = din("x1", [NTOK, 1024])
    h2_d = din("h2", [NTOK, 1024])
    modD = din("modD", [2, 6144])
    wr_d = din("w_r", [1024, 36])
    br_d = din("b_r", [1, 36])
    w1_d = din("moe_w1", [32 * 128, 4096])
    w3_d = din("moe_w3", [32 * 128, 4096])
    w2_d = din("moe_w2", [32 * 128, 4096])
    ln2g_d = din("ln2_g", [1, 1024])
    ln2b_d = din("ln2_b", [1, 1024])
    id32_d = din("ident", [128, 128])
    tris_d = din("triSlt", [128, 128])
    ones_d = din("ones128", [128, 128])
    thr_d = din("thr18", [128, 32, 18])
    thrj_d = din("thrj", [128, NBLK, 32])
    tokidx_d = din("tokidx", [128, NT], I32)
    wbase_d = din("wbase", [128, 8])
    x2_o = dout("x2", [NTOK, 1024], F32)
    h2b = dout("h2b", [NTOK + 128, 1024], BF16)
    tokslot = dout("tokslot", [NSLOT, 1], I32)
    yslot = dout("yslot", [NSLOT, 1024], F32)
    be_o = dout("be_o", [1, NBLK], I32)
    pos_o = dout("pos_o", [128, NT, 2], I32)
    gate_o = dout("gate_o", [128, NT, 2], F32)

    banks = Banks(P, 0, E.fb)
    pTb = E.bb

    id32 = P.sbuf("id32", [128, 128], F32)
    P.dma_copy("sp", id32[:], id32_d.t[:, :], writes=[id32.r])
    idb = P.sbuf("idb", [128, 128], BF16)
    P.dma_copy("pool", idb[:], id32_d.t[:, :], writes=[idb.r])
    tris = P.sbuf("tris", [128, 128], F32)
    P.dma_copy("sp", tris[:], tris_d.t[:, :], writes=[tris.r])
    ones = P.sbuf("ones", [128, 128], F32)
    P.dma_copy("sp", ones[:], ones_d.t[:, :], writes=[ones.r])
    thr = P.sbuf("thr", [128, 32, 18], F32)
    P.dma_copy("sp", thr[:], thr_d.t[:, :, :], writes=[thr.r])
    thrj = P.sbuf("thrj", [128, NBLK, 32], F32)
    P.dma_copy("sp", thrj[:], thrj_d.t[:, :, :], writes=[thrj.r])
    tokidx = P.sbuf("tokidx", [128, NT], I32)
    P.dma_copy("sp", tokidx[:], tokidx_d.t[:, :], writes=[tokidx.r])
    wr = P.sbuf("wr", [128, 8, 36], F32)
    P.dma_copy("sp", wr[:], wr_d.t.rearrange("(k p) c -> p k c", p=128), writes=[wr.r])
    br_bc = P.sbuf("br_bc", [128, 36], F32)
    P.dma_copy("sp", br_bc[:], br_d.t[0:1, :].partition_broadcast(128), writes=[br_bc.r])
    epsc = P.sbuf("epsc", [128, 1], F32)
    MS("dve", epsc[:], LN_EPS, [epsc.r])
    dum = P.sbuf("dum", [128, NBLK], I32)
    MS("pool", dum[:], NTOK, [dum.r])
    P.dma_copy("sp", tokslot.t.rearrange("(p j) o -> p (j o)", p=128), dum[:], reads=[dum.r], writes=[tokslot.r])
    zt = P.sbuf("zt", [128, 1024], BF16)
    MS("pool", zt[:], 0.0, [zt.r])
    P.dma_copy("sp", h2b.t[NTOK:NTOK + 128, :], zt[:], reads=[zt.r], writes=[h2b.r])

    oh1a = P.sbuf("oh1a", [128, NT, 32], F32)
    oh2a = P.sbuf("oh2a", [128, NT, 32], F32)
    Aall = P.sbuf("Aall", [128, NT, 32], F32)
    gates = P.sbuf("gates", [128, NT, 2], F32)
    posI = P.sbuf("posI", [128, NT, 2], I32)
    beI = P.sbuf("beI", [1, NBLK], I32)
    widx = P.sbuf("widx", [128, NBLK], I32)
    wbase = P.sbuf("wbase", [128, 8], F32)
    P.dma_copy("sp", wbase[:], wbase_d.t[:, :], writes=[wbase.r])
    carry = P.sbuf("carry", [128, 32], F32)
    MS("dve", carry[:], 0.0, [carry.r])
    excl = P.sbuf("excl", [128, NT, 32], F32)

    markR = P.mark()
    h2r = Ring([P.sbuf(f"h2t{i}", [128, 1024], F32) for i in range(2)])
    hTr = Ring([P.sbuf(f"h2T{i}", [128, 8, 128], F32) for i in range(2)])
    lgr = Ring([P.sbuf(f"lg{i}", [128, 36], F32) for i in range(2)])
    sm = Ring([P.sbuf(f"sm{i}", [128, 16], F32) for i in range(2)])
    elr = Ring([P.sbuf(f"elm{i}", [128, 32], F32) for i in range(2)])
    el2r = Ring([P.sbuf(f"elm2{i}", [128, 32], F32) for i in range(2)])
    for i in range(NT):
        ht = h2r.get()
        P.dma_copy("sp", ht[:], h2_d.t[i * 128:(i + 1) * 128, :], writes=[ht.r])
        P.dma_copy("pool", h2b.t[i * 128:(i + 1) * 128, :], ht[:], reads=[ht.r], writes=[h2b.r])
        hT = hTr.get()
        for half in range(2):
            pt = banks.get()
            for kk in range(4):
                k = half * 4 + kk
                TR(pt[:, kk * 128:(kk + 1) * 128], ht[:, k * 128:(k + 1) * 128], id32[:], [ht.r, id32.r], [pt.r])
            if half == 0:
                ACT(hT[:, 0:4, :].rearrange("p k t -> p (k t)"), pt[:, :], AF.Identity, [pt.r], [hT.r])
            else:
                CP("dve", hT[:, 4:8, :].rearrange("p k t -> p (k t)"), pt[:, :], [pt.r], [hT.r])
        pl = banks.get()
        for k in range(8):
            MM(pl[:, 0:36], hT[:, k, :], wr[:, k, :], k == 0, k == 7, [hT.r, wr.r], [pl.r])
        lg = lgr.get()
        TT("dve", lg[:], pl[:, 0:36], br_bc[:], ALU.add, [pl.r, br_bc.r], [lg.r])
        s = sm.get()
        P.op("dve", lambda e, s=s, lg=lg: e.reduce_max(out=s[:, 0:1], in_=lg[:, 0:4], axis=AX.X), [lg.r], [s.r])
        TS("dve", s[:, 1:2], s[:, 0:1], -1.0, None, ALU.mult, None, [s.r], [s.r])
        TS("dve", s[:, 8:12], lg[:, 0:4], s[:, 0:1], None, ALU.is_equal, None, [lg.r, s.r], [s.r])
        ACT(s[:, 12:16], lg[:, 0:4], AF.Exp, [lg.r, s.r], [s.r], bias=s[:, 1:2])
        P.op("dve", lambda e, s=s: e.reduce_sum(out=s[:, 2:3], in_=s[:, 12:16], axis=AX.X), [s.r], [s.r])
        P.op("dve", lambda e, s=s: e.reciprocal(out=s[:, 3:4], in_=s[:, 2:3]), [s.r], [s.r])
        TS("dve", s[:, 12:16], s[:, 8:12], 1.0, BIG, ALU.subtract, ALU.mult, [s.r], [s.r])
        elm = elr.get()
        TT("dve", elm[:].rearrange("p (g e) -> p g e", g=4), lg[:, 4:36].rearrange("p (g e) -> p g e", g=4),
           s[:, 12:16].unsqueeze(2).to_broadcast([128, 4, 8]), ALU.add, [lg.r, s.r], [elm.r])
        P.op("dve", lambda e, s=s, elm=elm: e.reduce_max(out=s[:, 4:5], in_=elm[:], axis=AX.X), [elm.r], [s.r])
        TS("dve", oh1a[:, i, :], elm[:], s[:, 4:5], None, ALU.is_equal, None, [elm.r, s.r], [oh1a.r])
        elm2 = el2r.get()
        STT("dve", elm2[:], oh1a[:, i, :], -BIG, elm[:], ALU.mult, ALU.add, [oh1a.r, elm.r], [elm2.r])
        P.op("dve", lambda e, s=s, elm2=elm2: e.reduce_max(out=s[:, 5:6], in_=elm2[:], axis=AX.X), [elm2.r], [s.r])
        TS("dve", oh2a[:, i, :], elm2[:], s[:, 5:6], None, ALU.is_equal, None, [elm2.r, s.r], [oh2a.r])
        TT("dve", Aall[:, i, :], oh1a[:, i, :], oh2a[:, i, :], ALU.add, [oh1a.r, oh2a.r], [Aall.r])
        TT("dve", s[:, 6:7], s[:, 5:6], s[:, 4:5], ALU.subtract, [s.r], [s.r])
        ACT(s[:, 6:7], s[:, 6:7], AF.Exp, [s.r], [s.r])
        TS("dve", s[:, 6:7], s[:, 6:7], 1.0, None, ALU.add, None, [s.r], [s.r])
        P.op("dve", lambda e, s=s: e.reciprocal(out=s[:, 7:8], in_=s[:, 6:7]), [s.r], [s.r])
        TT("dve", gates[:, i, 0:1], s[:, 3:4], s[:, 7:8], ALU.mult, [s.r], [gates.r])
        TT("dve", gates[:, i, 1:2], s[:, 3:4], gates[:, i, 0:1], ALU.subtract, [s.r, gates.r], [gates.r])
        pex = banks.get()
        MM(pex[:, 0:32], tris[:], Aall[:, i, :], True, True, [tris.r, Aall.r], [pex.r])
        TT("dve", excl[:, i, :], pex[:, 0:32], carry[:], ALU.add, [pex.r, carry.r], [excl.r])
        pcs = banks.get()
        MM(pcs[:, 0:32], ones[:], Aall[:, i, :], True, True, [ones.r, Aall.r], [pcs.r])
        TT("dve", carry[:], carry[:], pcs[:, 0:32], ALU.add, [carry.r, pcs.r], [carry.r])
    cmp18 = P.sbuf("cmp18", [128, 32, 18], F32)
    TT("dve", cmp18[:], carry[:].unsqueeze(2).to_broadcast([128, 32, 18]), thr[:], ALU.is_gt, [carry.r, thr.r], [cmp18.r])
    padded = P.sbuf("padded", [128, 32], F32)
    P.op("dve", lambda e: e.reduce_sum(out=padded[:], in_=cmp18[:], axis=AX.X), [cmp18.r], [padded.r])
    TS("dve", padded[:], padded[:], 128.0, None, ALU.mult, None, [padded.r], [padded.r])
    cs = [P.sbuf(f"cs{i}", [128, 32], F32) for i in range(2)]
    CP("dve", cs[0][:], padded[:], [padded.r], [cs[0].r])
    cur = 0
    for sh in (1, 2, 4, 8, 16):
        a, b_ = cs[cur], cs[1 - cur]
        CP("dve", b_[:, 0:sh], a[:, 0:sh], [a.r], [b_.r])
        TT("dve", b_[:, sh:32], a[:, sh:32], a[:, 0:32 - sh], ALU.add, [a.r], [b_.r])
        cur = 1 - cur
    pend = cs[cur]
    pstart = P.sbuf("pstart", [128, 32], F32)
    TT("dve", pstart[:], pend[:], padded[:], ALU.subtract, [pend.r, padded.r], [pstart.r])
    posf = P.sbuf("posf", [128, NT, 2], F32)
    tmp32 = Ring([P.sbuf(f"tmp32{i}", [128, 32], F32) for i in range(2)])
    for i in range(NT):
        sb_ = tmp32.get()
        TT("dve", sb_[:], excl[:, i, :], pstart[:], ALU.add, [excl.r, pstart.r], [sb_.r])
        for k, oh in enumerate((oh1a, oh2a)):
            t2 = tmp32.get()
            TT("dve", t2[:], sb_[:], oh[:, i, :], ALU.mult, [sb_.r, oh.r], [t2.r])
            P.op("dve", lambda e, t2=t2, i=i, k=k: e.reduce_sum(out=posf[:, i, k:k + 1], in_=t2[:], axis=AX.X),
                 [t2.r], [posf.r])
    CP("dve", posI[:], posf[:], [posf.r], [posI.r])
    cmpj = P.sbuf("cmpj", [128, NBLK, 32], F32)
    TT("dve", cmpj[:], pend[:].unsqueeze(1).to_broadcast([128, NBLK, 32]), thrj[:], ALU.is_le, [pend.r, thrj.r], [cmpj.r])
    bef = P.sbuf("bef", [128, NBLK], F32)
    P.op("dve", lambda e: e.reduce_sum(out=bef[:], in_=cmpj[:], axis=AX.X), [cmpj.r], [bef.r])
    TS("dve", bef[:], bef[:], 31.0, None, ALU.min, None, [bef.r], [bef.r])
    CP("dve", beI[:], bef[0:1, :], [bef.r], [beI.r])
    used = P.sbuf("used", [128, NBLK], F32)
    TS("dve", used[:], thrj[:, :, 0], pend[:, 31:32], None, ALU.is_lt, None, [thrj.r, pend.r], [used.r])
    TS("dve", used[:], used[:], -8192.0, 8192.0, ALU.mult, ALU.add, [used.r], [used.r])
    wif = P.sbuf("wif", [128, NBLK], F32)
    TS("dve", wif[:], bef[:], 128.0, wbase[:, 0:1], ALU.mult, ALU.add, [bef.r, wbase.r], [wif.r])
    TT("dve", wif[:], wif[:], used[:], ALU.add, [wif.r, used.r], [wif.r])
    CP("dve", widx[:], wif[:], [wif.r], [widx.r])
    P.dma_copy("sp", be_o.t[:, :], beI[:], reads=[beI.r], writes=[be_o.r])
    P.dma_copy("sp", pos_o.t[:, :, :], posI[:], reads=[posI.r], writes=[pos_o.r])
    P.dma_copy("sp", gate_o.t[:, :, :], gates[:], reads=[gates.r], writes=[gate_o.r])
    for i in range(NT):
        for k in range(2):
            P.dma("pool", lambda e, i=i, k=k: e.indirect_dma_start(
                out=tokslot.t[:, :], out_offset=bass.IndirectOffsetOnAxis(ap=posI[:, i, k:k + 1], axis=0),
                in_=tokidx[:, i:i + 1], in_offset=None), reads=[posI.r, tokidx.r], writes=[tokslot.r])
    P.release(markR)

    markE = P.mark()
    w1r = Ring([P.sbuf(f"w1b{i}", [128, 8, 512], BF16) for i in range(2)])
    w3r = Ring([P.sbuf(f"w3b{i}", [128, 8, 512], BF16) for i in range(2)])
    w2r = Ring([P.sbuf(f"w2b{i}", [128, 4, 1024], BF16) for i in range(2)])
    idxr = Ring([P.sbuf(f"idx{i}", [128, 1], I32) for i in range(2)])
    xgr = Ring([P.sbuf(f"xg{i}", [128, 1024], BF16) for i in range(2)])
    xTr = Ring([P.sbuf(f"xT{i}", [128, 8, 128], BF16) for i in range(2)])
    s1r = Ring([P.sbuf(f"s1{i}", [128, 512], F32) for i in range(2)])
    aTr = Ring([P.sbuf(f"aT{i}", [128, 4, 128], BF16) for i in range(2)])
    ysr = Ring([P.sbuf(f"ys{i}", [128, 1024], F32) for i in range(2)])

    bcreg = {}
    for j in range(NBLK):
        w1b, w3b, w2b = w1r.get(), w3r.get(), w2r.get()

        for (wb_, wd_) in ((w1b, w1_d), (w3b, w3_d), (w2b, w2_d)):
            def wgather(e, wb_=wb_, wd_=wd_, j=j):
                if not hasattr(P, "bnd_val"):
                    r_ = e.alloc_register("bnd")
                    e.reg_mov(r_, 32 * 128 - 1)
                    P.bnd_val = e.snap(r_)
                bcreg["v"] = P.bnd_val
                return e.indirect_dma_start(
                    out=wb_[:].rearrange("p a f -> p (a f)"), out_offset=None, in_=wd_.t[:, :],
                    in_offset=bass.IndirectOffsetOnAxis(ap=widx[:, j:j + 1], axis=0),
                    bounds_check=bcreg["v"], oob_is_err=False)
            P.dma("pool", wgather, reads=[widx.r], writes=[wb_.r])
        idx = idxr.get()
        P.dma_copy("sp", idx[:], tokslot.t[j * 128:(j + 1) * 128, :], reads=[tokslot.r], writes=[idx.r])
        xg = xgr.get()
        P.dma("pool", lambda e, xg=xg, idx=idx: e.indirect_dma_start(
            out=xg[:, :], out_offset=None, in_=h2b.t[:, :],
            in_offset=bass.IndirectOffsetOnAxis(ap=idx[:, 0:1], axis=0)), reads=[idx.r, h2b.r], writes=[xg.r])
        for k in range(8):
            TR(pTb[:, k, :], xg[:].rearrange("p (a i) -> p a i", i=8)[:, :, k], idb[:], [xg.r, idb.r], [pTb.r])
        xT = xTr.get()
        ACT(xT[:, 0:4, :], pTb[:, 0:4, :], AF.Identity, [pTb.r], [xT.r])
        CP("dve", xT[:, 4:8, :], pTb[:, 4:8, :], [pTb.r], [xT.r])
        p1, p3 = banks.get(), banks.get()
        for (pp, wb) in ((p1, w1b), (p3, w3b)):
            for fc in range(4):
                for k in range(8):
                    MM(pp[:, fc * 128:(fc + 1) * 128], wb[:, k, :].rearrange("p (a c) -> p a c", c=4)[:, :, fc], xT[:, k, :], k == 0, k == 7,
                       [wb.r, xT.r], [pp.r])
        s1 = s1r.get()
        ACT(s1[:], p1[:, :], AF.Silu, [p1.r], [s1.r])
        aT = aTr.get()
        TT("dve", aT[:].rearrange("p f t -> p (f t)"), s1[:], p3[:, :], ALU.mult, [s1.r, p3.r], [aT.r])
        ys = ysr.get()
        for half in range(2):
            py = banks.get()
            for fc in range(4):
                MM(py[:, :], aT[:, fc, :], w2b[:, fc, half * 512:(half + 1) * 512], fc == 0, fc == 3, [aT.r, w2b.r], [py.r])
            if half == 0:
                ACT(ys[:, 0:512], py[:, :], AF.Identity, [py.r], [ys.r])
            else:
                CP("dve", ys[:, 512:1024], py[:, :], [py.r], [ys.r])
        P.dma_copy("sp", yslot.t[j * 128:(j + 1) * 128, :], ys[:], reads=[ys.r], writes=[yslot.r])
    P.release(markE)

    g2bc = []
    for j in range(2):
        t = P.sbuf(f"g2bc{j}", [128, 1024], F32)
        P.dma_copy("sp", t[:], modD.t[j:j + 1, 5 * 1024:6 * 1024].partition_broadcast(128), writes=[t.r])
        g2bc.append(t)
    lg2 = P.sbuf("lg2", [128, 1024], F32)
    lb2 = P.sbuf("lb2", [128, 1024], F32)
    P.dma_copy("sp", lg2[:], ln2g_d.t[0:1, :].partition_broadcast(128), writes=[lg2.r])
    P.dma_copy("sp", lb2[:], ln2b_d.t[0:1, :].partition_broadcast(128), writes=[lb2.r])
    y1r = Ring([P.sbuf(f"y1{i}", [128, 1024], F32) for i in range(2)])
    y2r = Ring([P.sbuf(f"y2{i}", [128, 1024], F32) for i in range(2)])
    x1r = Ring([P.sbuf(f"x1t{i}", [128, 1024], F32) for i in range(2)])
    ur = Ring([P.sbuf(f"u{i}", [128, 1024], F32) for i in range(2)])
    str_ = Ring([P.sbuf(f"st{i}", [128, 2, 6], F32) for i in range(2)])
    mvr = Ring([P.sbuf(f"mv{i}", [128, 2], F32) for i in range(2)])
    for i in range(NT):
        j = 0 if i < 16 else 1
        y1, y2 = y1r.get(), y2r.get()
        for k, yy in enumerate((y1, y2)):
            P.dma("pool", lambda e, yy=yy, i=i, k=k: e.indirect_dma_start(
                out=yy[:, :], out_offset=None, in_=yslot.t[:, :],
                in_offset=bass.IndirectOffsetOnAxis(ap=posI[:, i, k:k + 1], axis=0)),
                reads=[posI.r, yslot.r], writes=[yy.r])
        xt = x1r.get()
        P.dma_copy("sp", xt[:], x1_d.t[i * 128:(i + 1) * 128, :], writes=[xt.r])
        TS("dve", y1[:], y1[:], gates[:, i, 0:1], None, ALU.mult, None, [y1.r, gates.r], [y1.r])
        STT("dve", y1[:], y2[:], gates[:, i, 1:2], y1[:], ALU.mult, ALU.add, [y2.r, gates.r, y1.r], [y1.r])
        u = ur.get()
        TT("pool", u[:], y1[:], g2bc[j][:], ALU.mult, [y1.r, g2bc[j].r], [u.r])
        STT("dve", u[:], xt[:], ALPHA, u[:], ALU.mult, ALU.add, [xt.r, u.r], [u.r])
        st = str_.get()
        for c in range(2):
            P.op("dve", lambda e, st=st, u=u, c=c: e.bn_stats(out=st[:, c, :], in_=u[:, c * 512:(c + 1) * 512]),
                 [u.r], [st.r])
        mv = mvr.get()
        P.op("dve", lambda e, st=st, mv=mv: e.bn_aggr(out=mv[:], in_=st[:].rearrange("p a b -> p (a b)")),
             [st.r], [mv.r])
        ACT(mv[:, 1:2], mv[:, 1:2], AF.Sqrt, [mv.r, epsc.r], [mv.r], bias=epsc[:, 0:1])
        P.op("dve", lambda e, mv=mv: e.reciprocal(out=mv[:, 1:2], in_=mv[:, 1:2]), [mv.r], [mv.r])
        TS("dve", u[:], u[:], mv[:, 0:1], mv[:, 1:2], ALU.subtract, ALU.mult, [u.r, mv.r], [u.r])
        TT("pool", u[:], u[:], lg2[:], ALU.mult, [u.r, lg2.r], [u.r])
        TT("pool", u[:], u[:], lb2[:], ALU.add, [u.r, lb2.r], [u.r])
        P.dma_copy("sp", x2_o.t[i * 128:(i + 1) * 128, :], u[:], reads=[u.r], writes=[x2_o.r])

    P.release(m_stage)


LAYER_W = {"w_mod", "b_mod", "w_in", "b_in", "qn_g", "kn_g", "wgate", "bgate", "gla_norm", "w_br_attn", "w_br_gla",
           "w_br_na", "w_out", "ln1_g", "ln1_b", "w_r", "b_r", "moe_w1", "moe_w3", "moe_w2", "ln2_g", "ln2_b", "tabNA"}


class Env:
    def __init__(self, P):
        self.P = P
        self.scratch = {}
        self.ext = {}
        self.last = 1
        self.fb = [P.psum(f"fb{i}", [128, 512], F32) for i in range(7)]
        self.bb = P.psum("bb", [128, 8, 128], BF16)

    def _mk(self, name, shape, dt, kind):
        b = self.P.dram(name, shape, dt, kind=kind)
        b.t = b.t.ap()
        return b

    def din(self, name, shape, dt, L):
        if name == "x_in":
            if L == 0:
                if "x_in" not in self.ext:
                    self.ext["x_in"] = self._mk("x_in", shape, dt, "ExternalInput")
                return self.ext["x_in"]
            return self.scratch["x2"]
        if name in self.scratch:
            return self.scratch[name]
        key = f"{name}_{L}" if name in LAYER_W else name
        if key not in self.ext:
            self.ext[key] = self._mk(key, shape, dt, "ExternalInput")
        return self.ext[key]

    def dout(self, name, shape, dt, L):
        if name == "x2" and L == self.last:
            return self._mk("x2out", shape, dt, "ExternalOutput")
        if name not in self.scratch:
            self.scratch[name] = self._mk("sc_" + name, shape, dt, "Internal")
        return self.scratch[name]


def emit_exchange(P, E, groups):
    S = E.scratch

    def sc(name, shape, dt):
        if name not in S:
            S[name] = E._mk("sc_" + name, shape, dt, "Internal")
        return S[name]
    pk_ak, g_ak = sc("pk_ak", [128, 2048], BF16), sc("g_ak", [256, 2048], BF16)
    pk_av, g_av = sc("pk_av", [2048, 128], BF16), sc("g_av", [4096, 128], BF16)
    pk_ck, g_ck = sc("pk_ck", [512, 768], BF16), sc("g_ck", [1024, 768], BF16)
    pk_cv, g_cv = sc("pk_cv", [768, 512], BF16), sc("g_cv", [1536, 512], BF16)
    pk_S, g_S = sc("pk_S", [128, 512], F32), sc("g_S", [256, 512], F32)
    P.dma_copy("sp", pk_ak.t[:, :], S["akT"].t[:, 0:2048], reads=[S["akT"].r], writes=[pk_ak.r])
    P.dma_copy("sp", pk_av.t[:, :], S["av"].t[0:2048, :], reads=[S["av"].r], writes=[pk_av.r])
    P.dma_copy("sp", pk_ck.t[:, 0:384], S["ckT"].t[:, 0:384], reads=[S["ckT"].r], writes=[pk_ck.r])
    P.dma_copy("sp", pk_ck.t[:, 384:768], S["ckT"].t[:, 1664:2048], reads=[S["ckT"].r], writes=[pk_ck.r])
    P.dma_copy("sp", pk_cv.t[0:384, :], S["cv"].t[0:384, :], reads=[S["cv"].r], writes=[pk_cv.r])
    P.dma_copy("sp", pk_cv.t[384:768, :], S["cv"].t[1664:2048, :], reads=[S["cv"].r], writes=[pk_cv.r])
    P.dma_copy("sp", pk_S.t[:, :], S["Sfin"].t.rearrange("d p f -> (d p) f"), reads=[S["Sfin"].r], writes=[pk_S.r])
    for a, b_ in ((pk_ak, g_ak), (pk_av, g_av), (pk_ck, g_ck), (pk_cv, g_cv), (pk_S, g_S)):
        P.coll(lambda e, a=a, b_=b_: e.collective_compute("AllGather", ALU.bypass, replica_groups=groups,
                                                           ins=[a.t[:, :]], outs=[b_.t[:, :]]),
               reads=[a.r], writes=[b_.r])


def build_fused(groups=None, n_layers=2):
    if groups is None:
        groups = [[0, 1], [2, 3], [4, 5], [6, 7]]
    nc = bass.Bass("TRN2", target_bir_lowering=False)
    P = Prog(nc)
    E = Env(P)
    E.last = n_layers - 1
    for L in range(n_layers):
        emit_l1(P, E, L)
        emit_exchange(P, E, groups)
        emit_l2(P, E, L)
        emit_l3(P, E, L)
    P.finalize()
    return nc, P


_BF = ml_dtypes.bfloat16


def _rope_tables(tok):
    row = (tok // 64).astype(np.float32)
    col = (tok % 64).astype(np.float32)
    inv = (10000.0 ** (-np.arange(16, dtype=np.float32) / 16)).astype(np.float32)
    ar = row[:, None] * inv
    ac = col[:, None] * inv
    ang = np.concatenate([ar, ar, ac, ac], -1)
    return np.cos(ang).astype(np.float32), np.sin(ang).astype(np.float32)


def _consts(s):
    tok = s * 2048 + np.arange(2048)
    cos, sin = _rope_tables(tok)
    cosf = np.concatenate([cos, np.ones((256, 64), np.float32)], 0).T
    sinf = np.concatenate([sin, np.zeros((256, 64), np.float32)], 0).T
    R = np.zeros((64, 64), np.float32)
    for d in range(16):
        R[d, d + 16] = -1
        R[d + 16, d] = 1
        R[d + 32, d + 48] = -1
        R[d + 48, d + 32] = 1
    rotM = np.zeros((128, 128), np.float32)
    rotM[:64, :64] = R.T
    rotM[64:, 64:] = R.T
    ob = np.zeros((128, 128), np.float32)
    ob[:64, :64] = 1
    ob[64:, 64:] = 1
    ii = np.arange(128)
    sI, tI = np.meshgrid(ii, ii, indexing="ij")
    f32 = np.float32
    return dict(cosT=np.ascontiguousarray(np.concatenate([cosf, cosf], 0)),
                sinT=np.ascontiguousarray(np.concatenate([sinf, sinf], 0)),
                rotM=rotM, onesblk=ob, ident=np.eye(128, dtype=f32),
                triInc=(sI <= tI).astype(f32), triDec=(sI >= tI).astype(f32),
                triSgt=(sI > tI).astype(f32), triSlt=(sI < tI).astype(f32),
                flags=np.tile(np.array([[1.0 if s == 0 else 0.0, 1.0 if s == 1 else 0.0]], f32), (64, 1)),
                fl1m=np.tile(np.array([[0.0 if s == 0 else 1.0, 0.0 if s == 1 else 1.0]], f32), (64, 1)),
                ones128=np.ones((128, 128), f32),
                thr18=np.tile((128.0 * np.arange(18, dtype=f32))[None, None, :], (128, 32, 1)),
                thrj=np.tile((128.0 * np.arange(NBLK, dtype=f32))[None, :, None], (128, 1, 32)),
                wbase=(np.arange(8)[None, :] * 128 + np.arange(128)[:, None]).astype(f32),
                tokidx=(np.arange(18)[None, :] * 128 + np.arange(128)[:, None]).astype(np.int32))


def _na_table7(rpb, s):
    tab = np.full((5, 7, 2, 128, 512), -30000.0, np.float32)
    kp = np.arange(128)
    qp = np.arange(128)
    for slot, j in enumerate((0, 1, 14, 15, 5)):
        J = 16 * s + j
        qr = 2 * J + qp // 64
        qc = qp % 64
        rs = np.clip(qr - 4, 0, 56)
        cs = np.clip(qc - 8, 0, 48)
        for kt in range(7):
            T = J - 3 + kt
            if T < 0 or T > 31:
                continue
            kr = 2 * T + kp // 64
            kc = kp % 64
            valid = ((kr[:, None] >= rs[None, :]) & (kr[:, None] < rs[None, :] + 8)
                     & (kc[:, None] >= cs[None, :]) & (kc[:, None] < cs[None, :] + 16))
            ri = np.clip(kr[:, None] - qr[None, :] + 7, 0, 14)
            ci = np.clip(kc[:, None] - qc[None, :] + 15, 0, 30)
            for h in range(8):
                tab[slot, kt, h // 4, :, (h % 4) * 128:(h % 4 + 1) * 128] = np.where(valid, rpb[h][ri, ci], -30000.0)
    return tab.astype(_BF)


def _core_map(inp, c, n_layers=2):
    f32 = np.float32
    b, s = c // 2, c % 2
    m = dict(x_in=np.concatenate([inp["x"][b, s * 2048:(s + 1) * 2048], inp["ctx"][b]], 0),
             cvec=np.stack([inp["c"][b], inp["c_ctx"]], 0), **_consts(s))
    for l in range(n_layers):
        lw = dict(w_mod=inp["w_mod"][l], b_mod=inp["b_mod"][l][None], w_in=inp["w_in"][l], b_in=inp["b_in"][l][None],
                  qn_g=inp["attn_q_norm"][l][:, None], kn_g=inp["attn_k_norm"][l][:, None],
                  wgate=inp["gla_w_gate"][l], bgate=inp["gla_b_gate"][l], gla_norm=inp["gla_norm"][l][:, None],
                  w_br_attn=inp["w_br_attn"][l], w_br_gla=inp["w_br_gla"][l], w_br_na=inp["w_br_na"][l],
                  w_out=inp["w_out"][l], ln1_g=inp["ln1_g"][l][None], ln1_b=inp["ln1_b"][l][None],
                  w_r=np.concatenate([inp["w_router_group"][l], inp["w_router_expert"][l]], 1),
                  b_r=np.concatenate([inp["b_router_group"][l], inp["b_router_expert"][l]])[None],
                  moe_w1=inp["moe_w1"][l].reshape(-1, 4096), moe_w3=inp["moe_w3"][l].reshape(-1, 4096),
                  moe_w2=inp["moe_w2"][l].reshape(-1, 4096), ln2_g=inp["ln2_g"][l][None], ln2_b=inp["ln2_b"][l][None],
                  tabNA=_na_table7(inp["na_rpb"][l].astype(f32), s))
        for k, v in lw.items():
            m[f"{k}_{l}"] = v
    out = {}
    for k, v in m.items():
        v = np.asarray(v)
        if v.dtype not in (np.int32, _BF):
            v = v.astype(f32)
        out[k] = np.ascontiguousarray(v)
    return out


_PROG = []


def kernel(**inp):
    inp = {k: np.asarray(v) for k, v in inp.items()}
    if not _PROG:
        _PROG.append(build_fused()[0])
    cores = list(range(8))
    maps = [_core_map(inp, c) for c in cores]
    res = run_bass_kernel_spmd(_PROG[0], maps, core_ids=cores).results
    out = np.zeros((4, 4096, 1024), np.float32)
    for c in cores:
        out[c // 2, (c % 2) * 2048:(c % 2 + 1) * 2048] = np.asarray(res[c]["x2out"], dtype=np.float32)[:2048]
    return out
```

```python
import os
import numpy as np
import ml_dtypes
import concourse.bass as bass
import concourse.mybir as mybir
from concourse.bass_utils import run_bass_kernel_spmd

F32 = mybir.dt.float32
BF16 = mybir.dt.bfloat16
I32 = mybir.dt.int32
AF = mybir.ActivationFunctionType
ALU = mybir.AluOpType
AX = mybir.AxisListType


class Res:
    __slots__ = ("name", "w", "r", "dram")

    def __init__(self, name=""):
        self.name = name
        self.w = None
        self.r = {}
        self.dram = False


class Buf:
    def __init__(self, t, nres=1, name=""):
        self.t = t
        self.rs = [Res(f"{name}{i}") for i in range(nres)]

    @property
    def r(self):
        return self.rs[0]

    def __getitem__(self, k):
        return self.t[k]


class Prog:
    ENGS = ("pe", "act", "dve", "pool", "sp")
    SAME_SYNC = {"pe": False, "act": "a" not in os.environ.get("SSOFF",""), "dve": "d" not in os.environ.get("SSOFF",""), "pool": "p" not in os.environ.get("SSOFF",""), "sp": False}

    def __init__(self, nc, n_dma_sems=48):
        self.nc = nc
        self.lists = {k: [] for k in self.ENGS}
        self.semobj = {}
        self.cnt = {}
        for k in self.ENGS:
            self.semobj[k] = nc.alloc_semaphore(f"sem_{k}")
            self.cnt[k] = 0
        self.seen = {k: {} for k in self.ENGS}
        self.nd = n_dma_sems
        self.dval = [0] * n_dma_sems
        for i in range(n_dma_sems):
            self.semobj[f"d{i}"] = nc.alloc_semaphore(f"sem_d{i}")
        self.dnext = 0
        self.dnext_sw = 0
        self.pending = []
        self.loads_since = 0
        self.defer_stores = os.environ.get("DEFER", "1") == "1"
        self.n_ops = 0
        self.arena0, self.arena1 = nc.bump_sbuf(212000)
        self.sb_off = self.arena0
        self.sb_peak = self.arena0
        self.n_alloc = 0

    def sbuf(self, name, shape, dtype, nres=1):
        esz = {F32: 4, BF16: 2, I32: 4}[dtype]
        nb = esz
        for d in shape[1:]:
            nb *= d
        nb = (nb + 31) // 32 * 32
        assert self.sb_off + nb <= self.arena1, f"SBUF arena overflow at {name}: {self.sb_off + nb - self.arena0}"
        self.n_alloc += 1
        t = self.nc.alloc_sbuf_tensor_at(f"s{self.n_alloc}_{name}", list(shape), dtype, offset=self.sb_off)
        self.sb_off += nb
        self.sb_peak = max(self.sb_peak, self.sb_off)
        return Buf(t, nres, name)

    def mark(self):
        return self.sb_off

    def release(self, mark):
        self.barrier()
        self.sb_off = mark

    def barrier(self):
        self._flush()
        self.loads_since = 0
        ev = [(k, self.cnt[k]) for k in self.ENGS if self.cnt[k] > 0]
        ev += [(f"d{i}", self.dval[i]) for i in range(self.nd) if self.dval[i] > 0]
        if "cc" in self.semobj:
            ev.append(("cc", self.ccval))
        for k in self.ENGS:
            self._wait(k, ev)

    def psum(self, name, shape, dtype=F32, nres=1):
        return Buf(self.nc.alloc_psum_tensor("p_" + name, list(shape), dtype), nres, name)

    def dram(self, name, shape, dtype, kind="Internal", nres=1):
        b = Buf(self.nc.dram_tensor(name, list(shape), dtype, kind=kind), nres, name)
        for r in b.rs:
            r.dram = True
        return b

    def _flush(self, upto=None):
        pend = self.pending
        n = len(pend) if upto is None else upto
        for (fn, reads, writes) in pend[:n]:
            self._dma_now("sp", fn, reads, writes)
        del pend[:n]

    def _check_pending(self, reads, writes, is_compute):
        if not self.pending:
            return
        last = -1
        for i, (fn, sr, sw) in enumerate(self.pending):
            hit = False
            for w in writes:
                if any(w is x for x in sr) or any(w is x for x in sw):
                    hit = True
            for r in reads:
                if any(r is x for x in sw):
                    hit = True
            if hit:
                last = i
        if is_compute and self.loads_since > 0:
            last = len(self.pending) - 1
        if last >= 0:
            self._flush(last + 1)
            if not self.pending:
                self.loads_since = 0

    def _wait(self, eng, deps):
        for key, val in deps:
            if key == eng and not self.SAME_SYNC[eng]:
                continue
            if self.seen[eng].get(key, 0) >= val:
                continue
            self.seen[eng][key] = val
            self.lists[eng].append(("w", key, val))

    @staticmethod
    def _deps(reads, writes):
        deps = []
        for r in reads:
            if r.w is not None:
                deps.append(r.w)
        for w in writes:
            if w.w is not None:
                deps.append(w.w)
            deps.extend(w.r.items())
        return deps

    @staticmethod
    def _commit(ev, reads, writes):
        for r in reads:
            if r.r.get(ev[0], 0) < ev[1]:
                r.r[ev[0]] = ev[1]
        for w in writes:
            w.w = ev
            w.r = {}

    def op(self, eng, fn, reads=(), writes=()):
        self._check_pending(reads, writes, True)
        self._wait(eng, self._deps(reads, writes))
        self.cnt[eng] += 1
        ev = (eng, self.cnt[eng])
        self.lists[eng].append(("o", fn, eng, 1))
        self._commit(ev, reads, writes)
        self.n_ops += 1
        return ev

    def dma(self, q, fn, reads=(), writes=()):
        reads, writes = list(reads), list(writes)
        if (self.defer_stores and q == "sp" and writes and all(w.dram for w in writes)
                and reads and not any(r.dram for r in reads)):
            self._check_pending(reads, writes, False)
            self.pending.append((fn, reads, writes))
            return None
        self._check_pending(reads, writes, False)
        if q == "sp":
            self.loads_since += 1 if self.pending else 0
        return self._dma_now(q, fn, reads, writes)

    def _dma_now(self, q, fn, reads=(), writes=()):
        half = self.nd // 2
        if q == "pool":
            i = half + self.dnext_sw
            self.dnext_sw = (self.dnext_sw + 1) % (self.nd - half)
        else:
            i = self.dnext
            self.dnext = (self.dnext + 1) % half
        key = f"d{i}"
        deps = self._deps(reads, writes)
        if self.dval[i] > 0:
            deps.append((key, self.dval[i]))
        self._wait(q, deps)
        self.dval[i] += 16
        ev = (key, self.dval[i])
        self.lists[q].append(("o", fn, key, 16))
        self._commit(ev, reads, writes)
        self.n_ops += 1
        return ev

    def coll(self, fn, reads=(), writes=()):
        self._flush()
        if "cc" not in self.semobj:
            self.semobj["cc"] = self.nc.alloc_semaphore("sem_cc")
            self.ccval = 0
        deps = self._deps(reads, writes)
        if self.ccval > 0:
            deps.append(("cc", self.ccval))
        self._wait("pool", deps)
        self.ccval += 1
        ev = ("cc", self.ccval)
        self.lists["pool"].append(("o", fn, "cc", 1))
        self._commit(ev, reads, writes)
        return ev

    def dma_copy(self, q, out, in_, reads=(), writes=(), **kw):
        return self.dma(q, lambda e: e.dma_start(out=out, in_=in_, **kw), reads, writes)

    def finalize(self):
        self._flush()
        final = [(k, self.cnt[k]) for k in self.ENGS if self.cnt[k] > 0 and k != "sp"]
        final += [(f"d{i}", self.dval[i]) for i in range(self.nd) if self.dval[i] > 0]
        if "cc" in self.semobj:
            final.append(("cc", self.ccval))
        self._wait("sp", final)
        nc = self.nc
        lists = self.lists
        semobj = self.semobj

        def run(e, items):
            for it in items:
                if it[0] == "w":
                    e.wait_ge(semobj[it[1]], it[2])
                else:
                    ins = it[1](e)
                    ins.then_inc(semobj[it[2]], it[3])

        with nc.Block() as block:
            @block.tensor
            def _(e):
                run(e, lists["pe"])

            @block.scalar
            def _(e):
                run(e, lists["act"])

            @block.vector
            def _(e):
                run(e, lists["dve"])

            @block.gpsimd
            def _(e):
                run(e, lists["pool"])

            @block.sync
            def _(e):
                run(e, lists["sp"])


PENG = os.environ.get('PENG', 'pool')
GM = int(os.environ.get('GM', '9'))
NT = 18
NTOK = 2304
BLKS = [(0, 512), (512, 512), (1024, 512), (1536, 512), (2048, 256)]
LN_EPS = 1e-6
RMS_EPS = 1e-6

C_AQ, C_AK, C_AV, C_BQ, C_BK, C_BV, C_BR, C_BA, C_CQ, C_CK, C_CV, C_G = (
    0, 512, 640, 768, 1024, 1280, 1792, 2304, 2336, 2848, 3360, 3872)


class Ctx:
    pass


def mk_helpers(P):
    H = Ctx()

    def MM(out, lhsT, rhs, start, stop, reads, writes):
        P.op("pe", lambda e: e.matmul(out, lhsT=lhsT, rhs=rhs, start=start, stop=stop), reads, writes)

    def TR(out, in_, ident, reads, writes):
        P.op("pe", lambda e: e.transpose(out, in_, ident), reads, writes)

    def ACT(out, in_, func, reads, writes, bias=None, scale=None):
        kw = {}
        if bias is not None:
            kw["bias"] = bias
        if scale is not None:
            kw["scale"] = scale
        P.op("act", lambda e: e.activation(out=out, in_=in_, func=func, **kw), reads, writes)

    def TT(eng, out, in0, in1, op, reads, writes):
        P.op(eng, lambda e: e.tensor_tensor(out=out, in0=in0, in1=in1, op=op), reads, writes)

    def TS(eng, out, in0, s1, s2, op0, op1, reads, writes):
        if op1 is None:
            P.op(eng, lambda e: e.tensor_scalar(out=out, in0=in0, scalar1=s1, scalar2=None, op0=op0), reads, writes)
        else:
            P.op(eng, lambda e: e.tensor_scalar(out=out, in0=in0, scalar1=s1, scalar2=s2, op0=op0, op1=op1), reads, writes)

    def STT(eng, out, in0, scalar, in1, op0, op1, reads, writes):
        P.op(eng, lambda e: e.scalar_tensor_tensor(out=out, in0=in0, scalar=scalar, in1=in1, op0=op0, op1=op1), reads, writes)

    def CP(eng, out, in_, reads, writes):
        P.op(eng, lambda e: e.tensor_copy(out=out, in_=in_), reads, writes)

    def MS(eng, ap, val, writes):
        P.op(eng, lambda e: e.memset(ap, val), (), writes)

    H.MM, H.TR, H.ACT, H.TT, H.TS, H.STT, H.CP, H.MS = MM, TR, ACT, TT, TS, STT, CP, MS
    return H


class Banks:
    def __init__(self, P, n=8, bufs=None):
        self.b = list(bufs) if bufs is not None else [P.psum(f"bank{i}", [128, 512], F32) for i in range(n)]
        self.i = 0
        self.n = len(self.b)

    def get(self):
        b = self.b[self.i]
        self.i = (self.i + 1) % self.n
        return b


class Ring:
    def __init__(self, bufs):
        self.bufs = bufs
        self.i = 0

    def get(self):
        b = self.bufs[self.i]
        self.i = (self.i + 1) % len(self.bufs)
        return b


def tile_res(buf, t0, n):
    return [buf.rs[i] for i in range(t0 // 128, (t0 + n + 127) // 128)]


ALPHA = 4 ** 0.25
NKT = 34
NBLK = 50
SB = 256
NSLOT = NBLK * SB
BIG = 1.0e9
def emit_l1(P, E, L):
    nc = P.nc
    H = mk_helpers(P)
    MM, TR, ACT, TT, TS, STT, CP, MS = H.MM, H.TR, H.ACT, H.TT, H.TS, H.STT, H.CP, H.MS
    din = lambda name, shape, dt=F32: E.din(name, shape, dt, L)
    dout = lambda name, shape, dt: E.dout(name, shape, dt, L)
    m_stage = P.mark()

    x_in = din("x_in", [NTOK, 1024])
    cvec = din("cvec", [2, 1024])
    w_mod = din("w_mod", [1024, 6144])
    b_mod = din("b_mod", [1, 6144])
    w_in = din("w_in", [1024, 6944])
    b_in = din("b_in", [1, 6944])
    qn_g = din("qn_g", [64, 1])
    kn_g = din("kn_g", [64, 1])
    wgate_d = din("wgate", [2, 16, 256])
    bgate_d = din("bgate", [2, 256])
    cos_d = din("cosT", [128, NTOK])
    sin_d = din("sinT", [128, NTOK])
    ident_d = din("ident", [128, 128])
    rot_d = din("rotM", [128, 128])
    oblk_d = din("onesblk", [128, 128])

    gT = dout("gT", [3072, NTOK], BF16)
    rT = dout("rT", [512, NTOK], BF16)
    cqT = dout("cqT", [512, NTOK], BF16)
    ckT = dout("ckT", [512, NTOK], BF16)
    cv = dout("cv", [NTOK, 512], BF16)
    bqT = dout("bqT", [256, NTOK], F32)
    bkT = dout("bkT", [256, NTOK], F32)
    bk = dout("bk", [NTOK, 256], F32)
    bv = dout("bv", [NTOK, 512], BF16)
    Gd = dout("Gd", [NTOK, 2, 256], F32)
    aqT = dout("aqT", [512, NTOK], BF16)
    akT = dout("akT", [128, NTOK], BF16)
    av = dout("av", [NTOK, 128], BF16)
    modD = dout("modD", [2, 6144], F32)

    banks = Banks(P, 0, E.fb)
    pT = E.bb

    ident = P.sbuf("ident", [128, 128], BF16)
    P.dma_copy("pool", ident[:], ident_d.t[:, :], writes=[ident.r])
    oblk = P.sbuf("oblk", [128, 128], BF16)
    P.dma_copy("pool", oblk[:], oblk_d.t[:, :], writes=[oblk.r])
    rotM = P.sbuf("rotM", [128, 128], F32)
    P.dma_copy("sp", rotM[:], rot_d.t[:, :], writes=[rotM.r])
    cosT = P.sbuf("cosT", [128, NTOK], F32)
    sinT = P.sbuf("sinT", [128, NTOK], F32)
    P.dma_copy("sp", cosT[:], cos_d.t[:, :], writes=[cosT.r])
    P.dma_copy("sp", sinT[:], sin_d.t[:, :], writes=[sinT.r])
    g8 = P.sbuf("g8", [128, 2], F32)
    for hh in range(2):
        P.dma_copy("sp", g8[hh * 64:(hh + 1) * 64, 0:1], qn_g.t[:, :], writes=[g8.r])
        P.dma_copy("sp", g8[hh * 64:(hh + 1) * 64, 1:2], kn_g.t[:, :], writes=[g8.r])
    TS("dve", g8[:], g8[:], 8.0, None, ALU.mult, None, [g8.r], [g8.r])
    wgate = P.sbuf("wgate", [16, 2, 256], BF16)
    P.dma_copy("pool", wgate[:], wgate_d.t.rearrange("d r c -> r d c"), writes=[wgate.r])
    bg_bc = P.sbuf("bg_bc", [128, 2, 256], F32)
    for d in range(2):
        P.dma_copy("sp", bg_bc[:, d, :], bgate_d.t[d:d + 1, :].partition_broadcast(128), writes=[bg_bc.r])

    epsc = P.sbuf("epsc", [128, 2], F32)
    MS("dve", epsc[:, 0:1], LN_EPS, [epsc.r])
    MS("dve", epsc[:, 1:2], 64.0 * RMS_EPS, [epsc.r])
    cT = P.sbuf("cT", [128, 8, 2], F32)
    for j in range(2):
        P.dma_copy("sp", cT[:, :, j], cvec.t[j:j + 1, :].rearrange("o (k p) -> p (o k)", p=128), writes=[cT.r],
                   allow_slow_non_contiguous=True)
    scT = P.sbuf("scT", [128, 8, 2], BF16)
    ACT(scT[:], cT[:], AF.Silu, [cT.r], [scT.r])
    bm_r = Ring([P.sbuf(f"bm{i}", [2, 512], F32) for i in range(2)])
    mr_r = Ring([P.sbuf(f"mr{i}", [2, 512], F32) for i in range(2)])
    wsl = Ring([P.sbuf(f"wsg{i}", [128, 8, 512], BF16) for i in range(2)])
    for g in range(12):
        wb = wsl.get()
        P.dma_copy("pool", wb[:], w_mod.t[:, g * 512:(g + 1) * 512].rearrange("(k p) c -> p k c", p=128),
                   writes=[wb.r])
        bm = bm_r.get()
        for j in range(2):
            P.dma_copy("sp", bm[j:j + 1, :], b_mod.t[0:1, g * 512:(g + 1) * 512], writes=[bm.r])
        ps = banks.get()
        for k in range(8):
            MM(ps[0:2, 0:512], scT[:, k, :], wb[:, k, :], k == 0, k == 7, [scT.r, wb.r], [ps.r])
        mr = mr_r.get()
        TT("dve", mr[:], ps[0:2, 0:512], bm[:], ALU.add, [ps.r, bm.r], [mr.r])
        P.dma_copy("sp", modD.t[:, g * 512:(g + 1) * 512], mr[:], reads=[mr.r], writes=[modD.r])
    modc = P.sbuf("modc", [128, 2, 6, 8], F32)
    for j in range(2):
        for m in range(2):
            P.dma_copy("sp", modc[:, j, m, :],
                       modD.t[j:j + 1, m * 1024:(m + 1) * 1024].rearrange("o (k p) -> p (o k)", p=128),
                       reads=[modD.r], writes=[modc.r], allow_slow_non_contiguous=True)
    TS("dve", modc[:, :, 1, :], modc[:, :, 1, :], 1.0, None, ALU.add, None, [modc.r], [modc.r])

    hT = P.sbuf("hT", [128, 8, NTOK], BF16, nres=NT)
    xin = Ring([P.sbuf(f"xin{i}", [128, 1024], F32) for i in range(2)])
    xnr = Ring([P.sbuf(f"xn{i}", [128, 1024], BF16) for i in range(2)])
    str_ = Ring([P.sbuf(f"st{i}", [128, 2, 6], F32) for i in range(2)])
    mvr = Ring([P.sbuf(f"mv{i}", [128, 2], F32) for i in range(2)])
    for i in range(NT):
        xt = xin.get()
        P.dma_copy("sp", xt[:], x_in.t[i * 128:(i + 1) * 128, :], writes=[xt.r])
        st = str_.get()
        for c in range(2):
            P.op("dve", lambda e, st=st, xt=xt, c=c: e.bn_stats(out=st[:, c, :], in_=xt[:, c * 512:(c + 1) * 512]),
                 [xt.r], [st.r])
        mv = mvr.get()
        P.op("dve", lambda e, st=st, mv=mv: e.bn_aggr(out=mv[:], in_=st[:].rearrange("p a b -> p (a b)")),
             [st.r], [mv.r])
        ACT(mv[:, 1:2], mv[:, 1:2], AF.Sqrt, [mv.r], [mv.r], bias=epsc[:, 0:1])
        P.op("dve", lambda e, mv=mv: e.reciprocal(out=mv[:, 1:2], in_=mv[:, 1:2]), [mv.r], [mv.r])
        xn = xnr.get()
        TS("dve", xn[:], xt[:], mv[:, 0:1], mv[:, 1:2], ALU.subtract, ALU.mult, [xt.r, mv.r], [xn.r])
        for k in range(8):
            TR(pT[:, k, :], xn[:, k * 128:(k + 1) * 128], ident[:], [xn.r, ident.r], [pT.r])
        j = 0 if i < 16 else 1
        for k in range(8):
            o = hT[:, k, i * 128:(i + 1) * 128]
            if k % 2 == 0:
                ACT(o, pT[:, k, :], AF.Identity, [pT.r, modc.r], [hT.rs[i]],
                    bias=modc[:, j, 0, k:k + 1], scale=modc[:, j, 1, k:k + 1])
            else:
                TS("dve", o, pT[:, k, :], modc[:, j, 1, k:k + 1], modc[:, j, 0, k:k + 1], ALU.mult, ALU.add,
                   [pT.r, modc.r], [hT.rs[i]])

    bcols = P.sbuf("bcols", [128, 48], F32)
    bcol_idx = {}
    nb = 0
    for (c0, ng) in [(C_AQ, 4), (C_AK, 1), (C_BQ, 2), (C_BK, 2), (C_BR, 4), (C_CQ, 4), (C_CK, 4), (C_G, 24)]:
        P.dma_copy("sp", bcols[:, nb:nb + ng],
                   b_in.t[0:1, c0:c0 + ng * 128].rearrange("o (g p) -> p (o g)", p=128),
                   writes=[bcols.r], allow_slow_non_contiguous=True)
        for g in range(ng):
            bcol_idx[c0 + g * 128] = nb + g
        nb += ng
    bacol = P.sbuf("bacol", [16, 2], F32)
    P.dma_copy("sp", bacol[:], b_in.t[0:1, C_BA:C_BA + 32].rearrange("o (d p) -> p (o d)", p=16),
               writes=[bacol.r], allow_slow_non_contiguous=True)
    bias_bc = P.sbuf("bias_bc", [128, 1408], F32)
    tm_groups = [(C_AV, 128, 0), (C_BK, 256, 128), (C_BV, 512, 384), (C_CV, 512, 896)]
    for (c0, n, o) in tm_groups:
        P.dma_copy("sp", bias_bc[:, o:o + n], b_in.t[0:1, c0:c0 + n].partition_broadcast(128), writes=[bias_bc.r])

    mark1b = P.mark()
    stg_bf = Ring([P.sbuf(f"stgbf{i}", [128, NTOK], BF16) for i in range(3)])
    stg_f = Ring([P.sbuf(f"stgf{i}", [128, NTOK], F32) for i in range(2)])
    aux = {n: Ring([P.sbuf(f"{n}{i}", [128, 512], dt) for i in range(2)])
           for n, dt in [("zq", F32), ("sq", BF16), ("rs", F32), ("qn", F32), ("t1", F32), ("t2", F32)]}
    stm_bf = Ring([P.sbuf(f"stmbf{i}", [128, 512], BF16) for i in range(3)])
    stm_f = Ring([P.sbuf(f"stmf{i}", [128, 256], F32) for i in range(2)])
    aT = P.sbuf("aT", [16, 2, NTOK], BF16)

    def load_w(c0, n):
        wb = wsl.get()
        P.dma_copy("pool", wb[:, :, 0:n], w_in.t[:, c0:c0 + n].rearrange("(k p) c -> p k c", p=128),
                   writes=[wb.r])
        return wb

    def fm_mm(wb, off, m, t0, n):
        ps = banks.get()
        rd = [wb.r] + tile_res(hT, t0, n)
        for k in range(8):
            MM(ps[0:m, 0:n], wb[:, k, off:off + m], hT[:, k, t0:t0 + n], k == 0, k == 7, rd, [ps.r])
        return ps

    def fm_simple(wb, off, c0, func, dst, row0, f32=False, scale=None):
        stg = stg_f.get() if f32 else stg_bf.get()
        bc = bcols[:, bcol_idx[c0]:bcol_idx[c0] + 1]
        for (t0, n) in BLKS:
            ps = fm_mm(wb, off, 128, t0, n)
            ACT(stg[:, t0:t0 + n], ps[:, 0:n], func, [ps.r, bcols.r], [stg.r], bias=bc)
        P.dma_copy("sp", dst.t[row0:row0 + 128, :], stg[:], reads=[stg.r], writes=[dst.r])

    def fm_qk(wb, off, c0, gcol, dst, row0):
        stg = stg_bf.get()
        bc = bcols[:, bcol_idx[c0]:bcol_idx[c0] + 1]
        for (t0, n) in BLKS:
            ps = fm_mm(wb, off, 128, t0, n)
            zq, sq, rs, qn, t1, t2 = (aux[k].get() for k in ("zq", "sq", "rs", "qn", "t1", "t2"))
            ACT(zq[:, 0:n], ps[:, 0:n], AF.Identity, [ps.r, bcols.r], [zq.r], bias=bc)
            ACT(sq[:, 0:n], ps[:, 0:n], AF.Square, [ps.r, bcols.r], [sq.r], bias=bc)
            ss = banks.get()
            MM(ss[:, 0:n], oblk[:], sq[:, 0:n], True, True, [oblk.r, sq.r], [ss.r])
            ACT(rs[:, 0:n], ss[:, 0:n], AF.Sqrt, [ss.r], [rs.r], bias=epsc[:, 1:2])
            P.op("dve", lambda e, rs=rs, n=n: e.reciprocal(out=rs[:, 0:n], in_=rs[:, 0:n]), [rs.r], [rs.r])
            STT("dve", qn[:, 0:n], zq[:, 0:n], g8[:, gcol:gcol + 1], rs[:, 0:n], ALU.mult, ALU.mult,
                [zq.r, g8.r, rs.r], [qn.r])
            rot = banks.get()
            MM(rot[:, 0:n], rotM[:], qn[:, 0:n], True, True, [rotM.r, qn.r], [rot.r])
            TT("pool", t1[:, 0:n], qn[:, 0:n], cosT[:, t0:t0 + n], ALU.mult, [qn.r, cosT.r], [t1.r])
            TT("dve", t2[:, 0:n], rot[:, 0:n], sinT[:, t0:t0 + n], ALU.mult, [rot.r, sinT.r], [t2.r])
            TT("dve", stg[:, t0:t0 + n], t1[:, 0:n], t2[:, 0:n], ALU.add, [t1.r, t2.r], [stg.r])
        P.dma_copy("sp", dst.t[row0:row0 + 128, :], stg[:], reads=[stg.r], writes=[dst.r])

    def tm_group(wb, off, n, bo, dst, f32=False):
        for i in range(NT):
            ps = banks.get()
            rd = [wb.r, hT.rs[i]]
            for k in range(8):
                MM(ps[:, 0:n], hT[:, k, i * 128:(i + 1) * 128], wb[:, k, off:off + n], k == 0, k == 7, rd, [ps.r])
            stg = stm_f.get() if f32 else stm_bf.get()
            TT("dve", stg[:, 0:n], ps[:, 0:n], bias_bc[:, bo:bo + n], ALU.add, [ps.r, bias_bc.r], [stg.r])
            P.dma_copy("sp", dst.t[i * 128:(i + 1) * 128, :], stg[:, 0:n], reads=[stg.r], writes=[dst.r])

    wb = load_w(C_AQ, 512)
    for s in range(4):
        fm_qk(wb, s * 128, C_AQ + s * 128, 0, aqT, s * 128)
    wb = load_w(C_AK, 256)
    fm_qk(wb, 0, C_AK, 1, akT, 0)
    tm_group(wb, 128, 128, 0, av)
    wb = load_w(C_BQ, 512)
    for s in range(2):
        fm_simple(wb, s * 128, C_BQ + s * 128, AF.Identity, bqT, s * 128, f32=True)
    for s in range(2):
        fm_simple(wb, 256 + s * 128, C_BK + s * 128, AF.Identity, bkT, s * 128, f32=True)
    tm_group(wb, 256, 256, 128, bk, f32=True)
    wb = load_w(C_BV, 512)
    tm_group(wb, 0, 512, 384, bv)
    wb = load_w(C_BR, 512)
    for s in range(4):
        fm_simple(wb, s * 128, C_BR + s * 128, AF.Silu, rT, s * 128)
    wb = load_w(C_BA, 32)
    for d in range(2):
        for (t0, n) in BLKS:
            ps = fm_mm(wb, d * 16, 16, t0, n)
            ACT(aT[0:16, d, t0:t0 + n], ps[0:16, 0:n], AF.Identity, [ps.r, bacol.r], [aT.r], bias=bacol[:, d:d + 1])
    tg_r = Ring([P.sbuf(f"tg{i}", [128, 256], F32) for i in range(2)])
    te_r = Ring([P.sbuf(f"te{i}", [128, 256], F32) for i in range(2)])
    gst_r = Ring([P.sbuf(f"gst{i}", [128, 2, 256], F32) for i in range(2)])
    for i in range(NT):
        gst = gst_r.get()
        for d in range(2):
            ps = banks.get()
            MM(ps[:, 0:256], aT[0:16, d, i * 128:(i + 1) * 128], wgate[0:16, d, :], True, True,
               [aT.r, wgate.r], [ps.r])
            tg = tg_r.get()
            te = te_r.get()
            TT("dve", tg[:], ps[:, 0:256], bg_bc[:, d, :], ALU.add, [ps.r, bg_bc.r], [tg.r])
            ACT(te[:], tg[:], AF.Exp, [tg.r], [te.r], scale=-1.0)
            ACT(gst[:, d, :], te[:], AF.Ln, [te.r], [gst.r], bias=1.0)
        P.dma_copy("sp", Gd.t[i * 128:(i + 1) * 128, :, :], gst[:], reads=[gst.r], writes=[Gd.r])
    wb = load_w(C_CQ, 512)
    for s in range(4):
        fm_simple(wb, s * 128, C_CQ + s * 128, AF.Identity, cqT, s * 128)
    wb = load_w(C_CK, 512)
    for s in range(4):
        fm_simple(wb, s * 128, C_CK + s * 128, AF.Identity, ckT, s * 128)
    wb = load_w(C_CV, 512)
    tm_group(wb, 0, 512, 896, cv)
    for gsup in range(6):
        wb = load_w(C_G + gsup * 512, 512)
        for s in range(4):
            fm_simple(wb, s * 128, C_G + gsup * 512 + s * 128, AF.Sigmoid, gT, gsup * 512 + s * 128)

    P.release(mark1b)
    tri_d = {n: din(n, [128, 128]) for n in ("triInc", "triDec", "triSgt", "triSlt")}
    flags_d = din("flags", [64, 2])
    Og = dout("Og", [2, 512, NTOK], F32)
    qBT = dout("qBT", [2, 256, 2048], BF16)
    Sfin = dout("Sfin", [2, 64, 512], F32)
    tri = {}
    for n in tri_d:
        tri[n] = P.sbuf("c_" + n, [128, 128], F32)
        P.dma_copy("sp", tri[n][:], tri_d[n].t[:, :], writes=[tri[n].r])
    flags = P.sbuf("flags", [64, 2], F32)
    P.dma_copy("sp", flags[:], flags_d.t[:, :], writes=[flags.r])
    Sst = [P.sbuf(f"S{d}", [64, 4, 128], F32) for d in range(2)]
    Sbf = [P.sbuf(f"Sbf{d}", [64, 4, 128], BF16) for d in range(2)]
    Dcum = [P.sbuf(f"Dcum{d}", [64, 4], F32) for d in range(2)]
    rq = [Ring([P.sbuf(f"gq{d}{i}", [64, 4, 128], F32) for i in range(2)]) for d in range(2)]
    rk = [Ring([P.sbuf(f"gk{d}{i}", [64, 4, 128], F32) for i in range(2)]) for d in range(2)]
    rkt = [Ring([P.sbuf(f"gkt{d}{i}", [128, 256], F32) for i in range(2)]) for d in range(2)]
    rv = [Ring([P.sbuf(f"gv{d}{i}", [128, 512], BF16) for i in range(2)]) for d in range(2)]
    rG = [Ring([P.sbuf(f"gG{d}{i}", [128, 256], F32) for i in range(2)]) for d in range(2)]
    reb = [Ring([P.sbuf(f"geb{d}{i}", [64, 4, 128], F32) for i in range(2)]) for d in range(2)]
    rei = [Ring([P.sbuf(f"gei{d}{i}", [64, 4, 128], F32) for i in range(2)]) for d in range(2)]
    rqb = [Ring([P.sbuf(f"gqb{d}{i}", [64, 4, 128], BF16) for i in range(2)]) for d in range(2)]
    rkb = [Ring([P.sbuf(f"gkb{d}{i}", [64, 4, 128], BF16) for i in range(2)]) for d in range(2)]
    rkr = [Ring([P.sbuf(f"gkr{d}{i}", [128, 256], F32) for i in range(2)]) for d in range(2)]
    rke = [Ring([P.sbuf(f"gke{d}{i}", [128, 256], BF16) for i in range(2)]) for d in range(2)]
    rsc = [Ring([P.sbuf(f"gsc{d}{i}", [128, 4, 128], BF16) for i in range(2)]) for d in range(2)]
    rO = [Ring([P.sbuf(f"gO{d}{i}", [128, 4, 128], F32) for i in range(2)]) for d in range(2)]
    rqB = [Ring([P.sbuf(f"gqB{d}{i}", [64, 4, 128], BF16) for i in range(2)]) for d in range(2)]

    def gla_step(d, i, lat):
        cumM = tri["triInc"] if d == 0 else tri["triDec"]
        remM = tri["triSgt"] if d == 0 else tri["triSlt"]
        endc = 127 if d == 0 else 0
        t0 = i * 128
        S, Sb, Dc = Sst[d], Sbf[d], Dcum[d]
        q_t, k_t, kt_t, v_t, G_t = rq[d].get(), rk[d].get(), rkt[d].get(), rv[d].get(), rG[d].get()
        P.dma_copy("sp", q_t[:], bqT.t[:, t0:t0 + 128].rearrange("(h p) t -> p h t", p=64), reads=[bqT.r], writes=[q_t.r])
        P.dma_copy("sp", k_t[:], bkT.t[:, t0:t0 + 128].rearrange("(h p) t -> p h t", p=64), reads=[bkT.r], writes=[k_t.r])
        P.dma_copy("sp", kt_t[:], bk.t[t0:t0 + 128, :], reads=[bk.r], writes=[kt_t.r])
        P.dma_copy("sp", v_t[:], bv.t[t0:t0 + 128, :], reads=[bv.r], writes=[v_t.r])
        P.dma_copy("sp", G_t[:], Gd.t[t0:t0 + 128, d, :], reads=[Gd.r], writes=[G_t.r])
        cps = banks.get()
        for h in range(4):
            MM(cps[0:64, h * 128:(h + 1) * 128], G_t[:, h * 64:(h + 1) * 64], cumM[:], True, True, [G_t.r, cumM.r], [cps.r])
        eb, ei = reb[d].get(), rei[d].get()
        ACT(eb[:].rearrange("p a t -> p (a t)"), cps[0:64, :], AF.Exp, [cps.r], [eb.r], scale=-1.0 / 16)
        ACT(ei[:].rearrange("p a t -> p (a t)"), cps[0:64, :], AF.Exp, [cps.r], [ei.r], scale=1.0 / 16)
        qb, kb = rqb[d].get(), rkb[d].get()
        STT("dve", qb[:], q_t[:], 0.125, eb[:], ALU.mult, ALU.mult, [q_t.r, eb.r], [qb.r])
        TT(PENG, kb[:], k_t[:], ei[:], ALU.mult, [k_t.r, ei.r], [kb.r])
        rps = banks.get()
        MM(rps[:, 0:256], remM[:], G_t[:], True, True, [remM.r, G_t.r], [rps.r])
        kr = rkr[d].get()
        ACT(kr[:], rps[:, 0:256], AF.Exp, [rps.r], [kr.r], scale=-1.0 / 16)
        ke = rke[d].get()
        TT(PENG, ke[:], kt_t[:], kr[:], ALU.mult, [kt_t.r, kr.r], [ke.r])
        sps = banks.get()
        for h in range(4):
            MM(sps[:, h * 128:(h + 1) * 128], kb[:, h, :], qb[:, h, :], True, True, [kb.r, qb.r], [sps.r])
        sc = rsc[d].get()
        TT("dve", sc[:], sps[:].rearrange("p (h t) -> p h t", h=4),
           cumM[:].unsqueeze(1).to_broadcast([128, 4, 128]), ALU.mult, [sps.r, cumM.r], [sc.r])
        ops_ = banks.get()
        for h in range(4):
            MM(ops_[:, h * 128:(h + 1) * 128], Sb[:, h, :], qb[:, h, :], True, False, [Sb.r, qb.r], [ops_.r])
            MM(ops_[:, h * 128:(h + 1) * 128], v_t[:, h * 128:(h + 1) * 128], sc[:, h, :],
               False, True, [v_t.r, sc.r], [ops_.r])
        Ot = rO[d].get()
        ACT(Ot[:].rearrange("p h t -> p (h t)"), ops_[:, :], AF.Identity, [ops_.r], [Ot.r])
        P.dma_copy("sp", Og.t[d, :, t0:t0 + 128].rearrange("(h p) t -> p h t", p=128), Ot[:], reads=[Ot.r], writes=[Og.r])
        if lat:
            qB = rqB[d].get()
            TT(PENG, qB[:], qb[:], Dc[:].unsqueeze(2).to_broadcast([64, 4, 128]), ALU.mult, [qb.r, Dc.r], [qB.r])
            P.dma_copy("sp", qBT.t[d, :, t0:t0 + 128].rearrange("(h p) t -> p h t", p=64), qB[:], reads=[qB.r], writes=[qBT.r])
            TT("dve", Dc[:], Dc[:], eb[:, :, endc], ALU.mult, [Dc.r, eb.r], [Dc.r])
        ups = banks.get()
        for h in range(4):
            MM(ups[0:64, h * 128:(h + 1) * 128], ke[:, h * 64:(h + 1) * 64], v_t[:, h * 128:(h + 1) * 128], True, True,
               [ke.r, v_t.r], [ups.r])
        TT("dve", S[:], S[:], eb[:, :, endc:endc + 1].to_broadcast([64, 4, 128]), ALU.mult, [S.r, eb.r], [S.r])
        TT("dve", S[:], S[:], ups[0:64, :].rearrange("p (h e) -> p h e", h=4), ALU.add, [S.r, ups.r], [S.r])
        CP(PENG, Sb[:], S[:], [S.r], [Sb.r])

    for d in range(2):
        MS("dve", Sst[d][:], 0.0, [Sst[d].r])
        MS(PENG, Sbf[d][:], 0.0, [Sbf[d].r])
        MS("dve", Dcum[d][:], 1.0, [Dcum[d].r])
    for j in range(2 if GM > 0 else 0):
        gla_step(0, 16 + j, False)
        gla_step(1, 17 - j, False)
    for d in range(2):
        TS("dve", Sst[d][:], Sst[d][:], flags[:, d:d + 1], None, ALU.mult, None, [Sst[d].r, flags.r], [Sst[d].r])
        CP(PENG, Sbf[d][:], Sst[d][:], [Sst[d].r], [Sbf[d].r])
    for j in range(16 if GM > 0 else 0):
        gla_step(0, j, True)
        gla_step(1, 15 - j, True)
    for d in range(2):
        P.dma_copy("sp", Sfin.t[d, :, :], Sst[d][:].rearrange("p h e -> p (h e)"), reads=[Sst[d].r], writes=[Sfin.r])

    P.release(m_stage)


def emit_l2(P, E, L):
    nc = P.nc
    H = mk_helpers(P)
    MM, TR, ACT, TT, TS, STT, CP, MS = H.MM, H.TR, H.ACT, H.TT, H.TS, H.STT, H.CP, H.MS
    din = lambda name, shape, dt=F32: E.din(name, shape, dt, L)
    dout = lambda name, shape, dt: E.dout(name, shape, dt, L)
    m_stage = P.mark()

    x_in = din("x_in", [NTOK, 1024])
    modD = din("modD", [2, 6144])
    aqT = din("aqT", [512, NTOK], BF16)
    akT = din("akT", [128, NTOK], BF16)
    av = din("av", [NTOK, 128], BF16)
    g_ak = din("g_ak", [256, 2048], BF16)
    g_av = din("g_av", [4096, 128], BF16)
    g_ck = din("g_ck", [1024, 768], BF16)
    g_cv = din("g_cv", [1536, 512], BF16)
    g_S = din("g_S", [256, 512])
    cqT = din("cqT", [512, NTOK], BF16)
    ckT = din("ckT", [512, NTOK], BF16)
    cv = din("cv", [NTOK, 512], BF16)
    tabNA = din("tabNA", [5, 7, 2, 128, 512], BF16)
    Og = din("Og", [2, 512, NTOK])
    qBT = din("qBT", [2, 256, 2048], BF16)
    fl1m = din("fl1m", [64, 2])
    rT = din("rT", [512, NTOK], BF16)
    gT = din("gT", [3072, NTOK], BF16)
    gn_d = din("gla_norm", [128, 1])
    wba_d = din("w_br_attn", [512, 1024])
    wbg_d = din("w_br_gla", [512, 1024])
    wbn_d = din("w_br_na", [512, 1024])
    wout_d = din("w_out", [1024, 1024])
    ln1g_d = din("ln1_g", [1, 1024])
    ln1b_d = din("ln1_b", [1, 1024])
    ones_d = din("ones128", [128, 128])
    x1_o = dout("x1", [NTOK, 1024], F32)
    h2_o = dout("h2", [NTOK, 1024], F32)

    bankS = Banks(P, 0, E.fb[0:3])
    bankA = Ring(E.fb[3:5])
    bankM = Ring(E.fb[5:7])

    oaD = dout("oaD", [512, NTOK], BF16)
    ocD = dout("ocD", [512, NTOK], BF16)
    obD = dout("obD", [512, NTOK], BF16)
    stgr = Ring([P.sbuf(f"ostg{i}", [128, 512], BF16) for i in range(3)])
    epsc = P.sbuf("epsc", [128, 2], F32)
    MS("dve", epsc[:, 0:1], LN_EPS, [epsc.r])
    MS("dve", epsc[:, 1:2], RMS_EPS, [epsc.r])
    rdr = Ring([P.sbuf(f"rd{i}", [128, 512], F32) for i in range(2)])
    rd0r = Ring([P.sbuf(f"rd0{i}", [64, 512], F32) for i in range(2)])
    ptr = Ring([P.sbuf(f"pt{i}", [128, 512], BF16) for i in range(8)])

    def blk_of(t0):
        return min(t0 // 512, 4)

    def normalize(po, n, dest, dres, view=None):
        rd = rdr.get()
        P.op("dve", lambda e: e.reciprocal(out=rd[64:128, 0:n], in_=po[64:128, 0:n]), [po.r], [rd.r])
        rd0 = rd0r.get()
        P.dma_copy("sp", rd0[0:64, 0:n], rd[64:128, 0:n], reads=[rd.r], writes=[rd0.r])
        a, b_ = po[0:64, 0:n], rd0[0:64, 0:n]
        if view is not None:
            a, b_ = view(a), view(b_)
        TT("dve", dest, a, b_, ALU.mult, [po.r, rd0.r], [dres])

    markA = P.mark()
    bankSA = Banks(P, 0, list(bankS.b) + list(bankM.bufs))
    KT = [P.sbuf(f"KT{g}", [64, NKT * 128], BF16) for g in range(2)]
    V1 = [P.sbuf(f"V1{g}", [128, NKT, 128], BF16) for g in range(2)]
    for g in range(2):
        for r_ in range(2):
            P.dma_copy("sp", KT[g][:, r_ * 2048:(r_ + 1) * 2048], g_ak.t[r_ * 128 + g * 64:r_ * 128 + (g + 1) * 64, :],
                       reads=[g_ak.r], writes=[KT[g].r])
        P.dma_copy("sp", KT[g][:, 4096:4352], akT.t[g * 64:(g + 1) * 64, 2048:2304], reads=[akT.r], writes=[KT[g].r])
        MS("pool", V1[g][:, :, 64:128], 1.0, [V1[g].r])
        P.dma_copy("sp", V1[g][:, 0:32, 0:64], g_av.t[:, g * 64:(g + 1) * 64].rearrange("(kt p) d -> p kt d", p=128),
                   reads=[g_av.r], writes=[V1[g].r])
        P.dma_copy("sp", V1[g][:, 32:34, 0:64], av.t[2048:2304, g * 64:(g + 1) * 64].rearrange("(kt p) d -> p kt d", p=128),
                   reads=[av.r], writes=[V1[g].r])
    qbr = Ring([P.sbuf(f"qblk{i}", [64, 8, 512], BF16) for i in range(2)])
    for bi, (t0, n) in enumerate(BLKS if 'A' not in os.environ.get('L2SKIP', '') else []):
        qb = qbr.get()
        P.dma_copy("sp", qb[:, :, 0:n], aqT.t[:, t0:t0 + n].rearrange("(h p) t -> p h t", p=64), writes=[qb.r])
        kts = list(range(NKT)) if bi < 4 else [32, 33]
        for h in range(8):
            g = h // 4
            po = bankA.get()
            LOOK = 4
            pend_ = []

            def issue_qk(kt):
                ps = bankSA.get()
                MM(ps[:, 0:n], KT[g][:, kt * 128:(kt + 1) * 128], qb[:, h, 0:n], True, True, [KT[g].r, qb.r], [ps.r])
                pt = ptr.get()
                ACT(pt[:, 0:n], ps[:, 0:n], AF.Exp, [ps.r], [pt.r], scale=0.125)
                pend_.append(pt)
            for kt in kts[:LOOK]:
                issue_qk(kt)
            for idx, kt in enumerate(kts):
                if idx + LOOK < len(kts):
                    issue_qk(kts[idx + LOOK])
                pt = pend_.pop(0)
                MM(po[:, 0:n], V1[g][:, kt, :], pt[:, 0:n], idx == 0, idx == len(kts) - 1, [V1[g].r, pt.r], [po.r])
            stg = stgr.get()
            normalize(po, n, stg[0:64, 0:n], stg.r)
            P.dma_copy("sp", oaD.t[h * 64:(h + 1) * 64, t0:t0 + n], stg[0:64, 0:n], reads=[stg.r], writes=[oaD.r])
    P.release(markA)

    markN = P.mark()
    ones128 = P.sbuf("ones128", [128, 128], BF16)
    P.dma_copy("pool", ones128[:], ones_d.t[:, :], writes=[ones128.r])
    gn = P.sbuf("gn", [128, 1], F32)
    P.dma_copy("sp", gn[:], gn_d.t[:, :], writes=[gn.r])
    f1 = P.sbuf("f1", [64, 2], F32)
    P.dma_copy("sp", f1[:], fl1m.t[:, :], writes=[f1.r])
    Sin = []
    for d in range(2):
        sp_ = P.sbuf(f"Spart{d}", [64, 512], F32)
        P.dma_copy("sp", sp_[:], g_S.t[d * 128 + d * 64:d * 128 + (d + 1) * 64, :], reads=[g_S.r], writes=[sp_.r])
        sb = P.sbuf(f"Sin{d}", [64, 4, 128], BF16)
        TS("dve", sb[:].rearrange("p h e -> p (h e)"), sp_[:], f1[:, d:d + 1], None, ALU.mult, None, [sp_.r, f1.r], [sb.r])
        Sin.append(sb)
    qBr = [Ring([P.sbuf(f"qB{d}{i}", [64, 4, 512], BF16) for i in range(2)]) for d in range(2)]
    ogr = [Ring([P.sbuf(f"og{d}{i}", [128, 512], F32) for i in range(2)]) for d in range(2)]
    osr = Ring([P.sbuf(f"os{i}", [128, 512], F32) for i in range(2)])
    sqr = Ring([P.sbuf(f"sq{i}", [128, 512], BF16) for i in range(2)])
    rsr = Ring([P.sbuf(f"rs{i}", [128, 512], F32) for i in range(2)])
    rtr = Ring([P.sbuf(f"rt{i}", [128, 512], BF16) for i in range(2)])
    t3r = Ring([P.sbuf(f"t3{i}", [128, 512], F32) for i in range(2)])

    def gla_units():
        for bi, (t0, n) in enumerate(BLKS):
            lat = bi < 4
            if lat:
                qB = [qBr[d].get() for d in range(2)]
                for d in range(2):
                    P.dma_copy("sp", qB[d][:, :, 0:n], qBT.t[d, :, t0:t0 + n].rearrange("(h p) t -> p h t", p=64),
                               writes=[qB[d].r])
            for h in range(4):
                og = [ogr[d].get() for d in range(2)]
                for d in range(2):
                    P.dma_copy("sp", og[d][:, 0:n], Og.t[d, h * 128:(h + 1) * 128, t0:t0 + n], writes=[og[d].r])
                osum = osr.get()
                TT("pool", osum[:, 0:n], og[0][:, 0:n], og[1][:, 0:n], ALU.add, [og[0].r, og[1].r], [osum.r])
                if lat:
                    pc = bankM.get()
                    MM(pc[:, 0:n], Sin[0][:, h, :], qB[0][:, h, 0:n], True, False, [Sin[0].r, qB[0].r], [pc.r])
                    MM(pc[:, 0:n], Sin[1][:, h, :], qB[1][:, h, 0:n], False, True, [Sin[1].r, qB[1].r], [pc.r])
                    TT("dve", osum[:, 0:n], osum[:, 0:n], pc[:, 0:n], ALU.add, [osum.r, pc.r], [osum.r])
                sq = sqr.get()
                ACT(sq[:, 0:n], osum[:, 0:n], AF.Square, [osum.r], [sq.r])
                ss = bankM.get()
                MM(ss[:, 0:n], ones128[:], sq[:, 0:n], True, True, [ones128.r, sq.r], [ss.r])
                rs = rsr.get()
                ACT(rs[:, 0:n], ss[:, 0:n], AF.Sqrt, [ss.r, epsc.r], [rs.r], bias=epsc[:, 1:2], scale=1.0 / 128)
                P.op("dve", lambda e, rs=rs, n=n: e.reciprocal(out=rs[:, 0:n], in_=rs[:, 0:n]), [rs.r], [rs.r])
                rt = rtr.get()
                P.dma_copy("sp", rt[:, 0:n], rT.t[h * 128:(h + 1) * 128, t0:t0 + n], writes=[rt.r])
                t3 = t3r.get()
                STT("dve", t3[:, 0:n], osum[:, 0:n], gn[:, 0:1], rs[:, 0:n], ALU.mult, ALU.mult, [osum.r, gn.r, rs.r], [t3.r])
                stg = stgr.get()
                TT("pool", stg[:, 0:n], t3[:, 0:n], rt[:, 0:n], ALU.mult, [t3.r, rt.r], [stg.r])
                P.dma_copy("sp", obD.t[h * 128:(h + 1) * 128, t0:t0 + n], stg[:, 0:n], reads=[stg.r], writes=[obD.r])
                yield

    gu = gla_units()
    KTc = P.sbuf("KTc", [64, 8, 256], BF16)
    V1c = P.sbuf("V1c", [128, 2, 8, 128], BF16)
    P.dma_copy("sp", KTc[:], ckT.t[:, 2048:2304].rearrange("(h p) t -> p h t", p=64), writes=[KTc.r])
    MS("pool", V1c[:, :, :, 64:128], 1.0, [V1c.r])
    for kt in range(2):
        P.dma_copy("sp", V1c[:, kt, :, 0:64],
                   cv.t[2048 + kt * 128:2048 + (kt + 1) * 128, :].rearrange("p (h d) -> p h d", d=64), writes=[V1c.r])
    qtr = Ring([P.sbuf(f"nq{i}", [64, 8, 128], BF16) for i in range(2)])
    kwin = P.sbuf("kwin", [64, 8, 8 * 128], BF16, nres=8)
    vwin = P.sbuf("vwin", [128, 8, 8, 128], BF16, nres=8)
    MS("pool", vwin[:, :, :, 64:128], 1.0, vwin.rs)
    tabI = P.sbuf("tabI", [128, 7, 2, 512], BF16)
    for kt in range(7):
        for grp in range(2):
            P.dma_copy("sp", tabI[:, kt, grp, :], tabNA.t[4, kt, grp], writes=[tabI.r])
    nloaded = [-1]

    def load_win_tile(t_):
        sl = t_ % 8
        if t_ < 3:
            ksrc, kres = g_ck.t[0:512, 384 + t_ * 128:384 + (t_ + 1) * 128], g_ck.r
            vsrc, vres = g_cv.t[384 + t_ * 128:384 + (t_ + 1) * 128, :], g_cv.r
        elif t_ < 19:
            ksrc, kres = ckT.t[:, (t_ - 3) * 128:(t_ - 2) * 128], ckT.r
            vsrc, vres = cv.t[(t_ - 3) * 128:(t_ - 2) * 128, :], cv.r
        else:
            ksrc, kres = g_ck.t[512:1024, (t_ - 19) * 128:(t_ - 18) * 128], g_ck.r
            vsrc, vres = g_cv.t[768 + (t_ - 19) * 128:768 + (t_ - 18) * 128, :], g_cv.r
        P.dma_copy("sp", kwin[:, :, sl * 128:(sl + 1) * 128], ksrc.rearrange("(h p) t -> p h t", p=64),
                   reads=[kres], writes=[kwin.rs[sl]])
        P.dma_copy("sp", vwin[:, sl, :, 0:64], vsrc.rearrange("p (h d) -> p h d", d=64), reads=[vres], writes=[vwin.rs[sl]])
    tbr = Ring([P.sbuf(f"ntb{i}", [128, 512], BF16) for i in range(3)])
    tmr = Ring([P.sbuf(f"ntm{i}", [128, 512], F32) for i in range(2)])
    nptr = Ring([P.sbuf(f"npt{i}", [128, 512], BF16) for i in range(18)])
    for j in range(18 if 'N' not in os.environ.get('L2SKIP', '') else 0):
        qt = qtr.get()
        P.dma_copy("sp", qt[:], cqT.t[:, j * 128:(j + 1) * 128].rearrange("(h p) t -> p h t", p=64), writes=[qt.r])
        if j < 16:
            while nloaded[0] < j + 6:
                nloaded[0] += 1
                load_win_tile(nloaded[0])
            kts = list(range(9))
            slot = j if j < 2 else (j - 12 if j >= 14 else 4)
        else:
            kts = [7, 8]
        for grp in range(2):
            pts = {}
            for idx, kt in enumerate(kts):
                sps = bankS.get()
                for hh in range(4):
                    h = grp * 4 + hh
                    if kt < 7:
                        lhs, rd_ = kwin[:, h, ((j + kt) % 8) * 128:((j + kt) % 8 + 1) * 128], kwin.rs[(j + kt) % 8]
                    else:
                        lhs, rd_ = KTc[:, h, (kt - 7) * 128:(kt - 6) * 128], KTc.r
                    MM(sps[:, hh * 128:(hh + 1) * 128], lhs, qt[:, h, :], True, True, [rd_, qt.r], [sps.r])
                pt = nptr.get()
                pts[kt] = pt
                if kt < 7:
                    tm = tmr.get()
                    if slot == 4:
                        STT("dve", tm[:], sps[:], 0.125, tabI[:, kt, grp, :], ALU.mult, ALU.add, [sps.r, tabI.r], [tm.r])
                    else:
                        tb = tbr.get()
                        P.dma_copy("sp", tb[:], tabNA.t[slot, kt, grp], writes=[tb.r])
                        STT("dve", tm[:], sps[:], 0.125, tb[:], ALU.mult, ALU.add, [sps.r, tb.r], [tm.r])
                    ACT(pt[:], tm[:], AF.Exp, [tm.r], [pt.r])
                else:
                    ACT(pt[:], sps[:], AF.Exp, [sps.r], [pt.r], scale=0.125)
            po = bankA.get()
            for hh in range(4):
                h = grp * 4 + hh
                for idx, kt in enumerate(kts):
                    pt = pts[kt]
                    if kt < 7:
                        lhs, rd_ = vwin[:, (j + kt) % 8, h, :], vwin.rs[(j + kt) % 8]
                    else:
                        lhs, rd_ = V1c[:, kt - 7, h, :], V1c.r
                    MM(po[:, hh * 128:(hh + 1) * 128], lhs, pt[:, hh * 128:(hh + 1) * 128], idx == 0, idx == len(kts) - 1,
                       [rd_, pt.r], [po.r])
            stg = stgr.get()
            normalize(po, 512, stg[0:64, :], stg.r)
            P.dma_copy("sp", ocD.t[grp * 256:(grp + 1) * 256, j * 128:(j + 1) * 128].rearrange("(h p) t -> p h t", p=64),
                       stg[0:64, :].rearrange("p (h t) -> p h t", h=4), reads=[stg.r], writes=[ocD.r])
        next(gu, None)
    for _ in gu:
        pass
    P.release(markN)


    wba = P.sbuf("wba", [64, 8, 1024], BF16)
    wbn = P.sbuf("wbn", [64, 8, 1024], BF16)
    wbg = P.sbuf("wbg", [128, 4, 1024], BF16)
    wo = P.sbuf("wo", [128, 8, 1024], BF16)
    P.dma_copy("pool", wba[:], wba_d.t.rearrange("(h p) f -> p h f", p=64), writes=[wba.r])
    P.dma_copy("pool", wbn[:], wbn_d.t.rearrange("(h p) f -> p h f", p=64), writes=[wbn.r])
    P.dma_copy("pool", wbg[:], wbg_d.t.rearrange("(h p) f -> p h f", p=128), writes=[wbg.r])
    P.dma_copy("pool", wo[:], wout_d.t.rearrange("(k p) f -> p k f", p=128), writes=[wo.r])
    bc = {}
    for nm, m in (("g1", 2), ("sh2", 3), ("sc2", 4)):
        for j in range(2):
            t = P.sbuf(f"bc_{nm}{j}", [128, 1024], F32)
            P.dma_copy("sp", t[:], modD.t[j:j + 1, m * 1024:(m + 1) * 1024].partition_broadcast(128), writes=[t.r])
            bc[(nm, j)] = t
    for j in range(2):
        TS("pool", bc[("sc2", j)][:], bc[("sc2", j)][:], 1.0, None, ALU.add, None, [bc[("sc2", j)].r], [bc[("sc2", j)].r])
    lg = P.sbuf("bc_ln1g", [128, 1024], F32)
    lb = P.sbuf("bc_ln1b", [128, 1024], F32)
    P.dma_copy("sp", lg[:], ln1g_d.t[0:1, :].partition_broadcast(128), writes=[lg.r])
    P.dma_copy("sp", lb[:], ln1b_d.t[0:1, :].partition_broadcast(128), writes=[lb.r])
    ymT = Ring([P.sbuf(f"ymT{i}", [128, 8, 512], BF16) for i in range(1)])
    ggr = Ring([P.sbuf(f"gg{i}", [128, 3, 512], BF16) for i in range(3)])
    tar = Ring([P.sbuf(f"ta{i}", [128, 512], F32) for i in range(2)])
    tbr2 = Ring([P.sbuf(f"tb2{i}", [128, 512], F32) for i in range(2)])
    xr = Ring([P.sbuf(f"xr{i}", [128, 1024], F32) for i in range(2)])
    ur = Ring([P.sbuf(f"ur{i}", [128, 1024], F32) for i in range(2)])
    x1r = Ring([P.sbuf(f"x1r{i}", [128, 1024], F32) for i in range(2)])
    h2r = Ring([P.sbuf(f"h2r{i}", [128, 1024], F32) for i in range(2)])
    str_ = Ring([P.sbuf(f"st{i}", [128, 2, 6], F32) for i in range(2)])
    mvr = Ring([P.sbuf(f"mv{i}", [128, 2], F32) for i in range(2)])

    def ln_stats(src):
        st = str_.get()
        for c in range(2):
            P.op("dve", lambda e, st=st, c=c: e.bn_stats(out=st[:, c, :], in_=src[:, c * 512:(c + 1) * 512]),
                 [src.r], [st.r])
        mv = mvr.get()
        P.op("dve", lambda e, st=st, mv=mv: e.bn_aggr(out=mv[:], in_=st[:].rearrange("p a b -> p (a b)")),
             [st.r], [mv.r])
        ACT(mv[:, 1:2], mv[:, 1:2], AF.Sqrt, [mv.r, epsc.r], [mv.r], bias=epsc[:, 0:1])
        P.op("dve", lambda e, mv=mv: e.reciprocal(out=mv[:, 1:2], in_=mv[:, 1:2]), [mv.r], [mv.r])
        return mv

    oabr = Ring([P.sbuf(f"oab{i}", [64, 8, 512], BF16) for i in range(2)])
    ocbr = Ring([P.sbuf(f"ocb{i}", [64, 8, 512], BF16) for i in range(2)])
    obbr = Ring([P.sbuf(f"obb{i}", [128, 4, 512], BF16) for i in range(2)])
    for bi, (t0, n) in enumerate(BLKS):
        ym = ymT.get()
        oab, ocb, obb = oabr.get(), ocbr.get(), obbr.get()
        P.dma_copy("sp", oab[:, :, 0:n], oaD.t[:, t0:t0 + n].rearrange("(h p) t -> p h t", p=64), reads=[oaD.r], writes=[oab.r])
        P.dma_copy("sp", ocb[:, :, 0:n], ocD.t[:, t0:t0 + n].rearrange("(h p) t -> p h t", p=64), reads=[ocD.r], writes=[ocb.r])
        P.dma_copy("sp", obb[:, :, 0:n], obD.t[:, t0:t0 + n].rearrange("(h p) t -> p h t", p=128), reads=[obD.r], writes=[obb.r])
        for fc in range(8):
            gg = ggr.get()
            P.dma_copy("sp", gg[:, :, 0:n],
                       gT.t[:, t0:t0 + n].rearrange("(b r) t -> r b t", b=3)[fc * 128:(fc + 1) * 128],
                       writes=[gg.r])
            fs = slice(fc * 128, (fc + 1) * 128)
            pa = bankS.get()
            for h in range(8):
                MM(pa[:, 0:n], wba[:, h, fs], oab[:, h, 0:n], h == 0, h == 7, [wba.r, oab.r], [pa.r])
            pb = bankS.get()
            for h in range(4):
                MM(pb[:, 0:n], wbg[:, h, fs], obb[:, h, 0:n], h == 0, h == 3, [wbg.r, obb.r], [pb.r])
            pcn = bankS.get()
            for h in range(8):
                MM(pcn[:, 0:n], wbn[:, h, fs], ocb[:, h, 0:n], h == 0, h == 7, [wbn.r, ocb.r], [pcn.r])
            ta, tb = tar.get(), tbr2.get()
            TT("dve", ta[:, 0:n], pa[:, 0:n], gg[:, 0, 0:n], ALU.mult, [pa.r, gg.r], [ta.r])
            TT("dve", tb[:, 0:n], pb[:, 0:n], gg[:, 1, 0:n], ALU.mult, [pb.r, gg.r], [tb.r])
            TT("pool", ta[:, 0:n], ta[:, 0:n], tb[:, 0:n], ALU.add, [ta.r, tb.r], [ta.r])
            TT("dve", tb[:, 0:n], pcn[:, 0:n], gg[:, 2, 0:n], ALU.mult, [pcn.r, gg.r], [tb.r])
            TT("pool", ym[:, fc, 0:n], ta[:, 0:n], tb[:, 0:n], ALU.add, [ta.r, tb.r], [ym.r])
        for ti in range(n // 128):
            i = t0 // 128 + ti
            j = 0 if i < 16 else 1
            xt = xr.get()
            P.dma_copy("sp", xt[:], x_in.t[i * 128:(i + 1) * 128, :], writes=[xt.r])
            u = ur.get()
            for hf in range(2):
                py = bankM.get()
                for fc in range(8):
                    MM(py[:, :], ym[:, fc, ti * 128:(ti + 1) * 128], wo[:, fc, hf * 512:(hf + 1) * 512], fc == 0, fc == 7,
                       [ym.r, wo.r], [py.r])
                TT("dve", u[:, hf * 512:(hf + 1) * 512], py[:, :], bc[("g1", j)][:, hf * 512:(hf + 1) * 512], ALU.mult,
                   [py.r, bc[("g1", j)].r], [u.r])
            STT("dve", u[:], xt[:], ALPHA, u[:], ALU.mult, ALU.add, [xt.r, u.r], [u.r])
            mv = ln_stats(u)
            x1 = x1r.get()
            TS("dve", x1[:], u[:], mv[:, 0:1], mv[:, 1:2], ALU.subtract, ALU.mult, [u.r, mv.r], [x1.r])
            TT("pool", x1[:], x1[:], lg[:], ALU.mult, [x1.r, lg.r], [x1.r])
            TT("pool", x1[:], x1[:], lb[:], ALU.add, [x1.r, lb.r], [x1.r])
            P.dma_copy("sp", x1_o.t[i * 128:(i + 1) * 128, :], x1[:], reads=[x1.r], writes=[x1_o.r])
            mv2 = ln_stats(x1)
            h2 = h2r.get()
            TS("dve", h2[:], x1[:], mv2[:, 0:1], mv2[:, 1:2], ALU.subtract, ALU.mult, [x1.r, mv2.r], [h2.r])
            TT("pool", h2[:], h2[:], bc[("sc2", j)][:], ALU.mult, [h2.r, bc[("sc2", j)].r], [h2.r])
            TT("pool", h2[:], h2[:], bc[("sh2", j)][:], ALU.add, [h2.r, bc[("sh2", j)].r], [h2.r])
            P.dma_copy("sp", h2_o.t[i * 128:(i + 1) * 128, :], h2[:], reads=[h2.r], writes=[h2_o.r])

    P.release(m_stage)


def emit_l3(P, E, L):
    nc = P.nc
    H = mk_helpers(P)
    MM, TR, ACT, TT, TS, STT, CP, MS = H.MM, H.TR, H.ACT, H.TT, H.TS, H.STT, H.CP, H.MS
    din = lambda name, shape, dt=F32: E.din(name, shape, dt, L)
    dout = lambda name, shape, dt: E.dout(name, shape, dt, L)
    m_stage = P.mark()

    x1_d = din("x1", [NTOK, 1024])
    h2_d = din("h2", [NTOK, 1024])
    modD = din("modD", [2, 6144])
    wr_d = din("w_r", [1024, 36])
    br_d = din("b_r", [1, 36])
    w1_d = din("moe_w1", [32 * 128, 4096])
    w3_d = din("moe_w3", [32 * 128, 4096])
    w2_d = din("moe_w2", [32 * 128, 4096])
    ln2g_d = din("ln2_g", [1, 1024])
    ln2b_d = din("ln2_b", [1, 1024])
    id32_d = din("ident", [128, 128])
    tris_d = din("triSlt", [128, 128])
    ones_d = din("ones128", [128, 128])
    thr_d = din("thr18", [128, 32, 9])
    thrj_d = din("thrj", [128, NBLK, 32])
    tokidx_d = din("tokidx", [128, NT], I32)
    wbase_d = din("wbase", [128, 8])
    x2_o = dout("x2", [NTOK, 1024], F32)
    h2b = dout("h2b", [NTOK + 128, 1024], BF16)
    tokslot = dout("tokslot", [NSLOT, 1], I32)
    yslot = dout("yslot", [NSLOT, 1024], F32)
    be_o = dout("be_o", [1, NBLK], I32)
    pos_o = dout("pos_o", [128, NT, 2], I32)
    gate_o = dout("gate_o", [128, NT, 2], F32)

    banks = Banks(P, 0, E.fb)
    pTb = E.bb

    id32 = P.sbuf("id32", [128, 128], F32)
    P.dma_copy("sp", id32[:], id32_d.t[:, :], writes=[id32.r])
    idb = P.sbuf("idb", [128, 128], BF16)
    P.dma_copy("pool", idb[:], id32_d.t[:, :], writes=[idb.r])
    tris = P.sbuf("tris", [128, 128], F32)
    P.dma_copy("sp", tris[:], tris_d.t[:, :], writes=[tris.r])
    ones = P.sbuf("ones", [128, 128], F32)
    P.dma_copy("sp", ones[:], ones_d.t[:, :], writes=[ones.r])
    thr = P.sbuf("thr", [128, 32, 9], F32)
    P.dma_copy("sp", thr[:], thr_d.t[:, :, :], writes=[thr.r])
    thrj = P.sbuf("thrj", [128, NBLK, 32], F32)
    P.dma_copy("sp", thrj[:], thrj_d.t[:, :, :], writes=[thrj.r])
    tokidx = P.sbuf("tokidx", [128, NT], I32)
    P.dma_copy("sp", tokidx[:], tokidx_d.t[:, :], writes=[tokidx.r])
    wr = P.sbuf("wr", [128, 8, 36], F32)
    P.dma_copy("sp", wr[:], wr_d.t.rearrange("(k p) c -> p k c", p=128), writes=[wr.r])
    br_bc = P.sbuf("br_bc", [128, 36], F32)
    P.dma_copy("sp", br_bc[:], br_d.t[0:1, :].partition_broadcast(128), writes=[br_bc.r])
    epsc = P.sbuf("epsc", [128, 1], F32)
    MS("dve", epsc[:], LN_EPS, [epsc.r])
    dum = P.sbuf("dum", [128, NSLOT // 128], I32)
    MS("pool", dum[:], NTOK, [dum.r])
    P.dma_copy("sp", tokslot.t.rearrange("(p j) o -> p (j o)", p=128), dum[:], reads=[dum.r], writes=[tokslot.r])
    zt = P.sbuf("zt", [128, 1024], BF16)
    MS("pool", zt[:], 0.0, [zt.r])
    P.dma_copy("sp", h2b.t[NTOK:NTOK + 128, :], zt[:], reads=[zt.r], writes=[h2b.r])

    oh1a = P.sbuf("oh1a", [128, NT, 32], F32)
    oh2a = P.sbuf("oh2a", [128, NT, 32], F32)
    Aall = P.sbuf("Aall", [128, NT, 32], F32)
    gates = P.sbuf("gates", [128, NT, 2], F32)
    posI = P.sbuf("posI", [128, NT, 2], I32)
    beI = P.sbuf("beI", [1, NBLK], I32)
    widx = P.sbuf("widx", [128, NBLK], I32)
    wbase = P.sbuf("wbase", [128, 8], F32)
    P.dma_copy("sp", wbase[:], wbase_d.t[:, :], writes=[wbase.r])
    carry = P.sbuf("carry", [128, 32], F32)
    MS("dve", carry[:], 0.0, [carry.r])
    excl = P.sbuf("excl", [128, NT, 32], F32)

    markR = P.mark()
    h2r = Ring([P.sbuf(f"h2t{i}", [128, 1024], F32) for i in range(2)])
    hTr = Ring([P.sbuf(f"h2T{i}", [128, 8, 128], F32) for i in range(2)])
    lgr = Ring([P.sbuf(f"lg{i}", [128, 36], F32) for i in range(2)])
    sm = Ring([P.sbuf(f"sm{i}", [128, 16], F32) for i in range(2)])
    elr = Ring([P.sbuf(f"elm{i}", [128, 32], F32) for i in range(2)])
    el2r = Ring([P.sbuf(f"elm2{i}", [128, 32], F32) for i in range(2)])
    for i in range(NT):
        ht = h2r.get()
        P.dma_copy("sp", ht[:], h2_d.t[i * 128:(i + 1) * 128, :], writes=[ht.r])
        P.dma_copy("pool", h2b.t[i * 128:(i + 1) * 128, :], ht[:], reads=[ht.r], writes=[h2b.r])
        hT = hTr.get()
        for half in range(2):
            pt = banks.get()
            for kk in range(4):
                k = half * 4 + kk
                TR(pt[:, kk * 128:(kk + 1) * 128], ht[:, k * 128:(k + 1) * 128], id32[:], [ht.r, id32.r], [pt.r])
            if half == 0:
                ACT(hT[:, 0:4, :].rearrange("p k t -> p (k t)"), pt[:, :], AF.Identity, [pt.r], [hT.r])
            else:
                CP("dve", hT[:, 4:8, :].rearrange("p k t -> p (k t)"), pt[:, :], [pt.r], [hT.r])
        pl = banks.get()
        for k in range(8):
            MM(pl[:, 0:36], hT[:, k, :], wr[:, k, :], k == 0, k == 7, [hT.r, wr.r], [pl.r])
        lg = lgr.get()
        TT("dve", lg[:], pl[:, 0:36], br_bc[:], ALU.add, [pl.r, br_bc.r], [lg.r])
        s = sm.get()
        P.op("dve", lambda e, s=s, lg=lg: e.reduce_max(out=s[:, 0:1], in_=lg[:, 0:4], axis=AX.X), [lg.r], [s.r])
        TS("dve", s[:, 1:2], s[:, 0:1], -1.0, None, ALU.mult, None, [s.r], [s.r])
        TS("dve", s[:, 8:12], lg[:, 0:4], s[:, 0:1], None, ALU.is_equal, None, [lg.r, s.r], [s.r])
        ACT(s[:, 12:16], lg[:, 0:4], AF.Exp, [lg.r, s.r], [s.r], bias=s[:, 1:2])
        P.op("dve", lambda e, s=s: e.reduce_sum(out=s[:, 2:3], in_=s[:, 12:16], axis=AX.X), [s.r], [s.r])
        P.op("dve", lambda e, s=s: e.reciprocal(out=s[:, 3:4], in_=s[:, 2:3]), [s.r], [s.r])
        TS("dve", s[:, 12:16], s[:, 8:12], 1.0, BIG, ALU.subtract, ALU.mult, [s.r], [s.r])
        elm = elr.get()
        TT("dve", elm[:].rearrange("p (g e) -> p g e", g=4), lg[:, 4:36].rearrange("p (g e) -> p g e", g=4),
           s[:, 12:16].unsqueeze(2).to_broadcast([128, 4, 8]), ALU.add, [lg.r, s.r], [elm.r])
        P.op("dve", lambda e, s=s, elm=elm: e.reduce_max(out=s[:, 4:5], in_=elm[:], axis=AX.X), [elm.r], [s.r])
        TS("dve", oh1a[:, i, :], elm[:], s[:, 4:5], None, ALU.is_equal, None, [elm.r, s.r], [oh1a.r])
        elm2 = el2r.get()
        STT("dve", elm2[:], oh1a[:, i, :], -BIG, elm[:], ALU.mult, ALU.add, [oh1a.r, elm.r], [elm2.r])
        P.op("dve", lambda e, s=s, elm2=elm2: e.reduce_max(out=s[:, 5:6], in_=elm2[:], axis=AX.X), [elm2.r], [s.r])
        TS("dve", oh2a[:, i, :], elm2[:], s[:, 5:6], None, ALU.is_equal, None, [elm2.r, s.r], [oh2a.r])
        TT("dve", Aall[:, i, :], oh1a[:, i, :], oh2a[:, i, :], ALU.add, [oh1a.r, oh2a.r], [Aall.r])
        TT("dve", s[:, 6:7], s[:, 5:6], s[:, 4:5], ALU.subtract, [s.r], [s.r])
        ACT(s[:, 6:7], s[:, 6:7], AF.Exp, [s.r], [s.r])
        TS("dve", s[:, 6:7], s[:, 6:7], 1.0, None, ALU.add, None, [s.r], [s.r])
        P.op("dve", lambda e, s=s: e.reciprocal(out=s[:, 7:8], in_=s[:, 6:7]), [s.r], [s.r])
        TT("dve", gates[:, i, 0:1], s[:, 3:4], s[:, 7:8], ALU.mult, [s.r], [gates.r])
        TT("dve", gates[:, i, 1:2], s[:, 3:4], gates[:, i, 0:1], ALU.subtract, [s.r, gates.r], [gates.r])
        pex = banks.get()
        MM(pex[:, 0:32], tris[:], Aall[:, i, :], True, True, [tris.r, Aall.r], [pex.r])
        TT("dve", excl[:, i, :], pex[:, 0:32], carry[:], ALU.add, [pex.r, carry.r], [excl.r])
        pcs = banks.get()
        MM(pcs[:, 0:32], ones[:], Aall[:, i, :], True, True, [ones.r, Aall.r], [pcs.r])
        TT("dve", carry[:], carry[:], pcs[:, 0:32], ALU.add, [carry.r, pcs.r], [carry.r])
    cmp18 = P.sbuf("cmp18", [128, 32, 9], F32)
    TT("dve", cmp18[:], carry[:].unsqueeze(2).to_broadcast([128, 32, 9]), thr[:], ALU.is_gt, [carry.r, thr.r], [cmp18.r])
    padded = P.sbuf("padded", [128, 32], F32)
    P.op("dve", lambda e: e.reduce_sum(out=padded[:], in_=cmp18[:], axis=AX.X), [cmp18.r], [padded.r])
    TS("dve", padded[:], padded[:], float(SB), None, ALU.mult, None, [padded.r], [padded.r])
    cs = [P.sbuf(f"cs{i}", [128, 32], F32) for i in range(2)]
    CP("dve", cs[0][:], padded[:], [padded.r], [cs[0].r])
    cur = 0
    for sh in (1, 2, 4, 8, 16):
        a, b_ = cs[cur], cs[1 - cur]
        CP("dve", b_[:, 0:sh], a[:, 0:sh], [a.r], [b_.r])
        TT("dve", b_[:, sh:32], a[:, sh:32], a[:, 0:32 - sh], ALU.add, [a.r], [b_.r])
        cur = 1 - cur
    pend = cs[cur]
    pstart = P.sbuf("pstart", [128, 32], F32)
    TT("dve", pstart[:], pend[:], padded[:], ALU.subtract, [pend.r, padded.r], [pstart.r])
    posf = P.sbuf("posf", [128, NT, 2], F32)
    tmp32 = Ring([P.sbuf(f"tmp32{i}", [128, 32], F32) for i in range(2)])
    for i in range(NT):
        sb_ = tmp32.get()
        TT("dve", sb_[:], excl[:, i, :], pstart[:], ALU.add, [excl.r, pstart.r], [sb_.r])
        for k, oh in enumerate((oh1a, oh2a)):
            t2 = tmp32.get()
            TT("dve", t2[:], sb_[:], oh[:, i, :], ALU.mult, [sb_.r, oh.r], [t2.r])
            P.op("dve", lambda e, t2=t2, i=i, k=k: e.reduce_sum(out=posf[:, i, k:k + 1], in_=t2[:], axis=AX.X),
                 [t2.r], [posf.r])
    CP("dve", posI[:], posf[:], [posf.r], [posI.r])
    cmpj = P.sbuf("cmpj", [128, NBLK, 32], F32)
    TT("dve", cmpj[:], pend[:].unsqueeze(1).to_broadcast([128, NBLK, 32]), thrj[:], ALU.is_le, [pend.r, thrj.r], [cmpj.r])
    bef = P.sbuf("bef", [128, NBLK], F32)
    P.op("dve", lambda e: e.reduce_sum(out=bef[:], in_=cmpj[:], axis=AX.X), [cmpj.r], [bef.r])
    TS("dve", bef[:], bef[:], 31.0, None, ALU.min, None, [bef.r], [bef.r])
    CP("dve", beI[:], bef[0:1, :], [bef.r], [beI.r])
    used = P.sbuf("used", [128, NBLK], F32)
    TS("dve", used[:], thrj[:, :, 0], pend[:, 31:32], None, ALU.is_lt, None, [thrj.r, pend.r], [used.r])
    TS("dve", used[:], used[:], -8192.0, 8192.0, ALU.mult, ALU.add, [used.r], [used.r])
    wif = P.sbuf("wif", [128, NBLK], F32)
    TS("dve", wif[:], bef[:], 128.0, wbase[:, 0:1], ALU.mult, ALU.add, [bef.r, wbase.r], [wif.r])
    TT("dve", wif[:], wif[:], used[:], ALU.add, [wif.r, used.r], [wif.r])
    CP("dve", widx[:], wif[:], [wif.r], [widx.r])
    P.dma_copy("sp", be_o.t[:, :], beI[:], reads=[beI.r], writes=[be_o.r])
    P.dma_copy("sp", pos_o.t[:, :, :], posI[:], reads=[posI.r], writes=[pos_o.r])
    P.dma_copy("sp", gate_o.t[:, :, :], gates[:], reads=[gates.r], writes=[gate_o.r])
    for i in range(NT):
        for k in range(2):
            P.dma("pool", lambda e, i=i, k=k: e.indirect_dma_start(
                out=tokslot.t[:, :], out_offset=bass.IndirectOffsetOnAxis(ap=posI[:, i, k:k + 1], axis=0),
                in_=tokidx[:, i:i + 1], in_offset=None), reads=[posI.r, tokidx.r], writes=[tokslot.r])
    P.release(markR)

    markE = P.mark()
    w1r = Ring([P.sbuf(f"w1b{i}", [128, 8, 512], BF16) for i in range(2)])
    w3r = Ring([P.sbuf(f"w3b{i}", [128, 8, 512], BF16) for i in range(2)])
    w2r = Ring([P.sbuf(f"w2b{i}", [128, 4, 1024], BF16) for i in range(2)])
    idxr = Ring([P.sbuf(f"idx{i}", [128, 1], I32) for i in range(4)])
    xgr = Ring([P.sbuf(f"xg{i}", [128, 1024], BF16) for i in range(4)])
    xTr = Ring([P.sbuf(f"xT{i}", [128, 8, SB], BF16) for i in range(2)])
    s1r = Ring([P.sbuf(f"s1{i}", [128, 4, SB], F32) for i in range(2)])
    aTr = Ring([P.sbuf(f"aT{i}", [128, 4, SB], BF16) for i in range(2)])
    ysr = Ring([P.sbuf(f"ys{i}", [128, 1024], F32) for i in range(3)])
    bcreg = {}
    for j in range(NBLK):
        w1b, w3b, w2b = w1r.get(), w3r.get(), w2r.get()
        for (wb_, wd_) in ((w1b, w1_d), (w3b, w3_d), (w2b, w2_d)):
            def wgather(e, wb_=wb_, wd_=wd_, j=j):
                if not hasattr(P, "bnd_val"):
                    r_ = e.alloc_register("bnd")
                    e.reg_mov(r_, 32 * 128 - 1)
                    P.bnd_val = e.snap(r_)
                return e.indirect_dma_start(
                    out=wb_[:].rearrange("p a f -> p (a f)"), out_offset=None, in_=wd_.t[:, :],
                    in_offset=bass.IndirectOffsetOnAxis(ap=widx[:, j:j + 1], axis=0),
                    bounds_check=P.bnd_val, oob_is_err=False)
            P.dma("pool", wgather, reads=[widx.r], writes=[wb_.r])
        xT = xTr.get()
        for hb in range(2):
            idx = idxr.get()
            r0 = j * SB + hb * 128
            P.dma_copy("sp", idx[:], tokslot.t[r0:r0 + 128, :], reads=[tokslot.r], writes=[idx.r])
            xg = xgr.get()
            P.dma("pool", lambda e, xg=xg, idx=idx: e.indirect_dma_start(
                out=xg[:, :], out_offset=None, in_=h2b.t[:, :],
                in_offset=bass.IndirectOffsetOnAxis(ap=idx[:, 0:1], axis=0)), reads=[idx.r, h2b.r], writes=[xg.r])
            for k in range(8):
                TR(pTb[:, k, :], xg[:].rearrange("p (a i) -> p a i", i=8)[:, :, k], idb[:], [xg.r, idb.r], [pTb.r])
            ACT(xT[:, 0:4, hb * 128:(hb + 1) * 128], pTb[:, 0:4, :], AF.Identity, [pTb.r], [xT.r])
            CP("dve", xT[:, 4:8, hb * 128:(hb + 1) * 128], pTb[:, 4:8, :], [pTb.r], [xT.r])
        s1 = s1r.get()
        aT = aTr.get()
        pb3 = []
        for (wb, which) in ((w1b, 1), (w3b, 3)):
            for fp in range(2):
                pp = banks.get()
                for f2 in range(2):
                    fc = fp * 2 + f2
                    for k in range(8):
                        MM(pp[:, f2 * SB:(f2 + 1) * SB], wb[:, k, :].rearrange("p (a c) -> p a c", c=4)[:, :, fc], xT[:, k, :],
                           k == 0, k == 7, [wb.r, xT.r], [pp.r])
                if which == 1:
                    ACT(s1[:, fp * 2:fp * 2 + 2, :].rearrange("p f t -> p (f t)"), pp[:, :], AF.Silu, [pp.r], [s1.r])
                else:
                    TT("dve", aT[:, fp * 2:fp * 2 + 2, :].rearrange("p f t -> p (f t)"),
                       s1[:, fp * 2:fp * 2 + 2, :].rearrange("p f t -> p (f t)"), pp[:, :], ALU.mult, [s1.r, pp.r], [aT.r])
        for hb in range(2):
            ys = ysr.get()
            for half in range(2):
                py = banks.get()
                for fc in range(4):
                    MM(py[:, :], aT[:, fc, hb * 128:(hb + 1) * 128], w2b[:, fc, half * 512:(half + 1) * 512], fc == 0, fc == 3,
                       [aT.r, w2b.r], [py.r])
                if half == 0:
                    ACT(ys[:, 0:512], py[:, :], AF.Identity, [py.r], [ys.r])
                else:
                    CP("dve", ys[:, 512:1024], py[:, :], [py.r], [ys.r])
            r0 = j * SB + hb * 128
            P.dma_copy("sp", yslot.t[r0:r0 + 128, :], ys[:], reads=[ys.r], writes=[yslot.r])
    P.release(markE)

    g2bc = []
    for j in range(2):
        t = P.sbuf(f"g2bc{j}", [128, 1024], F32)
        P.dma_copy("sp", t[:], modD.t[j:j + 1, 5 * 1024:6 * 1024].partition_broadcast(128), writes=[t.r])
        g2bc.append(t)
    lg2 = P.sbuf("lg2", [128, 1024], F32)
    lb2 = P.sbuf("lb2", [128, 1024], F32)
    P.dma_copy("sp", lg2[:], ln2g_d.t[0:1, :].partition_broadcast(128), writes=[lg2.r])
    P.dma_copy("sp", lb2[:], ln2b_d.t[0:1, :].partition_broadcast(128), writes=[lb2.r])
    y1r = Ring([P.sbuf(f"y1{i}", [128, 1024], F32) for i in range(2)])
    y2r = Ring([P.sbuf(f"y2{i}", [128, 1024], F32) for i in range(2)])
    x1r = Ring([P.sbuf(f"x1t{i}", [128, 1024], F32) for i in range(2)])
    ur = Ring([P.sbuf(f"u{i}", [128, 1024], F32) for i in range(2)])
    str_ = Ring([P.sbuf(f"st{i}", [128, 2, 6], F32) for i in range(2)])
    mvr = Ring([P.sbuf(f"mv{i}", [128, 2], F32) for i in range(2)])
    for i in range(NT):
        j = 0 if i < 16 else 1
        y1, y2 = y1r.get(), y2r.get()
        for k, yy in enumerate((y1, y2)):
            P.dma("pool", lambda e, yy=yy, i=i, k=k: e.indirect_dma_start(
                out=yy[:, :], out_offset=None, in_=yslot.t[:, :],
                in_offset=bass.IndirectOffsetOnAxis(ap=posI[:, i, k:k + 1], axis=0)),
                reads=[posI.r, yslot.r], writes=[yy.r])
        xt = x1r.get()
        P.dma_copy("sp", xt[:], x1_d.t[i * 128:(i + 1) * 128, :], writes=[xt.r])
        TS("dve", y1[:], y1[:], gates[:, i, 0:1], None, ALU.mult, None, [y1.r, gates.r], [y1.r])
        STT("dve", y1[:], y2[:], gates[:, i, 1:2], y1[:], ALU.mult, ALU.add, [y2.r, gates.r, y1.r], [y1.r])
        u = ur.get()
        TT("pool", u[:], y1[:], g2bc[j][:], ALU.mult, [y1.r, g2bc[j].r], [u.r])
        STT("dve", u[:], xt[:], ALPHA, u[:], ALU.mult, ALU.add, [xt.r, u.r], [u.r])
        st = str_.get()
        for c in range(2):
            P.op("dve", lambda e, st=st, u=u, c=c: e.bn_stats(out=st[:, c, :], in_=u[:, c * 512:(c + 1) * 512]),
                 [u.r], [st.r])
        mv = mvr.get()
        P.op("dve", lambda e, st=st, mv=mv: e.bn_aggr(out=mv[:], in_=st[:].rearrange("p a b -> p (a b)")),
             [st.r], [mv.r])
        ACT(mv[:, 1:2], mv[:, 1:2], AF.Sqrt, [mv.r, epsc.r], [mv.r], bias=epsc[:, 0:1])
        P.op("dve", lambda e, mv=mv: e.reciprocal(out=mv[:, 1:2], in_=mv[:, 1:2]), [mv.r], [mv.r])
        TS("dve", u[:], u[:], mv[:, 0:1], mv[:, 1:2], ALU.subtract, ALU.mult, [u.r, mv.r], [u.r])
        TT("pool", u[:], u[:], lg2[:], ALU.mult, [u.r, lg2.r], [u.r])
        TT("pool", u[:], u[:], lb2[:], ALU.add, [u.r, lb2.r], [u.r])
        P.dma_copy("sp", x2_o.t[i * 128:(i + 1) * 128, :], u[:], reads=[u.r], writes=[x2_o.r])

    P.release(m_stage)


LAYER_W = {"w_mod", "b_mod", "w_in", "b_in", "qn_g", "kn_g", "wgate", "bgate", "gla_norm", "w_br_attn", "w_br_gla",
           "w_br_na", "w_out", "ln1_g", "ln1_b", "w_r", "b_r", "moe_w1", "moe_w3", "moe_w2", "ln2_g", "ln2_b", "tabNA"}


class Env:
    def __init__(self, P):
        self.P = P
        self.scratch = {}
        self.ext = {}
        self.last = 1
        self.fb = [P.psum(f"fb{i}", [128, 512], F32) for i in range(7)]
        self.bb = P.psum("bb", [128, 8, 128], BF16)

    def _mk(self, name, shape, dt, kind):
        b = self.P.dram(name, shape, dt, kind=kind)
        b.t = b.t.ap()
        return b

    def din(self, name, shape, dt, L):
        if name == "x_in":
            if L == 0:
                if "x_in" not in self.ext:
                    self.ext["x_in"] = self._mk("x_in", shape, dt, "ExternalInput")
                return self.ext["x_in"]
            return self.scratch["x2"]
        if name in self.scratch:
            return self.scratch[name]
        key = f"{name}_{L}" if name in LAYER_W else name
        if key not in self.ext:
            self.ext[key] = self._mk(key, shape, dt, "ExternalInput")
        return self.ext[key]

    def dout(self, name, shape, dt, L):
        if name == "x2" and L == self.last:
            return self._mk("x2out", shape, dt, "ExternalOutput")
        if name not in self.scratch:
            self.scratch[name] = self._mk("sc_" + name, shape, dt, "Internal")
        return self.scratch[name]


def emit_exchange(P, E, groups):
    S = E.scratch

    def sc(name, shape, dt):
        if name not in S:
            S[name] = E._mk("sc_" + name, shape, dt, "Internal")
        return S[name]
    pk_ak, g_ak = sc("pk_ak", [128, 2048], BF16), sc("g_ak", [256, 2048], BF16)
    pk_av, g_av = sc("pk_av", [2048, 128], BF16), sc("g_av", [4096, 128], BF16)
    pk_ck, g_ck = sc("pk_ck", [512, 768], BF16), sc("g_ck", [1024, 768], BF16)
    pk_cv, g_cv = sc("pk_cv", [768, 512], BF16), sc("g_cv", [1536, 512], BF16)
    pk_S, g_S = sc("pk_S", [128, 512], F32), sc("g_S", [256, 512], F32)
    P.dma_copy("sp", pk_ak.t[:, :], S["akT"].t[:, 0:2048], reads=[S["akT"].r], writes=[pk_ak.r])
    P.dma_copy("sp", pk_av.t[:, :], S["av"].t[0:2048, :], reads=[S["av"].r], writes=[pk_av.r])
    P.dma_copy("sp", pk_ck.t[:, 0:384], S["ckT"].t[:, 0:384], reads=[S["ckT"].r], writes=[pk_ck.r])
    P.dma_copy("sp", pk_ck.t[:, 384:768], S["ckT"].t[:, 1664:2048], reads=[S["ckT"].r], writes=[pk_ck.r])
    P.dma_copy("sp", pk_cv.t[0:384, :], S["cv"].t[0:384, :], reads=[S["cv"].r], writes=[pk_cv.r])
    P.dma_copy("sp", pk_cv.t[384:768, :], S["cv"].t[1664:2048, :], reads=[S["cv"].r], writes=[pk_cv.r])
    P.dma_copy("sp", pk_S.t[:, :], S["Sfin"].t.rearrange("d p f -> (d p) f"), reads=[S["Sfin"].r], writes=[pk_S.r])
    for a, b_ in ((pk_ak, g_ak), (pk_av, g_av), (pk_ck, g_ck), (pk_cv, g_cv), (pk_S, g_S)):
        P.coll(lambda e, a=a, b_=b_: e.collective_compute("AllGather", ALU.bypass, replica_groups=groups,
                                                           ins=[a.t[:, :]], outs=[b_.t[:, :]]),
               reads=[a.r], writes=[b_.r])


def build_fused(groups=None, n_layers=2):
    if groups is None:
        groups = [[0, 1], [2, 3], [4, 5], [6, 7]]
    nc = bass.Bass("TRN2", target_bir_lowering=False)
    P = Prog(nc)
    E = Env(P)
    E.last = n_layers - 1
    for L in range(n_layers):
        emit_l1(P, E, L)
        emit_exchange(P, E, groups)
        emit_l2(P, E, L)
        emit_l3(P, E, L)
    P.finalize()
    return nc, P


_BF = ml_dtypes.bfloat16


def _rope_tables(tok):
    row = (tok // 64).astype(np.float32)
    col = (tok % 64).astype(np.float32)
    inv = (10000.0 ** (-np.arange(16, dtype=np.float32) / 16)).astype(np.float32)
    ar = row[:, None] * inv
    ac = col[:, None] * inv
    ang = np.concatenate([ar, ar, ac, ac], -1)
    return np.cos(ang).astype(np.float32), np.sin(ang).astype(np.float32)


def _consts(s):
    tok = s * 2048 + np.arange(2048)
    cos, sin = _rope_tables(tok)
    cosf = np.concatenate([cos, np.ones((256, 64), np.float32)], 0).T
    sinf = np.concatenate([sin, np.zeros((256, 64), np.float32)], 0).T
    R = np.zeros((64, 64), np.float32)
    for d in range(16):
        R[d, d + 16] = -1
        R[d + 16, d] = 1
        R[d + 32, d + 48] = -1
        R[d + 48, d + 32] = 1
    rotM = np.zeros((128, 128), np.float32)
    rotM[:64, :64] = R.T
    rotM[64:, 64:] = R.T
    ob = np.zeros((128, 128), np.float32)
    ob[:64, :64] = 1
    ob[64:, 64:] = 1
    ii = np.arange(128)
    sI, tI = np.meshgrid(ii, ii, indexing="ij")
    f32 = np.float32
    return dict(cosT=np.ascontiguousarray(np.concatenate([cosf, cosf], 0)),
                sinT=np.ascontiguousarray(np.concatenate([sinf, sinf], 0)),
                rotM=rotM, onesblk=ob, ident=np.eye(128, dtype=f32),
                triInc=(sI <= tI).astype(f32), triDec=(sI >= tI).astype(f32),
                triSgt=(sI > tI).astype(f32), triSlt=(sI < tI).astype(f32),
                flags=np.tile(np.array([[1.0 if s == 0 else 0.0, 1.0 if s == 1 else 0.0]], f32), (64, 1)),
                fl1m=np.tile(np.array([[0.0 if s == 0 else 1.0, 0.0 if s == 1 else 1.0]], f32), (64, 1)),
                ones128=np.ones((128, 128), f32),
                thr18=np.tile((256.0 * np.arange(9, dtype=f32))[None, None, :], (128, 32, 1)),
                thrj=np.tile((256.0 * np.arange(NBLK, dtype=f32))[None, :, None], (128, 1, 32)),
                wbase=(np.arange(8)[None, :] * 128 + np.arange(128)[:, None]).astype(f32),
                tokidx=(np.arange(18)[None, :] * 128 + np.arange(128)[:, None]).astype(np.int32))


def _na_table7(rpb, s):
    tab = np.full((5, 7, 2, 128, 512), -30000.0, np.float32)
    kp = np.arange(128)
    qp = np.arange(128)
    for slot, j in enumerate((0, 1, 14, 15, 5)):
        J = 16 * s + j
        qr = 2 * J + qp // 64
        qc = qp % 64
        rs = np.clip(qr - 4, 0, 56)
        cs = np.clip(qc - 8, 0, 48)
        for kt in range(7):
            T = J - 3 + kt
            if T < 0 or T > 31:
                continue
            kr = 2 * T + kp // 64
            kc = kp % 64
            valid = ((kr[:, None] >= rs[None, :]) & (kr[:, None] < rs[None, :] + 8)
                     & (kc[:, None] >= cs[None, :]) & (kc[:, None] < cs[None, :] + 16))
            ri = np.clip(kr[:, None] - qr[None, :] + 7, 0, 14)
            ci = np.clip(kc[:, None] - qc[None, :] + 15, 0, 30)
            for h in range(8):
                tab[slot, kt, h // 4, :, (h % 4) * 128:(h % 4 + 1) * 128] = np.where(valid, rpb[h][ri, ci], -30000.0)
    return tab.astype(_BF)


def _core_map(inp, c, n_layers=2):
    f32 = np.float32
    b, s = c // 2, c % 2
    m = dict(x_in=np.concatenate([inp["x"][b, s * 2048:(s + 1) * 2048], inp["ctx"][b]], 0),
             cvec=np.stack([inp["c"][b], inp["c_ctx"]], 0), **_consts(s))
    for l in range(n_layers):
        lw = dict(w_mod=inp["w_mod"][l], b_mod=inp["b_mod"][l][None], w_in=inp["w_in"][l], b_in=inp["b_in"][l][None],
                  qn_g=inp["attn_q_norm"][l][:, None], kn_g=inp["attn_k_norm"][l][:, None],
                  wgate=inp["gla_w_gate"][l], bgate=inp["gla_b_gate"][l], gla_norm=inp["gla_norm"][l][:, None],
                  w_br_attn=inp["w_br_attn"][l], w_br_gla=inp["w_br_gla"][l], w_br_na=inp["w_br_na"][l],
                  w_out=inp["w_out"][l], ln1_g=inp["ln1_g"][l][None], ln1_b=inp["ln1_b"][l][None],
                  w_r=np.concatenate([inp["w_router_group"][l], inp["w_router_expert"][l]], 1),
                  b_r=np.concatenate([inp["b_router_group"][l], inp["b_router_expert"][l]])[None],
                  moe_w1=inp["moe_w1"][l].reshape(-1, 4096), moe_w3=inp["moe_w3"][l].reshape(-1, 4096),
                  moe_w2=inp["moe_w2"][l].reshape(-1, 4096), ln2_g=inp["ln2_g"][l][None], ln2_b=inp["ln2_b"][l][None],
                  tabNA=_na_table7(inp["na_rpb"][l].astype(f32), s))
        for k, v in lw.items():
            m[f"{k}_{l}"] = v
    out = {}
    for k, v in m.items():
        v = np.asarray(v)
        if v.dtype not in (np.int32, _BF):
            v = v.astype(f32)
        out[k] = np.ascontiguousarray(v)
    return out


_PROG = []


def kernel(**inp):
    inp = {k: np.asarray(v) for k, v in inp.items()}
    if not _PROG:
        _PROG.append(build_fused()[0])
    cores = list(range(8))
    maps = [_core_map(inp, c) for c in cores]
    res = run_bass_kernel_spmd(_PROG[0], maps, core_ids=cores).results
    out = np.zeros((4, 4096, 1024), np.float32)
    for c in cores:
        out[c // 2, (c % 2) * 2048:(c % 2 + 1) * 2048] = np.asarray(res[c]["x2out"], dtype=np.float32)[:2048]
    return out
```

```python
import os
import numpy as np
import ml_dtypes
import concourse.bass as bass
import concourse.mybir as mybir
from concourse.bass_utils import run_bass_kernel_spmd

F32 = mybir.dt.float32
BF16 = mybir.dt.bfloat16
I32 = mybir.dt.int32
AF = mybir.ActivationFunctionType
ALU = mybir.AluOpType
AX = mybir.AxisListType


class Res:
    __slots__ = ("name", "w", "r", "dram")

    def __init__(self, name=""):
        self.name = name
        self.w = None
        self.r = {}
        self.dram = False


class Buf:
    def __init__(self, t, nres=1, name=""):
        self.t = t
        self.rs = [Res(f"{name}{i}") for i in range(nres)]

    @property
    def r(self):
        return self.rs[0]

    def __getitem__(self, k):
        return self.t[k]


class Prog:
    ENGS = ("pe", "act", "dve", "pool", "sp")
    SAME_SYNC = {"pe": False, "act": "a" not in os.environ.get("SSOFF",""), "dve": "d" not in os.environ.get("SSOFF",""), "pool": "p" not in os.environ.get("SSOFF",""), "sp": False}

    def __init__(self, nc, n_dma_sems=48):
        self.nc = nc
        self.lists = {k: [] for k in self.ENGS}
        self.semobj = {}
        self.cnt = {}
        for k in self.ENGS:
            self.semobj[k] = nc.alloc_semaphore(f"sem_{k}")
            self.cnt[k] = 0
        self.seen = {k: {} for k in self.ENGS}
        self.nd = n_dma_sems
        self.dval = [0] * n_dma_sems
        for i in range(n_dma_sems):
            self.semobj[f"d{i}"] = nc.alloc_semaphore(f"sem_d{i}")
        self.dnext = 0
        self.dnext_sw = 0
        self.pending = []
        self.loads_since = 0
        self.defer_stores = os.environ.get("DEFER", "1") == "1"
        self.n_ops = 0
        self.arena0, self.arena1 = nc.bump_sbuf(212000)
        self.sb_off = self.arena0
        self.sb_peak = self.arena0
        self.n_alloc = 0

    def sbuf(self, name, shape, dtype, nres=1):
        esz = {F32: 4, BF16: 2, I32: 4}[dtype]
        nb = esz
        for d in shape[1:]:
            nb *= d
        nb = (nb + 31) // 32 * 32
        assert self.sb_off + nb <= self.arena1, f"SBUF arena overflow at {name}: {self.sb_off + nb - self.arena0}"
        self.n_alloc += 1
        t = self.nc.alloc_sbuf_tensor_at(f"s{self.n_alloc}_{name}", list(shape), dtype, offset=self.sb_off)
        self.sb_off += nb
        self.sb_peak = max(self.sb_peak, self.sb_off)
        return Buf(t, nres, name)

    def mark(self):
        return self.sb_off

    def release(self, mark):
        self.barrier()
        self.sb_off = mark

    def barrier(self):
        self._flush()
        self.loads_since = 0
        ev = [(k, self.cnt[k]) for k in self.ENGS if self.cnt[k] > 0]
        ev += [(f"d{i}", self.dval[i]) for i in range(self.nd) if self.dval[i] > 0]
        if "cc" in self.semobj:
            ev.append(("cc", self.ccval))
        for k in self.ENGS:
            self._wait(k, ev)

    def psum(self, name, shape, dtype=F32, nres=1):
        return Buf(self.nc.alloc_psum_tensor("p_" + name, list(shape), dtype), nres, name)

    def dram(self, name, shape, dtype, kind="Internal", nres=1):
        b = Buf(self.nc.dram_tensor(name, list(shape), dtype, kind=kind), nres, name)
        for r in b.rs:
            r.dram = True
        return b

    def _flush(self, upto=None):
        pend = self.pending
        n = len(pend) if upto is None else upto
        for (fn, reads, writes) in pend[:n]:
            self._dma_now("sp", fn, reads, writes)
        del pend[:n]

    def _check_pending(self, reads, writes, is_compute):
        if not self.pending:
            return
        last = -1
        for i, (fn, sr, sw) in enumerate(self.pending):
            hit = False
            for w in writes:
                if any(w is x for x in sr) or any(w is x for x in sw):
                    hit = True
            for r in reads:
                if any(r is x for x in sw):
                    hit = True
            if hit:
                last = i
        if is_compute and self.loads_since > 0:
            last = len(self.pending) - 1
        if last >= 0:
            self._flush(last + 1)
            if not self.pending:
                self.loads_since = 0

    def _wait(self, eng, deps):
        for key, val in deps:
            if key == eng and not self.SAME_SYNC[eng]:
                continue
            if self.seen[eng].get(key, 0) >= val:
                continue
            self.seen[eng][key] = val
            self.lists[eng].append(("w", key, val))

    @staticmethod
    def _deps(reads, writes):
        deps = []
        for r in reads:
            if r.w is not None:
                deps.append(r.w)
        for w in writes:
            if w.w is not None:
                deps.append(w.w)
            deps.extend(w.r.items())
        return deps

    @staticmethod
    def _commit(ev, reads, writes):
        for r in reads:
            if r.r.get(ev[0], 0) < ev[1]:
                r.r[ev[0]] = ev[1]
        for w in writes:
            w.w = ev
            w.r = {}

    def op(self, eng, fn, reads=(), writes=()):
        self._check_pending(reads, writes, True)
        self._wait(eng, self._deps(reads, writes))
        self.cnt[eng] += 1
        ev = (eng, self.cnt[eng])
        self.lists[eng].append(("o", fn, eng, 1))
        self._commit(ev, reads, writes)
        self.n_ops += 1
        return ev

    def dma(self, q, fn, reads=(), writes=()):
        reads, writes = list(reads), list(writes)
        if (self.defer_stores and q == "sp" and writes and all(w.dram for w in writes)
                and reads and not any(r.dram for r in reads)):
            self._check_pending(reads, writes, False)
            self.pending.append((fn, reads, writes))
            return None
        self._check_pending(reads, writes, False)
        if q == "sp":
            self.loads_since += 1 if self.pending else 0
        return self._dma_now(q, fn, reads, writes)

    def _dma_now(self, q, fn, reads=(), writes=()):
        half = self.nd // 2
        if q == "pool":
            i = half + self.dnext_sw
            self.dnext_sw = (self.dnext_sw + 1) % (self.nd - half)
        else:
            i = self.dnext
            self.dnext = (self.dnext + 1) % half
        key = f"d{i}"
        deps = self._deps(reads, writes)
        if self.dval[i] > 0:
            deps.append((key, self.dval[i]))
        self._wait(q, deps)
        self.dval[i] += 16
        ev = (key, self.dval[i])
        self.lists[q].append(("o", fn, key, 16))
        self._commit(ev, reads, writes)
        self.n_ops += 1
        return ev

    def coll(self, fn, reads=(), writes=()):
        self._flush()
        if "cc" not in self.semobj:
            self.semobj["cc"] = self.nc.alloc_semaphore("sem_cc")
            self.ccval = 0
        deps = self._deps(reads, writes)
        if self.ccval > 0:
            deps.append(("cc", self.ccval))
        self._wait("pool", deps)
        self.ccval += 1
        ev = ("cc", self.ccval)
        self.lists["pool"].append(("o", fn, "cc", 1))
        self._commit(ev, reads, writes)
        return ev

    def dma_copy(self, q, out, in_, reads=(), writes=(), **kw):
        return self.dma(q, lambda e: e.dma_start(out=out, in_=in_, **kw), reads, writes)

    def finalize(self):
        self._flush()
        final = [(k, self.cnt[k]) for k in self.ENGS if self.cnt[k] > 0 and k != "sp"]
        final += [(f"d{i}", self.dval[i]) for i in range(self.nd) if self.dval[i] > 0]
        if "cc" in self.semobj:
            final.append(("cc", self.ccval))
        self._wait("sp", final)
        nc = self.nc
        lists = self.lists
        semobj = self.semobj

        def run(e, items):
            for it in items:
                if it[0] == "w":
                    e.wait_ge(semobj[it[1]], it[2])
                else:
                    ins = it[1](e)
                    ins.then_inc(semobj[it[2]], it[3])

        with nc.Block() as block:
            @block.tensor
            def _(e):
                run(e, lists["pe"])

            @block.scalar
            def _(e):
                run(e, lists["act"])

            @block.vector
            def _(e):
                run(e, lists["dve"])

            @block.gpsimd
            def _(e):
                run(e, lists["pool"])

            @block.sync
            def _(e):
                run(e, lists["sp"])


PENG = os.environ.get('PENG', 'pool')
GM = int(os.environ.get('GM', '9'))
NT = 18
NTOK = 2304
BLKS = [(0, 512), (512, 512), (1024, 512), (1536, 512), (2048, 256)]
LN_EPS = 1e-6
RMS_EPS = 1e-6

C_AQ, C_AK, C_AV, C_BQ, C_BK, C_BV, C_BR, C_BA, C_CQ, C_CK, C_CV, C_G = (
    0, 512, 640, 768, 1024, 1280, 1792, 2304, 2336, 2848, 3360, 3872)


class Ctx:
    pass


def mk_helpers(P):
    H = Ctx()

    def MM(out, lhsT, rhs, start, stop, reads, writes):
        P.op("pe", lambda e: e.matmul(out, lhsT=lhsT, rhs=rhs, start=start, stop=stop), reads, writes)

    def TR(out, in_, ident, reads, writes):
        P.op("pe", lambda e: e.transpose(out, in_, ident), reads, writes)

    def ACT(out, in_, func, reads, writes, bias=None, scale=None):
        kw = {}
        if bias is not None:
            kw["bias"] = bias
        if scale is not None:
            kw["scale"] = scale
        P.op("act", lambda e: e.activation(out=out, in_=in_, func=func, **kw), reads, writes)

    def TT(eng, out, in0, in1, op, reads, writes):
        P.op(eng, lambda e: e.tensor_tensor(out=out, in0=in0, in1=in1, op=op), reads, writes)

    def TS(eng, out, in0, s1, s2, op0, op1, reads, writes):
        if op1 is None:
            P.op(eng, lambda e: e.tensor_scalar(out=out, in0=in0, scalar1=s1, scalar2=None, op0=op0), reads, writes)
        else:
            P.op(eng, lambda e: e.tensor_scalar(out=out, in0=in0, scalar1=s1, scalar2=s2, op0=op0, op1=op1), reads, writes)

    def STT(eng, out, in0, scalar, in1, op0, op1, reads, writes):
        P.op(eng, lambda e: e.scalar_tensor_tensor(out=out, in0=in0, scalar=scalar, in1=in1, op0=op0, op1=op1), reads, writes)

    def CP(eng, out, in_, reads, writes):
        P.op(eng, lambda e: e.tensor_copy(out=out, in_=in_), reads, writes)

    def MS(eng, ap, val, writes):
        P.op(eng, lambda e: e.memset(ap, val), (), writes)

    H.MM, H.TR, H.ACT, H.TT, H.TS, H.STT, H.CP, H.MS = MM, TR, ACT, TT, TS, STT, CP, MS
    return H


class Banks:
    def __init__(self, P, n=8, bufs=None):
        self.b = list(bufs) if bufs is not None else [P.psum(f"bank{i}", [128, 512], F32) for i in range(n)]
        self.i = 0
        self.n = len(self.b)

    def get(self):
        b = self.b[self.i]
        self.i = (self.i + 1) % self.n
        return b


class Ring:
    def __init__(self, bufs):
        self.bufs = bufs
        self.i = 0

    def get(self):
        b = self.bufs[self.i]
        self.i = (self.i + 1) % len(self.bufs)
        return b


def tile_res(buf, t0, n):
    return [buf.rs[i] for i in range(t0 // 128, (t0 + n + 127) // 128)]


ALPHA = 4 ** 0.25
NKT = 34
NBLK = 50
SB = 256
NSLOT = NBLK * SB
BIG = 1.0e9
def emit_l1(P, E, L):
    nc = P.nc
    H = mk_helpers(P)
    MM, TR, ACT, TT, TS, STT, CP, MS = H.MM, H.TR, H.ACT, H.TT, H.TS, H.STT, H.CP, H.MS
    din = lambda name, shape, dt=F32: E.din(name, shape, dt, L)
    dout = lambda name, shape, dt: E.dout(name, shape, dt, L)
    m_stage = P.mark()

    x_in = din("x_in", [NTOK, 1024])
    cvec = din("cvec", [2, 1024])
    w_mod = din("w_mod", [1024, 6144])
    b_mod = din("b_mod", [1, 6144])
    w_in = din("w_in", [1024, 6944])
    b_in = din("b_in", [1, 6944])
    qn_g = din("qn_g", [64, 1])
    kn_g = din("kn_g", [64, 1])
    wgate_d = din("wgate", [2, 16, 256])
    bgate_d = din("bgate", [2, 256])
    cos_d = din("cosT", [128, NTOK])
    sin_d = din("sinT", [128, NTOK])
    ident_d = din("ident", [128, 128])
    rot_d = din("rotM", [128, 128])
    oblk_d = din("onesblk", [128, 128])

    gT = dout("gT", [3072, NTOK], BF16)
    rT = dout("rT", [512, NTOK], BF16)
    cqT = dout("cqT", [512, NTOK], BF16)
    ckT = dout("ckT", [512, NTOK], BF16)
    cv = dout("cv", [NTOK, 512], BF16)
    bqT = dout("bqT", [256, NTOK], F32)
    bkT = dout("bkT", [256, NTOK], F32)
    bk = dout("bk", [NTOK, 256], F32)
    bv = dout("bv", [NTOK, 512], BF16)
    Gd = dout("Gd", [NTOK, 2, 256], F32)
    aqT = dout("aqT", [512, NTOK], BF16)
    akT = dout("akT", [128, NTOK], BF16)
    av = dout("av", [NTOK, 128], BF16)
    modD = dout("modD", [2, 6144], F32)

    banks = Banks(P, 0, E.fb)
    pT = E.bb

    ident = P.sbuf("ident", [128, 128], BF16)
    P.dma_copy("pool", ident[:], ident_d.t[:, :], writes=[ident.r])
    oblk = P.sbuf("oblk", [128, 128], BF16)
    P.dma_copy("pool", oblk[:], oblk_d.t[:, :], writes=[oblk.r])
    rotM = P.sbuf("rotM", [128, 128], F32)
    P.dma_copy("sp", rotM[:], rot_d.t[:, :], writes=[rotM.r])
    cosT = P.sbuf("cosT", [128, NTOK], F32)
    sinT = P.sbuf("sinT", [128, NTOK], F32)
    P.dma_copy("sp", cosT[:], cos_d.t[:, :], writes=[cosT.r])
    P.dma_copy("sp", sinT[:], sin_d.t[:, :], writes=[sinT.r])
    g8 = P.sbuf("g8", [128, 2], F32)
    for hh in range(2):
        P.dma_copy("sp", g8[hh * 64:(hh + 1) * 64, 0:1], qn_g.t[:, :], writes=[g8.r])
        P.dma_copy("sp", g8[hh * 64:(hh + 1) * 64, 1:2], kn_g.t[:, :], writes=[g8.r])
    TS("dve", g8[:], g8[:], 8.0, None, ALU.mult, None, [g8.r], [g8.r])
    wgate = P.sbuf("wgate", [16, 2, 256], BF16)
    P.dma_copy("pool", wgate[:], wgate_d.t.rearrange("d r c -> r d c"), writes=[wgate.r])
    bg_bc = P.sbuf("bg_bc", [128, 2, 256], F32)
    for d in range(2):
        P.dma_copy("sp", bg_bc[:, d, :], bgate_d.t[d:d + 1, :].partition_broadcast(128), writes=[bg_bc.r])

    epsc = P.sbuf("epsc", [128, 2], F32)
    MS("dve", epsc[:, 0:1], LN_EPS, [epsc.r])
    MS("dve", epsc[:, 1:2], 64.0 * RMS_EPS, [epsc.r])
    cT = P.sbuf("cT", [128, 8, 2], F32)
    for j in range(2):
        P.dma_copy("sp", cT[:, :, j], cvec.t[j:j + 1, :].rearrange("o (k p) -> p (o k)", p=128), writes=[cT.r],
                   allow_slow_non_contiguous=True)
    scT = P.sbuf("scT", [128, 8, 2], BF16)
    ACT(scT[:], cT[:], AF.Silu, [cT.r], [scT.r])
    bm_r = Ring([P.sbuf(f"bm{i}", [2, 512], F32) for i in range(2)])
    mr_r = Ring([P.sbuf(f"mr{i}", [2, 512], F32) for i in range(2)])
    wsl = Ring([P.sbuf(f"wsg{i}", [128, 8, 512], BF16) for i in range(2)])
    for g in range(12):
        wb = wsl.get()
        P.dma_copy("pool", wb[:], w_mod.t[:, g * 512:(g + 1) * 512].rearrange("(k p) c -> p k c", p=128),
                   writes=[wb.r])
        bm = bm_r.get()
        for j in range(2):
            P.dma_copy("sp", bm[j:j + 1, :], b_mod.t[0:1, g * 512:(g + 1) * 512], writes=[bm.r])
        ps = banks.get()
        for k in range(8):
            MM(ps[0:2, 0:512], scT[:, k, :], wb[:, k, :], k == 0, k == 7, [scT.r, wb.r], [ps.r])
        mr = mr_r.get()
        TT("dve", mr[:], ps[0:2, 0:512], bm[:], ALU.add, [ps.r, bm.r], [mr.r])
        P.dma_copy("sp", modD.t[:, g * 512:(g + 1) * 512], mr[:], reads=[mr.r], writes=[modD.r])
    modc = P.sbuf("modc", [128, 2, 6, 8], F32)
    for j in range(2):
        for m in range(2):
            P.dma_copy("sp", modc[:, j, m, :],
                       modD.t[j:j + 1, m * 1024:(m + 1) * 1024].rearrange("o (k p) -> p (o k)", p=128),
                       reads=[modD.r], writes=[modc.r], allow_slow_non_contiguous=True)
    TS("dve", modc[:, :, 1, :], modc[:, :, 1, :], 1.0, None, ALU.add, None, [modc.r], [modc.r])

    hT = P.sbuf("hT", [128, 8, NTOK], BF16, nres=NT)
    xin = Ring([P.sbuf(f"xin{i}", [128, 1024], F32) for i in range(2)])
    xnr = Ring([P.sbuf(f"xn{i}", [128, 1024], BF16) for i in range(2)])
    str_ = Ring([P.sbuf(f"st{i}", [128, 2, 6], F32) for i in range(2)])
    mvr = Ring([P.sbuf(f"mv{i}", [128, 2], F32) for i in range(2)])
    for i in range(NT):
        xt = xin.get()
        P.dma_copy("sp", xt[:], x_in.t[i * 128:(i + 1) * 128, :], writes=[xt.r])
        st = str_.get()
        for c in range(2):
            P.op("dve", lambda e, st=st, xt=xt, c=c: e.bn_stats(out=st[:, c, :], in_=xt[:, c * 512:(c + 1) * 512]),
                 [xt.r], [st.r])
        mv = mvr.get()
        P.op("dve", lambda e, st=st, mv=mv: e.bn_aggr(out=mv[:], in_=st[:].rearrange("p a b -> p (a b)")),
             [st.r], [mv.r])
        ACT(mv[:, 1:2], mv[:, 1:2], AF.Sqrt, [mv.r], [mv.r], bias=epsc[:, 0:1])
        P.op("dve", lambda e, mv=mv: e.reciprocal(out=mv[:, 1:2], in_=mv[:, 1:2]), [mv.r], [mv.r])
        xn = xnr.get()
        TS("dve", xn[:], xt[:], mv[:, 0:1], mv[:, 1:2], ALU.subtract, ALU.mult, [xt.r, mv.r], [xn.r])
        for k in range(8):
            TR(pT[:, k, :], xn[:, k * 128:(k + 1) * 128], ident[:], [xn.r, ident.r], [pT.r])
        j = 0 if i < 16 else 1
        for k in range(8):
            o = hT[:, k, i * 128:(i + 1) * 128]
            if k % 2 == 0:
                ACT(o, pT[:, k, :], AF.Identity, [pT.r, modc.r], [hT.rs[i]],
                    bias=modc[:, j, 0, k:k + 1], scale=modc[:, j, 1, k:k + 1])
            else:
                TS("dve", o, pT[:, k, :], modc[:, j, 1, k:k + 1], modc[:, j, 0, k:k + 1], ALU.mult, ALU.add,
                   [pT.r, modc.r], [hT.rs[i]])

    bcols = P.sbuf("bcols", [128, 48], F32)
    bcol_idx = {}
    nb = 0
    for (c0, ng) in [(C_AQ, 4), (C_AK, 1), (C_BQ, 2), (C_BK, 2), (C_BR, 4), (C_CQ, 4), (C_CK, 4), (C_G, 24)]:
        P.dma_copy("sp", bcols[:, nb:nb + ng],
                   b_in.t[0:1, c0:c0 + ng * 128].rearrange("o (g p) -> p (o g)", p=128),
                   writes=[bcols.r], allow_slow_non_contiguous=True)
        for g in range(ng):
            bcol_idx[c0 + g * 128] = nb + g
        nb += ng
    bacol = P.sbuf("bacol", [16, 2], F32)
    P.dma_copy("sp", bacol[:], b_in.t[0:1, C_BA:C_BA + 32].rearrange("o (d p) -> p (o d)", p=16),
               writes=[bacol.r], allow_slow_non_contiguous=True)
    bias_bc = P.sbuf("bias_bc", [128, 1408], F32)
    tm_groups = [(C_AV, 128, 0), (C_BK, 256, 128), (C_BV, 512, 384), (C_CV, 512, 896)]
    for (c0, n, o) in tm_groups:
        P.dma_copy("sp", bias_bc[:, o:o + n], b_in.t[0:1, c0:c0 + n].partition_broadcast(128), writes=[bias_bc.r])

    mark1b = P.mark()
    stg_bf = Ring([P.sbuf(f"stgbf{i}", [128, NTOK], BF16) for i in range(3)])
    stg_f = Ring([P.sbuf(f"stgf{i}", [128, NTOK], F32) for i in range(2)])
    aux = {n: Ring([P.sbuf(f"{n}{i}", [128, 512], dt) for i in range(2)])
           for n, dt in [("zq", F32), ("sq", BF16), ("rs", F32), ("qn", F32), ("t1", F32), ("t2", F32)]}
    stm_bf = Ring([P.sbuf(f"stmbf{i}", [128, 512], BF16) for i in range(3)])
    stm_f = Ring([P.sbuf(f"stmf{i}", [128, 256], F32) for i in range(2)])
    aT = P.sbuf("aT", [16, 2, NTOK], BF16)

    def load_w(c0, n):
        wb = wsl.get()
        P.dma_copy("pool", wb[:, :, 0:n], w_in.t[:, c0:c0 + n].rearrange("(k p) c -> p k c", p=128),
                   writes=[wb.r])
        return wb

    def fm_mm(wb, off, m, t0, n):
        ps = banks.get()
        rd = [wb.r] + tile_res(hT, t0, n)
        for k in range(8):
            MM(ps[0:m, 0:n], wb[:, k, off:off + m], hT[:, k, t0:t0 + n], k == 0, k == 7, rd, [ps.r])
        return ps

    def fm_simple(wb, off, c0, func, dst, row0, f32=False, scale=None):
        stg = stg_f.get() if f32 else stg_bf.get()
        bc = bcols[:, bcol_idx[c0]:bcol_idx[c0] + 1]
        for (t0, n) in BLKS:
            ps = fm_mm(wb, off, 128, t0, n)
            ACT(stg[:, t0:t0 + n], ps[:, 0:n], func, [ps.r, bcols.r], [stg.r], bias=bc)
        P.dma_copy("sp", dst.t[row0:row0 + 128, :], stg[:], reads=[stg.r], writes=[dst.r])

    def fm_qk(wb, off, c0, gcol, dst, row0):
        stg = stg_bf.get()
        bc = bcols[:, bcol_idx[c0]:bcol_idx[c0] + 1]
        for (t0, n) in BLKS:
            ps = fm_mm(wb, off, 128, t0, n)
            zq, sq, rs, qn, t1, t2 = (aux[k].get() for k in ("zq", "sq", "rs", "qn", "t1", "t2"))
            ACT(zq[:, 0:n], ps[:, 0:n], AF.Identity, [ps.r, bcols.r], [zq.r], bias=bc)
            ACT(sq[:, 0:n], ps[:, 0:n], AF.Square, [ps.r, bcols.r], [sq.r], bias=bc)
            ss = banks.get()
            MM(ss[:, 0:n], oblk[:], sq[:, 0:n], True, True, [oblk.r, sq.r], [ss.r])
            ACT(rs[:, 0:n], ss[:, 0:n], AF.Sqrt, [ss.r], [rs.r], bias=epsc[:, 1:2])
            P.op("dve", lambda e, rs=rs, n=n: e.reciprocal(out=rs[:, 0:n], in_=rs[:, 0:n]), [rs.r], [rs.r])
            STT("dve", qn[:, 0:n], zq[:, 0:n], g8[:, gcol:gcol + 1], rs[:, 0:n], ALU.mult, ALU.mult,
                [zq.r, g8.r, rs.r], [qn.r])
            rot = banks.get()
            MM(rot[:, 0:n], rotM[:], qn[:, 0:n], True, True, [rotM.r, qn.r], [rot.r])
            TT("pool", t1[:, 0:n], qn[:, 0:n], cosT[:, t0:t0 + n], ALU.mult, [qn.r, cosT.r], [t1.r])
            TT("dve", t2[:, 0:n], rot[:, 0:n], sinT[:, t0:t0 + n], ALU.mult, [rot.r, sinT.r], [t2.r])
            TT("dve", stg[:, t0:t0 + n], t1[:, 0:n], t2[:, 0:n], ALU.add, [t1.r, t2.r], [stg.r])
        P.dma_copy("sp", dst.t[row0:row0 + 128, :], stg[:], reads=[stg.r], writes=[dst.r])

    def tm_group(wb, off, n, bo, dst, f32=False):
        for i in range(NT):
            ps = banks.get()
            rd = [wb.r, hT.rs[i]]
            for k in range(8):
                MM(ps[:, 0:n], hT[:, k, i * 128:(i + 1) * 128], wb[:, k, off:off + n], k == 0, k == 7, rd, [ps.r])
            stg = stm_f.get() if f32 else stm_bf.get()
            TT("dve", stg[:, 0:n], ps[:, 0:n], bias_bc[:, bo:bo + n], ALU.add, [ps.r, bias_bc.r], [stg.r])
            P.dma_copy("sp", dst.t[i * 128:(i + 1) * 128, :], stg[:, 0:n], reads=[stg.r], writes=[dst.r])

    wb = load_w(C_AQ, 512)
    for s in range(4):
        fm_qk(wb, s * 128, C_AQ + s * 128, 0, aqT, s * 128)
    wb = load_w(C_AK, 256)
    fm_qk(wb, 0, C_AK, 1, akT, 0)
    tm_group(wb, 128, 128, 0, av)
    wb = load_w(C_BQ, 512)
    for s in range(2):
        fm_simple(wb, s * 128, C_BQ + s * 128, AF.Identity, bqT, s * 128, f32=True)
    for s in range(2):
        fm_simple(wb, 256 + s * 128, C_BK + s * 128, AF.Identity, bkT, s * 128, f32=True)
    tm_group(wb, 256, 256, 128, bk, f32=True)
    wb = load_w(C_BV, 512)
    tm_group(wb, 0, 512, 384, bv)
    wb = load_w(C_BR, 512)
    for s in range(4):
        fm_simple(wb, s * 128, C_BR + s * 128, AF.Silu, rT, s * 128)
    wb = load_w(C_BA, 32)
    for d in range(2):
        for (t0, n) in BLKS:
            ps = fm_mm(wb, d * 16, 16, t0, n)
            ACT(aT[0:16, d, t0:t0 + n], ps[0:16, 0:n], AF.Identity, [ps.r, bacol.r], [aT.r], bias=bacol[:, d:d + 1])
    tg_r = Ring([P.sbuf(f"tg{i}", [128, 256], F32) for i in range(2)])
    te_r = Ring([P.sbuf(f"te{i}", [128, 256], F32) for i in range(2)])
    gst_r = Ring([P.sbuf(f"gst{i}", [128, 2, 256], F32) for i in range(2)])
    for i in range(NT):
        gst = gst_r.get()
        for d in range(2):
            ps = banks.get()
            MM(ps[:, 0:256], aT[0:16, d, i * 128:(i + 1) * 128], wgate[0:16, d, :], True, True,
               [aT.r, wgate.r], [ps.r])
            tg = tg_r.get()
            te = te_r.get()
            TT("dve", tg[:], ps[:, 0:256], bg_bc[:, d, :], ALU.add, [ps.r, bg_bc.r], [tg.r])
            ACT(te[:], tg[:], AF.Exp, [tg.r], [te.r], scale=-1.0)
            ACT(gst[:, d, :], te[:], AF.Ln, [te.r], [gst.r], bias=1.0)
        P.dma_copy("sp", Gd.t[i * 128:(i + 1) * 128, :, :], gst[:], reads=[gst.r], writes=[Gd.r])
    wb = load_w(C_CQ, 512)
    for s in range(4):
        fm_simple(wb, s * 128, C_CQ + s * 128, AF.Identity, cqT, s * 128)
    wb = load_w(C_CK, 512)
    for s in range(4):
        fm_simple(wb, s * 128, C_CK + s * 128, AF.Identity, ckT, s * 128)
    wb = load_w(C_CV, 512)
    tm_group(wb, 0, 512, 896, cv)
    for gsup in range(6):
        wb = load_w(C_G + gsup * 512, 512)
        for s in range(4):
            fm_simple(wb, s * 128, C_G + gsup * 512 + s * 128, AF.Sigmoid, gT, gsup * 512 + s * 128)

    P.release(mark1b)
    tri_d = {n: din(n, [128, 128]) for n in ("triInc", "triDec", "triSgt", "triSlt")}
    flags_d = din("flags", [64, 2])
    Og = dout("Og", [2, 512, NTOK], F32)
    qBT = dout("qBT", [2, 256, 2048], BF16)
    Sfin = dout("Sfin", [2, 64, 512], F32)
    tri = {}
    for n in tri_d:
        tri[n] = P.sbuf("c_" + n, [128, 128], F32)
        P.dma_copy("sp", tri[n][:], tri_d[n].t[:, :], writes=[tri[n].r])
    flags = P.sbuf("flags", [64, 2], F32)
    P.dma_copy("sp", flags[:], flags_d.t[:, :], writes=[flags.r])
    Sst = [P.sbuf(f"S{d}", [64, 4, 128], F32) for d in range(2)]
    Sbf = [P.sbuf(f"Sbf{d}", [64, 4, 128], BF16) for d in range(2)]
    Dcum = [P.sbuf(f"Dcum{d}", [64, 4], F32) for d in range(2)]
    rq = [Ring([P.sbuf(f"gq{d}{i}", [64, 4, 128], F32) for i in range(2)]) for d in range(2)]
    rk = [Ring([P.sbuf(f"gk{d}{i}", [64, 4, 128], F32) for i in range(2)]) for d in range(2)]
    rkt = [Ring([P.sbuf(f"gkt{d}{i}", [128, 256], F32) for i in range(2)]) for d in range(2)]
    rv = [Ring([P.sbuf(f"gv{d}{i}", [128, 512], BF16) for i in range(2)]) for d in range(2)]
    rG = [Ring([P.sbuf(f"gG{d}{i}", [128, 256], F32) for i in range(2)]) for d in range(2)]
    reb = [Ring([P.sbuf(f"geb{d}{i}", [64, 4, 128], F32) for i in range(2)]) for d in range(2)]
    rei = [Ring([P.sbuf(f"gei{d}{i}", [64, 4, 128], F32) for i in range(2)]) for d in range(2)]
    rqb = [Ring([P.sbuf(f"gqb{d}{i}", [64, 4, 128], BF16) for i in range(2)]) for d in range(2)]
    rkb = [Ring([P.sbuf(f"gkb{d}{i}", [64, 4, 128], BF16) for i in range(2)]) for d in range(2)]
    rkr = [Ring([P.sbuf(f"gkr{d}{i}", [128, 256], F32) for i in range(2)]) for d in range(2)]
    rke = [Ring([P.sbuf(f"gke{d}{i}", [128, 256], BF16) for i in range(2)]) for d in range(2)]
    rsc = [Ring([P.sbuf(f"gsc{d}{i}", [128, 4, 128], BF16) for i in range(2)]) for d in range(2)]
    rO = [Ring([P.sbuf(f"gO{d}{i}", [128, 4, 128], F32) for i in range(2)]) for d in range(2)]
    rqB = [Ring([P.sbuf(f"gqB{d}{i}", [64, 4, 128], BF16) for i in range(2)]) for d in range(2)]

    def gla_step(d, i, lat):
        cumM = tri["triInc"] if d == 0 else tri["triDec"]
        remM = tri["triSgt"] if d == 0 else tri["triSlt"]
        endc = 127 if d == 0 else 0
        t0 = i * 128
        S, Sb, Dc = Sst[d], Sbf[d], Dcum[d]
        q_t, k_t, kt_t, v_t, G_t = rq[d].get(), rk[d].get(), rkt[d].get(), rv[d].get(), rG[d].get()
        P.dma_copy("sp", q_t[:], bqT.t[:, t0:t0 + 128].rearrange("(h p) t -> p h t", p=64), reads=[bqT.r], writes=[q_t.r])
        P.dma_copy("sp", k_t[:], bkT.t[:, t0:t0 + 128].rearrange("(h p) t -> p h t", p=64), reads=[bkT.r], writes=[k_t.r])
        P.dma_copy("sp", kt_t[:], bk.t[t0:t0 + 128, :], reads=[bk.r], writes=[kt_t.r])
        P.dma_copy("sp", v_t[:], bv.t[t0:t0 + 128, :], reads=[bv.r], writes=[v_t.r])
        P.dma_copy("sp", G_t[:], Gd.t[t0:t0 + 128, d, :], reads=[Gd.r], writes=[G_t.r])
        cps = banks.get()
        for h in range(4):
            MM(cps[0:64, h * 128:(h + 1) * 128], G_t[:, h * 64:(h + 1) * 64], cumM[:], True, True, [G_t.r, cumM.r], [cps.r])
        eb, ei = reb[d].get(), rei[d].get()
        ACT(eb[:].rearrange("p a t -> p (a t)"), cps[0:64, :], AF.Exp, [cps.r], [eb.r], scale=-1.0 / 16)
        ACT(ei[:].rearrange("p a t -> p (a t)"), cps[0:64, :], AF.Exp, [cps.r], [ei.r], scale=1.0 / 16)
        qb, kb = rqb[d].get(), rkb[d].get()
        STT("dve", qb[:], q_t[:], 0.125, eb[:], ALU.mult, ALU.mult, [q_t.r, eb.r], [qb.r])
        TT(PENG, kb[:], k_t[:], ei[:], ALU.mult, [k_t.r, ei.r], [kb.r])
        rps = banks.get()
        MM(rps[:, 0:256], remM[:], G_t[:], True, True, [remM.r, G_t.r], [rps.r])
        kr = rkr[d].get()
        ACT(kr[:], rps[:, 0:256], AF.Exp, [rps.r], [kr.r], scale=-1.0 / 16)
        ke = rke[d].get()
        TT(PENG, ke[:], kt_t[:], kr[:], ALU.mult, [kt_t.r, kr.r], [ke.r])
        sps = banks.get()
        for h in range(4):
            MM(sps[:, h * 128:(h + 1) * 128], kb[:, h, :], qb[:, h, :], True, True, [kb.r, qb.r], [sps.r])
        sc = rsc[d].get()
        TT("dve", sc[:], sps[:].rearrange("p (h t) -> p h t", h=4),
           cumM[:].unsqueeze(1).to_broadcast([128, 4, 128]), ALU.mult, [sps.r, cumM.r], [sc.r])
        ops_ = banks.get()
        for h in range(4):
            MM(ops_[:, h * 128:(h + 1) * 128], Sb[:, h, :], qb[:, h, :], True, False, [Sb.r, qb.r], [ops_.r])
            MM(ops_[:, h * 128:(h + 1) * 128], v_t[:, h * 128:(h + 1) * 128], sc[:, h, :],
               False, True, [v_t.r, sc.r], [ops_.r])
        Ot = rO[d].get()
        ACT(Ot[:].rearrange("p h t -> p (h t)"), ops_[:, :], AF.Identity, [ops_.r], [Ot.r])
        P.dma_copy("sp", Og.t[d, :, t0:t0 + 128].rearrange("(h p) t -> p h t", p=128), Ot[:], reads=[Ot.r], writes=[Og.r])
        if lat:
            qB = rqB[d].get()
            TT(PENG, qB[:], qb[:], Dc[:].unsqueeze(2).to_broadcast([64, 4, 128]), ALU.mult, [qb.r, Dc.r], [qB.r])
            P.dma_copy("sp", qBT.t[d, :, t0:t0 + 128].rearrange("(h p) t -> p h t", p=64), qB[:], reads=[qB.r], writes=[qBT.r])
            TT("dve", Dc[:], Dc[:], eb[:, :, endc], ALU.mult, [Dc.r, eb.r], [Dc.r])
        ups = banks.get()
        for h in range(4):
            MM(ups[0:64, h * 128:(h + 1) * 128], ke[:, h * 64:(h + 1) * 64], v_t[:, h * 128:(h + 1) * 128], True, True,
               [ke.r, v_t.r], [ups.r])
        TT("dve", S[:], S[:], eb[:, :, endc:endc + 1].to_broadcast([64, 4, 128]), ALU.mult, [S.r, eb.r], [S.r])
        TT("dve", S[:], S[:], ups[0:64, :].rearrange("p (h e) -> p h e", h=4), ALU.add, [S.r, ups.r], [S.r])
        CP(PENG, Sb[:], S[:], [S.r], [Sb.r])

    for d in range(2):
        MS("dve", Sst[d][:], 0.0, [Sst[d].r])
        MS(PENG, Sbf[d][:], 0.0, [Sbf[d].r])
        MS("dve", Dcum[d][:], 1.0, [Dcum[d].r])
    for j in range(2 if GM > 0 else 0):
        gla_step(0, 16 + j, False)
        gla_step(1, 17 - j, False)
    for d in range(2):
        TS("dve", Sst[d][:], Sst[d][:], flags[:, d:d + 1], None, ALU.mult, None, [Sst[d].r, flags.r], [Sst[d].r])
        CP(PENG, Sbf[d][:], Sst[d][:], [Sst[d].r], [Sbf[d].r])
    for j in range(16 if GM > 0 else 0):
        gla_step(0, j, True)
        gla_step(1, 15 - j, True)
    for d in range(2):
        P.dma_copy("sp", Sfin.t[d, :, :], Sst[d][:].rearrange("p h e -> p (h e)"), reads=[Sst[d].r], writes=[Sfin.r])

    P.release(m_stage)


def emit_l2(P, E, L):
    nc = P.nc
    H = mk_helpers(P)
    MM, TR, ACT, TT, TS, STT, CP, MS = H.MM, H.TR, H.ACT, H.TT, H.TS, H.STT, H.CP, H.MS
    din = lambda name, shape, dt=F32: E.din(name, shape, dt, L)
    dout = lambda name, shape, dt: E.dout(name, shape, dt, L)
    m_stage = P.mark()

    x_in = din("x_in", [NTOK, 1024])
    modD = din("modD", [2, 6144])
    aqT = din("aqT", [512, NTOK], BF16)
    akT = din("akT", [128, NTOK], BF16)
    av = din("av", [NTOK, 128], BF16)
    g_ak = din("g_ak", [256, 2048], BF16)
    g_av = din("g_av", [4096, 128], BF16)
    g_ck = din("g_ck", [1024, 768], BF16)
    g_cv = din("g_cv", [1536, 512], BF16)
    g_S = din("g_S", [256, 512])
    cqT = din("cqT", [512, NTOK], BF16)
    ckT = din("ckT", [512, NTOK], BF16)
    cv = din("cv", [NTOK, 512], BF16)
    tabNA = din("tabNA", [5, 7, 2, 128, 512], BF16)
    Og = din("Og", [2, 512, NTOK])
    qBT = din("qBT", [2, 256, 2048], BF16)
    fl1m = din("fl1m", [64, 2])
    rT = din("rT", [512, NTOK], BF16)
    gT = din("gT", [3072, NTOK], BF16)
    gn_d = din("gla_norm", [128, 1])
    wba_d = din("w_br_attn", [512, 1024])
    wbg_d = din("w_br_gla", [512, 1024])
    wbn_d = din("w_br_na", [512, 1024])
    wout_d = din("w_out", [1024, 1024])
    ln1g_d = din("ln1_g", [1, 1024])
    ln1b_d = din("ln1_b", [1, 1024])
    ones_d = din("ones128", [128, 128])
    x1_o = dout("x1", [NTOK, 1024], F32)
    h2_o = dout("h2", [NTOK, 1024], F32)

    bankS = Banks(P, 0, E.fb[0:3])
    bankA = Ring(E.fb[3:5])
    bankM = Ring(E.fb[5:7])

    oaD = dout("oaD", [512, NTOK], BF16)
    ocD = dout("ocD", [512, NTOK], BF16)
    obD = dout("obD", [512, NTOK], BF16)
    stgr = Ring([P.sbuf(f"ostg{i}", [128, 512], BF16) for i in range(3)])
    epsc = P.sbuf("epsc", [128, 2], F32)
    MS("dve", epsc[:, 0:1], LN_EPS, [epsc.r])
    MS("dve", epsc[:, 1:2], RMS_EPS, [epsc.r])
    rdr = Ring([P.sbuf(f"rd{i}", [128, 512], F32) for i in range(2)])
    rd0r = Ring([P.sbuf(f"rd0{i}", [64, 512], F32) for i in range(2)])
    ptr = Ring([P.sbuf(f"pt{i}", [128, 512], BF16) for i in range(8)])

    def blk_of(t0):
        return min(t0 // 512, 4)

    def normalize(po, n, dest, dres, view=None):
        rd = rdr.get()
        P.op("dve", lambda e: e.reciprocal(out=rd[64:128, 0:n], in_=po[64:128, 0:n]), [po.r], [rd.r])
        rd0 = rd0r.get()
        P.dma_copy("sp", rd0[0:64, 0:n], rd[64:128, 0:n], reads=[rd.r], writes=[rd0.r])
        a, b_ = po[0:64, 0:n], rd0[0:64, 0:n]
        if view is not None:
            a, b_ = view(a), view(b_)
        TT("dve", dest, a, b_, ALU.mult, [po.r, rd0.r], [dres])

    markA = P.mark()
    bankSA = Banks(P, 0, list(bankS.b) + list(bankM.bufs))
    KT = [P.sbuf(f"KT{g}", [64, NKT * 128], BF16) for g in range(2)]
    V1 = [P.sbuf(f"V1{g}", [128, NKT, 128], BF16) for g in range(2)]
    for g in range(2):
        for r_ in range(2):
            P.dma_copy("sp", KT[g][:, r_ * 2048:(r_ + 1) * 2048], g_ak.t[r_ * 128 + g * 64:r_ * 128 + (g + 1) * 64, :],
                       reads=[g_ak.r], writes=[KT[g].r])
        P.dma_copy("sp", KT[g][:, 4096:4352], akT.t[g * 64:(g + 1) * 64, 2048:2304], reads=[akT.r], writes=[KT[g].r])
        MS("pool", V1[g][:, :, 64:128], 1.0, [V1[g].r])
        P.dma_copy("sp", V1[g][:, 0:32, 0:64], g_av.t[:, g * 64:(g + 1) * 64].rearrange("(kt p) d -> p kt d", p=128),
                   reads=[g_av.r], writes=[V1[g].r])
        P.dma_copy("sp", V1[g][:, 32:34, 0:64], av.t[2048:2304, g * 64:(g + 1) * 64].rearrange("(kt p) d -> p kt d", p=128),
                   reads=[av.r], writes=[V1[g].r])
    qbr = Ring([P.sbuf(f"qblk{i}", [64, 8, 512], BF16) for i in range(2)])
    for bi, (t0, n) in enumerate(BLKS if 'A' not in os.environ.get('L2SKIP', '') else []):
        qb = qbr.get()
        P.dma_copy("sp", qb[:, :, 0:n], aqT.t[:, t0:t0 + n].rearrange("(h p) t -> p h t", p=64), writes=[qb.r])
        kts = list(range(NKT)) if bi < 4 else [32, 33]
        for h in range(8):
            g = h // 4
            po = bankA.get()
            LOOK = 4
            pend_ = []

            def issue_qk(kt):
                ps = bankSA.get()
                MM(ps[:, 0:n], KT[g][:, kt * 128:(kt + 1) * 128], qb[:, h, 0:n], True, True, [KT[g].r, qb.r], [ps.r])
                pt = ptr.get()
                ACT(pt[:, 0:n], ps[:, 0:n], AF.Exp, [ps.r], [pt.r], scale=0.125)
                pend_.append(pt)
            for kt in kts[:LOOK]:
                issue_qk(kt)
            for idx, kt in enumerate(kts):
                if idx + LOOK < len(kts):
                    issue_qk(kts[idx + LOOK])
                pt = pend_.pop(0)
                MM(po[:, 0:n], V1[g][:, kt, :], pt[:, 0:n], idx == 0, idx == len(kts) - 1, [V1[g].r, pt.r], [po.r])
            stg = stgr.get()
            normalize(po, n, stg[0:64, 0:n], stg.r)
            P.dma_copy("sp", oaD.t[h * 64:(h + 1) * 64, t0:t0 + n], stg[0:64, 0:n], reads=[stg.r], writes=[oaD.r])
    P.release(markA)

    markN = P.mark()
    ones128 = P.sbuf("ones128", [128, 128], BF16)
    P.dma_copy("pool", ones128[:], ones_d.t[:, :], writes=[ones128.r])
    gn = P.sbuf("gn", [128, 1], F32)
    P.dma_copy("sp", gn[:], gn_d.t[:, :], writes=[gn.r])
    f1 = P.sbuf("f1", [64, 2], F32)
    P.dma_copy("sp", f1[:], fl1m.t[:, :], writes=[f1.r])
    Sin = []
    for d in range(2):
        sp_ = P.sbuf(f"Spart{d}", [64, 512], F32)
        P.dma_copy("sp", sp_[:], g_S.t[d * 128 + d * 64:d * 128 + (d + 1) * 64, :], reads=[g_S.r], writes=[sp_.r])
        sb = P.sbuf(f"Sin{d}", [64, 4, 128], BF16)
        TS("dve", sb[:].rearrange("p h e -> p (h e)"), sp_[:], f1[:, d:d + 1], None, ALU.mult, None, [sp_.r, f1.r], [sb.r])
        Sin.append(sb)
    qBr = [Ring([P.sbuf(f"qB{d}{i}", [64, 4, 512], BF16) for i in range(2)]) for d in range(2)]
    ogr = [Ring([P.sbuf(f"og{d}{i}", [128, 512], F32) for i in range(2)]) for d in range(2)]
    osr = Ring([P.sbuf(f"os{i}", [128, 512], F32) for i in range(2)])
    sqr = Ring([P.sbuf(f"sq{i}", [128, 512], BF16) for i in range(2)])
    rsr = Ring([P.sbuf(f"rs{i}", [128, 512], F32) for i in range(2)])
    rtr = Ring([P.sbuf(f"rt{i}", [128, 512], BF16) for i in range(2)])
    t3r = Ring([P.sbuf(f"t3{i}", [128, 512], F32) for i in range(2)])

    def gla_units():
        for bi, (t0, n) in enumerate(BLKS):
            lat = bi < 4
            if lat:
                qB = [qBr[d].get() for d in range(2)]
                for d in range(2):
                    P.dma_copy("sp", qB[d][:, :, 0:n], qBT.t[d, :, t0:t0 + n].rearrange("(h p) t -> p h t", p=64),
                               writes=[qB[d].r])
            for h in range(4):
                og = [ogr[d].get() for d in range(2)]
                for d in range(2):
                    P.dma_copy("sp", og[d][:, 0:n], Og.t[d, h * 128:(h + 1) * 128, t0:t0 + n], writes=[og[d].r])
                osum = osr.get()
                TT("pool", osum[:, 0:n], og[0][:, 0:n], og[1][:, 0:n], ALU.add, [og[0].r, og[1].r], [osum.r])
                if lat:
                    pc = bankM.get()
                    MM(pc[:, 0:n], Sin[0][:, h, :], qB[0][:, h, 0:n], True, False, [Sin[0].r, qB[0].r], [pc.r])
                    MM(pc[:, 0:n], Sin[1][:, h, :], qB[1][:, h, 0:n], False, True, [Sin[1].r, qB[1].r], [pc.r])
                    TT("dve", osum[:, 0:n], osum[:, 0:n], pc[:, 0:n], ALU.add, [osum.r, pc.r], [osum.r])
                sq = sqr.get()
                ACT(sq[:, 0:n], osum[:, 0:n], AF.Square, [osum.r], [sq.r])
                ss = bankM.get()
                MM(ss[:, 0:n], ones128[:], sq[:, 0:n], True, True, [ones128.r, sq.r], [ss.r])
                rs = rsr.get()
                ACT(rs[:, 0:n], ss[:, 0:n], AF.Sqrt, [ss.r, epsc.r], [rs.r], bias=epsc[:, 1:2], scale=1.0 / 128)
                P.op("dve", lambda e, rs=rs, n=n: e.reciprocal(out=rs[:, 0:n], in_=rs[:, 0:n]), [rs.r], [rs.r])
                rt = rtr.get()
                P.dma_copy("sp", rt[:, 0:n], rT.t[h * 128:(h + 1) * 128, t0:t0 + n], writes=[rt.r])
                t3 = t3r.get()
                STT("dve", t3[:, 0:n], osum[:, 0:n], gn[:, 0:1], rs[:, 0:n], ALU.mult, ALU.mult, [osum.r, gn.r, rs.r], [t3.r])
                stg = stgr.get()
                TT("pool", stg[:, 0:n], t3[:, 0:n], rt[:, 0:n], ALU.mult, [t3.r, rt.r], [stg.r])
                P.dma_copy("sp", obD.t[h * 128:(h + 1) * 128, t0:t0 + n], stg[:, 0:n], reads=[stg.r], writes=[obD.r])
                yield

    gu = gla_units()
    KTc = P.sbuf("KTc", [64, 8, 256], BF16)
    V1c = P.sbuf("V1c", [128, 2, 8, 128], BF16)
    P.dma_copy("sp", KTc[:], ckT.t[:, 2048:2304].rearrange("(h p) t -> p h t", p=64), writes=[KTc.r])
    MS("pool", V1c[:, :, :, 64:128], 1.0, [V1c.r])
    for kt in range(2):
        P.dma_copy("sp", V1c[:, kt, :, 0:64],
                   cv.t[2048 + kt * 128:2048 + (kt + 1) * 128, :].rearrange("p (h d) -> p h d", d=64), writes=[V1c.r])
    qtr = Ring([P.sbuf(f"nq{i}", [64, 8, 128], BF16) for i in range(2)])
    kwin = P.sbuf("kwin", [64, 8, 8 * 128], BF16, nres=8)
    vwin = P.sbuf("vwin", [128, 8, 8, 128], BF16, nres=8)
    MS("pool", vwin[:, :, :, 64:128], 1.0, vwin.rs)
    tabI = P.sbuf("tabI", [128, 7, 2, 512], BF16)
    for kt in range(7):
        for grp in range(2):
            P.dma_copy("sp", tabI[:, kt, grp, :], tabNA.t[4, kt, grp], writes=[tabI.r])
    nloaded = [-1]

    def load_win_tile(t_):
        sl = t_ % 8
        if t_ < 3:
            ksrc, kres = g_ck.t[0:512, 384 + t_ * 128:384 + (t_ + 1) * 128], g_ck.r
            vsrc, vres = g_cv.t[384 + t_ * 128:384 + (t_ + 1) * 128, :], g_cv.r
        elif t_ < 19:
            ksrc, kres = ckT.t[:, (t_ - 3) * 128:(t_ - 2) * 128], ckT.r
            vsrc, vres = cv.t[(t_ - 3) * 128:(t_ - 2) * 128, :], cv.r
        else:
            ksrc, kres = g_ck.t[512:1024, (t_ - 19) * 128:(t_ - 18) * 128], g_ck.r
            vsrc, vres = g_cv.t[768 + (t_ - 19) * 128:768 + (t_ - 18) * 128, :], g_cv.r
        P.dma_copy("sp", kwin[:, :, sl * 128:(sl + 1) * 128], ksrc.rearrange("(h p) t -> p h t", p=64),
                   reads=[kres], writes=[kwin.rs[sl]])
        P.dma_copy("sp", vwin[:, sl, :, 0:64], vsrc.rearrange("p (h d) -> p h d", d=64), reads=[vres], writes=[vwin.rs[sl]])
    tbr = Ring([P.sbuf(f"ntb{i}", [128, 512], BF16) for i in range(3)])
    tmr = Ring([P.sbuf(f"ntm{i}", [128, 512], F32) for i in range(2)])
    nptr = Ring([P.sbuf(f"npt{i}", [128, 512], BF16) for i in range(18)])
    for j in range(18 if 'N' not in os.environ.get('L2SKIP', '') else 0):
        qt = qtr.get()
        P.dma_copy("sp", qt[:], cqT.t[:, j * 128:(j + 1) * 128].rearrange("(h p) t -> p h t", p=64), writes=[qt.r])
        if j < 16:
            while nloaded[0] < j + 6:
                nloaded[0] += 1
                load_win_tile(nloaded[0])
            kts = list(range(9))
            slot = j if j < 2 else (j - 12 if j >= 14 else 4)
        else:
            kts = [7, 8]
        ptsg = [{}, {}]
        for grp in range(2):
            pts = ptsg[grp]
            for idx, kt in enumerate(kts):
                sps = bankS.get()
                for hh in range(4):
                    h = grp * 4 + hh
                    if kt < 7:
                        lhs, rd_ = kwin[:, h, ((j + kt) % 8) * 128:((j + kt) % 8 + 1) * 128], kwin.rs[(j + kt) % 8]
                    else:
                        lhs, rd_ = KTc[:, h, (kt - 7) * 128:(kt - 6) * 128], KTc.r
                    MM(sps[:, hh * 128:(hh + 1) * 128], lhs, qt[:, h, :], True, True, [rd_, qt.r], [sps.r])
                pt = nptr.get()
                pts[kt] = pt
                if kt < 7:
                    tm = tmr.get()
                    if slot == 4:
                        STT("dve", tm[:], sps[:], 0.125, tabI[:, kt, grp, :], ALU.mult, ALU.add, [sps.r, tabI.r], [tm.r])
                    else:
                        tb = tbr.get()
                        P.dma_copy("sp", tb[:], tabNA.t[slot, kt, grp], writes=[tb.r])
                        STT("dve", tm[:], sps[:], 0.125, tb[:], ALU.mult, ALU.add, [sps.r, tb.r], [tm.r])
                    ACT(pt[:], tm[:], AF.Exp, [tm.r], [pt.r])
                else:
                    ACT(pt[:], sps[:], AF.Exp, [sps.r], [pt.r], scale=0.125)
        for grp in range(2):
            pts = ptsg[grp]
            po = bankA.get()
            for hh in range(4):
                h = grp * 4 + hh
                for idx, kt in enumerate(kts):
                    pt = pts[kt]
                    if kt < 7:
                        lhs, rd_ = vwin[:, (j + kt) % 8, h, :], vwin.rs[(j + kt) % 8]
                    else:
                        lhs, rd_ = V1c[:, kt - 7, h, :], V1c.r
                    MM(po[:, hh * 128:(hh + 1) * 128], lhs, pt[:, hh * 128:(hh + 1) * 128], idx == 0, idx == len(kts) - 1,
                       [rd_, pt.r], [po.r])
            stg = stgr.get()
            normalize(po, 512, stg[0:64, :], stg.r)
            P.dma_copy("sp", ocD.t[grp * 256:(grp + 1) * 256, j * 128:(j + 1) * 128].rearrange("(h p) t -> p h t", p=64),
                       stg[0:64, :].rearrange("p (h t) -> p h t", h=4), reads=[stg.r], writes=[ocD.r])
        next(gu, None)
    for _ in gu:
        pass
    P.release(markN)


    wba = P.sbuf("wba", [64, 8, 1024], BF16)
    wbn = P.sbuf("wbn", [64, 8, 1024], BF16)
    wbg = P.sbuf("wbg", [128, 4, 1024], BF16)
    wo = P.sbuf("wo", [128, 8, 1024], BF16)
    P.dma_copy("pool", wba[:], wba_d.t.rearrange("(h p) f -> p h f", p=64), writes=[wba.r])
    P.dma_copy("pool", wbn[:], wbn_d.t.rearrange("(h p) f -> p h f", p=64), writes=[wbn.r])
    P.dma_copy("pool", wbg[:], wbg_d.t.rearrange("(h p) f -> p h f", p=128), writes=[wbg.r])
    P.dma_copy("pool", wo[:], wout_d.t.rearrange("(k p) f -> p k f", p=128), writes=[wo.r])
    bc = {}
    for nm, m in (("g1", 2), ("sh2", 3), ("sc2", 4)):
        for j in range(2):
            t = P.sbuf(f"bc_{nm}{j}", [128, 1024], F32)
            P.dma_copy("sp", t[:], modD.t[j:j + 1, m * 1024:(m + 1) * 1024].partition_broadcast(128), writes=[t.r])
            bc[(nm, j)] = t
    for j in range(2):
        TS("pool", bc[("sc2", j)][:], bc[("sc2", j)][:], 1.0, None, ALU.add, None, [bc[("sc2", j)].r], [bc[("sc2", j)].r])
    lg = P.sbuf("bc_ln1g", [128, 1024], F32)
    lb = P.sbuf("bc_ln1b", [128, 1024], F32)
    P.dma_copy("sp", lg[:], ln1g_d.t[0:1, :].partition_broadcast(128), writes=[lg.r])
    P.dma_copy("sp", lb[:], ln1b_d.t[0:1, :].partition_broadcast(128), writes=[lb.r])
    ymT = Ring([P.sbuf(f"ymT{i}", [128, 8, 512], BF16) for i in range(1)])
    ggr = Ring([P.sbuf(f"gg{i}", [128, 3, 512], BF16) for i in range(3)])
    tar = Ring([P.sbuf(f"ta{i}", [128, 512], F32) for i in range(2)])
    tbr2 = Ring([P.sbuf(f"tb2{i}", [128, 512], F32) for i in range(2)])
    xr = Ring([P.sbuf(f"xr{i}", [128, 1024], F32) for i in range(2)])
    ur = Ring([P.sbuf(f"ur{i}", [128, 1024], F32) for i in range(2)])
    x1r = Ring([P.sbuf(f"x1r{i}", [128, 1024], F32) for i in range(2)])
    h2r = Ring([P.sbuf(f"h2r{i}", [128, 1024], F32) for i in range(2)])
    str_ = Ring([P.sbuf(f"st{i}", [128, 2, 6], F32) for i in range(2)])
    mvr = Ring([P.sbuf(f"mv{i}", [128, 2], F32) for i in range(2)])

    def ln_stats(src):
        st = str_.get()
        for c in range(2):
            P.op("dve", lambda e, st=st, c=c: e.bn_stats(out=st[:, c, :], in_=src[:, c * 512:(c + 1) * 512]),
                 [src.r], [st.r])
        mv = mvr.get()
        P.op("dve", lambda e, st=st, mv=mv: e.bn_aggr(out=mv[:], in_=st[:].rearrange("p a b -> p (a b)")),
             [st.r], [mv.r])
        ACT(mv[:, 1:2], mv[:, 1:2], AF.Sqrt, [mv.r, epsc.r], [mv.r], bias=epsc[:, 0:1])
        P.op("dve", lambda e, mv=mv: e.reciprocal(out=mv[:, 1:2], in_=mv[:, 1:2]), [mv.r], [mv.r])
        return mv

    oabr = Ring([P.sbuf(f"oab{i}", [64, 8, 512], BF16) for i in range(2)])
    ocbr = Ring([P.sbuf(f"ocb{i}", [64, 8, 512], BF16) for i in range(2)])
    obbr = Ring([P.sbuf(f"obb{i}", [128, 4, 512], BF16) for i in range(2)])
    for bi, (t0, n) in enumerate(BLKS):
        ym = ymT.get()
        oab, ocb, obb = oabr.get(), ocbr.get(), obbr.get()
        P.dma_copy("sp", oab[:, :, 0:n], oaD.t[:, t0:t0 + n].rearrange("(h p) t -> p h t", p=64), reads=[oaD.r], writes=[oab.r])
        P.dma_copy("sp", ocb[:, :, 0:n], ocD.t[:, t0:t0 + n].rearrange("(h p) t -> p h t", p=64), reads=[ocD.r], writes=[ocb.r])
        P.dma_copy("sp", obb[:, :, 0:n], obD.t[:, t0:t0 + n].rearrange("(h p) t -> p h t", p=128), reads=[obD.r], writes=[obb.r])
        for fc in range(8):
            gg = ggr.get()
            P.dma_copy("sp", gg[:, :, 0:n],
                       gT.t[:, t0:t0 + n].rearrange("(b r) t -> r b t", b=3)[fc * 128:(fc + 1) * 128],
                       writes=[gg.r])
            fs = slice(fc * 128, (fc + 1) * 128)
            pa = bankS.get()
            for h in range(8):
                MM(pa[:, 0:n], wba[:, h, fs], oab[:, h, 0:n], h == 0, h == 7, [wba.r, oab.r], [pa.r])
            pb = bankS.get()
            for h in range(4):
                MM(pb[:, 0:n], wbg[:, h, fs], obb[:, h, 0:n], h == 0, h == 3, [wbg.r, obb.r], [pb.r])
            pcn = bankS.get()
            for h in range(8):
                MM(pcn[:, 0:n], wbn[:, h, fs], ocb[:, h, 0:n], h == 0, h == 7, [wbn.r, ocb.r], [pcn.r])
            ta, tb = tar.get(), tbr2.get()
            TT("dve", ta[:, 0:n], pa[:, 0:n], gg[:, 0, 0:n], ALU.mult, [pa.r, gg.r], [ta.r])
            TT("dve", tb[:, 0:n], pb[:, 0:n], gg[:, 1, 0:n], ALU.mult, [pb.r, gg.r], [tb.r])
            TT("pool", ta[:, 0:n], ta[:, 0:n], tb[:, 0:n], ALU.add, [ta.r, tb.r], [ta.r])
            TT("dve", tb[:, 0:n], pcn[:, 0:n], gg[:, 2, 0:n], ALU.mult, [pcn.r, gg.r], [tb.r])
            TT("pool", ym[:, fc, 0:n], ta[:, 0:n], tb[:, 0:n], ALU.add, [ta.r, tb.r], [ym.r])
        for ti in range(n // 128):
            i = t0 // 128 + ti
            j = 0 if i < 16 else 1
            xt = xr.get()
            P.dma_copy("sp", xt[:], x_in.t[i * 128:(i + 1) * 128, :], writes=[xt.r])
            u = ur.get()
            for hf in range(2):
                py = bankM.get()
                for fc in range(8):
                    MM(py[:, :], ym[:, fc, ti * 128:(ti + 1) * 128], wo[:, fc, hf * 512:(hf + 1) * 512], fc == 0, fc == 7,
                       [ym.r, wo.r], [py.r])
                TT("dve", u[:, hf * 512:(hf + 1) * 512], py[:, :], bc[("g1", j)][:, hf * 512:(hf + 1) * 512], ALU.mult,
                   [py.r, bc[("g1", j)].r], [u.r])
            STT("dve", u[:], xt[:], ALPHA, u[:], ALU.mult, ALU.add, [xt.r, u.r], [u.r])
            mv = ln_stats(u)
            x1 = x1r.get()
            TS("dve", x1[:], u[:], mv[:, 0:1], mv[:, 1:2], ALU.subtract, ALU.mult, [u.r, mv.r], [x1.r])
            TT("pool", x1[:], x1[:], lg[:], ALU.mult, [x1.r, lg.r], [x1.r])
            TT("pool", x1[:], x1[:], lb[:], ALU.add, [x1.r, lb.r], [x1.r])
            P.dma_copy("sp", x1_o.t[i * 128:(i + 1) * 128, :], x1[:], reads=[x1.r], writes=[x1_o.r])
            mv2 = ln_stats(x1)
            h2 = h2r.get()
            TS("dve", h2[:], x1[:], mv2[:, 0:1], mv2[:, 1:2], ALU.subtract, ALU.mult, [x1.r, mv2.r], [h2.r])
            TT("pool", h2[:], h2[:], bc[("sc2", j)][:], ALU.mult, [h2.r, bc[("sc2", j)].r], [h2.r])
            TT("pool", h2[:], h2[:], bc[("sh2", j)][:], ALU.add, [h2.r, bc[("sh2", j)].r], [h2.r])
            P.dma_copy("sp", h2_o.t[i * 128:(i + 1) * 128, :], h2[:], reads=[h2.r], writes=[h2_o.r])

    P.release(m_stage)


def emit_l3(P, E, L):
    nc = P.nc
    H = mk_helpers(P)
    MM, TR, ACT, TT, TS, STT, CP, MS = H.MM, H.TR, H.ACT, H.TT, H.TS, H.STT, H.CP, H.MS
    din = lambda name, shape, dt=F32: E.din(name, shape, dt, L)
    dout = lambda name, shape, dt: E.dout(name, shape, dt, L)
    m_stage = P.mark()

    x1_d = din("x1", [NTOK, 1024])
    h2_d = din("h2", [NTOK, 1024])
    modD = din("modD", [2, 6144])
    wr_d = din("w_r", [1024, 36])
    br_d = din("b_r", [1, 36])
    w1_d = din("moe_w1", [32 * 128, 4096])
    w3_d = din("moe_w3", [32 * 128, 4096])
    w2_d = din("moe_w2", [32 * 128, 4096])
    ln2g_d = din("ln2_g", [1, 1024])
    ln2b_d = din("ln2_b", [1, 1024])
    id32_d = din("ident", [128, 128])
    tris_d = din("triSlt", [128, 128])
    ones_d = din("ones128", [128, 128])
    thr_d = din("thr18", [128, 32, 9])
    thrj_d = din("thrj", [128, NBLK, 32])
    tokidx_d = din("tokidx", [128, NT], I32)
    wbase_d = din("wbase", [128, 8])
    x2_o = dout("x2", [NTOK, 1024], F32)
    h2b = dout("h2b", [NTOK + 128, 1024], BF16)
    tokslot = dout("tokslot", [NSLOT, 1], I32)
    yslot = dout("yslot", [NSLOT, 1024], F32)
    be_o = dout("be_o", [1, NBLK], I32)
    pos_o = dout("pos_o", [128, NT, 2], I32)
    gate_o = dout("gate_o", [128, NT, 2], F32)

    banks = Banks(P, 0, E.fb)
    pTb = E.bb

    id32 = P.sbuf("id32", [128, 128], F32)
    P.dma_copy("sp", id32[:], id32_d.t[:, :], writes=[id32.r])
    idb = P.sbuf("idb", [128, 128], BF16)
    P.dma_copy("pool", idb[:], id32_d.t[:, :], writes=[idb.r])
    tris = P.sbuf("tris", [128, 128], F32)
    P.dma_copy("sp", tris[:], tris_d.t[:, :], writes=[tris.r])
    ones = P.sbuf("ones", [128, 128], F32)
    P.dma_copy("sp", ones[:], ones_d.t[:, :], writes=[ones.r])
    thr = P.sbuf("thr", [128, 32, 9], F32)
    P.dma_copy("sp", thr[:], thr_d.t[:, :, :], writes=[thr.r])
    thrj = P.sbuf("thrj", [128, NBLK, 32], F32)
    P.dma_copy("sp", thrj[:], thrj_d.t[:, :, :], writes=[thrj.r])
    tokidx = P.sbuf("tokidx", [128, NT], I32)
    P.dma_copy("sp", tokidx[:], tokidx_d.t[:, :], writes=[tokidx.r])
    wr = P.sbuf("wr", [128, 8, 36], F32)
    P.dma_copy("sp", wr[:], wr_d.t.rearrange("(k p) c -> p k c", p=128), writes=[wr.r])
    br_bc = P.sbuf("br_bc", [128, 36], F32)
    P.dma_copy("sp", br_bc[:], br_d.t[0:1, :].partition_broadcast(128), writes=[br_bc.r])
    epsc = P.sbuf("epsc", [128, 1], F32)
    MS("dve", epsc[:], LN_EPS, [epsc.r])
    dum = P.sbuf("dum", [128, NSLOT // 128], I32)
    MS("pool", dum[:], NTOK, [dum.r])
    P.dma_copy("sp", tokslot.t.rearrange("(p j) o -> p (j o)", p=128), dum[:], reads=[dum.r], writes=[tokslot.r])
    zt = P.sbuf("zt", [128, 1024], BF16)
    MS("pool", zt[:], 0.0, [zt.r])
    P.dma_copy("sp", h2b.t[NTOK:NTOK + 128, :], zt[:], reads=[zt.r], writes=[h2b.r])

    oh1a = P.sbuf("oh1a", [128, NT, 32], F32)
    oh2a = P.sbuf("oh2a", [128, NT, 32], F32)
    Aall = P.sbuf("Aall", [128, NT, 32], F32)
    gates = P.sbuf("gates", [128, NT, 2], F32)
    posI = P.sbuf("posI", [128, NT, 2], I32)
    beI = P.sbuf("beI", [1, NBLK], I32)
    widx = P.sbuf("widx", [128, NBLK], I32)
    wbase = P.sbuf("wbase", [128, 8], F32)
    P.dma_copy("sp", wbase[:], wbase_d.t[:, :], writes=[wbase.r])
    carry = P.sbuf("carry", [128, 32], F32)
    MS("dve", carry[:], 0.0, [carry.r])
    excl = P.sbuf("excl", [128, NT, 32], F32)

    markR = P.mark()
    h2r = Ring([P.sbuf(f"h2t{i}", [128, 1024], F32) for i in range(2)])
    hTr = Ring([P.sbuf(f"h2T{i}", [128, 8, 128], F32) for i in range(2)])
    lgr = Ring([P.sbuf(f"lg{i}", [128, 36], F32) for i in range(2)])
    sm = Ring([P.sbuf(f"sm{i}", [128, 16], F32) for i in range(2)])
    elr = Ring([P.sbuf(f"elm{i}", [128, 32], F32) for i in range(2)])
    el2r = Ring([P.sbuf(f"elm2{i}", [128, 32], F32) for i in range(2)])
    for i in range(NT):
        ht = h2r.get()
        P.dma_copy("sp", ht[:], h2_d.t[i * 128:(i + 1) * 128, :], writes=[ht.r])
        P.dma_copy("pool", h2b.t[i * 128:(i + 1) * 128, :], ht[:], reads=[ht.r], writes=[h2b.r])
        hT = hTr.get()
        for half in range(2):
            pt = banks.get()
            for kk in range(4):
                k = half * 4 + kk
                TR(pt[:, kk * 128:(kk + 1) * 128], ht[:, k * 128:(k + 1) * 128], id32[:], [ht.r, id32.r], [pt.r])
            if half == 0:
                ACT(hT[:, 0:4, :].rearrange("p k t -> p (k t)"), pt[:, :], AF.Identity, [pt.r], [hT.r])
            else:
                CP("dve", hT[:, 4:8, :].rearrange("p k t -> p (k t)"), pt[:, :], [pt.r], [hT.r])
        pl = banks.get()
        for k in range(8):
            MM(pl[:, 0:36], hT[:, k, :], wr[:, k, :], k == 0, k == 7, [hT.r, wr.r], [pl.r])
        lg = lgr.get()
        TT("dve", lg[:], pl[:, 0:36], br_bc[:], ALU.add, [pl.r, br_bc.r], [lg.r])
        s = sm.get()
        P.op("dve", lambda e, s=s, lg=lg: e.reduce_max(out=s[:, 0:1], in_=lg[:, 0:4], axis=AX.X), [lg.r], [s.r])
        TS("dve", s[:, 1:2], s[:, 0:1], -1.0, None, ALU.mult, None, [s.r], [s.r])
        TS("dve", s[:, 8:12], lg[:, 0:4], s[:, 0:1], None, ALU.is_equal, None, [lg.r, s.r], [s.r])
        ACT(s[:, 12:16], lg[:, 0:4], AF.Exp, [lg.r, s.r], [s.r], bias=s[:, 1:2])
        P.op("dve", lambda e, s=s: e.reduce_sum(out=s[:, 2:3], in_=s[:, 12:16], axis=AX.X), [s.r], [s.r])
        P.op("dve", lambda e, s=s: e.reciprocal(out=s[:, 3:4], in_=s[:, 2:3]), [s.r], [s.r])
        TS("dve", s[:, 12:16], s[:, 8:12], 1.0, BIG, ALU.subtract, ALU.mult, [s.r], [s.r])
        elm = elr.get()
        TT("dve", elm[:].rearrange("p (g e) -> p g e", g=4), lg[:, 4:36].rearrange("p (g e) -> p g e", g=4),
           s[:, 12:16].unsqueeze(2).to_broadcast([128, 4, 8]), ALU.add, [lg.r, s.r], [elm.r])
        P.op("dve", lambda e, s=s, elm=elm: e.reduce_max(out=s[:, 4:5], in_=elm[:], axis=AX.X), [elm.r], [s.r])
        TS("dve", oh1a[:, i, :], elm[:], s[:, 4:5], None, ALU.is_equal, None, [elm.r, s.r], [oh1a.r])
        elm2 = el2r.get()
        STT("dve", elm2[:], oh1a[:, i, :], -BIG, elm[:], ALU.mult, ALU.add, [oh1a.r, elm.r], [elm2.r])
        P.op("dve", lambda e, s=s, elm2=elm2: e.reduce_max(out=s[:, 5:6], in_=elm2[:], axis=AX.X), [elm2.r], [s.r])
        TS("dve", oh2a[:, i, :], elm2[:], s[:, 5:6], None, ALU.is_equal, None, [elm2.r, s.r], [oh2a.r])
        TT("dve", Aall[:, i, :], oh1a[:, i, :], oh2a[:, i, :], ALU.add, [oh1a.r, oh2a.r], [Aall.r])
        TT("dve", s[:, 6:7], s[:, 5:6], s[:, 4:5], ALU.subtract, [s.r], [s.r])
        ACT(s[:, 6:7], s[:, 6:7], AF.Exp, [s.r], [s.r])
        TS("dve", s[:, 6:7], s[:, 6:7], 1.0, None, ALU.add, None, [s.r], [s.r])
        P.op("dve", lambda e, s=s: e.reciprocal(out=s[:, 7:8], in_=s[:, 6:7]), [s.r], [s.r])
        TT("dve", gates[:, i, 0:1], s[:, 3:4], s[:, 7:8], ALU.mult, [s.r], [gates.r])
        TT("dve", gates[:, i, 1:2], s[:, 3:4], gates[:, i, 0:1], ALU.subtract, [s.r, gates.r], [gates.r])
        pex = banks.get()
        MM(pex[:, 0:32], tris[:], Aall[:, i, :], True, True, [tris.r, Aall.r], [pex.r])
        TT("dve", excl[:, i, :], pex[:, 0:32], carry[:], ALU.add, [pex.r, carry.r], [excl.r])
        pcs = banks.get()
        MM(pcs[:, 0:32], ones[:], Aall[:, i, :], True, True, [ones.r, Aall.r], [pcs.r])
        TT("dve", carry[:], carry[:], pcs[:, 0:32], ALU.add, [carry.r, pcs.r], [carry.r])
    cmp18 = P.sbuf("cmp18", [128, 32, 9], F32)
    TT("dve", cmp18[:], carry[:].unsqueeze(2).to_broadcast([128, 32, 9]), thr[:], ALU.is_gt, [carry.r, thr.r], [cmp18.r])
    padded = P.sbuf("padded", [128, 32], F32)
    P.op("dve", lambda e: e.reduce_sum(out=padded[:], in_=cmp18[:], axis=AX.X), [cmp18.r], [padded.r])
    TS("dve", padded[:], padded[:], float(SB), None, ALU.mult, None, [padded.r], [padded.r])
    cs = [P.sbuf(f"cs{i}", [128, 32], F32) for i in range(2)]
    CP("dve", cs[0][:], padded[:], [padded.r], [cs[0].r])
    cur = 0
    for sh in (1, 2, 4, 8, 16):
        a, b_ = cs[cur], cs[1 - cur]
        CP("dve", b_[:, 0:sh], a[:, 0:sh], [a.r], [b_.r])
        TT("dve", b_[:, sh:32], a[:, sh:32], a[:, 0:32 - sh], ALU.add, [a.r], [b_.r])
        cur = 1 - cur
    pend = cs[cur]
    pstart = P.sbuf("pstart", [128, 32], F32)
    TT("dve", pstart[:], pend[:], padded[:], ALU.subtract, [pend.r, padded.r], [pstart.r])
    posf = P.sbuf("posf", [128, NT, 2], F32)
    tmp32 = Ring([P.sbuf(f"tmp32{i}", [128, 32], F32) for i in range(2)])
    for i in range(NT):
        sb_ = tmp32.get()
        TT("dve", sb_[:], excl[:, i, :], pstart[:], ALU.add, [excl.r, pstart.r], [sb_.r])
        for k, oh in enumerate((oh1a, oh2a)):
            t2 = tmp32.get()
            TT("dve", t2[:], sb_[:], oh[:, i, :], ALU.mult, [sb_.r, oh.r], [t2.r])
            P.op("dve", lambda e, t2=t2, i=i, k=k: e.reduce_sum(out=posf[:, i, k:k + 1], in_=t2[:], axis=AX.X),
                 [t2.r], [posf.r])
    CP("dve", posI[:], posf[:], [posf.r], [posI.r])
    cmpj = P.sbuf("cmpj", [128, NBLK, 32], F32)
    TT("dve", cmpj[:], pend[:].unsqueeze(1).to_broadcast([128, NBLK, 32]), thrj[:], ALU.is_le, [pend.r, thrj.r], [cmpj.r])
    bef = P.sbuf("bef", [128, NBLK], F32)
    P.op("dve", lambda e: e.reduce_sum(out=bef[:], in_=cmpj[:], axis=AX.X), [cmpj.r], [bef.r])
    TS("dve", bef[:], bef[:], 31.0, None, ALU.min, None, [bef.r], [bef.r])
    CP("dve", beI[:], bef[0:1, :], [bef.r], [beI.r])
    used = P.sbuf("used", [128, NBLK], F32)
    TS("dve", used[:], thrj[:, :, 0], pend[:, 31:32], None, ALU.is_lt, None, [thrj.r, pend.r], [used.r])
    TS("dve", used[:], used[:], -8192.0, 8192.0, ALU.mult, ALU.add, [used.r], [used.r])
    wif = P.sbuf("wif", [128, NBLK], F32)
    TS("dve", wif[:], bef[:], 128.0, wbase[:, 0:1], ALU.mult, ALU.add, [bef.r, wbase.r], [wif.r])
    TT("dve", wif[:], wif[:], used[:], ALU.add, [wif.r, used.r], [wif.r])
    CP("dve", widx[:], wif[:], [wif.r], [widx.r])
    P.dma_copy("sp", be_o.t[:, :], beI[:], reads=[beI.r], writes=[be_o.r])
    P.dma_copy("sp", pos_o.t[:, :, :], posI[:], reads=[posI.r], writes=[pos_o.r])
    P.dma_copy("sp", gate_o.t[:, :, :], gates[:], reads=[gates.r], writes=[gate_o.r])
    for i in range(NT):
        for k in range(2):
            P.dma("pool", lambda e, i=i, k=k: e.indirect_dma_start(
                out=tokslot.t[:, :], out_offset=bass.IndirectOffsetOnAxis(ap=posI[:, i, k:k + 1], axis=0),
                in_=tokidx[:, i:i + 1], in_offset=None), reads=[posI.r, tokidx.r], writes=[tokslot.r])
    P.release(markR)

    markE = P.mark()
    w1r = Ring([P.sbuf(f"w1b{i}", [128, 8, 512], BF16) for i in range(2)])
    w3r = Ring([P.sbuf(f"w3b{i}", [128, 8, 512], BF16) for i in range(2)])
    w2r = Ring([P.sbuf(f"w2b{i}", [128, 4, 1024], BF16) for i in range(2)])
    idxr = Ring([P.sbuf(f"idx{i}", [128, 1], I32) for i in range(4)])
    xgr = Ring([P.sbuf(f"xg{i}", [128, 1024], BF16) for i in range(4)])
    xTr = Ring([P.sbuf(f"xT{i}", [128, 8, SB], BF16) for i in range(2)])
    s1r = Ring([P.sbuf(f"s1{i}", [128, 4, SB], F32) for i in range(2)])
    aTr = Ring([P.sbuf(f"aT{i}", [128, 4, SB], BF16) for i in range(2)])
    ysr = Ring([P.sbuf(f"ys{i}", [128, 1024], F32) for i in range(3)])
    bcreg = {}
    for j in range(NBLK):
        w1b, w3b, w2b = w1r.get(), w3r.get(), w2r.get()
        for (wb_, wd_) in ((w1b, w1_d), (w3b, w3_d), (w2b, w2_d)):
            def wgather(e, wb_=wb_, wd_=wd_, j=j):
                if not hasattr(P, "bnd_val"):
                    r_ = e.alloc_register("bnd")
                    e.reg_mov(r_, 32 * 128 - 1)
                    P.bnd_val = e.snap(r_)
                return e.indirect_dma_start(
                    out=wb_[:].rearrange("p a f -> p (a f)"), out_offset=None, in_=wd_.t[:, :],
                    in_offset=bass.IndirectOffsetOnAxis(ap=widx[:, j:j + 1], axis=0),
                    bounds_check=P.bnd_val, oob_is_err=False)
            P.dma("pool", wgather, reads=[widx.r], writes=[wb_.r])
        xT = xTr.get()
        for hb in range(2):
            idx = idxr.get()
            r0 = j * SB + hb * 128
            P.dma_copy("sp", idx[:], tokslot.t[r0:r0 + 128, :], reads=[tokslot.r], writes=[idx.r])
            xg = xgr.get()
            P.dma("pool", lambda e, xg=xg, idx=idx: e.indirect_dma_start(
                out=xg[:, :], out_offset=None, in_=h2b.t[:, :],
                in_offset=bass.IndirectOffsetOnAxis(ap=idx[:, 0:1], axis=0)), reads=[idx.r, h2b.r], writes=[xg.r])
            for k in range(8):
                TR(pTb[:, k, :], xg[:].rearrange("p (a i) -> p a i", i=8)[:, :, k], idb[:], [xg.r, idb.r], [pTb.r])
            ACT(xT[:, 0:4, hb * 128:(hb + 1) * 128], pTb[:, 0:4, :], AF.Identity, [pTb.r], [xT.r])
            CP("dve", xT[:, 4:8, hb * 128:(hb + 1) * 128], pTb[:, 4:8, :], [pTb.r], [xT.r])
        s1 = s1r.get()
        aT = aTr.get()
        pb3 = []
        for (wb, which) in ((w1b, 1), (w3b, 3)):
            for fp in range(2):
                pp = banks.get()
                for f2 in range(2):
                    fc = fp * 2 + f2
                    for k in range(8):
                        MM(pp[:, f2 * SB:(f2 + 1) * SB], wb[:, k, :].rearrange("p (a c) -> p a c", c=4)[:, :, fc], xT[:, k, :],
                           k == 0, k == 7, [wb.r, xT.r], [pp.r])
                if which == 1:
                    ACT(s1[:, fp * 2:fp * 2 + 2, :].rearrange("p f t -> p (f t)"), pp[:, :], AF.Silu, [pp.r], [s1.r])
                else:
                    TT("dve", aT[:, fp * 2:fp * 2 + 2, :].rearrange("p f t -> p (f t)"),
                       s1[:, fp * 2:fp * 2 + 2, :].rearrange("p f t -> p (f t)"), pp[:, :], ALU.mult, [s1.r, pp.r], [aT.r])
        for hb in range(2):
            ys = ysr.get()
            for half in range(2):
                py = banks.get()
                for fc in range(4):
                    MM(py[:, :], aT[:, fc, hb * 128:(hb + 1) * 128], w2b[:, fc, half * 512:(half + 1) * 512], fc == 0, fc == 3,
                       [aT.r, w2b.r], [py.r])
                if half == 0:
                    ACT(ys[:, 0:512], py[:, :], AF.Identity, [py.r], [ys.r])
                else:
                    CP("dve", ys[:, 512:1024], py[:, :], [py.r], [ys.r])
            r0 = j * SB + hb * 128
            P.dma_copy("sp", yslot.t[r0:r0 + 128, :], ys[:], reads=[ys.r], writes=[yslot.r])
    P.release(markE)

    g2bc = []
    for j in range(2):
        t = P.sbuf(f"g2bc{j}", [128, 1024], F32)
        P.dma_copy("sp", t[:], modD.t[j:j + 1, 5 * 1024:6 * 1024].partition_broadcast(128), writes=[t.r])
        g2bc.append(t)
    lg2 = P.sbuf("lg2", [128, 1024], F32)
    lb2 = P.sbuf("lb2", [128, 1024], F32)
    P.dma_copy("sp", lg2[:], ln2g_d.t[0:1, :].partition_broadcast(128), writes=[lg2.r])
    P.dma_copy("sp", lb2[:], ln2b_d.t[0:1, :].partition_broadcast(128), writes=[lb2.r])
    y1r = Ring([P.sbuf(f"y1{i}", [128, 1024], F32) for i in range(2)])
    y2r = Ring([P.sbuf(f"y2{i}", [128, 1024], F32) for i in range(2)])
    x1r = Ring([P.sbuf(f"x1t{i}", [128, 1024], F32) for i in range(2)])
    ur = Ring([P.sbuf(f"u{i}", [128, 1024], F32) for i in range(2)])
    str_ = Ring([P.sbuf(f"st{i}", [128, 2, 6], F32) for i in range(2)])
    mvr = Ring([P.sbuf(f"mv{i}", [128, 2], F32) for i in range(2)])
    for i in range(NT):
        j = 0 if i < 16 else 1
        y1, y2 = y1r.get(), y2r.get()
        for k, yy in enumerate((y1, y2)):
            P.dma("pool", lambda e, yy=yy, i=i, k=k: e.indirect_dma_start(
                out=yy[:, :], out_offset=None, in_=yslot.t[:, :],
                in_offset=bass.IndirectOffsetOnAxis(ap=posI[:, i, k:k + 1], axis=0)),
                reads=[posI.r, yslot.r], writes=[yy.r])
        xt = x1r.get()
        P.dma_copy("sp", xt[:], x1_d.t[i * 128:(i + 1) * 128, :], writes=[xt.r])
        TS("dve", y1[:], y1[:], gates[:, i, 0:1], None, ALU.mult, None, [y1.r, gates.r], [y1.r])
        STT("dve", y1[:], y2[:], gates[:, i, 1:2], y1[:], ALU.mult, ALU.add, [y2.r, gates.r, y1.r], [y1.r])
        u = ur.get()
        TT("pool", u[:], y1[:], g2bc[j][:], ALU.mult, [y1.r, g2bc[j].r], [u.r])
        STT("dve", u[:], xt[:], ALPHA, u[:], ALU.mult, ALU.add, [xt.r, u.r], [u.r])
        st = str_.get()
        for c in range(2):
            P.op("dve", lambda e, st=st, u=u, c=c: e.bn_stats(out=st[:, c, :], in_=u[:, c * 512:(c + 1) * 512]),
                 [u.r], [st.r])
        mv = mvr.get()
        P.op("dve", lambda e, st=st, mv=mv: e.bn_aggr(out=mv[:], in_=st[:].rearrange("p a b -> p (a b)")),
             [st.r], [mv.r])
        ACT(mv[:, 1:2], mv[:, 1:2], AF.Sqrt, [mv.r, epsc.r], [mv.r], bias=epsc[:, 0:1])
        P.op("dve", lambda e, mv=mv: e.reciprocal(out=mv[:, 1:2], in_=mv[:, 1:2]), [mv.r], [mv.r])
        TS("dve", u[:], u[:], mv[:, 0:1], mv[:, 1:2], ALU.subtract, ALU.mult, [u.r, mv.r], [u.r])
        TT("pool", u[:], u[:], lg2[:], ALU.mult, [u.r, lg2.r], [u.r])
        TT("pool", u[:], u[:], lb2[:], ALU.add, [u.r, lb2.r], [u.r])
        P.dma_copy("sp", x2_o.t[i * 128:(i + 1) * 128, :], u[:], reads=[u.r], writes=[x2_o.r])

    P.release(m_stage)


LAYER_W = {"w_mod", "b_mod", "w_in", "b_in", "qn_g", "kn_g", "wgate", "bgate", "gla_norm", "w_br_attn", "w_br_gla",
           "w_br_na", "w_out", "ln1_g", "ln1_b", "w_r", "b_r", "moe_w1", "moe_w3", "moe_w2", "ln2_g", "ln2_b", "tabNA"}


class Env:
    def __init__(self, P):
        self.P = P
        self.scratch = {}
        self.ext = {}
        self.last = 1
        self.fb = [P.psum(f"fb{i}", [128, 512], F32) for i in range(7)]
        self.bb = P.psum("bb", [128, 8, 128], BF16)

    def _mk(self, name, shape, dt, kind):
        b = self.P.dram(name, shape, dt, kind=kind)
        b.t = b.t.ap()
        return b

    def din(self, name, shape, dt, L):
        if name == "x_in":
            if L == 0:
                if "x_in" not in self.ext:
                    self.ext["x_in"] = self._mk("x_in", shape, dt, "ExternalInput")
                return self.ext["x_in"]
            return self.scratch["x2"]
        if name in self.scratch:
            return self.scratch[name]
        key = f"{name}_{L}" if name in LAYER_W else name
        if key not in self.ext:
            self.ext[key] = self._mk(key, shape, dt, "ExternalInput")
        return self.ext[key]

    def dout(self, name, shape, dt, L):
        if name == "x2" and L == self.last:
            return self._mk("x2out", shape, dt, "ExternalOutput")
        if name not in self.scratch:
            self.scratch[name] = self._mk("sc_" + name, shape, dt, "Internal")
        return self.scratch[name]


def emit_exchange(P, E, groups):
    S = E.scratch

    def sc(name, shape, dt):
        if name not in S:
            S[name] = E._mk("sc_" + name, shape, dt, "Internal")
        return S[name]
    pk_ak, g_ak = sc("pk_ak", [128, 2048], BF16), sc("g_ak", [256, 2048], BF16)
    pk_av, g_av = sc("pk_av", [2048, 128], BF16), sc("g_av", [4096, 128], BF16)
    pk_ck, g_ck = sc("pk_ck", [512, 768], BF16), sc("g_ck", [1024, 768], BF16)
    pk_cv, g_cv = sc("pk_cv", [768, 512], BF16), sc("g_cv", [1536, 512], BF16)
    pk_S, g_S = sc("pk_S", [128, 512], F32), sc("g_S", [256, 512], F32)
    P.dma_copy("sp", pk_ak.t[:, :], S["akT"].t[:, 0:2048], reads=[S["akT"].r], writes=[pk_ak.r])
    P.dma_copy("sp", pk_av.t[:, :], S["av"].t[0:2048, :], reads=[S["av"].r], writes=[pk_av.r])
    P.dma_copy("sp", pk_ck.t[:, 0:384], S["ckT"].t[:, 0:384], reads=[S["ckT"].r], writes=[pk_ck.r])
    P.dma_copy("sp", pk_ck.t[:, 384:768], S["ckT"].t[:, 1664:2048], reads=[S["ckT"].r], writes=[pk_ck.r])
    P.dma_copy("sp", pk_cv.t[0:384, :], S["cv"].t[0:384, :], reads=[S["cv"].r], writes=[pk_cv.r])
    P.dma_copy("sp", pk_cv.t[384:768, :], S["cv"].t[1664:2048, :], reads=[S["cv"].r], writes=[pk_cv.r])
    P.dma_copy("sp", pk_S.t[:, :], S["Sfin"].t.rearrange("d p f -> (d p) f"), reads=[S["Sfin"].r], writes=[pk_S.r])
    for a, b_ in ((pk_ak, g_ak), (pk_av, g_av), (pk_ck, g_ck), (pk_cv, g_cv), (pk_S, g_S)):
        P.coll(lambda e, a=a, b_=b_: e.collective_compute("AllGather", ALU.bypass, replica_groups=groups,
                                                           ins=[a.t[:, :]], outs=[b_.t[:, :]]),
               reads=[a.r], writes=[b_.r])


def build_fused(groups=None, n_layers=2):
    if groups is None:
        groups = [[0, 1], [2, 3], [4, 5], [6, 7]]
    nc = bass.Bass("TRN2", target_bir_lowering=False)
    P = Prog(nc)
    E = Env(P)
    E.last = n_layers - 1
    for L in range(n_layers):
        emit_l1(P, E, L)
        emit_exchange(P, E, groups)
        emit_l2(P, E, L)
        emit_l3(P, E, L)
    P.finalize()
    return nc, P


_BF = ml_dtypes.bfloat16


def _rope_tables(tok):
    row = (tok // 64).astype(np.float32)
    col = (tok % 64).astype(np.float32)
    inv = (10000.0 ** (-np.arange(16, dtype=np.float32) / 16)).astype(np.float32)
    ar = row[:, None] * inv
    ac = col[:, None] * inv
    ang = np.concatenate([ar, ar, ac, ac], -1)
    return np.cos(ang).astype(np.float32), np.sin(ang).astype(np.float32)


def _consts(s):
    tok = s * 2048 + np.arange(2048)
    cos, sin = _rope_tables(tok)
    cosf = np.concatenate([cos, np.ones((256, 64), np.float32)], 0).T
    sinf = np.concatenate([sin, np.zeros((256, 64), np.float32)], 0).T
    R = np.zeros((64, 64), np.float32)
    for d in range(16):
        R[d, d + 16] = -1
        R[d + 16, d] = 1
        R[d + 32, d + 48] = -1
        R[d + 48, d + 32] = 1
    rotM = np.zeros((128, 128), np.float32)
    rotM[:64, :64] = R.T
    rotM[64:, 64:] = R.T
    ob = np.zeros((128, 128), np.float32)
    ob[:64, :64] = 1
    ob[64:, 64:] = 1
    ii = np.arange(128)
    sI, tI = np.meshgrid(ii, ii, indexing="ij")
    f32 = np.float32
    return dict(cosT=np.ascontiguousarray(np.concatenate([cosf, cosf], 0)),
                sinT=np.ascontiguousarray(np.concatenate([sinf, sinf], 0)),
                rotM=rotM, onesblk=ob, ident=np.eye(128, dtype=f32),
                triInc=(sI <= tI).astype(f32), triDec=(sI >= tI).astype(f32),
                triSgt=(sI > tI).astype(f32), triSlt=(sI < tI).astype(f32),
                flags=np.tile(np.array([[1.0 if s == 0 else 0.0, 1.0 if s == 1 else 0.0]], f32), (64, 1)),
                fl1m=np.tile(np.array([[0.0 if s == 0 else 1.0, 0.0 if s == 1 else 1.0]], f32), (64, 1)),
                ones128=np.ones((128, 128), f32),
                thr18=np.tile((256.0 * np.arange(9, dtype=f32))[None, None, :], (128, 32, 1)),
                thrj=np.tile((256.0 * np.arange(NBLK, dtype=f32))[None, :, None], (128, 1, 32)),
                wbase=(np.arange(8)[None, :] * 128 + np.arange(128)[:, None]).astype(f32),
                tokidx=(np.arange(18)[None, :] * 128 + np.arange(128)[:, None]).astype(np.int32))


def _na_table7(rpb, s):
    tab = np.full((5, 7, 2, 128, 512), -30000.0, np.float32)
    kp = np.arange(128)
    qp = np.arange(128)
    for slot, j in enumerate((0, 1, 14, 15, 5)):
        J = 16 * s + j
        qr = 2 * J + qp // 64
        qc = qp % 64
        rs = np.clip(qr - 4, 0, 56)
        cs = np.clip(qc - 8, 0, 48)
        for kt in range(7):
            T = J - 3 + kt
            if T < 0 or T > 31:
                continue
            kr = 2 * T + kp // 64
            kc = kp % 64
            valid = ((kr[:, None] >= rs[None, :]) & (kr[:, None] < rs[None, :] + 8)
                     & (kc[:, None] >= cs[None, :]) & (kc[:, None] < cs[None, :] + 16))
            ri = np.clip(kr[:, None] - qr[None, :] + 7, 0, 14)
            ci = np.clip(kc[:, None] - qc[None, :] + 15, 0, 30)
            for h in range(8):
                tab[slot, kt, h // 4, :, (h % 4) * 128:(h % 4 + 1) * 128] = np.where(valid, rpb[h][ri, ci], -30000.0)
    return tab.astype(_BF)


def _core_map(inp, c, n_layers=2):
    f32 = np.float32
    b, s = c // 2, c % 2
    m = dict(x_in=np.concatenate([inp["x"][b, s * 2048:(s + 1) * 2048], inp["ctx"][b]], 0),
             cvec=np.stack([inp["c"][b], inp["c_ctx"]], 0), **_consts(s))
    for l in range(n_layers):
        lw = dict(w_mod=inp["w_mod"][l], b_mod=inp["b_mod"][l][None], w_in=inp["w_in"][l], b_in=inp["b_in"][l][None],
                  qn_g=inp["attn_q_norm"][l][:, None], kn_g=inp["attn_k_norm"][l][:, None],
                  wgate=inp["gla_w_gate"][l], bgate=inp["gla_b_gate"][l], gla_norm=inp["gla_norm"][l][:, None],
                  w_br_attn=inp["w_br_attn"][l], w_br_gla=inp["w_br_gla"][l], w_br_na=inp["w_br_na"][l],
                  w_out=inp["w_out"][l], ln1_g=inp["ln1_g"][l][None], ln1_b=inp["ln1_b"][l][None],
                  w_r=np.concatenate([inp["w_router_group"][l], inp["w_router_expert"][l]], 1),
                  b_r=np.concatenate([inp["b_router_group"][l], inp["b_router_expert"][l]])[None],
                  moe_w1=inp["moe_w1"][l].reshape(-1, 4096), moe_w3=inp["moe_w3"][l].reshape(-1, 4096),
                  moe_w2=inp["moe_w2"][l].reshape(-1, 4096), ln2_g=inp["ln2_g"][l][None], ln2_b=inp["ln2_b"][l][None],
                  tabNA=_na_table7(inp["na_rpb"][l].astype(f32), s))
        for k, v in lw.items():
            m[f"{k}_{l}"] = v
    out = {}
    for k, v in m.items():
        v = np.asarray(v)
        if v.dtype not in (np.int32, _BF):
            v = v.astype(f32)
        out[k] = np.ascontiguousarray(v)
    return out


_PROG = []


def kernel(**inp):
    inp = {k: np.asarray(v) for k, v in inp.items()}
    if not _PROG:
        _PROG.append(build_fused()[0])
    cores = list(range(8))
    maps = [_core_map(inp, c) for c in cores]
    res = run_bass_kernel_spmd(_PROG[0], maps, core_ids=cores).results
    out = np.zeros((4, 4096, 1024), np.float32)
    for c in cores:
        out[c // 2, (c % 2) * 2048:(c % 2 + 1) * 2048] = np.asarray(res[c]["x2out"], dtype=np.float32)[:2048]
    return out
```
